# Optimizing a Trainium2 kernel written in Bass

```python
import jax, jax.numpy as jnp
from jax import lax
import numpy as np

D_MODEL = 1024
BATCH = 8
SEQ = 4096
DEPTH = 4

HEAD_DIM = 64
N_HEADS = D_MODEL // HEAD_DIM
N_A = max(1, DEPTH // 2)
N_B = DEPTH - N_A
DECAY_LORA = 64
AAA_LORA = 64
MV_LORA = 32
GATE_LORA = 160
N_GROUPS = 4
EXP_PER_GROUP = 8
N_EXPERTS = N_GROUPS * EXP_PER_GROUP
TOP_K = 2
D_EXPERT = 512
EXPERT_BLOCK = 128
Q_BLOCK = 128
DEEPNORM_ALPHA = (2 * DEPTH) ** 0.25
DEEPNORM_BETA = (8 * DEPTH) ** -0.25
LN_EPS = 1e-5
GN_EPS = 64e-5
QK_EPS = 1e-6

kernel_name = 'yoco_rwkv7_fox_hier_moe_deepnorm_adaln'


def layer_norm(x, g, b, eps=LN_EPS):
    xf = x.astype(jnp.float32)
    mu = xf.mean(-1, keepdims=True)
    var = jnp.square(xf - mu).mean(-1, keepdims=True)
    return ((xf - mu) * lax.rsqrt(var + eps)).astype(x.dtype) * g + b


def head_rms(t, g, eps=QK_EPS):
    tf = t.astype(jnp.float32)
    return (tf * lax.rsqrt(jnp.mean(tf * tf, -1, keepdims=True) + eps)).astype(t.dtype) * g


def wkv7_scan(r, w, k, v, a, b):
    B, S, H, N = r.shape

    def step(state, inp):
        r_t, w_t, k_t, v_t, a_t, b_t = inp
        sa = jnp.einsum('bhij,bhj->bhi', state, a_t)
        state = (state * w_t[:, :, None, :] + sa[..., None] * b_t[:, :, None, :]
                 + v_t[..., None] * k_t[:, :, None, :])
        return state, jnp.einsum('bhij,bhj->bhi', state, r_t)

    tm = lambda t: jnp.moveaxis(t, 1, 0)
    state0 = jnp.zeros((B, H, N, N), jnp.float32)
    _, y = lax.scan(step, state0, (tm(r), tm(w), tm(k), tm(v), tm(a), tm(b)))
    return jnp.moveaxis(y, 0, 1)


def rwkv7_time_mix(h, v_first, mu, w_rkv, w0, w1, w2, a0, a1, a2, g1, g2,
                   k_k, k_a, r_k, gn_g, gn_b, w_o, v_mix):
    B, S, D = h.shape
    f32 = jnp.float32
    xx = jnp.pad(h, ((0, 0), (1, 0), (0, 0)))[:, :-1] - h
    xr, xw, xk, xv, xa, xg = [h + xx * mu[i] for i in range(6)]
    r, k, v = jnp.einsum('nbsd,nde->nbse', jnp.stack([xr, xk, xv]), w_rkv)
    w_log = -jnp.exp((-jax.nn.softplus(-(w0 + jnp.tanh(xw @ w1) @ w2)) - 0.5).astype(f32))
    if v_mix is None:
        v_first = v
    else:
        v0, v1, v2 = v_mix
        v = v + (v_first - v) * jax.nn.sigmoid(v0 + (xv @ v1) @ v2)
    a = jax.nn.sigmoid(a0 + (xa @ a1) @ a2)
    g = jax.nn.sigmoid(xg @ g1) @ g2
    heads = lambda t: t.reshape(B, S, N_HEADS, HEAD_DIM).astype(f32)
    kk = heads(k * k_k)
    kk = kk / jnp.maximum(jnp.sqrt(jnp.sum(kk * kk, -1, keepdims=True)), 1e-12)
    k = k * (1 + (a - 1) * k_a)
    rh, kh, vh, ah = heads(r), heads(k), heads(v), heads(a)
    y = wkv7_scan(rh, jnp.exp(heads(w_log)), kh, vh, -kk, kk * ah)
    ym = y.mean(-1, keepdims=True)
    yv = jnp.square(y - ym).mean(-1, keepdims=True)
    y = ((y - ym) * lax.rsqrt(yv + GN_EPS)).reshape(B, S, D).astype(h.dtype) * gn_g + gn_b
    bonus = (jnp.sum(rh * kh * r_k, -1, keepdims=True) * vh).reshape(B, S, D).astype(h.dtype)
    return ((y + bonus) * g) @ w_o, v_first


def shared_kv(x, cs, ada_w, ada_b, w_kvf, b_f, k_norm):
    B, S, D = x.shape
    shift, scale = jnp.split(cs @ ada_w + ada_b, 2, axis=-1)
    hk = x * (1 + scale[:, None]) + shift[:, None]
    kvf = hk @ w_kvf
    k = head_rms(kvf[..., :D].reshape(B, S, N_HEADS, HEAD_DIM), k_norm)
    v = kvf[..., D:2 * D].reshape(B, S, N_HEADS, HEAD_DIM)
    log_f = jax.nn.log_sigmoid((kvf[..., 2 * D:] + b_f).astype(jnp.float32))
    fcum = jnp.cumsum(log_f, axis=1)
    return (k.transpose(0, 2, 1, 3), v.transpose(0, 2, 1, 3), fcum.transpose(0, 2, 1))


def forgetting_attention(q, k, v, fcum):
    S = q.shape[2]
    scale = HEAD_DIM ** -0.5
    outs = []
    for i in range(S // Q_BLOCK):
        lo, hi = i * Q_BLOCK, (i + 1) * Q_BLOCK
        logits = (jnp.einsum('bhqd,bhkd->bhqk', q[:, :, lo:hi], k[:, :, :hi]).astype(jnp.float32) * scale
                  + (fcum[:, :, lo:hi, None] - fcum[:, :, None, :hi]))
        causal = (lo + jnp.arange(Q_BLOCK))[:, None] >= jnp.arange(hi)[None, :]
        logits = jnp.where(causal, logits, -jnp.inf)
        p = jax.nn.softmax(logits, axis=-1).astype(v.dtype)
        outs.append(jnp.einsum('bhqk,bhkd->bhqd', p, v[:, :, :hi]))
    return jnp.concatenate(outs, axis=2)


def fox_layer(h, kv, w_qg, q_norm, w_o):
    B, S, D = h.shape
    k, v, fcum = kv
    qg = h @ w_qg
    q = head_rms(qg[..., :D].reshape(B, S, N_HEADS, HEAD_DIM), q_norm).transpose(0, 2, 1, 3)
    o = forgetting_attention(q, k, v, fcum).transpose(0, 2, 1, 3).reshape(B, S, D)
    return (o * jax.nn.sigmoid(qg[..., D:])) @ w_o


def grouped_experts(xf, expert_idx, gates, w_gate, w_up, w_down):
    T, D = xf.shape
    A = T * TOP_K
    n_blocks = -(-(A + N_EXPERTS * EXPERT_BLOCK) // EXPERT_BLOCK)
    n_slots = n_blocks * EXPERT_BLOCK
    flat_e = expert_idx.reshape(A)
    order = jnp.argsort(flat_e)
    sorted_e = flat_e[order]
    counts = jnp.bincount(flat_e, length=N_EXPERTS)
    padded = (counts + EXPERT_BLOCK - 1) // EXPERT_BLOCK * EXPERT_BLOCK
    pad_end = jnp.cumsum(padded)
    pad_start = pad_end - padded
    start = jnp.cumsum(counts) - counts
    dest_sorted = pad_start[sorted_e] + (jnp.arange(A) - start[sorted_e])
    dest = jnp.zeros((A,), jnp.int32).at[order].set(dest_sorted.astype(jnp.int32))
    slot_tok = jnp.zeros((n_slots,), jnp.int32).at[dest].set(jnp.arange(A, dtype=jnp.int32) // TOP_K)
    block_exp = jnp.minimum(jnp.searchsorted(pad_end, jnp.arange(n_blocks) * EXPERT_BLOCK, side='right'),
                            N_EXPERTS - 1)
    xs = xf[slot_tok].reshape(n_blocks, EXPERT_BLOCK, D)

    def one_block(args):
        xb, e = args
        hb = jax.nn.silu(xb @ w_gate[e]) * (xb @ w_up[e])
        return hb @ w_down[e]

    ys = lax.map(one_block, (xs, block_exp)).reshape(n_slots, D)
    y_assign = ys[dest].reshape(T, TOP_K, D)
    return jnp.einsum('tk,tkd->td', gates, y_assign)


def hier_moe(h, w_grp, b_grp, w_exp, b_exp, w_gate, w_up, w_down):
    B, S, D = h.shape
    T = B * S
    hf = h.reshape(T, D)
    p_grp = jax.nn.softmax((hf @ w_grp).astype(jnp.float32) + b_grp, axis=-1)
    g_sel = jnp.argmax(p_grp, axis=-1)
    p_g = jnp.take_along_axis(p_grp, g_sel[:, None], axis=-1)
    e_logits = ((hf @ w_exp).astype(jnp.float32) + b_exp).reshape(T, N_GROUPS, EXP_PER_GROUP)
    e_logits = jnp.take_along_axis(e_logits, g_sel[:, None, None], axis=1)[:, 0]
    top_p, top_i = lax.top_k(jax.nn.softmax(e_logits, axis=-1), TOP_K)
    gates = (p_g * top_p / jnp.sum(top_p, -1, keepdims=True)).astype(h.dtype)
    expert_idx = (g_sel[:, None] * EXP_PER_GROUP + top_i).astype(jnp.int32)
    return grouped_experts(hf, expert_idx, gates, w_gate, w_up, w_down).reshape(B, S, D)


def setup_inputs(seed: int = 0) -> dict:
    key = jax.random.key(seed)
    ks = iter(jax.random.split(key, 64))
    nrm = lambda shape, s: jax.random.normal(next(ks), shape, jnp.float32) * s
    uni = lambda shape, lo, hi: jax.random.uniform(next(ks), shape, jnp.float32, lo, hi)
    D, H, N = D_MODEL, N_HEADS, HEAD_DIM
    s_in = D ** -0.5
    beta = DEEPNORM_BETA
    nv = N_A - 1
    return {
        'x': nrm((BATCH, SEQ, D), 1.0),
        'c': nrm((BATCH, D), 1.0),
        'ada_w': nrm((DEPTH, D, 6 * D), 0.1 * s_in),
        'ada_b': nrm((DEPTH, 6 * D), 0.01),
        'ln_g': 1.0 + nrm((DEPTH, 2, D), 0.02),
        'ln_b': nrm((DEPTH, 2, D), 0.02),
        'rw_mu': uni((N_A, 6, D), 0.0, 1.0),
        'rw_rkv': nrm((N_A, 3, D, D), s_in) * jnp.array([1.0, 1.0, beta], jnp.float32)[None, :, None, None],
        'rw_w0': uni((N_A, D), -6.0, -1.0),
        'rw_w1': nrm((N_A, D, DECAY_LORA), s_in),
        'rw_w2': nrm((N_A, DECAY_LORA, D), 0.1 * DECAY_LORA ** -0.5),
        'rw_a0': nrm((N_A, D), 0.1),
        'rw_a1': nrm((N_A, D, AAA_LORA), s_in),
        'rw_a2': nrm((N_A, AAA_LORA, D), 0.1 * AAA_LORA ** -0.5),
        'rw_g1': nrm((N_A, D, GATE_LORA), s_in),
        'rw_g2': nrm((N_A, GATE_LORA, D), GATE_LORA ** -0.5),
        'rw_kk': 0.85 + nrm((N_A, D), 0.02),
        'rw_ka': 1.0 + nrm((N_A, D), 0.02),
        'rw_rk': nrm((N_A, H, N), 0.1),
        'rw_gn_g': 1.0 + nrm((N_A, D), 0.02),
        'rw_gn_b': nrm((N_A, D), 0.02),
        'rw_wo': nrm((N_A, D, D), s_in * beta),
        'rw_v0': 1.0 + nrm((nv, D), 0.1),
        'rw_v1': nrm((nv, D, MV_LORA), s_in),
        'rw_v2': nrm((nv, MV_LORA, D), 0.1 * MV_LORA ** -0.5),
        'kv_ada_w': nrm((D, 2 * D), 0.1 * s_in),
        'kv_ada_b': nrm((2 * D,), 0.01),
        'kv_w': jnp.concatenate([nrm((D, D), s_in), nrm((D, D), s_in * beta), nrm((D, H), s_in)], axis=1),
        'kv_fb': uni((H,), 0.0, 5.0),
        'kv_knorm': 1.0 + nrm((N,), 0.02),
        'fx_wqg': nrm((N_B, D, 2 * D), s_in),
        'fx_qnorm': 1.0 + nrm((N_B, N), 0.02),
        'fx_wo': nrm((N_B, D, D), s_in * beta),
        'moe_wgrp': nrm((DEPTH, D, N_GROUPS), s_in),
        'moe_bgrp': nrm((DEPTH, N_GROUPS), 0.01),
        'moe_wexp': nrm((DEPTH, D, N_EXPERTS), s_in),
        'moe_bexp': nrm((DEPTH, N_EXPERTS), 0.01),
        'moe_wgate': nrm((DEPTH, N_EXPERTS, D, D_EXPERT), s_in),
        'moe_wup': nrm((DEPTH, N_EXPERTS, D, D_EXPERT), s_in),
        'moe_wdown': nrm((DEPTH, N_EXPERTS, D_EXPERT, D), D_EXPERT ** -0.5 * beta),
    }


def reference(x, c, ada_w, ada_b, ln_g, ln_b, rw_mu, rw_rkv, rw_w0, rw_w1, rw_w2, rw_a0, rw_a1, rw_a2,
              rw_g1, rw_g2, rw_kk, rw_ka, rw_rk, rw_gn_g, rw_gn_b, rw_wo, rw_v0, rw_v1, rw_v2,
              kv_ada_w, kv_ada_b, kv_w, kv_fb, kv_knorm, fx_wqg, fx_qnorm, fx_wo,
              moe_wgrp, moe_bgrp, moe_wexp, moe_bexp, moe_wgate, moe_wup, moe_wdown):
    cs = jax.nn.silu(c)
    kv = None
    v_first = None
    for l in range(DEPTH):
        mod = cs @ ada_w[l] + ada_b[l]
        sh_m, sc_m, gt_m, sh_f, sc_f, gt_f = [m[:, None, :] for m in jnp.split(mod, 6, axis=-1)]
        h = x * (1 + sc_m) + sh_m
        if l < N_A:
            v_mix = None if l == 0 else (rw_v0[l - 1], rw_v1[l - 1], rw_v2[l - 1])
            y, v_first = rwkv7_time_mix(h, v_first, rw_mu[l], rw_rkv[l], rw_w0[l], rw_w1[l], rw_w2[l],
                                        rw_a0[l], rw_a1[l], rw_a2[l], rw_g1[l], rw_g2[l], rw_kk[l],
                                        rw_ka[l], rw_rk[l], rw_gn_g[l], rw_gn_b[l], rw_wo[l], v_mix)
        else:
            j = l - N_A
            y = fox_layer(h, kv, fx_wqg[j], fx_qnorm[j], fx_wo[j])
        x = layer_norm(DEEPNORM_ALPHA * x + (1 + gt_m) * y, ln_g[l, 0], ln_b[l, 0])
        h = x * (1 + sc_f) + sh_f
        y = hier_moe(h, moe_wgrp[l], moe_bgrp[l], moe_wexp[l], moe_bexp[l],
                     moe_wgate[l], moe_wup[l], moe_wdown[l])
        x = layer_norm(DEEPNORM_ALPHA * x + (1 + gt_f) * y, ln_g[l, 1], ln_b[l, 1])
        if l == N_A - 1:
            kv = shared_kv(x, cs, kv_ada_w, kv_ada_b, kv_w, kv_fb, kv_knorm)
    return x
```

```python
import numpy as np
from contextlib import ExitStack
import concourse.bass as bass
import concourse.mybir as mybir
from concourse.bass_utils import run_bass_kernel_spmd

F32 = mybir.dt.float32
BF16 = mybir.dt.bfloat16
AF = mybir.ActivationFunctionType
ALU = mybir.AluOpType
AX = mybir.AxisListType

D = 1024
KC = 8
HD = 64
NH = 16
DEPTH = 4
N_A = 2
ALPHA = (2 * DEPTH) ** 0.25
LN_EPS = 1e-5
GN_EPS = 64e-5
QK_EPS = 1e-6
DEXP = 512


class Tk:
    __slots__ = ("w", "r", "excl")

    def __init__(s):
        s.w = None
        s.r = {}
        s.excl = False


class Buf:
    def __init__(s, t, n=1):
        s.t = t
        s.k = [Tk() for _ in range(n)]

    def __getitem__(s, idx):
        return s.t[idx]


class _KL(list):
    def __getitem__(s, i):
        return list.__getitem__(s, 0)


class PBuf(Buf):
    def __init__(s, t):
        s.t = t
        s.k = _KL([Tk()])
        s.k[0].excl = True


class K:
    NS = 24

    def __init__(s, nc, es):
        s.nc = nc
        s.eng = {'pe': nc.tensor, 'act': nc.scalar, 'dve': nc.vector, 'pool': nc.gpsimd, 'sp': nc.sync}
        s.esem = {n: es.enter_context(nc.semaphore("s_" + n)) for n in s.eng}
        s.ecnt = {n: 0 for n in s.eng}
        s.seen = {n: {} for n in s.eng}
        s.dsem = [es.enter_context(nc.semaphore("d%d" % i)) for i in range(s.NS)]
        s.dcnt = [0] * s.NS
        s.dnext = {'hw': 0, 'sw': 0}
        s.dpool = {'hw': list(range(0, 16)), 'sw': list(range(16, s.NS))}
        s.same_sync = {'pe': False, 'act': True, 'dve': True, 'pool': True, 'sp': False}
        s.nins = 0

    def _wait(s, en, key, val):
        if s.seen[en].get(key, 0) >= val:
            return
        if key[0] == 'E':
            if key[1] == en and not s.same_sync[en]:
                return
            sem = s.esem[key[1]]
        else:
            sem = s.dsem[key[1]]
        s.eng[en].wait_ge(sem, val)
        s.seen[en][key] = val
        s.nins += 1

    def _need(s, reads, writes):
        need = {}
        for t in reads:
            if t.w is not None:
                k_, v = t.w
                if need.get(k_, 0) < v:
                    need[k_] = v
        for t in writes:
            if t.w is not None:
                k_, v = t.w
                if need.get(k_, 0) < v:
                    need[k_] = v
            for k_, v in t.r.items():
                if need.get(k_, 0) < v:
                    need[k_] = v
        return need

    def op(s, en, fn, reads=(), writes=()):
        ex = [t for t in reads if t.excl]
        if ex:
            reads = [t for t in reads if not t.excl]
            writes = list(writes) + ex
        for k_, v in s._need(reads, writes).items():
            s._wait(en, k_, v)
        ins = fn(s.eng[en])
        s.ecnt[en] += 1
        c = s.ecnt[en]
        ins.then_inc(s.esem[en], 1)
        key = ('E', en)
        for t in reads:
            t.r[key] = c
        for t in writes:
            t.w = (key, c)
            t.r = {}
        s.nins += 1

    def dma(s, q, out, in_, reads=(), writes=(), **kw):
        for k_, v in s._need(reads, writes).items():
            s._wait(q, k_, v)
        pn = 'sw' if q == 'pool' else 'hw'
        pl = s.dpool[pn]
        i = pl[s.dnext[pn] % len(pl)]
        s.dnext[pn] += 1
        if s.dcnt[i]:
            s._wait(q, ('D', i), s.dcnt[i] * 16)
        s.eng[q].dma_start(out=out, in_=in_, **kw).then_inc(s.dsem[i], 16)
        s.dcnt[i] += 1
        key = ('D', i)
        val = s.dcnt[i] * 16
        for t in reads:
            t.r[key] = val
        for t in writes:
            t.w = (key, val)
            t.r = {}
        s.nins += 1

    def barrier(s):
        for en in s.eng:
            for o in s.eng:
                if o != en and s.ecnt[o] > 0:
                    s._wait(en, ('E', o), s.ecnt[o])
            for i in range(s.NS):
                if s.dcnt[i] > 0:
                    s._wait(en, ('D', i), s.dcnt[i] * 16)

    def mm(s, out, lhsT, rhs, start, stop, reads, writes):
        s.op('pe', lambda e: e.matmul(out, lhsT, rhs, start=start, stop=stop), reads, writes)

    def tr(s, out, in_, ident, reads, writes):
        s.op('pe', lambda e: e.transpose(out, in_, ident), reads, writes)

    def act(s, out, in_, func, reads, writes, bias=None, scale=None, accum_out=None, en='act'):
        kw = {}
        if bias is not None:
            kw['bias'] = bias
        if scale is not None:
            kw['scale'] = scale
        if accum_out is not None:
            kw['accum_out'] = accum_out
        s.op('act', lambda e: e.activation(out, in_, func, **kw), reads, writes)

    def tt(s, en, out, in0, in1, op, reads, writes):
        s.op(en, lambda e: e.tensor_tensor(out=out, in0=in0, in1=in1, op=op), reads, writes)

    def ts(s, en, out, in0, s1, s2, op0, op1, reads, writes):
        if op1 is None:
            s.op(en, lambda e: e.tensor_scalar(out=out, in0=in0, scalar1=s1, scalar2=None, op0=op0), reads, writes)
        else:
            s.op(en, lambda e: e.tensor_scalar(out=out, in0=in0, scalar1=s1, scalar2=s2, op0=op0, op1=op1), reads, writes)

    def stt(s, out, in0, scalar, in1, op0, op1, reads, writes):
        s.op('dve', lambda e: e.scalar_tensor_tensor(out=out, in0=in0, scalar=scalar, in1=in1, op0=op0, op1=op1),
             reads, writes)

    def cp(s, en, out, in_, reads, writes):
        if en == 'act':
            s.op('act', lambda e: e.copy(out, in_), reads, writes)
        else:
            s.op(en, lambda e: e.tensor_copy(out=out, in_=in_), reads, writes)


class Ctx:
    pass


def build(T, NG, EPG, layers=DEPTH, dbg=None):
    NE = NG * EPG
    NR = NG + NE
    nc = bass.Bass("TRN2", target_bir_lowering=False)
    g = Ctx()
    g.T, g.NG, g.EPG, g.NE, g.NR = T, NG, EPG, NE, NR
    g.dbg = dbg
    n_a = min(N_A, layers)
    n_b = layers - n_a
    nv = max(n_a - 1, 0)

    def din(name, shape):
        return nc.dram_tensor(name, list(shape), F32, kind="ExternalInput").ap()

    I = {}
    I['x'] = din('x', [T, D])
    I['c'] = din('c', [KC, 128])
    I['ada_w'] = din('ada_w', [DEPTH, D, 6 * D])
    I['ada_b'] = din('ada_b', [DEPTH, 48, 128])
    I['ln_g'] = din('ln_g', [DEPTH, 16, 128])
    I['ln_b'] = din('ln_b', [DEPTH, 16, 128])
    I['rw_mu'] = din('rw_mu', [N_A, 48, 128])
    I['rw_rkv'] = din('rw_rkv', [N_A, 3, D, D])
    for nm in ['rw_w0', 'rw_a0', 'rw_kk', 'rw_ka', 'rw_rk', 'rw_gn_g', 'rw_gn_b']:
        I[nm] = din(nm, [N_A, 8, 128])
    I['rw_w1'] = din('rw_w1', [N_A, D, 64])
    I['rw_w2'] = din('rw_w2', [N_A, 64, D])
    I['rw_a1'] = din('rw_a1', [N_A, D, 64])
    I['rw_a2'] = din('rw_a2', [N_A, 64, D])
    I['rw_g1'] = din('rw_g1', [N_A, D, 160])
    I['rw_g2'] = din('rw_g2', [N_A, 160, D])
    I['rw_wo'] = din('rw_wo', [N_A, D, D])
    I['rw_v0'] = din('rw_v0', [1, 8, 128])
    I['rw_v1'] = din('rw_v1', [1, D, 32])
    I['rw_v2'] = din('rw_v2', [1, 32, D])
    I['kv_ada_w'] = din('kv_ada_w', [D, 2 * D])
    I['kv_ada_b'] = din('kv_ada_b', [16, 128])
    I['kv_w'] = din('kv_w', [D, 2 * D + NH])
    I['kv_fb'] = din('kv_fb', [NH, 1])
    I['kv_knorm'] = din('kv_knorm', [HD, 1])
    I['fx_wqg'] = din('fx_wqg', [2, D, 2 * D])
    I['fx_qnorm'] = din('fx_qnorm', [2, HD, 1])
    I['fx_wo'] = din('fx_wo', [2, D, D])
    I['moe_wgrp'] = din('moe_wgrp', [DEPTH, D, NG])
    I['moe_bgrp'] = din('moe_bgrp', [DEPTH, NG])
    I['moe_wexp'] = din('moe_wexp', [DEPTH, D, NE])
    I['moe_bexp'] = din('moe_bexp', [DEPTH, NE])
    I['moe_wgate'] = din('moe_wgate', [DEPTH, NE, D, DEXP])
    I['moe_wup'] = din('moe_wup', [DEPTH, NE, D, DEXP])
    I['moe_wdown'] = din('moe_wdown', [DEPTH, NE, DEXP, D])
    out_ap = nc.dram_tensor('out', [T, D], F32, kind="ExternalOutput").ap()

    def dscr(name, shape, dt=F32):
        kind = "ExternalOutput" if (dbg and name in dbg) else "Internal"
        return Buf(nc.dram_tensor(name, list(shape), dt, kind=kind).ap())

    S = {}
    S['XT'] = dscr('XT', [D, T])
    S['X1'] = dscr('X1', [D, T])
    S['H2'] = dscr('H2', [D, T], BF16)
    S['MO'] = dscr('MO', [D, T], BF16)
    S['VF'] = dscr('VF', [D, T])
    S['KT'] = dscr('KT', [D, T], BF16)
    S['VK'] = dscr('VK', [T, D], BF16)
    S['FC'] = dscr('FC', [NH, T])
    S['QT'] = dscr('QT', [D, T], BF16)
    S['SG'] = dscr('SG', [D, T], BF16)

    with ExitStack() as es:
        k = K(nc, es)
        g.es = es
        g.k, g.nc, g.I, g.S, g.out = k, nc, I, S, out_ap
        g.ps = [PBuf(es.enter_context(nc.psum_tensor("ps%d" % i, [128, 512], F32))) for i in range(8)]
        setup_consts(g, es)
        g.GT = sb(g, es, "GT", [128, T])
        phase_mod(g, layers, n_a)
        phase_in(g)
        for l in range(layers):
            if l < n_a:
                phase_rwkv(g, l)
            else:
                phase_fox(g, l, l - n_a)
            phase_moe(g, l, last=(l == layers - 1))
            if l == n_a - 1 and n_b > 0:
                phase_kv(g)
        k.barrier()
    g.nc = nc
    return nc, g


_UID = [0]


def sb(g, es, name, shape, dt=F32, n=1):
    _UID[0] += 1
    return Buf(es.enter_context(g.nc.sbuf_tensor("%s_%d" % (name, _UID[0]), list(shape), dt)), n)


def setup_consts(g, es):
    k, nc = g.k, g.nc
    ones = sb(g, es, "c_ones", [128, 512])
    g.ones = ones
    k.op('pool', lambda e: e.memset(ones[:], 1.0), [], ones.k)
    ident = sb(g, es, "c_ident", [128, 128])
    g.ident = ident
    k.op('pool', lambda e: e.affine_select(out=ident[:], in_=ones[:, 0:128], pattern=[[-1, 128]],
                                           compare_op=ALU.is_equal, fill=0.0, base=0, channel_multiplier=1),
         ones.k, ident.k)
    mmean = sb(g, es, "c_mmean", [128, 128])
    g.mmean = mmean
    k.op('pool', lambda e: e.memset(mmean[:], 1.0 / D), [], mmean.k)
    blk = sb(g, es, "c_blk", [128, 128])
    g.blk = blk
    k.op('pool', lambda e: e.memset(blk[:], 0.0), [], blk.k)
    k.op('pool', lambda e: e.memset(blk[0:64, 0:64], 1.0), [], blk.k)
    k.op('pool', lambda e: e.memset(blk[64:128, 64:128], 1.0), [], blk.k)


def dump(g, name, ap, reads):
    if not g.dbg or name not in g.dbg:
        return
    d = g.nc.dram_tensor("dbg_" + name, list(ap.shape), ap.dtype, kind="ExternalOutput").ap()
    g.k.dma('sp', d, ap, reads, [])


def load_vecT(g, out, src2d, R, st):
    k = g.k
    k.dma('sp', st[0:R, :], src2d, [], st.k)
    ps = g.ps[0]
    k.tr(ps[:, 0:R], st[0:R, :], g.ident[0:R, 0:R], st.k + g.ident.k, [ps.k[0]])
    k.cp('dve', out[:, 0:R], ps[:, 0:R], [ps.k[0]], out.k)
    return out


def phase_mod(g, layers, n_a):
    k, nc, I = g.k, g.nc, g.I
    es = g.es
    g.modT = [sb(g, es, "modT%d" % l, [128, 48]) for l in range(layers)]
    g.mod1 = [sb(g, es, "mod1_%d" % l, [128, 48]) for l in range(layers)]
    g.lng = [sb(g, es, "lng%d" % l, [128, 16]) for l in range(layers)]
    g.lnb = [sb(g, es, "lnb%d" % l, [128, 16]) for l in range(layers)]
    if layers > n_a:
        g.kvmod = sb(g, es, "kvmod", [128, 16])
        g.kvmod1 = sb(g, es, "kvmod1", [128, 16])
    with ExitStack() as ph:
        st = sb(g, ph, "lv_st", [128, 128])
        cT = sb(g, ph, "cT", [128, KC])
        bT = sb(g, ph, "bT", [128, 48])
        load_vecT(g, cT, I['c'], KC, st)
        cs2 = sb(g, ph, "cs2", [128, KC, 2])
        k.act(cs2[:, :, 0], cT[:], AF.Silu, cT.k, cs2.k)
        k.act(cs2[:, :, 1], cT[:], AF.Silu, cT.k, cs2.k)
        wb = [sb(g, ph, "adaw%d" % i, [128, KC, 1024]) for i in range(2)]
        nblk = 0

        def matvec(w_ap, ncols, outT):
            nonlocal nblk
            wv = w_ap.rearrange("(kc p) n -> p kc n", p=128)
            for b0 in range(0, ncols, 1024):
                bw = min(1024, ncols - b0)
                w = wb[nblk % 2]
                nblk += 1
                k.dma('sp', w[:, :, 0:bw], wv[:, :, b0:b0 + bw], [], w.k)
                ps = g.ps[1 + (nblk % 2)]
                for j in range(bw // 128):
                    for kc in range(KC):
                        k.mm(ps[:, 2 * j:2 * j + 2], w[:, kc, j * 128:(j + 1) * 128], cs2[:, kc, :],
                             kc == 0, kc == KC - 1, w.k + cs2.k, [ps.k[0]])
                nj = bw // 128
                k.cp('dve', outT[:, b0 // 128:b0 // 128 + nj],
                     ps[:, 0:2 * nj].rearrange("p (j t) -> p j t", t=2)[:, :, 0], [ps.k[0]], outT.k)

        for l in range(layers):
            mt, m1 = g.modT[l], g.mod1[l]
            matvec(I['ada_w'][l], 6 * D, mt)
            load_vecT(g, bT, I['ada_b'][l], 48, st)
            k.tt('dve', mt[:], mt[:], bT[:], ALU.add, mt.k + bT.k, mt.k)
            k.ts('dve', m1[:], mt[:], 1.0, None, ALU.add, None, mt.k, m1.k)
            dump(g, "modT%d" % l, mt[:], mt.k)
            load_vecT(g, g.lng[l], I['ln_g'][l], 16, st)
            load_vecT(g, g.lnb[l], I['ln_b'][l], 16, st)
        if layers > n_a:
            kt, k1 = g.kvmod, g.kvmod1
            matvec(I['kv_ada_w'], 2 * D, kt)
            load_vecT(g, bT, I['kv_ada_b'], 16, st)
            k.tt('dve', kt[:], kt[:], bT[:, 0:16], ALU.add, kt.k + bT.k, kt.k)
            k.ts('dve', k1[:], kt[:], 1.0, None, ALU.add, None, kt.k, k1.k)
        k.barrier()


def phase_in(g):
    k, I, S = g.k, g.I, g.S
    T = g.T
    with ExitStack() as ph:
        xt = [sb(g, ph, "in_x%d" % i, [128, D]) for i in range(2)]
        st = [sb(g, ph, "in_st%d" % i, [128, KC, 512]) for i in range(2)]
        XTv = S['XT'].t.rearrange("(kc p) t -> p kc t", p=128)
        for gi in range(T // 128):
            xb = xt[gi % 2]
            k.dma('sp', xb[:], I['x'][gi * 128:(gi + 1) * 128, :], [], xb.k)
            sg = st[(gi // 4) % 2]
            for half in range(2):
                ps = g.ps[(gi * 2 + half) % 4]
                for j in range(4):
                    kc = half * 4 + j
                    k.tr(ps[:, j * 128:(j + 1) * 128], xb[:, kc * 128:(kc + 1) * 128], g.ident[:],
                         xb.k + g.ident.k, [ps.k[0]])
                dst = sg[:, half * 4:half * 4 + 4, (gi % 4) * 128:(gi % 4 + 1) * 128]
                src = ps[:, :].rearrange("p (j t) -> p j t", t=128)
                k.cp('act' if half else 'dve', dst, src, [ps.k[0]], sg.k)
            if gi % 4 == 3:
                t0 = (gi // 4) * 512
                k.dma('sp', XTv[:, :, t0:t0 + 512], sg[:], sg.k, S['XT'].k)
        k.barrier()


STUB = {'rwkv': False, 'fox': False}
RW_STOP = [99]
RW_SUB = [9]


def bc_rows(ap2d_row, nparts, ncols):
    return bass.AP(tensor=ap2d_row.tensor, offset=ap2d_row.offset, ap=[[0, nparts], [1, ncols]])


def mixer_zero(g):
    k, S, T = g.k, g.S, g.T
    with ExitStack() as ph:
        z = sb(g, ph, "mz", [128, KC, 512], BF16)
        k.op('pool', lambda e: e.memset(z[:], 0.0), [], z.k)
        MOv = S['MO'].t.rearrange("(kc p) t -> p kc t", p=128)
        for t0 in range(0, T, 512):
            k.dma('sp', MOv[:, :, t0:t0 + 512], z[:], z.k, S['MO'].k)
        k.barrier()


def phase_rwkv(g, l):
    if STUB['rwkv']:
        mixer_zero(g)
    else:
        rwkv_mixer(g, l)
    phase_tail(g, l, g.I['rw_wo'][l])


def phase_fox(g, l, j):
    if STUB['fox']:
        mixer_zero(g)
    else:
        fox_mixer(g, l, j)
    phase_tail(g, l, g.I['fx_wo'][j])


def ln_tile(g, zf, zk, sq, mean, rstd, gam, bet, outf, outk, w):
    k = g.k
    psm, psq = g.ps[6], g.ps[7]
    for kc in range(KC):
        k.act(sq[:, kc, 0:w], zf(kc), AF.Square, zk, sq.k)
    for kc in range(KC):
        k.mm(psm[:, 0:w], g.mmean[:], zf(kc), kc == 0, kc == KC - 1, g.mmean.k + zk, [psm.k[0]])
    for kc in range(KC):
        k.mm(psq[:, 0:w], g.mmean[:], sq[:, kc, 0:w], kc == 0, kc == KC - 1, g.mmean.k + sq.k, [psq.k[0]])
    k.cp('act', mean[:, 0:w], psm[:, 0:w], [psm.k[0]], mean.k)
    k.tt('dve', rstd[:, 0:w], mean[:, 0:w], mean[:, 0:w], ALU.mult, mean.k, rstd.k)
    k.tt('dve', rstd[:, 0:w], psq[:, 0:w], rstd[:, 0:w], ALU.subtract, [psq.k[0]] + rstd.k, rstd.k)
    k.ts('dve', rstd[:, 0:w], rstd[:, 0:w], LN_EPS, None, ALU.add, None, rstd.k, rstd.k)
    k.act(rstd[:, 0:w], rstd[:, 0:w], AF.Sqrt, rstd.k, rstd.k)
    k.op('dve', lambda e: e.reciprocal(out=rstd[:, 0:w], in_=rstd[:, 0:w]), rstd.k, rstd.k)
    for kc in range(KC):
        k.tt('dve', zf(kc), zf(kc), mean[:, 0:w], ALU.subtract, zk + mean.k, zk)
        k.tt('pool', zf(kc), zf(kc), rstd[:, 0:w], ALU.mult, zk + rstd.k, zk)
        k.act(outf(kc), zf(kc), AF.Identity, zk, outk, scale=gam(kc), bias=bet(kc))


def phase_tail(g, l, wo_ap):
    k, I, S, T = g.k, g.I, g.S, g.T
    NE, NG, NR, EPG = g.NE, g.NG, g.NR, g.EPG
    mt, m1 = g.modT[l], g.mod1[l]
    GT = g.GT
    with ExitStack() as ph:
        wo = sb(g, ph, "t_wo", [128, KC, D], BF16)
        k.dma('pool', wo[:], wo_ap.rearrange("(kc p) n -> p kc n", p=128), [], wo.k)
        wr = sb(g, ph, "t_wr", [128, KC, NR])
        k.dma('sp', wr[:, :, 0:NG], I['moe_wgrp'][l].rearrange("(kc p) n -> p kc n", p=128), [], wr.k)
        k.dma('sp', wr[:, :, NG:NR], I['moe_wexp'][l].rearrange("(kc p) n -> p kc n", p=128), [], wr.k)
        rb = sb(g, ph, "t_rb", [128, NR])
        k.dma('sp', rb[:, 0:NG], bc_rows(I['moe_bgrp'][l], 128, NG), [], rb.k)
        k.dma('sp', rb[:, NG:NR], bc_rows(I['moe_bexp'][l], 128, NE), [], rb.k)
        xt = [sb(g, ph, "t_x%d" % i, [128, KC, 512]) for i in range(2)]
        zt = [sb(g, ph, "t_z%d" % i, [128, KC, 512]) for i in range(2)]
        mo = [sb(g, ph, "t_mo%d" % i, [128, KC, 512], BF16) for i in range(2)]
        hb = [sb(g, ph, "t_hb%d" % i, [128, KC, 512], BF16) for i in range(2)]
        sq = sb(g, ph, "t_sq", [128, KC, 512])
        mean = sb(g, ph, "t_mean", [128, 512])
        rstd = sb(g, ph, "t_rstd", [128, 512])
        sm = {n: sb(g, ph, "t_r_" + n, [128, w_]) for n, w_ in
              [('lg', NR), ('mg', 1), ('nmg', 1), ('eg', NG), ('sg', 1), ('pg', 1), ('oh', NG), ('pen', NG),
               ('le', NE), ('t8', 8), ('d12', 1), ('s12', 1), ('g1', 1), ('g2', 1), ('G1', NE), ('G2', NE)]}
        XTv = S['XT'].t.rearrange("(kc p) t -> p kc t", p=128)
        X1v = S['X1'].t.rearrange("(kc p) t -> p kc t", p=128)
        MOv = S['MO'].t.rearrange("(kc p) t -> p kc t", p=128)
        H2v = S['H2'].t.rearrange("(kc p) t -> p kc t", p=128)
        for ti in range(T // 512):
            t0 = ti * 512
            x, z, m, h = xt[ti % 2], zt[ti % 2], mo[ti % 2], hb[ti % 2]
            k.dma('sp', x[:], XTv[:, :, t0:t0 + 512], S['XT'].k, x.k)
            k.dma('sp', m[:], MOv[:, :, t0:t0 + 512], S['MO'].k, m.k)
            for oc in range(KC):
                ps = g.ps[oc % 2]
                for kc in range(KC):
                    k.mm(ps[:, :], wo[:, kc, oc * 128:(oc + 1) * 128], m[:, kc, :], kc == 0, kc == KC - 1,
                         wo.k + m.k, [ps.k[0]])
                k.act(z[:, oc, :], ps[:, :], AF.Identity, [ps.k[0]], z.k, scale=m1[:, 16 + oc:17 + oc])
                k.stt(z[:, oc, :], x[:, oc, :], ALPHA, z[:, oc, :], ALU.mult, ALU.add, x.k + z.k, z.k)
            ln_tile(g, lambda kc: z[:, kc, :], z.k, sq, mean, rstd,
                    lambda kc: g.lng[l][:, kc:kc + 1], lambda kc: g.lnb[l][:, kc:kc + 1],
                    lambda kc: x[:, kc, :], x.k, 512)
            k.dma('sp', X1v[:, :, t0:t0 + 512], x[:], x.k, S['X1'].k)
            for kc in range(KC):
                k.act(z[:, kc, :], x[:, kc, :], AF.Identity, x.k, z.k,
                      scale=m1[:, 32 + kc:33 + kc], bias=mt[:, 24 + kc:25 + kc])
            k.cp('pool', h[:], z[:], z.k, h.k)
            k.dma('sp', H2v[:, :, t0:t0 + 512], h[:], h.k, S['H2'].k)
            for tg in range(4):
                pr = g.ps[2 + tg % 2]
                for kc in range(KC):
                    k.mm(pr[:, 0:NR], z[:, kc, tg * 128:(tg + 1) * 128], wr[:, kc, :], kc == 0, kc == KC - 1,
                         z.k + wr.k, [pr.k[0]])
                lg, mg, nmg, eg, sg, pg = sm['lg'], sm['mg'], sm['nmg'], sm['eg'], sm['sg'], sm['pg']
                oh, pen, le, t8 = sm['oh'], sm['pen'], sm['le'], sm['t8']
                k.tt('dve', lg[:], pr[:, 0:NR], rb[:], ALU.add, [pr.k[0]] + rb.k, lg.k)
                k.op('dve', lambda e: e.tensor_reduce(out=mg[:], in_=lg[:, 0:NG], axis=AX.X, op=ALU.max), lg.k, mg.k)
                k.ts('dve', nmg[:], mg[:], -1.0, None, ALU.mult, None, mg.k, nmg.k)
                k.act(eg[:], lg[:, 0:NG], AF.Exp, lg.k + nmg.k, eg.k + sg.k, bias=nmg[:, 0:1], accum_out=sg[:])
                k.op('dve', lambda e: e.reciprocal(out=pg[:], in_=sg[:]), sg.k, pg.k)
                k.ts('dve', oh[:], lg[:, 0:NG], mg[:, 0:1], None, ALU.is_equal, None, lg.k + mg.k, oh.k)
                k.ts('dve', pen[:], oh[:], -1.0, 1e30, ALU.add, ALU.mult, oh.k, pen.k)
                for gi in range(NG):
                    k.ts('dve', le[:, gi * EPG:(gi + 1) * EPG], lg[:, NG + gi * EPG:NG + (gi + 1) * EPG],
                         pen[:, gi:gi + 1], None, ALU.add, None, lg.k + pen.k, le.k)
                k.op('dve', lambda e: e.max(out=t8[:], in_=le[:]), le.k, t8.k)
                d12, s12, g1, g2, G1, G2 = sm['d12'], sm['s12'], sm['g1'], sm['g2'], sm['G1'], sm['G2']
                k.tt('dve', d12[:], t8[:, 0:1], t8[:, 1:2], ALU.subtract, t8.k, d12.k)
                k.act(s12[:], d12[:], AF.Sigmoid, d12.k, s12.k)
                k.tt('dve', g1[:], s12[:], pg[:], ALU.mult, s12.k + pg.k, g1.k)
                k.tt('dve', g2[:], pg[:], g1[:], ALU.subtract, pg.k + g1.k, g2.k)
                k.ts('dve', G1[:], le[:], t8[:, 0:1], g1[:, 0:1], ALU.is_equal, ALU.mult, le.k + t8.k + g1.k, G1.k)
                k.ts('dve', G2[:], le[:], t8[:, 1:2], g2[:, 0:1], ALU.is_equal, ALU.mult, le.k + t8.k + g2.k, G2.k)
                k.tt('dve', G1[:], G1[:], G2[:], ALU.add, G1.k + G2.k, G1.k)
                pt = g.ps[4 + tg % 2]
                k.tr(pt[0:NE, 0:128], G1[:], g.ident[:], G1.k + g.ident.k, [pt.k[0]])
                c0 = t0 + tg * 128
                k.cp('dve', GT[0:NE, c0:c0 + 128], pt[0:NE, 0:128], [pt.k[0]], GT.k)
        dump(g, "GT%d" % l, GT[0:NE, :], GT.k)
        k.barrier()


def phase_moe(g, l, last):
    k, I, S, T = g.k, g.I, g.S, g.T
    NE = g.NE
    mt, m1 = g.modT[l], g.mod1[l]
    GT = g.GT
    HT = min(2048, T)
    NTT = HT // 512
    X1v = S['X1'].t.rearrange("(kc p) t -> p kc t", p=128)
    XTv = S['XT'].t.rearrange("(kc p) t -> p kc t", p=128)
    H2v = S['H2'].t.rearrange("(kc p) t -> p kc t", p=128)
    with ExitStack() as ph:
        h2 = sb(g, ph, "m_h2", [128, KC, HT], BF16)
        yacc = sb(g, ph, "m_y", [128, KC, HT], F32, n=KC * NTT)
        for hf in range(T // HT):
            tb = hf * HT
            for tt in range(NTT):
                k.dma('sp', h2[:, :, tt * 512:(tt + 1) * 512], H2v[:, :, tb + tt * 512:tb + (tt + 1) * 512],
                      S['H2'].k, h2.k)
            with ExitStack() as ex:
                wg = [sb(g, ex, "m_wg%d" % i, [128, KC, DEXP], BF16) for i in range(2)]
                wu = [sb(g, ex, "m_wu%d" % i, [128, KC, DEXP], BF16) for i in range(2)]
                wd = [sb(g, ex, "m_wd%d" % i, [128, 4, D], BF16) for i in range(2)]
                sel = [sb(g, ex, "m_sel%d" % i, [128, 128]) for i in range(2)]
                gbc = sb(g, ex, "m_gbc", [128, 512])
                sl = [sb(g, ex, "m_sl%d" % i, [128, 512]) for i in range(2)]
                tl = sb(g, ex, "m_tl", [128, 512])
                hT = [sb(g, ex, "m_hT%d" % i, [128, 4, 512], BF16) for i in range(2)]
                it = 0

                def load_w(e):
                    bi = e % 2
                    k.dma('pool', wg[bi][:], I['moe_wgate'][l, e].rearrange("(kc p) n -> p kc n", p=128), [], wg[bi].k)
                    k.dma('pool', wu[bi][:], I['moe_wup'][l, e].rearrange("(kc p) n -> p kc n", p=128), [], wu[bi].k)
                    k.dma('pool', wd[bi][:], I['moe_wdown'][l, e].rearrange("(dc p) n -> p dc n", p=128), [], wd[bi].k)
                load_w(0)
                for e in range(NE):
                    bi = e % 2
                    if e + 1 < NE:
                        load_w(e + 1)
                    se = sel[bi]
                    k.op('pool', lambda e_, se=se, e=e: e_.affine_select(
                        out=se[0:NE, :], in_=g.ones[0:NE, 0:128], pattern=[[0, 128]], compare_op=ALU.is_equal,
                        fill=0.0, base=-e, channel_multiplier=1), g.ones.k, se.k)
                    for tt in range(NTT):
                        c0 = tt * 512
                        psg = g.ps[0]
                        k.mm(psg[:, :], se[0:NE, :], GT[0:NE, tb + c0:tb + c0 + 512], True, True,
                             se.k + GT.k, [psg.k[0]])
                        k.cp('act', gbc[:], psg[:, :], [psg.k[0]], gbc.k)
                        hh = hT[it % 2]
                        it += 1
                        for dc in range(4):
                            pg_, pu_ = g.ps[1 + dc % 2], g.ps[3 + dc % 2]
                            for kc in range(KC):
                                k.mm(pg_[:, :], wg[bi][:, kc, dc * 128:(dc + 1) * 128], h2[:, kc, c0:c0 + 512],
                                     kc == 0, kc == KC - 1, wg[bi].k + h2.k, [pg_.k[0]])
                            for kc in range(KC):
                                k.mm(pu_[:, :], wu[bi][:, kc, dc * 128:(dc + 1) * 128], h2[:, kc, c0:c0 + 512],
                                     kc == 0, kc == KC - 1, wu[bi].k + h2.k, [pu_.k[0]])
                            s_ = sl[dc % 2]
                            k.act(s_[:], pg_[:, :], AF.Silu, [pg_.k[0]], s_.k)
                            k.tt('dve', tl[:], pu_[:, :], s_[:], ALU.mult, [pu_.k[0]] + s_.k, tl.k)
                            k.tt('dve', hh[:, dc, :], tl[:], gbc[:], ALU.mult, tl.k + gbc.k, hh.k)
                        for oc in range(KC):
                            py = g.ps[5 + oc % 2]
                            for dc in range(4):
                                k.mm(py[:, :], wd[bi][:, dc, oc * 128:(oc + 1) * 128], hh[:, dc, :],
                                     dc == 0, dc == 3, wd[bi].k + hh.k, [py.k[0]])
                            yk = [yacc.k[oc * NTT + tt]]
                            ya = yacc[:, oc, c0:c0 + 512]
                            if e == 0:
                                k.cp('dve', ya, py[:, :], [py.k[0]], yk)
                            else:
                                k.tt('dve', ya, py[:, :], ya, ALU.add, [py.k[0]] + yk, yk)
                k.barrier()
            with ExitStack() as ex:
                xt = [sb(g, ex, "m_x%d" % i, [128, KC, 512]) for i in range(2)]
                sq = sb(g, ex, "m_sq", [128, KC, 512])
                mean = sb(g, ex, "m_mean", [128, 512])
                rstd = sb(g, ex, "m_rstd", [128, 512])
                ot = [sb(g, ex, "m_ot%d" % i, [128, D]) for i in range(2)] if last else None
                for tt in range(NTT):
                    c0 = tt * 512
                    t0 = tb + c0
                    x = xt[tt % 2]
                    k.dma('sp', x[:], X1v[:, :, t0:t0 + 512], S['X1'].k, x.k)
                    zk = [yacc.k[oc * NTT + tt] for oc in range(KC)]
                    for oc in range(KC):
                        ya = yacc[:, oc, c0:c0 + 512]
                        k.act(ya, ya, AF.Identity, zk, zk, scale=m1[:, 40 + oc:41 + oc])
                        k.stt(ya, x[:, oc, :], ALPHA, ya, ALU.mult, ALU.add, x.k + zk, zk)
                    ln_tile(g, lambda kc: yacc[:, kc, c0:c0 + 512], zk, sq, mean, rstd,
                            lambda kc: g.lng[l][:, 8 + kc:9 + kc], lambda kc: g.lnb[l][:, 8 + kc:9 + kc],
                            lambda kc: x[:, kc, :], x.k, 512)
                    if not last:
                        k.dma('sp', XTv[:, :, t0:t0 + 512], x[:], x.k, S['XT'].k)
                    else:
                        for tg in range(4):
                            o = ot[tg % 2]
                            for half in range(2):
                                ps = g.ps[half]
                                for j in range(4):
                                    kc = half * 4 + j
                                    k.tr(ps[:, j * 128:(j + 1) * 128], x[:, kc, tg * 128:(tg + 1) * 128], g.ident[:],
                                         x.k + g.ident.k, [ps.k[0]])
                                k.cp('act' if half else 'dve', o[:, half * 512:(half + 1) * 512], ps[:, :],
                                     [ps.k[0]], o.k)
                            k.dma('sp', g.out[t0 + tg * 128:t0 + (tg + 1) * 128, :], o[:], o.k, [])
                k.barrier()
        k.barrier()


def head_rms_fm(g, k, pin, outap, outk, sqt, rst, nrm, extra_scale):
    pss = g.ps[7]
    k.act(sqt[:], pin[:, :], AF.Square, [pin.k[0]], sqt.k)
    k.mm(pss[:, :], g.blk[:], sqt[:], True, True, g.blk.k + sqt.k, [pss.k[0]])
    k.ts('dve', rst[:], pss[:, :], 1.0 / 64, QK_EPS, ALU.mult, ALU.add, [pss.k[0]], rst.k)
    k.act(rst[:], rst[:], AF.Sqrt, rst.k, rst.k)
    k.op('dve', lambda e: e.reciprocal(out=rst[:], in_=rst[:]), rst.k, rst.k)
    k.tt('dve', sqt[:], pin[:, :], rst[:], ALU.mult, [pin.k[0]] + rst.k, sqt.k)
    k.ts('pool', outap, sqt[:], nrm[:, 0:1], extra_scale, ALU.mult, ALU.mult, sqt.k + nrm.k, outk)


def phase_kv(g):
    k, I, S, T = g.k, g.I, g.S, g.T
    g.FQ = Buf(g.nc.dram_tensor("FQ", [NH, 2, T], F32, kind="Internal").ap())
    g.FK = Buf(g.nc.dram_tensor("FKn", [NH, 2, T], F32, kind="Internal").ap())
    with ExitStack() as ph:
        wv_ = lambda ap: ap.rearrange("(kc p) n -> p kc n", p=128)
        wk = sb(g, ph, "kv_wk", [128, KC, D], BF16)
        wv = sb(g, ph, "kv_wv", [128, KC, D], BF16)
        wf = sb(g, ph, "kv_wf", [128, KC, NH], BF16)
        k.dma('pool', wk[:], wv_(I['kv_w'][:, 0:D]), [], wk.k)
        k.dma('pool', wv[:], wv_(I['kv_w'][:, D:2 * D]), [], wv.k)
        k.dma('pool', wf[:], wv_(I['kv_w'][:, 2 * D:2 * D + NH]), [], wf.k)
        kn = sb(g, ph, "kv_kn", [128, 1])
        k.dma('sp', kn[0:64, :], I['kv_knorm'], [], kn.k)
        k.dma('sp', kn[64:128, :], I['kv_knorm'], [], kn.k)
        fb = sb(g, ph, "kv_fb", [NH, 1])
        k.dma('sp', fb[:], I['kv_fb'], [], fb.k)
        k.barrier()
        xb = [sb(g, ph, "kv_x%d" % i, [128, KC, 512]) for i in range(2)]
        hk = [sb(g, ph, "kv_h%d" % i, [128, KC, 512], BF16) for i in range(2)]
        kst = [sb(g, ph, "kv_ks%d" % i, [128, KC, 512], BF16) for i in range(2)]
        vst = [sb(g, ph, "kv_vs%d" % i, [128, D], BF16) for i in range(2)]
        sqt = sb(g, ph, "kv_sq", [128, 512])
        rst = sb(g, ph, "kv_rs", [128, 512])
        lf = sb(g, ph, "kv_lf", [NH, 512])
        fc = [sb(g, ph, "kv_fc%d" % i, [NH, 512]) for i in range(2)]
        nfc = [sb(g, ph, "kv_nfc%d" % i, [NH, 512]) for i in range(2)]
        XTv = S['XT'].t.rearrange("(kc p) t -> p kc t", p=128)
        KTv = S['KT'].t.rearrange("(kc p) t -> p kc t", p=128)
        for ti in range(T // 512):
            t0 = ti * 512
            x, h, ks = xb[ti % 2], hk[ti % 2], kst[ti % 2]
            k.dma('sp', x[:], XTv[:, :, t0:t0 + 512], S['XT'].k, x.k)
            for kc in range(KC):
                k.act(h[:, kc, :], x[:, kc, :], AF.Identity, x.k, h.k,
                      scale=g.kvmod1[:, 8 + kc:9 + kc], bias=g.kvmod[:, kc:kc + 1])
            for oc in range(KC):
                p = g.ps[oc % 2]
                for kc in range(KC):
                    k.mm(p[:, :], wk[:, kc, oc * 128:(oc + 1) * 128], h[:, kc, :], kc == 0, kc == KC - 1,
                         wk.k + h.k, [p.k[0]])
                head_rms_fm(g, k, p, ks[:, oc, :], ks.k, sqt, rst, kn, 1.0)
            k.dma('sp', KTv[:, :, t0:t0 + 512], ks[:], ks.k, S['KT'].k)
            for tg in range(4):
                vs = vst[tg % 2]
                for half in range(2):
                    p = g.ps[2 + half]
                    for kc in range(KC):
                        k.mm(p[:, :], h[:, kc, tg * 128:(tg + 1) * 128], wv[:, kc, half * 512:(half + 1) * 512],
                             kc == 0, kc == KC - 1, wv.k + h.k, [p.k[0]])
                    k.cp('act' if half else 'dve', vs[:, half * 512:(half + 1) * 512], p[:, :], [p.k[0]], vs.k)
                k.dma('sp', S['VK'].t[t0 + tg * 128:t0 + (tg + 1) * 128, :], vs[:], vs.k, S['VK'].k)
            p = g.ps[4]
            for kc in range(KC):
                k.mm(p[0:NH, :], wf[:, kc, :], h[:, kc, :], kc == 0, kc == KC - 1, wf.k + h.k, [p.k[0]])
            k.act(lf[:], p[0:NH, :], AF.Sigmoid, [p.k[0]], lf.k, bias=fb[:, 0:1])
            k.act(lf[:], lf[:], AF.Ln, lf.k, lf.k)
            f, fp_, nf = fc[ti % 2], fc[(ti + 1) % 2], nfc[ti % 2]
            init = 0.0 if ti == 0 else fp_[:, 511:512]
            k.op('dve', lambda e, f=f, init=init: e.tensor_tensor_scan(
                out=f[:], data0=g.ones[0:NH, 0:512], data1=lf[:], initial=init, op0=ALU.mult, op1=ALU.add),
                g.ones.k + lf.k + fp_.k, f.k)
            k.ts('pool', nf[:], f[:], -1.0, None, ALU.mult, None, f.k, nf.k)
            k.dma('sp', g.FQ.t[:, 0, t0:t0 + 512], f[:], f.k, g.FQ.k)
            k.dma('sp', g.FQ.t[:, 1, t0:t0 + 512], g.ones[0:NH, 0:512], g.ones.k, g.FQ.k)
            k.dma('sp', g.FK.t[:, 0, t0:t0 + 512], g.ones[0:NH, 0:512], g.ones.k, g.FK.k)
            k.dma('sp', g.FK.t[:, 1, t0:t0 + 512], nf[:], nf.k, g.FK.k)
        k.barrier()


def fox_mixer(g, l, j):
    k, I, S, T = g.k, g.I, g.S, g.T
    mt, m1 = g.modT[l], g.mod1[l]
    XTv = S['XT'].t.rearrange("(kc p) t -> p kc t", p=128)
    QTv = S['QT'].t.rearrange("(kc p) t -> p kc t", p=128)
    SGv = S['SG'].t.rearrange("(kc p) t -> p kc t", p=128)
    with ExitStack() as ph:
        wv_ = lambda ap: ap.rearrange("(kc p) n -> p kc n", p=128)
        wq = sb(g, ph, "f_wq", [128, KC, D], BF16)
        wg = sb(g, ph, "f_wg", [128, KC, D], BF16)
        k.dma('pool', wq[:], wv_(I['fx_wqg'][j][:, 0:D]), [], wq.k)
        k.dma('pool', wg[:], wv_(I['fx_wqg'][j][:, D:2 * D]), [], wg.k)
        qn = sb(g, ph, "f_qn", [128, 1])
        k.dma('sp', qn[0:64, :], I['fx_qnorm'][j], [], qn.k)
        k.dma('sp', qn[64:128, :], I['fx_qnorm'][j], [], qn.k)
        k.barrier()
        xb = [sb(g, ph, "f_x%d" % i, [128, KC, 512]) for i in range(2)]
        hb = [sb(g, ph, "f_h%d" % i, [128, KC, 512], BF16) for i in range(2)]
        qst = [sb(g, ph, "f_qs%d" % i, [128, KC, 512], BF16) for i in range(2)]
        gst = [sb(g, ph, "f_gs%d" % i, [128, KC, 512], BF16) for i in range(2)]
        sqt = sb(g, ph, "f_sq", [128, 512])
        rst = sb(g, ph, "f_rs", [128, 512])
        for ti in range(T // 512):
            t0 = ti * 512
            x, h, qs, gs = xb[ti % 2], hb[ti % 2], qst[ti % 2], gst[ti % 2]
            k.dma('sp', x[:], XTv[:, :, t0:t0 + 512], S['XT'].k, x.k)
            for kc in range(KC):
                k.act(h[:, kc, :], x[:, kc, :], AF.Identity, x.k, h.k,
                      scale=m1[:, 8 + kc:9 + kc], bias=mt[:, kc:kc + 1])
            for oc in range(KC):
                p = g.ps[oc % 2]
                for kc in range(KC):
                    k.mm(p[:, :], wq[:, kc, oc * 128:(oc + 1) * 128], h[:, kc, :], kc == 0, kc == KC - 1,
                         wq.k + h.k, [p.k[0]])
                head_rms_fm(g, k, p, qs[:, oc, :], qs.k, sqt, rst, qn, HD ** -0.5)
                p2 = g.ps[2 + oc % 2]
                for kc in range(KC):
                    k.mm(p2[:, :], wg[:, kc, oc * 128:(oc + 1) * 128], h[:, kc, :], kc == 0, kc == KC - 1,
                         wg.k + h.k, [p2.k[0]])
                k.act(gs[:, oc, :], p2[:, :], AF.Sigmoid, [p2.k[0]], gs.k)
            k.dma('sp', QTv[:, :, t0:t0 + 512], qs[:], qs.k, S['QT'].k)
            k.dma('sp', SGv[:, :, t0:t0 + 512], gs[:], gs.k, S['SG'].k)
        k.barrier()
    with ExitStack() as ph:
        identb = sb(g, ph, "a_identb", [128, 128], BF16)
        k.cp('pool', identb[:], g.ident[:], g.ident.k, identb.k)
        zer = sb(g, ph, "a_zer", [128, 128])
        k.op('pool', lambda e: e.memset(zer[:], 0.0), [], zer.k)
        nmask = sb(g, ph, "a_nmask", [128, 128], BF16)
        k.op('pool', lambda e: e.affine_select(out=nmask[:], in_=zer[:], pattern=[[1, 128]], compare_op=ALU.is_ge,
                                               fill=-30000.0, base=0, channel_multiplier=-1), zer.k, nmask.k)
        onesb = sb(g, ph, "a_onesb", [128, 64], BF16)
        k.op('pool', lambda e: e.memset(onesb[:], 1.0), [], onesb.k)
        k.barrier()
        NTG = T // 128
        KTh = [sb(g, ph, "a_k%d" % i, [64, T], BF16) for i in range(2)]
        QTh = [sb(g, ph, "a_q%d" % i, [64, T], BF16) for i in range(2)]
        SGh = [sb(g, ph, "a_g%d" % i, [64, T], BF16) for i in range(2)]
        Vh = [sb(g, ph, "a_v%d" % i, [128, NTG, 64], BF16) for i in range(2)]
        FQh = [sb(g, ph, "a_fq%d" % i, [2, T]) for i in range(2)]
        FKh = [sb(g, ph, "a_fk%d" % i, [2, T]) for i in range(2)]
        Pb = [sb(g, ph, "a_p%d" % i, [128, 512], BF16) for i in range(3)]
        rl = sb(g, ph, "a_rl", [64, 512])
        of = sb(g, ph, "a_of", [64, 512])
        ost = [sb(g, ph, "a_os%d" % i, [64, 512], BF16) for i in range(2)]
        it = 0
        for hd in range(NH):
            b = hd % 2
            hr = slice(hd * 64, (hd + 1) * 64)
            k.dma('sp', KTh[b][:], S['KT'].t[hr, :], S['KT'].k, KTh[b].k)
            k.dma('sp', QTh[b][:], S['QT'].t[hr, :], S['QT'].k, QTh[b].k)
            k.dma('sp', SGh[b][:], S['SG'].t[hr, :], S['SG'].k, SGh[b].k)
            k.dma('sp', Vh[b][:], S['VK'].t[:, hr].rearrange("(tg p) d -> p tg d", p=128), S['VK'].k, Vh[b].k)
            k.dma('sp', FQh[b][:], g.FQ.t[hd], g.FQ.k, FQh[b].k)
            k.dma('sp', FKh[b][:], g.FK.t[hd], g.FK.k, FKh[b].k)
            for qg in range(T // 512):
                q0 = qg * 512
                Op, Lp = g.ps[2 + 2 * (qg % 2)], g.ps[3 + 2 * (qg % 2)]
                kts = list(range(4 * qg + 4))
                for idx, kt in enumerate(kts):
                    diag = kt >= 4 * qg
                    qlo = (kt - 4 * qg) * 128 if diag else 0
                    n = 512 - qlo
                    Sp = g.ps[idx % 2]
                    kc_ = slice(kt * 128, (kt + 1) * 128)
                    qc_ = slice(q0 + qlo, q0 + 512)
                    k.mm(Sp[:, 0:n], KTh[b][:, kc_], QTh[b][:, qc_], True, False, KTh[b].k + QTh[b].k, [Sp.k[0]])
                    k.mm(Sp[:, 0:n], FKh[b][:, kc_], FQh[b][:, qc_], False, not diag, FKh[b].k + FQh[b].k, [Sp.k[0]])
                    if diag:
                        k.mm(Sp[:, 0:128], identb[:], nmask[:], False, True, identb.k + nmask.k, [Sp.k[0]])
                    P = Pb[it % 3]
                    it += 1
                    k.act(P[:, 0:n], Sp[:, 0:n], AF.Exp, [Sp.k[0]], P.k)
                    first, lastf = idx == 0, idx == len(kts) - 1
                    k.mm(Op[0:64, qlo:512], Vh[b][:, kt, :], P[:, 0:n], first, lastf, Vh[b].k + P.k, [Op.k[0]])
                    k.mm(Lp[0:64, qlo:512], onesb[:], P[:, 0:n], first, lastf, onesb.k + P.k, [Lp.k[0]])
                k.op('dve', lambda e, Lp=Lp: e.reciprocal(out=rl[:], in_=Lp[0:64, :]), [Lp.k[0]], rl.k)
                k.tt('dve', of[:], Op[0:64, :], rl[:], ALU.mult, [Op.k[0]] + rl.k, of.k)
                o = ost[qg % 2]
                k.tt('pool', o[:], of[:], SGh[b][:, q0:q0 + 512], ALU.mult, of.k + SGh[b].k, o.k)
                k.dma('sp', S['MO'].t[hr, q0:q0 + 512], o[:], o.k, S['MO'].k)
        k.barrier()


def setup_rwkv_consts(g, es):
    k = g.k
    ones = g.ones
    su = sb(g, es, "c_su", [128, 128])
    iu = sb(g, es, "c_iu", [128, 128])
    g.mask4 = sb(g, es, "c_mask4", [128, 512])
    g.sl = sb(g, es, "c_sl", [128, 128])
    g.rm = sb(g, es, "c_rm", [128, 256])
    k.op('pool', lambda e: e.affine_select(out=su[:], in_=ones[:, 0:128], pattern=[[1, 128]], compare_op=ALU.is_gt,
                                           fill=0.0, base=0, channel_multiplier=-1), ones.k, su.k)
    k.op('pool', lambda e: e.affine_select(out=iu[:], in_=ones[:, 0:128], pattern=[[1, 128]], compare_op=ALU.is_ge,
                                           fill=0.0, base=0, channel_multiplier=-1), ones.k, iu.k)
    k.op('pool', lambda e: e.affine_select(out=g.sl[:], in_=ones[:, 0:128], pattern=[[-1, 128]], compare_op=ALU.is_gt,
                                           fill=0.0, base=0, channel_multiplier=1), ones.k, g.sl.k)
    k.tt('pool', g.sl[:], g.sl[:], g.blk[:], ALU.mult, g.sl.k + g.blk.k, g.sl.k)
    for q in range(4):
        src = su if q < 2 else iu
        k.tt('pool', g.mask4[:, q * 128:(q + 1) * 128], src[:], g.blk[:], ALU.mult, src.k + g.blk.k, g.mask4.k)
    k.op('pool', lambda e: e.memset(g.rm[:], 1.0), [], g.rm.k)
    for q in range(4):
        k.op('pool', lambda e, q=q: e.memset(g.rm[:, q * 64:q * 64 + 1], 0.0), [], g.rm.k)
    k.barrier()


def rwkv_mixer(g, l):
    k, I, S, T = g.k, g.I, g.S, g.T
    mt, m1 = g.modT[l], g.mod1[l]
    W = 256
    ps = g.ps
    with ExitStack() as ph:
        setup_rwkv_consts(g, ph)
        wv_ = lambda ap: ap.rearrange("(kc p) n -> p kc n", p=128)
        wr_ = sb(g, ph, "r_wr", [128, KC, D], BF16)
        wk_ = sb(g, ph, "r_wk", [128, KC, D], BF16)
        wvv = sb(g, ph, "r_wv", [128, KC, D], BF16)
        k.dma('pool', wr_[:], wv_(I['rw_rkv'][l, 0]), [], wr_.k)
        k.dma('pool', wk_[:], wv_(I['rw_rkv'][l, 1]), [], wk_.k)
        k.dma('pool', wvv[:], wv_(I['rw_rkv'][l, 2]), [], wvv.k)
        w1 = sb(g, ph, "r_w1", [128, KC, 64], BF16)
        a1 = sb(g, ph, "r_a1", [128, KC, 64], BF16)
        g1 = sb(g, ph, "r_g1", [128, KC, 160], BF16)
        k.dma('pool', w1[:], wv_(I['rw_w1'][l]), [], w1.k)
        k.dma('pool', a1[:], wv_(I['rw_a1'][l]), [], a1.k)
        k.dma('pool', g1[:], wv_(I['rw_g1'][l]), [], g1.k)
        w2 = sb(g, ph, "r_w2", [64, D], BF16)
        a2 = sb(g, ph, "r_a2", [64, D], BF16)
        g2 = sb(g, ph, "r_g2", [128, 2, D], BF16)
        k.dma('pool', w2[:], I['rw_w2'][l], [], w2.k)
        k.dma('pool', a2[:], I['rw_a2'][l], [], a2.k)
        k.dma('pool', g2[:, 0, :], I['rw_g2'][l, 0:128, :], [], g2.k)
        k.dma('pool', g2[0:32, 1, :], I['rw_g2'][l, 128:160, :], [], g2.k)
        if l > 0:
            v1 = sb(g, ph, "r_v1", [128, KC, 32], BF16)
            v2 = sb(g, ph, "r_v2", [32, D], BF16)
            k.dma('pool', v1[:], wv_(I['rw_v1'][l - 1]), [], v1.k)
            k.dma('pool', v2[:], I['rw_v2'][l - 1], [], v2.k)
        st = sb(g, ph, "r_lvst", [128, 128])
        pv = {}
        for nm, R in [('rw_mu', 48), ('rw_w0', 8), ('rw_a0', 8), ('rw_kk', 8), ('rw_ka', 8), ('rw_rk', 8),
                      ('rw_gn_g', 8), ('rw_gn_b', 8)]:
            pv[nm] = sb(g, ph, "r_p_" + nm, [128, R])
            load_vecT(g, pv[nm], I[nm][l], R, st)
        if l > 0:
            pv['rw_v0'] = sb(g, ph, "r_p_v0", [128, 8])
            load_vecT(g, pv['rw_v0'], I['rw_v0'][l - 1], 8, st)
        hm = sb(g, ph, "r_hm", [128, 2])
        k.cp('pool', hm[:, 0:1], g.blk[:, 0:1], g.blk.k, hm.k)
        k.cp('pool', hm[:, 1:2], g.blk[:, 127:128], g.blk.k, hm.k)
        Bth = [sb(g, ph, "r_Bth%d" % i, [128, W]) for i in range(2)]
        Kth = [sb(g, ph, "r_Kth%d" % i, [128, W]) for i in range(2)]
        Ath = [sb(g, ph, "r_Ath%d" % i, [128, W]) for i in range(2)]
        VTc = [sb(g, ph, "r_VTc%d" % i, [128, 128]) for i in range(2)]
        VTm = [sb(g, ph, "r_VTm%d" % i, [128, 128]) for i in range(2)]
        cmk = [sb(g, ph, "r_cmk%d" % i, [128, 128]) for i in range(2)]
        Upad = [sb(g, ph, "r_Upad%d" % i, [128, 128]) for i in range(2)]
        Spad = [sb(g, ph, "r_Spad%d" % i, [128, 128]) for i in range(2)]
        for i in range(2):
            k.op('pool', lambda e, i=i: e.memset(cmk[i][:], 0.0), [], cmk[i].k)
            k.op('pool', lambda e, i=i: e.memset(cmk[i][:, i * 64:(i + 1) * 64], 1.0), [], cmk[i].k)
            k.op('pool', lambda e, i=i: e.memset(Upad[i][:], 0.0), [], Upad[i].k)
            k.op('pool', lambda e, i=i: e.memset(Spad[i][:], 0.0), [], Spad[i].k)
        omka = sb(g, ph, "r_omka", [128, 8])
        k.ts('dve', omka[:], pv['rw_ka'][:], -1.0, 1.0, ALU.mult, ALU.add, pv['rw_ka'].k, omka.k)
        k.barrier()
        state = [sb(g, ph, "r_state%d" % i, [128, 8, 64], F32, n=8) for i in range(2)]
        k.op('pool', lambda e: e.memset(state[0][:], 0.0), [], state[0].k)
        spar = [0] * 8
        xb = sb(g, ph, "r_x", [128, KC, W])
        hbuf = [sb(g, ph, "r_h%d" % i, [128, KC, W + 1]) for i in range(2)]
        k.op('pool', lambda e: e.memset(hbuf[1][:, :, W:W + 1], 0.0), [], hbuf[1].k)
        xx = sb(g, ph, "r_xx", [128, KC, W])
        xm = [sb(g, ph, "r_xm%d" % i, [128, KC, W], BF16) for i in range(6)]
        tw = sb(g, ph, "r_tw", [64, W], BF16)
        ta = sb(g, ph, "r_ta", [64, W], BF16)
        tg0 = sb(g, ph, "r_tg0", [128, W], BF16)
        tg1 = sb(g, ph, "r_tg1", [32, W], BF16)
        tv = sb(g, ph, "r_tv", [32, W], BF16)
        names = ['r', 'k', 'v', 'wl', 'sa', 'g', 'kk', 't1', 'rs', 'kkn', 'kf', 'b', 'c', 'd', 'eg', 'em', 'ed', 'ep',
                 'Rt', 'At', 'Kt', 'Bt', 'Kh', 'Bh', 'y', 'y2', 'mn', 'bon', 'vf']
        t = {n: sb(g, ph, "r_t_" + n, [128, W]) for n in names}
        gl_ = sb(g, ph, "r_gl", [128, 4])
        KhT = sb(g, ph, "r_KhT", [128, 2, 128])
        BhT = sb(g, ph, "r_BhT", [128, 2, 128])
        VT = sb(g, ph, "r_VT", [128, 2, 128])
        Msb = [sb(g, ph, "r_M%d" % i, [128, 512]) for i in range(2)]
        Nb = [sb(g, ph, "r_N%d" % i, [128, 128]) for i in range(2)]
        Ntb = [sb(g, ph, "r_Nt%d" % i, [128, 128]) for i in range(2)]
        PT = [sb(g, ph, "r_PT%d" % i, [128, 128]) for i in range(2)]
        Xs = sb(g, ph, "r_Xs", [128, 64], F32)
        mob = sb(g, ph, "r_mo", [128, W], BF16)
        XTv = S['XT'].t.rearrange("(kc p) t -> p kc t", p=128)
        unit_i = 0
        for ti in range(T // W):
            t0 = ti * W
            h, hp_ = hbuf[ti % 2], hbuf[(ti + 1) % 2]
            k.dma('sp', xb[:], XTv[:, :, t0:t0 + W], S['XT'].k, xb.k)
            for kc in range(KC):
                k.act(h[:, kc, 1:W + 1], xb[:, kc, :], AF.Identity, xb.k, h.k,
                      scale=m1[:, 8 + kc:9 + kc], bias=mt[:, kc:kc + 1])
            k.cp('pool', h[:, :, 0:1], hp_[:, :, W:W + 1], hp_.k, h.k)
            k.tt('dve', xx[:], h[:, :, 0:W], h[:, :, 1:W + 1], ALU.subtract, h.k, xx.k)
            for i in range(6):
                if i == 3 and False:
                    continue
                for kc in range(KC):
                    k.stt(xm[i][:, kc, :], xx[:, kc, :], pv['rw_mu'][:, i * 8 + kc:i * 8 + kc + 1], h[:, kc, 1:W + 1],
                          ALU.mult, ALU.add, xx.k + h.k, xm[i].k)
            p = ps[3]
            for kc in range(KC):
                k.mm(p[0:64, 0:W], w1[:, kc, :], xm[1][:, kc, :], kc == 0, kc == KC - 1, w1.k + xm[1].k, [p.k[0]])
            k.act(tw[:], p[0:64, 0:W], AF.Tanh, [p.k[0]], tw.k)
            for kc in range(KC):
                k.mm(p[0:64, W:2 * W], a1[:, kc, :], xm[4][:, kc, :], kc == 0, kc == KC - 1, a1.k + xm[4].k, [p.k[2]])
            k.cp('act', ta[:], p[0:64, W:2 * W], [p.k[2]], ta.k)
            p = ps[2]
            for kc in range(KC):
                k.mm(p[:, 0:W], g1[:, kc, 0:128], xm[5][:, kc, :], kc == 0, kc == KC - 1, g1.k + xm[5].k, [p.k[0]])
            k.act(tg0[:], p[:, 0:W], AF.Sigmoid, [p.k[0]], tg0.k)
            for kc in range(KC):
                k.mm(p[0:32, W:2 * W], g1[:, kc, 128:160], xm[5][:, kc, :], kc == 0, kc == KC - 1, g1.k + xm[5].k,
                     [p.k[2]])
            k.act(tg1[:], p[0:32, W:2 * W], AF.Sigmoid, [p.k[2]], tg1.k)
            if l > 0:
                p = ps[1]
                for kc in range(KC):
                    k.mm(p[0:32, 0:W], v1[:, kc, :], xm[3][:, kc, :], kc == 0, kc == KC - 1, v1.k + xm[3].k, [p.k[0]])
                k.cp('act', tv[:], p[0:32, 0:W], [p.k[0]], tv.k)
            for hp in range(8):
                if RW_STOP[0] <= 1:
                    break
                oc = hp
                ocs = slice(oc * 128, (oc + 1) * 128)
                col = lambda buf, j: buf[:, j:j + 1]

                def proj(pb, c0, kk_, wt, xi):
                    for kc in range(KC):
                        k.mm(pb[:, c0:c0 + W], wt[:, kc, ocs], xm[xi][:, kc, :], kc == 0, kc == KC - 1,
                             wt.k + xm[xi].k, [pb.k[kk_]])
                proj(ps[0], 0, 0, wr_, 0)
                proj(ps[0], W, 2, wk_, 2)
                proj(ps[1], 0, 0, wvv, 3)
                k.mm(ps[1][:, W:2 * W], w2[:, ocs], tw[:], True, True, w2.k + tw.k, [ps[1].k[2]])
                k.mm(ps[2][:, 0:W], a2[:, ocs], ta[:], True, True, a2.k + ta.k, [ps[2].k[0]])
                k.mm(ps[2][:, W:2 * W], g2[:, 0, ocs], tg0[:], True, False, g2.k + tg0.k, [ps[2].k[2]])
                k.mm(ps[2][:, W:2 * W], g2[0:32, 1, ocs], tg1[:], False, True, g2.k + tg1.k, [ps[2].k[2]])
                if l > 0:
                    k.mm(ps[3][:, 0:W], v2[:, ocs], tv[:], True, True, v2.k + tv.k, [ps[3].k[0]])
                k.cp('act', t['r'][:], ps[0][:, 0:W], [ps[0].k[0]], t['r'].k)
                k.cp('act', t['k'][:], ps[0][:, W:2 * W], [ps[0].k[2]], t['k'].k)
                k.cp('dve', t['v'][:], ps[1][:, 0:W], [ps[1].k[0]], t['v'].k)
                k.cp('act', t['g'][:], ps[2][:, W:2 * W], [ps[2].k[2]], t['g'].k)
                VFv = S['VF'].t[ocs, t0:t0 + W]
                if l == 0:
                    k.dma('sp', VFv, t['v'][:], t['v'].k, S['VF'].k)
                else:
                    k.dma('sp', t['vf'][:], VFv, S['VF'].k, t['vf'].k)
                    k.act(t['y2'][:], ps[3][:, 0:W], AF.Sigmoid, [ps[3].k[0]], t['y2'].k, bias=col(pv['rw_v0'], oc))
                    k.tt('pool', t['vf'][:], t['vf'][:], t['v'][:], ALU.subtract, t['vf'].k + t['v'].k, t['vf'].k)
                    k.tt('pool', t['vf'][:], t['vf'][:], t['y2'][:], ALU.mult, t['vf'].k + t['y2'].k, t['vf'].k)
                    k.tt('pool', t['v'][:], t['v'][:], t['vf'][:], ALU.add, t['vf'].k + t['v'].k, t['v'].k)
                k.act(t['wl'][:], ps[1][:, W:2 * W], AF.Sigmoid, [ps[1].k[2]], t['wl'].k, bias=col(pv['rw_w0'], oc))
                k.act(t['sa'][:], ps[2][:, 0:W], AF.Sigmoid, [ps[2].k[0]], t['sa'].k, bias=col(pv['rw_a0'], oc))
                k.ts('pool', t['wl'][:], t['wl'][:], -0.6065306597126334, None, ALU.mult, None, t['wl'].k, t['wl'].k)
                k.ts('dve', t['kk'][:], t['k'][:], col(pv['rw_kk'], oc), None, ALU.mult, None, t['k'].k, t['kk'].k)
                k.tt('pool', t['t1'][:], t['kk'][:], t['kk'][:], ALU.mult, t['kk'].k, t['t1'].k)
                k.mm(ps[3][:, W:2 * W], g.blk[:], t['t1'][:], True, True, g.blk.k + t['t1'].k, [ps[3].k[2]])
                k.act(t['rs'][:], ps[3][:, W:2 * W], AF.Sqrt, [ps[3].k[2]], t['rs'].k)
                k.ts('dve', t['rs'][:], t['rs'][:], 1e-12, None, ALU.max, None, t['rs'].k, t['rs'].k)
                k.op('dve', lambda e: e.reciprocal(out=t['rs'][:], in_=t['rs'][:]), t['rs'].k, t['rs'].k)
                k.tt('dve', t['kkn'][:], t['kk'][:], t['rs'][:], ALU.mult, t['kk'].k + t['rs'].k, t['kkn'].k)
                k.ts('dve', t['t1'][:], t['sa'][:], col(pv['rw_ka'], oc), col(omka, oc), ALU.mult, ALU.add,
                     t['sa'].k, t['t1'].k)
                k.tt('pool', t['kf'][:], t['k'][:], t['t1'][:], ALU.mult, t['k'].k + t['t1'].k, t['kf'].k)
                k.tt('pool', t['b'][:], t['kkn'][:], t['sa'][:], ALU.mult, t['kkn'].k + t['sa'].k, t['b'].k)
                k.op('dve', lambda e: e.tensor_tensor_scan(out=t['c'][:], data0=g.rm[:], data1=t['wl'][:], initial=0.0,
                                                           op0=ALU.mult, op1=ALU.add),
                     g.rm.k + t['wl'].k, t['c'].k)
                for j in range(4):
                    cs_ = slice(j * 64, (j + 1) * 64)
                    k.ts('dve', t['d'][:, cs_], t['c'][:, cs_], -1.0, t['c'][:, j * 64 + 63:j * 64 + 64],
                         ALU.mult, ALU.add, t['c'].k, t['d'].k)
                k.tt('pool', t['ep'][:], t['c'][:], t['wl'][:], ALU.subtract, t['c'].k + t['wl'].k, t['ep'].k)
                k.act(t['eg'][:], t['c'][:], AF.Exp, t['c'].k, t['eg'].k)
                k.act(t['em'][:], t['c'][:], AF.Exp, t['c'].k, t['em'].k, scale=-1.0)
                k.act(t['ed'][:], t['d'][:], AF.Exp, t['d'].k, t['ed'].k)
                k.act(t['ep'][:], t['ep'][:], AF.Exp, t['ep'].k, t['ep'].k)
                k.act(gl_[:], t['c'][:, :].rearrange("p (j t) -> p j t", t=64)[:, :, 63], AF.Exp, t['c'].k, gl_.k)
                k.tt('dve', t['Rt'][:], t['r'][:], t['eg'][:], ALU.mult, t['r'].k + t['eg'].k, t['Rt'].k)
                k.stt(t['At'][:], t['kkn'][:], -1.0, t['ep'][:], ALU.mult, ALU.mult, t['kkn'].k + t['ep'].k, t['At'].k)
                k.tt('pool', t['Kt'][:], t['kf'][:], t['em'][:], ALU.mult, t['kf'].k + t['em'].k, t['Kt'].k)
                k.tt('pool', t['Bt'][:], t['b'][:], t['em'][:], ALU.mult, t['b'].k + t['em'].k, t['Bt'].k)
                k.tt('dve', t['Kh'][:], t['kf'][:], t['ed'][:], ALU.mult, t['kf'].k + t['ed'].k, t['Kh'].k)
                k.tt('pool', t['Bh'][:], t['b'][:], t['ed'][:], ALU.mult, t['b'].k + t['ed'].k, t['Bh'].k)
                if RW_STOP[0] <= 2:
                    continue
                for hh in range(2):
                    k.ts('pool', Bth[hh][:], t['Bt'][:], hm[:, hh:hh + 1], None, ALU.mult, None, t['Bt'].k + hm.k, Bth[hh].k)
                    k.ts('pool', Kth[hh][:], t['Kt'][:], hm[:, hh:hh + 1], None, ALU.mult, None, t['Kt'].k + hm.k, Kth[hh].k)
                    k.ts('dve', Ath[hh][:], t['At'][:], hm[:, hh:hh + 1], None, ALU.mult, None, t['At'].k + hm.k, Ath[hh].k)
                for cp in range(2):
                    cc = slice(cp * 128, (cp + 1) * 128)
                    k.tr(ps[6][:, cp * 128:(cp + 1) * 128], t['Kh'][:, cc], g.ident[:], t['Kh'].k + g.ident.k, [ps[6].k[0]])
                    k.tr(ps[6][:, 256 + cp * 128:256 + (cp + 1) * 128], t['Bh'][:, cc], g.ident[:],
                         t['Bh'].k + g.ident.k, [ps[6].k[0]])
                    k.tr(ps[7][:, 256 + cp * 128:256 + (cp + 1) * 128], t['v'][:, cc], g.ident[:],
                         t['v'].k + g.ident.k, [ps[7].k[3]])
                k.cp('act', KhT[:], ps[6][:, 0:256].rearrange("p (a b) -> p a b", b=128), [ps[6].k[0]], KhT.k)
                k.cp('dve', BhT[:], ps[6][:, 256:512].rearrange("p (a b) -> p a b", b=128), [ps[6].k[0]], BhT.k)
                k.cp('act', VT[:], ps[7][:, 256:512].rearrange("p (a b) -> p a b", b=128), [ps[7].k[3]], VT.k)
                Yp = ps[0]
                for cp in range(2):
                    if RW_STOP[0] == 3 and RW_SUB[0] == 0:
                        break
                    cc = slice(cp * 128, (cp + 1) * 128)
                    units = []
                    for hh in range(2):
                        hb = slice(hh * 64, (hh + 1) * 64)
                        M, Mp = Msb[hh], ps[4]
                        rd = Bth[hh].k + t['At'].k + Kth[hh].k + t['Rt'].k
                        k.mm(Mp[:, 0:128], Bth[hh][:, cc], t['At'][:, cc], True, True, rd, [Mp.k[0]])
                        k.mm(Mp[:, 128:256], Kth[hh][:, cc], t['At'][:, cc], True, True, rd, [Mp.k[0]])
                        k.mm(Mp[:, 256:384], Bth[hh][:, cc], t['Rt'][:, cc], True, True, rd, [Mp.k[0]])
                        k.mm(Mp[:, 384:512], Kth[hh][:, cc], t['Rt'][:, cc], True, True, rd, [Mp.k[0]])
                        k.mm(ps[5][:, 0:128], t['At'][:, cc], Bth[hh][:, cc], True, True, rd, [ps[5].k[0]])
                        k.tt('dve', M[:], Mp[:, :], g.mask4[:], ALU.mult, [Mp.k[0]] + g.mask4.k, M.k)
                        N0, Nt0 = Nb[0], Ntb[0]
                        k.tt('dve', N0[:], ps[5][:, 0:128], g.sl[:], ALU.mult, [ps[5].k[0]] + g.sl.k, N0.k)
                        k.cp('pool', Nt0[:], M[:, 0:128], M.k, Nt0.k)
                        P = PT[hh]
                        k.tt('pool', P[:], M[:, 0:128], g.ident[:], ALU.add, M.k + g.ident.k, P.k)
                        for lev in range(1, 6):
                            if RW_STOP[0] == 3 and RW_SUB[0] == 1:
                                break
                            Np, Ntp = Nb[(lev - 1) % 2], Ntb[(lev - 1) % 2]
                            Nn, Ntn = Nb[lev % 2], Ntb[lev % 2]
                            s1, s2, s3 = ps[5][:, 128:256], ps[5][:, 256:384], ps[5][:, 384:512]
                            k.mm(s1, Ntp[:], Np[:], True, True, Ntp.k + Np.k, [ps[5].k[1]])
                            if lev < 5:
                                k.mm(s2, Np[:], Ntp[:], True, True, Ntp.k + Np.k, [ps[5].k[2]])
                            k.cp('act', Nn[:], s1, [ps[5].k[1]], Nn.k)
                            if lev < 5:
                                k.cp('dve', Ntn[:], s2, [ps[5].k[2]], Ntn.k)
                            k.mm(s3, Nn[:], P[:], True, True, Nn.k + P.k, [ps[5].k[3]])
                            k.tt('dve', P[:], s3, P[:], ALU.add, [ps[5].k[3]] + P.k, P.k)
                    for q in range(2):
                        k.ts('pool', VTc[q][:], VT[:, cp, :], hm[:, q:q + 1], None, ALU.mult, None, VT.k + hm.k, VTc[q].k)
                        k.tt('pool', VTm[q][:], VT[:, cp, :], cmk[q][:], ALU.mult, VT.k + cmk[q].k, VTm[q].k)
                    for ch in range(2):
                        if RW_STOP[0] <= 3:
                            break
                        j = cp * 2 + ch
                        c64 = slice(j * 64, (j + 1) * 64)
                        so, sn = state[spar[hp]], state[1 - spar[hp]]
                        sk = [so.k[hp]]
                        for hh in range(2):
                            hb = slice(hh * 64, (hh + 1) * 64)
                            hcol = slice(hh * 64, (hh + 1) * 64)
                            M, P = Msb[hh], PT[hh]
                            X_, U_, T_ = ps[7][:, 0:64], ps[7][:, 64:128], ps[7][:, 128:192]
                            k.mm(X_, Ath[hh][:, cc], so[:, hp, :], True, False, Ath[hh].k + sk, [ps[7].k[0]])
                            k.mm(X_, M[:, 128:256], VT[:, cp, hcol], False, True, M.k + VT.k, [ps[7].k[0]])
                            k.cp('act', Xs[:, :], X_, [ps[7].k[0]], Xs.k)
                            k.mm(U_, P[:, :], Xs[:, :], True, True, P.k + Xs.k, [ps[7].k[0]])
                            k.ts('dve', Upad[hh][:, hcol], U_, hm[:, ch:ch + 1], None, ALU.mult, None,
                                 [ps[7].k[0]] + hm.k, Upad[hh].k)
                            k.ts('pool', Spad[hh][:, hcol], so[:, hp, :], hm[:, hh:hh + 1], None, ALU.mult, None,
                                 sk + hm.k, Spad[hh].k)
                            k.mm(T_, BhT[:, cp, :], Upad[hh][:, hcol], True, False, BhT.k + Upad[hh].k, [ps[7].k[0]])
                            k.mm(T_, KhT[:, cp, :], VTc[ch][:, hcol], False, True, KhT.k + VTc[ch].k, [ps[7].k[0]])
                            k.stt(sn[hb, hp, :], so[hb, hp, :], gl_[hb, j:j + 1], ps[7][hb, 128:192], ALU.mult, ALU.add,
                                  sk + gl_.k + [ps[7].k[0]], [sn.k[hp]])
                        Y_ = Yp[:, c64]
                        for hh in range(2):
                            M = Msb[hh]
                            k.mm(Y_, Spad[hh][:], t['Rt'][:, c64], hh == 0, False, Spad[hh].k + t['Rt'].k, [Yp.k[0]])
                            k.mm(Y_, Upad[hh][:], M[:, 256 + ch * 64:256 + (ch + 1) * 64], False, False,
                                 Upad[hh].k + M.k, [Yp.k[0]])
                            k.mm(Y_, VTm[hh][:], M[:, 384 + ch * 64:384 + (ch + 1) * 64], False, hh == 1,
                                 VTm[hh].k + M.k, [Yp.k[0]])
                        spar[hp] = 1 - spar[hp]
                if RW_STOP[0] <= 4:
                    continue
                k.cp('act', t['y'][:], Yp[:, 0:W], [Yp.k[0]], t['y'].k)
                k.tt('pool', t['y2'][:], t['y'][:], t['y'][:], ALU.mult, t['y'].k, t['y2'].k)
                k.mm(ps[1][:, 0:W], g.blk[:], t['y'][:], True, True, g.blk.k + t['y'].k, [ps[1].k[0]])
                k.mm(ps[1][:, W:2 * W], g.blk[:], t['y2'][:], True, True, g.blk.k + t['y2'].k, [ps[1].k[2]])
                k.ts('dve', t['mn'][:], ps[1][:, 0:W], 1.0 / 64, None, ALU.mult, None, [ps[1].k[0]], t['mn'].k)
                k.tt('pool', t['y2'][:], t['mn'][:], t['mn'][:], ALU.mult, t['mn'].k, t['y2'].k)
                k.stt(t['y2'][:], ps[1][:, W:2 * W], 1.0 / 64, t['y2'][:], ALU.mult, ALU.subtract,
                      [ps[1].k[2]] + t['y2'].k, t['y2'].k)
                k.ts('dve', t['y2'][:], t['y2'][:], GN_EPS, None, ALU.add, None, t['y2'].k, t['y2'].k)
                k.act(t['y2'][:], t['y2'][:], AF.Sqrt, t['y2'].k, t['y2'].k)
                k.op('dve', lambda e: e.reciprocal(out=t['y2'][:], in_=t['y2'][:]), t['y2'].k, t['y2'].k)
                k.tt('pool', t['y'][:], t['y'][:], t['mn'][:], ALU.subtract, t['y'].k + t['mn'].k, t['y'].k)
                k.tt('pool', t['y'][:], t['y'][:], t['y2'][:], ALU.mult, t['y'].k + t['y2'].k, t['y'].k)
                k.act(t['y'][:], t['y'][:], AF.Identity, t['y'].k, t['y'].k, scale=col(pv['rw_gn_g'], oc),
                      bias=col(pv['rw_gn_b'], oc))
                k.tt('pool', t['bon'][:], t['r'][:], t['kf'][:], ALU.mult, t['r'].k + t['kf'].k, t['bon'].k)
                k.ts('dve', t['bon'][:], t['bon'][:], col(pv['rw_rk'], oc), None, ALU.mult, None, t['bon'].k, t['bon'].k)
                k.mm(ps[3][:, W:2 * W], g.blk[:], t['bon'][:], True, True, g.blk.k + t['bon'].k, [ps[3].k[2]])
                k.tt('dve', t['bon'][:], ps[3][:, W:2 * W], t['v'][:], ALU.mult, [ps[3].k[2]] + t['v'].k, t['bon'].k)
                k.tt('pool', t['y'][:], t['y'][:], t['bon'][:], ALU.add, t['y'].k + t['bon'].k, t['y'].k)
                k.tt('pool', mob[:], t['y'][:], t['g'][:], ALU.mult, t['y'].k + t['g'].k, mob.k)
                k.dma('sp', S['MO'].t[ocs, t0:t0 + W], mob[:], mob.k, S['MO'].k)
        k.barrier()


_CACHE = {}


def prep_inputs(inp, b, T):
    f = lambda a: np.ascontiguousarray(np.asarray(a, dtype=np.float32))
    m = {}
    m['x'] = f(inp['x'][b][:T])
    m['c'] = f(inp['c'][b]).reshape(KC, 128)
    m['ada_w'] = f(inp['ada_w'])
    m['ada_b'] = f(inp['ada_b']).reshape(DEPTH, 48, 128)
    m['ln_g'] = f(inp['ln_g']).reshape(DEPTH, 16, 128)
    m['ln_b'] = f(inp['ln_b']).reshape(DEPTH, 16, 128)
    m['rw_mu'] = f(inp['rw_mu']).reshape(N_A, 48, 128)
    m['rw_rkv'] = f(inp['rw_rkv'])
    for nm in ['rw_w0', 'rw_a0', 'rw_kk', 'rw_ka', 'rw_rk', 'rw_gn_g', 'rw_gn_b']:
        m[nm] = f(inp[nm]).reshape(N_A, 8, 128)
    for nm in ['rw_w1', 'rw_w2', 'rw_a1', 'rw_a2', 'rw_g1', 'rw_g2', 'rw_wo', 'rw_v1', 'rw_v2',
               'kv_ada_w', 'kv_w', 'fx_wqg', 'fx_wo', 'moe_wgrp', 'moe_bgrp', 'moe_wexp', 'moe_bexp',
               'moe_wgate', 'moe_wup', 'moe_wdown']:
        m[nm] = f(inp[nm])
    m['rw_v0'] = f(inp['rw_v0']).reshape(1, 8, 128)
    m['kv_ada_b'] = f(inp['kv_ada_b']).reshape(16, 128)
    m['kv_fb'] = f(inp['kv_fb']).reshape(NH, 1)
    m['kv_knorm'] = f(inp['kv_knorm']).reshape(HD, 1)
    m['fx_qnorm'] = f(inp['fx_qnorm']).reshape(2, HD, 1)
    return m


def kernel(**inputs):
    B, T = inputs['x'].shape[0], inputs['x'].shape[1]
    NG = inputs['moe_wgrp'].shape[-1]
    NE = inputs['moe_wexp'].shape[-1]
    key = (T, NG, NE)
    if key not in _CACHE:
        _CACHE[key] = build(T, NG, NE // NG)[0]
    nc = _CACHE[key]
    shared = None
    maps = []
    for b in range(B):
        m = prep_inputs(inputs, b, T) if shared is None else dict(shared)
        if shared is None:
            shared = m
        else:
            m['x'] = np.ascontiguousarray(np.asarray(inputs['x'][b], dtype=np.float32))
            m['c'] = np.ascontiguousarray(np.asarray(inputs['c'][b], dtype=np.float32)).reshape(KC, 128)
        maps.append(m)
    res = run_bass_kernel_spmd(nc, maps, core_ids=list(range(B)))
    return np.stack([np.asarray(r['out']) for r in res.results]).astype(np.float32)
```

```python
import numpy as np
from contextlib import ExitStack
import concourse.bass as bass
import concourse.mybir as mybir
from concourse.bass_utils import run_bass_kernel_spmd

F32 = mybir.dt.float32
BF16 = mybir.dt.bfloat16
AF = mybir.ActivationFunctionType
ALU = mybir.AluOpType
AX = mybir.AxisListType

D = 1024
KC = 8
HD = 64
NH = 16
DEPTH = 4
N_A = 2
ALPHA = (2 * DEPTH) ** 0.25
LN_EPS = 1e-5
GN_EPS = 64e-5
QK_EPS = 1e-6
DEXP = 512


class Tk:
    __slots__ = ("w", "r", "excl")

    def __init__(s):
        s.w = None
        s.r = {}
        s.excl = False


class Buf:
    def __init__(s, t, n=1):
        s.t = t
        s.k = [Tk() for _ in range(n)]

    def __getitem__(s, idx):
        return s.t[idx]


class _KL(list):
    def __getitem__(s, i):
        return list.__getitem__(s, 0)


class PBuf(Buf):
    def __init__(s, t):
        s.t = t
        s.k = _KL([Tk()])
        s.k[0].excl = True


class K:
    NS = 24

    def __init__(s, nc, es):
        s.nc = nc
        s.eng = {'pe': nc.tensor, 'act': nc.scalar, 'dve': nc.vector, 'pool': nc.gpsimd, 'sp': nc.sync}
        s.esem = {n: es.enter_context(nc.semaphore("s_" + n)) for n in s.eng}
        s.ecnt = {n: 0 for n in s.eng}
        s.seen = {n: {} for n in s.eng}
        s.dsem = [es.enter_context(nc.semaphore("d%d" % i)) for i in range(s.NS)]
        s.dcnt = [0] * s.NS
        s.dnext = {'hw': 0, 'sw': 0}
        s.dpool = {'hw': list(range(0, 16)), 'sw': list(range(16, s.NS))}
        s.same_sync = {'pe': False, 'act': True, 'dve': True, 'pool': True, 'sp': False}
        s.nins = 0

    def _wait(s, en, key, val):
        if s.seen[en].get(key, 0) >= val:
            return
        if key[0] == 'E':
            if key[1] == en and not s.same_sync[en]:
                return
            sem = s.esem[key[1]]
        else:
            sem = s.dsem[key[1]]
        s.eng[en].wait_ge(sem, val)
        s.seen[en][key] = val
        s.nins += 1

    def _need(s, reads, writes):
        need = {}
        for t in reads:
            if t.w is not None:
                k_, v = t.w
                if need.get(k_, 0) < v:
                    need[k_] = v
        for t in writes:
            if t.w is not None:
                k_, v = t.w
                if need.get(k_, 0) < v:
                    need[k_] = v
            for k_, v in t.r.items():
                if need.get(k_, 0) < v:
                    need[k_] = v
        return need

    def op(s, en, fn, reads=(), writes=()):
        ex = [t for t in reads if t.excl]
        if ex:
            reads = [t for t in reads if not t.excl]
            writes = list(writes) + ex
        for k_, v in s._need(reads, writes).items():
            s._wait(en, k_, v)
        ins = fn(s.eng[en])
        s.ecnt[en] += 1
        c = s.ecnt[en]
        ins.then_inc(s.esem[en], 1)
        key = ('E', en)
        for t in reads:
            t.r[key] = c
        for t in writes:
            t.w = (key, c)
            t.r = {}
        s.nins += 1

    def dma(s, q, out, in_, reads=(), writes=(), **kw):
        for k_, v in s._need(reads, writes).items():
            s._wait(q, k_, v)
        pn = 'sw' if q == 'pool' else 'hw'
        pl = s.dpool[pn]
        i = pl[s.dnext[pn] % len(pl)]
        s.dnext[pn] += 1
        if s.dcnt[i]:
            s._wait(q, ('D', i), s.dcnt[i] * 16)
        s.eng[q].dma_start(out=out, in_=in_, **kw).then_inc(s.dsem[i], 16)
        s.dcnt[i] += 1
        key = ('D', i)
        val = s.dcnt[i] * 16
        for t in reads:
            t.r[key] = val
        for t in writes:
            t.w = (key, val)
            t.r = {}
        s.nins += 1

    def barrier(s):
        for en in s.eng:
            for o in s.eng:
                if o != en and s.ecnt[o] > 0:
                    s._wait(en, ('E', o), s.ecnt[o])
            for i in range(s.NS):
                if s.dcnt[i] > 0:
                    s._wait(en, ('D', i), s.dcnt[i] * 16)

    def mm(s, out, lhsT, rhs, start, stop, reads, writes):
        s.op('pe', lambda e: e.matmul(out, lhsT, rhs, start=start, stop=stop), reads, writes)

    def tr(s, out, in_, ident, reads, writes):
        s.op('pe', lambda e: e.transpose(out, in_, ident), reads, writes)

    def act(s, out, in_, func, reads, writes, bias=None, scale=None, accum_out=None, en='act'):
        kw = {}
        if bias is not None:
            kw['bias'] = bias
        if scale is not None:
            kw['scale'] = scale
        if accum_out is not None:
            kw['accum_out'] = accum_out
        s.op('act', lambda e: e.activation(out, in_, func, **kw), reads, writes)

    def tt(s, en, out, in0, in1, op, reads, writes):
        s.op(en, lambda e: e.tensor_tensor(out=out, in0=in0, in1=in1, op=op), reads, writes)

    def ts(s, en, out, in0, s1, s2, op0, op1, reads, writes):
        if op1 is None:
            s.op(en, lambda e: e.tensor_scalar(out=out, in0=in0, scalar1=s1, scalar2=None, op0=op0), reads, writes)
        else:
            s.op(en, lambda e: e.tensor_scalar(out=out, in0=in0, scalar1=s1, scalar2=s2, op0=op0, op1=op1), reads, writes)

    def stt(s, out, in0, scalar, in1, op0, op1, reads, writes):
        s.op('dve', lambda e: e.scalar_tensor_tensor(out=out, in0=in0, scalar=scalar, in1=in1, op0=op0, op1=op1),
             reads, writes)

    def cp(s, en, out, in_, reads, writes):
        if en == 'act':
            s.op('act', lambda e: e.copy(out, in_), reads, writes)
        else:
            s.op(en, lambda e: e.tensor_copy(out=out, in_=in_), reads, writes)


class Ctx:
    pass


def build(T, NG, EPG, layers=DEPTH, dbg=None):
    NE = NG * EPG
    NR = NG + NE
    nc = bass.Bass("TRN2", target_bir_lowering=False)
    g = Ctx()
    g.T, g.NG, g.EPG, g.NE, g.NR = T, NG, EPG, NE, NR
    g.dbg = dbg
    n_a = min(N_A, layers)
    n_b = layers - n_a
    nv = max(n_a - 1, 0)

    def din(name, shape):
        return nc.dram_tensor(name, list(shape), F32, kind="ExternalInput").ap()

    I = {}
    I['x'] = din('x', [T, D])
    I['c'] = din('c', [KC, 128])
    I['ada_w'] = din('ada_w', [DEPTH, D, 6 * D])
    I['ada_b'] = din('ada_b', [DEPTH, 48, 128])
    I['ln_g'] = din('ln_g', [DEPTH, 16, 128])
    I['ln_b'] = din('ln_b', [DEPTH, 16, 128])
    I['rw_mu'] = din('rw_mu', [N_A, 48, 128])
    I['rw_rkv'] = din('rw_rkv', [N_A, 3, D, D])
    for nm in ['rw_w0', 'rw_a0', 'rw_kk', 'rw_ka', 'rw_rk', 'rw_gn_g', 'rw_gn_b']:
        I[nm] = din(nm, [N_A, 8, 128])
    I['rw_w1'] = din('rw_w1', [N_A, D, 64])
    I['rw_w2'] = din('rw_w2', [N_A, 64, D])
    I['rw_a1'] = din('rw_a1', [N_A, D, 64])
    I['rw_a2'] = din('rw_a2', [N_A, 64, D])
    I['rw_g1'] = din('rw_g1', [N_A, D, 160])
    I['rw_g2'] = din('rw_g2', [N_A, 160, D])
    I['rw_wo'] = din('rw_wo', [N_A, D, D])
    I['rw_v0'] = din('rw_v0', [1, 8, 128])
    I['rw_v1'] = din('rw_v1', [1, D, 32])
    I['rw_v2'] = din('rw_v2', [1, 32, D])
    I['kv_ada_w'] = din('kv_ada_w', [D, 2 * D])
    I['kv_ada_b'] = din('kv_ada_b', [16, 128])
    I['kv_w'] = din('kv_w', [D, 2 * D + NH])
    I['kv_fb'] = din('kv_fb', [NH, 1])
    I['kv_knorm'] = din('kv_knorm', [HD, 1])
    I['fx_wqg'] = din('fx_wqg', [2, D, 2 * D])
    I['fx_qnorm'] = din('fx_qnorm', [2, HD, 1])
    I['fx_wo'] = din('fx_wo', [2, D, D])
    I['moe_wgrp'] = din('moe_wgrp', [DEPTH, D, NG])
    I['moe_bgrp'] = din('moe_bgrp', [DEPTH, NG])
    I['moe_wexp'] = din('moe_wexp', [DEPTH, D, NE])
    I['moe_bexp'] = din('moe_bexp', [DEPTH, NE])
    I['moe_wgate'] = din('moe_wgate', [DEPTH, NE, D, DEXP])
    I['moe_wup'] = din('moe_wup', [DEPTH, NE, D, DEXP])
    I['moe_wdown'] = din('moe_wdown', [DEPTH, NE, DEXP, D])
    out_ap = nc.dram_tensor('out', [T, D], F32, kind="ExternalOutput").ap()

    def dscr(name, shape, dt=F32):
        kind = "ExternalOutput" if (dbg and name in dbg) else "Internal"
        return Buf(nc.dram_tensor(name, list(shape), dt, kind=kind).ap())

    S = {}
    S['XT'] = dscr('XT', [D, T])
    S['X1'] = dscr('X1', [D, T])
    S['H2'] = dscr('H2', [D, T], BF16)
    S['MO'] = dscr('MO', [D, T], BF16)
    S['VF'] = dscr('VF', [D, T])
    S['KT'] = dscr('KT', [D, T], BF16)
    S['VK'] = dscr('VK', [T, D], BF16)
    S['FC'] = dscr('FC', [NH, T])
    S['QT'] = dscr('QT', [D, T], BF16)
    S['SG'] = dscr('SG', [D, T], BF16)

    with ExitStack() as es:
        k = K(nc, es)
        g.es = es
        g.k, g.nc, g.I, g.S, g.out = k, nc, I, S, out_ap
        g.ps = [PBuf(es.enter_context(nc.psum_tensor("ps%d" % i, [128, 512], F32))) for i in range(8)]
        setup_consts(g, es)
        g.GT = sb(g, es, "GT", [128, T])
        phase_mod(g, layers, n_a)
        phase_in(g)
        for l in range(layers):
            if l < n_a:
                phase_rwkv(g, l)
            else:
                phase_fox(g, l, l - n_a)
            phase_moe(g, l, last=(l == layers - 1))
            if l == n_a - 1 and n_b > 0:
                phase_kv(g)
        k.barrier()
    g.nc = nc
    return nc, g


_UID = [0]


def sb(g, es, name, shape, dt=F32, n=1):
    _UID[0] += 1
    return Buf(es.enter_context(g.nc.sbuf_tensor("%s_%d" % (name, _UID[0]), list(shape), dt)), n)


def setup_consts(g, es):
    k, nc = g.k, g.nc
    ones = sb(g, es, "c_ones", [128, 512])
    g.ones = ones
    k.op('pool', lambda e: e.memset(ones[:], 1.0), [], ones.k)
    ident = sb(g, es, "c_ident", [128, 128])
    g.ident = ident
    k.op('pool', lambda e: e.affine_select(out=ident[:], in_=ones[:, 0:128], pattern=[[-1, 128]],
                                           compare_op=ALU.is_equal, fill=0.0, base=0, channel_multiplier=1),
         ones.k, ident.k)
    mmean = sb(g, es, "c_mmean", [128, 128])
    g.mmean = mmean
    k.op('pool', lambda e: e.memset(mmean[:], 1.0 / D), [], mmean.k)
    blk = sb(g, es, "c_blk", [128, 128])
    g.blk = blk
    k.op('pool', lambda e: e.memset(blk[:], 0.0), [], blk.k)
    k.op('pool', lambda e: e.memset(blk[0:64, 0:64], 1.0), [], blk.k)
    k.op('pool', lambda e: e.memset(blk[64:128, 64:128], 1.0), [], blk.k)


def dump(g, name, ap, reads):
    if not g.dbg or name not in g.dbg:
        return
    d = g.nc.dram_tensor("dbg_" + name, list(ap.shape), ap.dtype, kind="ExternalOutput").ap()
    g.k.dma('sp', d, ap, reads, [])


def load_vecT(g, out, src2d, R, st):
    k = g.k
    k.dma('sp', st[0:R, :], src2d, [], st.k)
    ps = g.ps[0]
    k.tr(ps[:, 0:R], st[0:R, :], g.ident[0:R, 0:R], st.k + g.ident.k, [ps.k[0]])
    k.cp('dve', out[:, 0:R], ps[:, 0:R], [ps.k[0]], out.k)
    return out


def phase_mod(g, layers, n_a):
    k, nc, I = g.k, g.nc, g.I
    es = g.es
    g.modT = [sb(g, es, "modT%d" % l, [128, 48]) for l in range(layers)]
    g.mod1 = [sb(g, es, "mod1_%d" % l, [128, 48]) for l in range(layers)]
    g.lng = [sb(g, es, "lng%d" % l, [128, 16]) for l in range(layers)]
    g.lnb = [sb(g, es, "lnb%d" % l, [128, 16]) for l in range(layers)]
    if layers > n_a:
        g.kvmod = sb(g, es, "kvmod", [128, 16])
        g.kvmod1 = sb(g, es, "kvmod1", [128, 16])
    with ExitStack() as ph:
        st = sb(g, ph, "lv_st", [128, 128])
        cT = sb(g, ph, "cT", [128, KC])
        bT = sb(g, ph, "bT", [128, 48])
        load_vecT(g, cT, I['c'], KC, st)
        cs2 = sb(g, ph, "cs2", [128, KC, 2])
        k.act(cs2[:, :, 0], cT[:], AF.Silu, cT.k, cs2.k)
        k.act(cs2[:, :, 1], cT[:], AF.Silu, cT.k, cs2.k)
        wb = [sb(g, ph, "adaw%d" % i, [128, KC, 1024]) for i in range(2)]
        nblk = 0

        def matvec(w_ap, ncols, outT):
            nonlocal nblk
            wv = w_ap.rearrange("(kc p) n -> p kc n", p=128)
            for b0 in range(0, ncols, 1024):
                bw = min(1024, ncols - b0)
                w = wb[nblk % 2]
                nblk += 1
                k.dma('sp', w[:, :, 0:bw], wv[:, :, b0:b0 + bw], [], w.k)
                ps = g.ps[1 + (nblk % 2)]
                for j in range(bw // 128):
                    for kc in range(KC):
                        k.mm(ps[:, 2 * j:2 * j + 2], w[:, kc, j * 128:(j + 1) * 128], cs2[:, kc, :],
                             kc == 0, kc == KC - 1, w.k + cs2.k, [ps.k[0]])
                nj = bw // 128
                k.cp('dve', outT[:, b0 // 128:b0 // 128 + nj],
                     ps[:, 0:2 * nj].rearrange("p (j t) -> p j t", t=2)[:, :, 0], [ps.k[0]], outT.k)

        for l in range(layers):
            mt, m1 = g.modT[l], g.mod1[l]
            matvec(I['ada_w'][l], 6 * D, mt)
            load_vecT(g, bT, I['ada_b'][l], 48, st)
            k.tt('dve', mt[:], mt[:], bT[:], ALU.add, mt.k + bT.k, mt.k)
            k.ts('dve', m1[:], mt[:], 1.0, None, ALU.add, None, mt.k, m1.k)
            dump(g, "modT%d" % l, mt[:], mt.k)
            load_vecT(g, g.lng[l], I['ln_g'][l], 16, st)
            load_vecT(g, g.lnb[l], I['ln_b'][l], 16, st)
        if layers > n_a:
            kt, k1 = g.kvmod, g.kvmod1
            matvec(I['kv_ada_w'], 2 * D, kt)
            load_vecT(g, bT, I['kv_ada_b'], 16, st)
            k.tt('dve', kt[:], kt[:], bT[:, 0:16], ALU.add, kt.k + bT.k, kt.k)
            k.ts('dve', k1[:], kt[:], 1.0, None, ALU.add, None, kt.k, k1.k)
        k.barrier()


def phase_in(g):
    k, I, S = g.k, g.I, g.S
    T = g.T
    with ExitStack() as ph:
        xt = [sb(g, ph, "in_x%d" % i, [128, D]) for i in range(2)]
        st = [sb(g, ph, "in_st%d" % i, [128, KC, 512]) for i in range(2)]
        XTv = S['XT'].t.rearrange("(kc p) t -> p kc t", p=128)
        for gi in range(T // 128):
            xb = xt[gi % 2]
            k.dma('sp', xb[:], I['x'][gi * 128:(gi + 1) * 128, :], [], xb.k)
            sg = st[(gi // 4) % 2]
            for half in range(2):
                ps = g.ps[(gi * 2 + half) % 4]
                for j in range(4):
                    kc = half * 4 + j
                    k.tr(ps[:, j * 128:(j + 1) * 128], xb[:, kc * 128:(kc + 1) * 128], g.ident[:],
                         xb.k + g.ident.k, [ps.k[0]])
                dst = sg[:, half * 4:half * 4 + 4, (gi % 4) * 128:(gi % 4 + 1) * 128]
                src = ps[:, :].rearrange("p (j t) -> p j t", t=128)
                k.cp('act' if half else 'dve', dst, src, [ps.k[0]], sg.k)
            if gi % 4 == 3:
                t0 = (gi // 4) * 512
                k.dma('sp', XTv[:, :, t0:t0 + 512], sg[:], sg.k, S['XT'].k)
        k.barrier()


STUB = {'rwkv': False, 'fox': False}
RW_STOP = [99]
RW_SUB = [9]


def bc_rows(ap2d_row, nparts, ncols):
    return bass.AP(tensor=ap2d_row.tensor, offset=ap2d_row.offset, ap=[[0, nparts], [1, ncols]])


def mixer_zero(g):
    k, S, T = g.k, g.S, g.T
    with ExitStack() as ph:
        z = sb(g, ph, "mz", [128, KC, 512], BF16)
        k.op('pool', lambda e: e.memset(z[:], 0.0), [], z.k)
        MOv = S['MO'].t.rearrange("(kc p) t -> p kc t", p=128)
        for t0 in range(0, T, 512):
            k.dma('sp', MOv[:, :, t0:t0 + 512], z[:], z.k, S['MO'].k)
        k.barrier()


def phase_rwkv(g, l):
    if STUB['rwkv']:
        mixer_zero(g)
    else:
        rwkv_mixer(g, l)
    phase_tail(g, l, g.I['rw_wo'][l])


def phase_fox(g, l, j):
    if STUB['fox']:
        mixer_zero(g)
    else:
        fox_mixer(g, l, j)
    phase_tail(g, l, g.I['fx_wo'][j])


def ln_tile(g, zf, zk, sq, mean, rstd, gam, bet, outf, outk, w):
    k = g.k
    psm, psq = g.ps[6], g.ps[7]
    for kc in range(KC):
        k.act(sq[:, kc, 0:w], zf(kc), AF.Square, zk, sq.k)
    for kc in range(KC):
        k.mm(psm[:, 0:w], g.mmean[:], zf(kc), kc == 0, kc == KC - 1, g.mmean.k + zk, [psm.k[0]])
    for kc in range(KC):
        k.mm(psq[:, 0:w], g.mmean[:], sq[:, kc, 0:w], kc == 0, kc == KC - 1, g.mmean.k + sq.k, [psq.k[0]])
    k.cp('act', mean[:, 0:w], psm[:, 0:w], [psm.k[0]], mean.k)
    k.tt('dve', rstd[:, 0:w], mean[:, 0:w], mean[:, 0:w], ALU.mult, mean.k, rstd.k)
    k.tt('dve', rstd[:, 0:w], psq[:, 0:w], rstd[:, 0:w], ALU.subtract, [psq.k[0]] + rstd.k, rstd.k)
    k.ts('dve', rstd[:, 0:w], rstd[:, 0:w], LN_EPS, None, ALU.add, None, rstd.k, rstd.k)
    k.act(rstd[:, 0:w], rstd[:, 0:w], AF.Sqrt, rstd.k, rstd.k)
    k.op('dve', lambda e: e.reciprocal(out=rstd[:, 0:w], in_=rstd[:, 0:w]), rstd.k, rstd.k)
    for kc in range(KC):
        k.tt('dve', zf(kc), zf(kc), mean[:, 0:w], ALU.subtract, zk + mean.k, zk)
        k.tt('pool', zf(kc), zf(kc), rstd[:, 0:w], ALU.mult, zk + rstd.k, zk)
        k.act(outf(kc), zf(kc), AF.Identity, zk, outk, scale=gam(kc), bias=bet(kc))


def phase_tail(g, l, wo_ap):
    k, I, S, T = g.k, g.I, g.S, g.T
    NE, NG, NR, EPG = g.NE, g.NG, g.NR, g.EPG
    mt, m1 = g.modT[l], g.mod1[l]
    GT = g.GT
    with ExitStack() as ph:
        wo = sb(g, ph, "t_wo", [128, KC, D], BF16)
        k.dma('pool', wo[:], wo_ap.rearrange("(kc p) n -> p kc n", p=128), [], wo.k)
        wr = sb(g, ph, "t_wr", [128, KC, NR])
        k.dma('sp', wr[:, :, 0:NG], I['moe_wgrp'][l].rearrange("(kc p) n -> p kc n", p=128), [], wr.k)
        k.dma('sp', wr[:, :, NG:NR], I['moe_wexp'][l].rearrange("(kc p) n -> p kc n", p=128), [], wr.k)
        rb = sb(g, ph, "t_rb", [128, NR])
        k.dma('sp', rb[:, 0:NG], bc_rows(I['moe_bgrp'][l], 128, NG), [], rb.k)
        k.dma('sp', rb[:, NG:NR], bc_rows(I['moe_bexp'][l], 128, NE), [], rb.k)
        xt = [sb(g, ph, "t_x%d" % i, [128, KC, 512]) for i in range(2)]
        zt = [sb(g, ph, "t_z%d" % i, [128, KC, 512]) for i in range(2)]
        mo = [sb(g, ph, "t_mo%d" % i, [128, KC, 512], BF16) for i in range(2)]
        hb = [sb(g, ph, "t_hb%d" % i, [128, KC, 512], BF16) for i in range(2)]
        sq = sb(g, ph, "t_sq", [128, KC, 512])
        mean = sb(g, ph, "t_mean", [128, 512])
        rstd = sb(g, ph, "t_rstd", [128, 512])
        sm = {n: sb(g, ph, "t_r_" + n, [128, w_]) for n, w_ in
              [('lg', NR), ('mg', 1), ('nmg', 1), ('eg', NG), ('sg', 1), ('pg', 1), ('oh', NG), ('pen', NG),
               ('le', NE), ('t8', 8), ('d12', 1), ('s12', 1), ('g1', 1), ('g2', 1), ('G1', NE), ('G2', NE)]}
        XTv = S['XT'].t.rearrange("(kc p) t -> p kc t", p=128)
        X1v = S['X1'].t.rearrange("(kc p) t -> p kc t", p=128)
        MOv = S['MO'].t.rearrange("(kc p) t -> p kc t", p=128)
        H2v = S['H2'].t.rearrange("(kc p) t -> p kc t", p=128)
        for ti in range(T // 512):
            t0 = ti * 512
            x, z, m, h = xt[ti % 2], zt[ti % 2], mo[ti % 2], hb[ti % 2]
            k.dma('sp', x[:], XTv[:, :, t0:t0 + 512], S['XT'].k, x.k)
            k.dma('sp', m[:], MOv[:, :, t0:t0 + 512], S['MO'].k, m.k)
            for oc in range(KC):
                ps = g.ps[oc % 2]
                for kc in range(KC):
                    k.mm(ps[:, :], wo[:, kc, oc * 128:(oc + 1) * 128], m[:, kc, :], kc == 0, kc == KC - 1,
                         wo.k + m.k, [ps.k[0]])
                k.act(z[:, oc, :], ps[:, :], AF.Identity, [ps.k[0]], z.k, scale=m1[:, 16 + oc:17 + oc])
                k.stt(z[:, oc, :], x[:, oc, :], ALPHA, z[:, oc, :], ALU.mult, ALU.add, x.k + z.k, z.k)
            ln_tile(g, lambda kc: z[:, kc, :], z.k, sq, mean, rstd,
                    lambda kc: g.lng[l][:, kc:kc + 1], lambda kc: g.lnb[l][:, kc:kc + 1],
                    lambda kc: x[:, kc, :], x.k, 512)
            k.dma('sp', X1v[:, :, t0:t0 + 512], x[:], x.k, S['X1'].k)
            for kc in range(KC):
                k.act(z[:, kc, :], x[:, kc, :], AF.Identity, x.k, z.k,
                      scale=m1[:, 32 + kc:33 + kc], bias=mt[:, 24 + kc:25 + kc])
            k.cp('pool', h[:], z[:], z.k, h.k)
            k.dma('sp', H2v[:, :, t0:t0 + 512], h[:], h.k, S['H2'].k)
            for tg in range(4):
                pr = g.ps[2 + tg % 2]
                for kc in range(KC):
                    k.mm(pr[:, 0:NR], z[:, kc, tg * 128:(tg + 1) * 128], wr[:, kc, :], kc == 0, kc == KC - 1,
                         z.k + wr.k, [pr.k[0]])
                lg, mg, nmg, eg, sg, pg = sm['lg'], sm['mg'], sm['nmg'], sm['eg'], sm['sg'], sm['pg']
                oh, pen, le, t8 = sm['oh'], sm['pen'], sm['le'], sm['t8']
                k.tt('dve', lg[:], pr[:, 0:NR], rb[:], ALU.add, [pr.k[0]] + rb.k, lg.k)
                k.op('dve', lambda e: e.tensor_reduce(out=mg[:], in_=lg[:, 0:NG], axis=AX.X, op=ALU.max), lg.k, mg.k)
                k.ts('dve', nmg[:], mg[:], -1.0, None, ALU.mult, None, mg.k, nmg.k)
                k.act(eg[:], lg[:, 0:NG], AF.Exp, lg.k + nmg.k, eg.k + sg.k, bias=nmg[:, 0:1], accum_out=sg[:])
                k.op('dve', lambda e: e.reciprocal(out=pg[:], in_=sg[:]), sg.k, pg.k)
                k.ts('dve', oh[:], lg[:, 0:NG], mg[:, 0:1], None, ALU.is_equal, None, lg.k + mg.k, oh.k)
                k.ts('dve', pen[:], oh[:], -1.0, 1e30, ALU.add, ALU.mult, oh.k, pen.k)
                for gi in range(NG):
                    k.ts('dve', le[:, gi * EPG:(gi + 1) * EPG], lg[:, NG + gi * EPG:NG + (gi + 1) * EPG],
                         pen[:, gi:gi + 1], None, ALU.add, None, lg.k + pen.k, le.k)
                k.op('dve', lambda e: e.max(out=t8[:], in_=le[:]), le.k, t8.k)
                d12, s12, g1, g2, G1, G2 = sm['d12'], sm['s12'], sm['g1'], sm['g2'], sm['G1'], sm['G2']
                k.tt('dve', d12[:], t8[:, 0:1], t8[:, 1:2], ALU.subtract, t8.k, d12.k)
                k.act(s12[:], d12[:], AF.Sigmoid, d12.k, s12.k)
                k.tt('dve', g1[:], s12[:], pg[:], ALU.mult, s12.k + pg.k, g1.k)
                k.tt('dve', g2[:], pg[:], g1[:], ALU.subtract, pg.k + g1.k, g2.k)
                k.ts('dve', G1[:], le[:], t8[:, 0:1], g1[:, 0:1], ALU.is_equal, ALU.mult, le.k + t8.k + g1.k, G1.k)
                k.ts('dve', G2[:], le[:], t8[:, 1:2], g2[:, 0:1], ALU.is_equal, ALU.mult, le.k + t8.k + g2.k, G2.k)
                k.tt('dve', G1[:], G1[:], G2[:], ALU.add, G1.k + G2.k, G1.k)
                pt = g.ps[4 + tg % 2]
                k.tr(pt[0:NE, 0:128], G1[:], g.ident[:], G1.k + g.ident.k, [pt.k[0]])
                c0 = t0 + tg * 128
                k.cp('dve', GT[0:NE, c0:c0 + 128], pt[0:NE, 0:128], [pt.k[0]], GT.k)
        dump(g, "GT%d" % l, GT[0:NE, :], GT.k)
        k.barrier()


def phase_moe(g, l, last):
    k, I, S, T = g.k, g.I, g.S, g.T
    NE = g.NE
    mt, m1 = g.modT[l], g.mod1[l]
    GT = g.GT
    HT = min(2048, T)
    NTT = HT // 512
    X1v = S['X1'].t.rearrange("(kc p) t -> p kc t", p=128)
    XTv = S['XT'].t.rearrange("(kc p) t -> p kc t", p=128)
    H2v = S['H2'].t.rearrange("(kc p) t -> p kc t", p=128)
    with ExitStack() as ph:
        h2 = sb(g, ph, "m_h2", [128, KC, HT], BF16)
        yacc = sb(g, ph, "m_y", [128, KC, HT], F32, n=KC * NTT)
        for hf in range(T // HT):
            tb = hf * HT
            for tt in range(NTT):
                k.dma('sp', h2[:, :, tt * 512:(tt + 1) * 512], H2v[:, :, tb + tt * 512:tb + (tt + 1) * 512],
                      S['H2'].k, h2.k)
            with ExitStack() as ex:
                wg = [sb(g, ex, "m_wg%d" % i, [128, KC, DEXP], BF16) for i in range(2)]
                wu = [sb(g, ex, "m_wu%d" % i, [128, KC, DEXP], BF16) for i in range(2)]
                wd = [sb(g, ex, "m_wd%d" % i, [128, 4, D], BF16) for i in range(2)]
                sel = [sb(g, ex, "m_sel%d" % i, [128, 128]) for i in range(2)]
                gbc = sb(g, ex, "m_gbc", [128, 512])
                sl = [sb(g, ex, "m_sl%d" % i, [128, 512]) for i in range(2)]
                tl = sb(g, ex, "m_tl", [128, 512])
                hT = [sb(g, ex, "m_hT%d" % i, [128, 4, 512], BF16) for i in range(2)]
                it = 0

                def load_w(e):
                    bi = e % 2
                    k.dma('pool', wg[bi][:], I['moe_wgate'][l, e].rearrange("(kc p) n -> p kc n", p=128), [], wg[bi].k)
                    k.dma('pool', wu[bi][:], I['moe_wup'][l, e].rearrange("(kc p) n -> p kc n", p=128), [], wu[bi].k)
                    k.dma('pool', wd[bi][:], I['moe_wdown'][l, e].rearrange("(dc p) n -> p dc n", p=128), [], wd[bi].k)
                load_w(0)
                for e in range(NE):
                    bi = e % 2
                    if e + 1 < NE:
                        load_w(e + 1)
                    se = sel[bi]
                    k.op('pool', lambda e_, se=se, e=e: e_.affine_select(
                        out=se[0:NE, :], in_=g.ones[0:NE, 0:128], pattern=[[0, 128]], compare_op=ALU.is_equal,
                        fill=0.0, base=-e, channel_multiplier=1), g.ones.k, se.k)
                    for tt in range(NTT):
                        c0 = tt * 512
                        psg = g.ps[0]
                        k.mm(psg[:, :], se[0:NE, :], GT[0:NE, tb + c0:tb + c0 + 512], True, True,
                             se.k + GT.k, [psg.k[0]])
                        k.cp('act', gbc[:], psg[:, :], [psg.k[0]], gbc.k)
                        hh = hT[it % 2]
                        it += 1
                        for dc in range(4):
                            pg_, pu_ = g.ps[1 + dc % 2], g.ps[3 + dc % 2]
                            for kc in range(KC):
                                k.mm(pg_[:, :], wg[bi][:, kc, dc * 128:(dc + 1) * 128], h2[:, kc, c0:c0 + 512],
                                     kc == 0, kc == KC - 1, wg[bi].k + h2.k, [pg_.k[0]])
                            for kc in range(KC):
                                k.mm(pu_[:, :], wu[bi][:, kc, dc * 128:(dc + 1) * 128], h2[:, kc, c0:c0 + 512],
                                     kc == 0, kc == KC - 1, wu[bi].k + h2.k, [pu_.k[0]])
                            s_ = sl[dc % 2]
                            k.act(s_[:], pg_[:, :], AF.Silu, [pg_.k[0]], s_.k)
                            k.tt('dve', tl[:], pu_[:, :], s_[:], ALU.mult, [pu_.k[0]] + s_.k, tl.k)
                            k.tt('dve', hh[:, dc, :], tl[:], gbc[:], ALU.mult, tl.k + gbc.k, hh.k)
                        for oc in range(KC):
                            py = g.ps[5 + oc % 2]
                            for dc in range(4):
                                k.mm(py[:, :], wd[bi][:, dc, oc * 128:(oc + 1) * 128], hh[:, dc, :],
                                     dc == 0, dc == 3, wd[bi].k + hh.k, [py.k[0]])
                            yk = [yacc.k[oc * NTT + tt]]
                            ya = yacc[:, oc, c0:c0 + 512]
                            if e == 0:
                                k.cp('dve', ya, py[:, :], [py.k[0]], yk)
                            else:
                                k.tt('dve', ya, py[:, :], ya, ALU.add, [py.k[0]] + yk, yk)
                k.barrier()
            with ExitStack() as ex:
                xt = [sb(g, ex, "m_x%d" % i, [128, KC, 512]) for i in range(2)]
                sq = sb(g, ex, "m_sq", [128, KC, 512])
                mean = sb(g, ex, "m_mean", [128, 512])
                rstd = sb(g, ex, "m_rstd", [128, 512])
                ot = [sb(g, ex, "m_ot%d" % i, [128, D]) for i in range(2)] if last else None
                for tt in range(NTT):
                    c0 = tt * 512
                    t0 = tb + c0
                    x = xt[tt % 2]
                    k.dma('sp', x[:], X1v[:, :, t0:t0 + 512], S['X1'].k, x.k)
                    zk = [yacc.k[oc * NTT + tt] for oc in range(KC)]
                    for oc in range(KC):
                        ya = yacc[:, oc, c0:c0 + 512]
                        k.act(ya, ya, AF.Identity, zk, zk, scale=m1[:, 40 + oc:41 + oc])
                        k.stt(ya, x[:, oc, :], ALPHA, ya, ALU.mult, ALU.add, x.k + zk, zk)
                    ln_tile(g, lambda kc: yacc[:, kc, c0:c0 + 512], zk, sq, mean, rstd,
                            lambda kc: g.lng[l][:, 8 + kc:9 + kc], lambda kc: g.lnb[l][:, 8 + kc:9 + kc],
                            lambda kc: x[:, kc, :], x.k, 512)
                    if not last:
                        k.dma('sp', XTv[:, :, t0:t0 + 512], x[:], x.k, S['XT'].k)
                    else:
                        for tg in range(4):
                            o = ot[tg % 2]
                            for half in range(2):
                                ps = g.ps[half]
                                for j in range(4):
                                    kc = half * 4 + j
                                    k.tr(ps[:, j * 128:(j + 1) * 128], x[:, kc, tg * 128:(tg + 1) * 128], g.ident[:],
                                         x.k + g.ident.k, [ps.k[0]])
                                k.cp('act' if half else 'dve', o[:, half * 512:(half + 1) * 512], ps[:, :],
                                     [ps.k[0]], o.k)
                            k.dma('sp', g.out[t0 + tg * 128:t0 + (tg + 1) * 128, :], o[:], o.k, [])
                k.barrier()
        k.barrier()


def head_rms_fm(g, k, pin, outap, outk, sqt, rst, nrm, extra_scale):
    pss = g.ps[7]
    k.act(sqt[:], pin[:, :], AF.Square, [pin.k[0]], sqt.k)
    k.mm(pss[:, :], g.blk[:], sqt[:], True, True, g.blk.k + sqt.k, [pss.k[0]])
    k.ts('dve', rst[:], pss[:, :], 1.0 / 64, QK_EPS, ALU.mult, ALU.add, [pss.k[0]], rst.k)
    k.act(rst[:], rst[:], AF.Sqrt, rst.k, rst.k)
    k.op('dve', lambda e: e.reciprocal(out=rst[:], in_=rst[:]), rst.k, rst.k)
    k.tt('dve', sqt[:], pin[:, :], rst[:], ALU.mult, [pin.k[0]] + rst.k, sqt.k)
    k.ts('pool', outap, sqt[:], nrm[:, 0:1], extra_scale, ALU.mult, ALU.mult, sqt.k + nrm.k, outk)


def phase_kv(g):
    k, I, S, T = g.k, g.I, g.S, g.T
    g.FQ = Buf(g.nc.dram_tensor("FQ", [NH, 2, T], F32, kind="Internal").ap())
    g.FK = Buf(g.nc.dram_tensor("FKn", [NH, 2, T], F32, kind="Internal").ap())
    with ExitStack() as ph:
        wv_ = lambda ap: ap.rearrange("(kc p) n -> p kc n", p=128)
        wk = sb(g, ph, "kv_wk", [128, KC, D], BF16)
        wv = sb(g, ph, "kv_wv", [128, KC, D], BF16)
        wf = sb(g, ph, "kv_wf", [128, KC, NH], BF16)
        k.dma('pool', wk[:], wv_(I['kv_w'][:, 0:D]), [], wk.k)
        k.dma('pool', wv[:], wv_(I['kv_w'][:, D:2 * D]), [], wv.k)
        k.dma('pool', wf[:], wv_(I['kv_w'][:, 2 * D:2 * D + NH]), [], wf.k)
        kn = sb(g, ph, "kv_kn", [128, 1])
        k.dma('sp', kn[0:64, :], I['kv_knorm'], [], kn.k)
        k.dma('sp', kn[64:128, :], I['kv_knorm'], [], kn.k)
        fb = sb(g, ph, "kv_fb", [NH, 1])
        k.dma('sp', fb[:], I['kv_fb'], [], fb.k)
        k.barrier()
        xb = [sb(g, ph, "kv_x%d" % i, [128, KC, 512]) for i in range(2)]
        hk = [sb(g, ph, "kv_h%d" % i, [128, KC, 512], BF16) for i in range(2)]
        kst = [sb(g, ph, "kv_ks%d" % i, [128, KC, 512], BF16) for i in range(2)]
        vst = [sb(g, ph, "kv_vs%d" % i, [128, D], BF16) for i in range(2)]
        sqt = sb(g, ph, "kv_sq", [128, 512])
        rst = sb(g, ph, "kv_rs", [128, 512])
        lf = sb(g, ph, "kv_lf", [NH, 512])
        fc = [sb(g, ph, "kv_fc%d" % i, [NH, 512]) for i in range(2)]
        nfc = [sb(g, ph, "kv_nfc%d" % i, [NH, 512]) for i in range(2)]
        XTv = S['XT'].t.rearrange("(kc p) t -> p kc t", p=128)
        KTv = S['KT'].t.rearrange("(kc p) t -> p kc t", p=128)
        for ti in range(T // 512):
            t0 = ti * 512
            x, h, ks = xb[ti % 2], hk[ti % 2], kst[ti % 2]
            k.dma('sp', x[:], XTv[:, :, t0:t0 + 512], S['XT'].k, x.k)
            for kc in range(KC):
                k.act(h[:, kc, :], x[:, kc, :], AF.Identity, x.k, h.k,
                      scale=g.kvmod1[:, 8 + kc:9 + kc], bias=g.kvmod[:, kc:kc + 1])
            for oc in range(KC):
                p = g.ps[oc % 2]
                for kc in range(KC):
                    k.mm(p[:, :], wk[:, kc, oc * 128:(oc + 1) * 128], h[:, kc, :], kc == 0, kc == KC - 1,
                         wk.k + h.k, [p.k[0]])
                head_rms_fm(g, k, p, ks[:, oc, :], ks.k, sqt, rst, kn, 1.0)
            k.dma('sp', KTv[:, :, t0:t0 + 512], ks[:], ks.k, S['KT'].k)
            for tg in range(4):
                vs = vst[tg % 2]
                for half in range(2):
                    p = g.ps[2 + half]
                    for kc in range(KC):
                        k.mm(p[:, :], h[:, kc, tg * 128:(tg + 1) * 128], wv[:, kc, half * 512:(half + 1) * 512],
                             kc == 0, kc == KC - 1, wv.k + h.k, [p.k[0]])
                    k.cp('act' if half else 'dve', vs[:, half * 512:(half + 1) * 512], p[:, :], [p.k[0]], vs.k)
                k.dma('sp', S['VK'].t[t0 + tg * 128:t0 + (tg + 1) * 128, :], vs[:], vs.k, S['VK'].k)
            p = g.ps[4]
            for kc in range(KC):
                k.mm(p[0:NH, :], wf[:, kc, :], h[:, kc, :], kc == 0, kc == KC - 1, wf.k + h.k, [p.k[0]])
            k.act(lf[:], p[0:NH, :], AF.Sigmoid, [p.k[0]], lf.k, bias=fb[:, 0:1])
            k.act(lf[:], lf[:], AF.Ln, lf.k, lf.k)
            f, fp_, nf = fc[ti % 2], fc[(ti + 1) % 2], nfc[ti % 2]
            init = 0.0 if ti == 0 else fp_[:, 511:512]
            k.op('dve', lambda e, f=f, init=init: e.tensor_tensor_scan(
                out=f[:], data0=g.ones[0:NH, 0:512], data1=lf[:], initial=init, op0=ALU.mult, op1=ALU.add),
                g.ones.k + lf.k + fp_.k, f.k)
            k.ts('pool', nf[:], f[:], -1.0, None, ALU.mult, None, f.k, nf.k)
            k.dma('sp', g.FQ.t[:, 0, t0:t0 + 512], f[:], f.k, g.FQ.k)
            k.dma('sp', g.FQ.t[:, 1, t0:t0 + 512], g.ones[0:NH, 0:512], g.ones.k, g.FQ.k)
            k.dma('sp', g.FK.t[:, 0, t0:t0 + 512], g.ones[0:NH, 0:512], g.ones.k, g.FK.k)
            k.dma('sp', g.FK.t[:, 1, t0:t0 + 512], nf[:], nf.k, g.FK.k)
        k.barrier()


def fox_mixer(g, l, j):
    k, I, S, T = g.k, g.I, g.S, g.T
    mt, m1 = g.modT[l], g.mod1[l]
    XTv = S['XT'].t.rearrange("(kc p) t -> p kc t", p=128)
    QTv = S['QT'].t.rearrange("(kc p) t -> p kc t", p=128)
    SGv = S['SG'].t.rearrange("(kc p) t -> p kc t", p=128)
    with ExitStack() as ph:
        wv_ = lambda ap: ap.rearrange("(kc p) n -> p kc n", p=128)
        wq = sb(g, ph, "f_wq", [128, KC, D], BF16)
        wg = sb(g, ph, "f_wg", [128, KC, D], BF16)
        k.dma('pool', wq[:], wv_(I['fx_wqg'][j][:, 0:D]), [], wq.k)
        k.dma('pool', wg[:], wv_(I['fx_wqg'][j][:, D:2 * D]), [], wg.k)
        qn = sb(g, ph, "f_qn", [128, 1])
        k.dma('sp', qn[0:64, :], I['fx_qnorm'][j], [], qn.k)
        k.dma('sp', qn[64:128, :], I['fx_qnorm'][j], [], qn.k)
        k.barrier()
        xb = [sb(g, ph, "f_x%d" % i, [128, KC, 512]) for i in range(2)]
        hb = [sb(g, ph, "f_h%d" % i, [128, KC, 512], BF16) for i in range(2)]
        qst = [sb(g, ph, "f_qs%d" % i, [128, KC, 512], BF16) for i in range(2)]
        gst = [sb(g, ph, "f_gs%d" % i, [128, KC, 512], BF16) for i in range(2)]
        sqt = sb(g, ph, "f_sq", [128, 512])
        rst = sb(g, ph, "f_rs", [128, 512])
        for ti in range(T // 512):
            t0 = ti * 512
            x, h, qs, gs = xb[ti % 2], hb[ti % 2], qst[ti % 2], gst[ti % 2]
            k.dma('sp', x[:], XTv[:, :, t0:t0 + 512], S['XT'].k, x.k)
            for kc in range(KC):
                k.act(h[:, kc, :], x[:, kc, :], AF.Identity, x.k, h.k,
                      scale=m1[:, 8 + kc:9 + kc], bias=mt[:, kc:kc + 1])
            for oc in range(KC):
                p = g.ps[oc % 2]
                for kc in range(KC):
                    k.mm(p[:, :], wq[:, kc, oc * 128:(oc + 1) * 128], h[:, kc, :], kc == 0, kc == KC - 1,
                         wq.k + h.k, [p.k[0]])
                head_rms_fm(g, k, p, qs[:, oc, :], qs.k, sqt, rst, qn, HD ** -0.5)
                p2 = g.ps[2 + oc % 2]
                for kc in range(KC):
                    k.mm(p2[:, :], wg[:, kc, oc * 128:(oc + 1) * 128], h[:, kc, :], kc == 0, kc == KC - 1,
                         wg.k + h.k, [p2.k[0]])
                k.act(gs[:, oc, :], p2[:, :], AF.Sigmoid, [p2.k[0]], gs.k)
            k.dma('sp', QTv[:, :, t0:t0 + 512], qs[:], qs.k, S['QT'].k)
            k.dma('sp', SGv[:, :, t0:t0 + 512], gs[:], gs.k, S['SG'].k)
        k.barrier()
    with ExitStack() as ph:
        identb = sb(g, ph, "a_identb", [128, 128], BF16)
        k.cp('pool', identb[:], g.ident[:], g.ident.k, identb.k)
        zer = sb(g, ph, "a_zer", [128, 128])
        k.op('pool', lambda e: e.memset(zer[:], 0.0), [], zer.k)
        nmask = sb(g, ph, "a_nmask", [128, 128], BF16)
        k.op('pool', lambda e: e.affine_select(out=nmask[:], in_=zer[:], pattern=[[1, 128]], compare_op=ALU.is_ge,
                                               fill=-30000.0, base=0, channel_multiplier=-1), zer.k, nmask.k)
        onesb = sb(g, ph, "a_onesb", [128, 64], BF16)
        k.op('pool', lambda e: e.memset(onesb[:], 1.0), [], onesb.k)
        k.barrier()
        NTG = T // 128
        KTh = [sb(g, ph, "a_k%d" % i, [64, T], BF16) for i in range(2)]
        QTh = [sb(g, ph, "a_q%d" % i, [64, T], BF16) for i in range(2)]
        SGh = [sb(g, ph, "a_g%d" % i, [64, T], BF16) for i in range(2)]
        Vh = [sb(g, ph, "a_v%d" % i, [128, NTG, 64], BF16) for i in range(2)]
        FQh = [sb(g, ph, "a_fq%d" % i, [2, T]) for i in range(2)]
        FKh = [sb(g, ph, "a_fk%d" % i, [2, T]) for i in range(2)]
        Pb = [sb(g, ph, "a_p%d" % i, [128, 512], BF16) for i in range(3)]
        rl = sb(g, ph, "a_rl", [64, 512])
        of = sb(g, ph, "a_of", [64, 512])
        ost = [sb(g, ph, "a_os%d" % i, [64, 512], BF16) for i in range(2)]
        it = 0
        for hd in range(NH):
            b = hd % 2
            hr = slice(hd * 64, (hd + 1) * 64)
            k.dma('sp', KTh[b][:], S['KT'].t[hr, :], S['KT'].k, KTh[b].k)
            k.dma('sp', QTh[b][:], S['QT'].t[hr, :], S['QT'].k, QTh[b].k)
            k.dma('sp', SGh[b][:], S['SG'].t[hr, :], S['SG'].k, SGh[b].k)
            k.dma('sp', Vh[b][:], S['VK'].t[:, hr].rearrange("(tg p) d -> p tg d", p=128), S['VK'].k, Vh[b].k)
            k.dma('sp', FQh[b][:], g.FQ.t[hd], g.FQ.k, FQh[b].k)
            k.dma('sp', FKh[b][:], g.FK.t[hd], g.FK.k, FKh[b].k)
            for qg in range(T // 512):
                q0 = qg * 512
                Op, Lp = g.ps[2 + 2 * (qg % 2)], g.ps[3 + 2 * (qg % 2)]
                kts = list(range(4 * qg + 4))
                for idx, kt in enumerate(kts):
                    diag = kt >= 4 * qg
                    qlo = (kt - 4 * qg) * 128 if diag else 0
                    n = 512 - qlo
                    Sp = g.ps[idx % 2]
                    kc_ = slice(kt * 128, (kt + 1) * 128)
                    qc_ = slice(q0 + qlo, q0 + 512)
                    k.mm(Sp[:, 0:n], KTh[b][:, kc_], QTh[b][:, qc_], True, False, KTh[b].k + QTh[b].k, [Sp.k[0]])
                    k.mm(Sp[:, 0:n], FKh[b][:, kc_], FQh[b][:, qc_], False, not diag, FKh[b].k + FQh[b].k, [Sp.k[0]])
                    if diag:
                        k.mm(Sp[:, 0:128], identb[:], nmask[:], False, True, identb.k + nmask.k, [Sp.k[0]])
                    P = Pb[it % 3]
                    it += 1
                    k.act(P[:, 0:n], Sp[:, 0:n], AF.Exp, [Sp.k[0]], P.k)
                    first, lastf = idx == 0, idx == len(kts) - 1
                    k.mm(Op[0:64, qlo:512], Vh[b][:, kt, :], P[:, 0:n], first, lastf, Vh[b].k + P.k, [Op.k[0]])
                    k.mm(Lp[0:64, qlo:512], onesb[:], P[:, 0:n], first, lastf, onesb.k + P.k, [Lp.k[0]])
                k.op('dve', lambda e, Lp=Lp: e.reciprocal(out=rl[:], in_=Lp[0:64, :]), [Lp.k[0]], rl.k)
                k.tt('dve', of[:], Op[0:64, :], rl[:], ALU.mult, [Op.k[0]] + rl.k, of.k)
                o = ost[qg % 2]
                k.tt('pool', o[:], of[:], SGh[b][:, q0:q0 + 512], ALU.mult, of.k + SGh[b].k, o.k)
                k.dma('sp', S['MO'].t[hr, q0:q0 + 512], o[:], o.k, S['MO'].k)
        k.barrier()


def setup_rwkv_consts(g, es):
    k = g.k
    ones = g.ones
    su = sb(g, es, "c_su", [128, 128])
    iu = sb(g, es, "c_iu", [128, 128])
    g.mask4 = sb(g, es, "c_mask4", [128, 512])
    g.sl = sb(g, es, "c_sl", [128, 128])
    g.rm = sb(g, es, "c_rm", [128, 256])
    k.op('pool', lambda e: e.affine_select(out=su[:], in_=ones[:, 0:128], pattern=[[1, 128]], compare_op=ALU.is_gt,
                                           fill=0.0, base=0, channel_multiplier=-1), ones.k, su.k)
    k.op('pool', lambda e: e.affine_select(out=iu[:], in_=ones[:, 0:128], pattern=[[1, 128]], compare_op=ALU.is_ge,
                                           fill=0.0, base=0, channel_multiplier=-1), ones.k, iu.k)
    k.op('pool', lambda e: e.affine_select(out=g.sl[:], in_=ones[:, 0:128], pattern=[[-1, 128]], compare_op=ALU.is_gt,
                                           fill=0.0, base=0, channel_multiplier=1), ones.k, g.sl.k)
    k.tt('pool', g.sl[:], g.sl[:], g.blk[:], ALU.mult, g.sl.k + g.blk.k, g.sl.k)
    for q in range(4):
        src = su if q < 2 else iu
        k.tt('pool', g.mask4[:, q * 128:(q + 1) * 128], src[:], g.blk[:], ALU.mult, src.k + g.blk.k, g.mask4.k)
    k.op('pool', lambda e: e.memset(g.rm[:], 1.0), [], g.rm.k)
    for q in range(4):
        k.op('pool', lambda e, q=q: e.memset(g.rm[:, q * 64:q * 64 + 1], 0.0), [], g.rm.k)
    k.barrier()


def rwkv_mixer(g, l):
    k, I, S, T = g.k, g.I, g.S, g.T
    mt, m1 = g.modT[l], g.mod1[l]
    W = 256
    ps = g.ps
    with ExitStack() as ph:
        setup_rwkv_consts(g, ph)
        wv_ = lambda ap: ap.rearrange("(kc p) n -> p kc n", p=128)
        wr_ = sb(g, ph, "r_wr", [128, KC, D], BF16)
        wk_ = sb(g, ph, "r_wk", [128, KC, D], BF16)
        wvv = sb(g, ph, "r_wv", [128, KC, D], BF16)
        k.dma('pool', wr_[:], wv_(I['rw_rkv'][l, 0]), [], wr_.k)
        k.dma('pool', wk_[:], wv_(I['rw_rkv'][l, 1]), [], wk_.k)
        k.dma('pool', wvv[:], wv_(I['rw_rkv'][l, 2]), [], wvv.k)
        w1 = sb(g, ph, "r_w1", [128, KC, 64], BF16)
        a1 = sb(g, ph, "r_a1", [128, KC, 64], BF16)
        g1 = sb(g, ph, "r_g1", [128, KC, 160], BF16)
        k.dma('pool', w1[:], wv_(I['rw_w1'][l]), [], w1.k)
        k.dma('pool', a1[:], wv_(I['rw_a1'][l]), [], a1.k)
        k.dma('pool', g1[:], wv_(I['rw_g1'][l]), [], g1.k)
        w2 = sb(g, ph, "r_w2", [64, D], BF16)
        a2 = sb(g, ph, "r_a2", [64, D], BF16)
        g2 = sb(g, ph, "r_g2", [128, 2, D], BF16)
        k.dma('pool', w2[:], I['rw_w2'][l], [], w2.k)
        k.dma('pool', a2[:], I['rw_a2'][l], [], a2.k)
        k.dma('pool', g2[:, 0, :], I['rw_g2'][l, 0:128, :], [], g2.k)
        k.dma('pool', g2[0:32, 1, :], I['rw_g2'][l, 128:160, :], [], g2.k)
        if l > 0:
            v1 = sb(g, ph, "r_v1", [128, KC, 32], BF16)
            v2 = sb(g, ph, "r_v2", [32, D], BF16)
            k.dma('pool', v1[:], wv_(I['rw_v1'][l - 1]), [], v1.k)
            k.dma('pool', v2[:], I['rw_v2'][l - 1], [], v2.k)
        st = sb(g, ph, "r_lvst", [128, 128])
        pv = {}
        for nm, R in [('rw_mu', 48), ('rw_w0', 8), ('rw_a0', 8), ('rw_kk', 8), ('rw_ka', 8), ('rw_rk', 8),
                      ('rw_gn_g', 8), ('rw_gn_b', 8)]:
            pv[nm] = sb(g, ph, "r_p_" + nm, [128, R])
            load_vecT(g, pv[nm], I[nm][l], R, st)
        if l > 0:
            pv['rw_v0'] = sb(g, ph, "r_p_v0", [128, 8])
            load_vecT(g, pv['rw_v0'], I['rw_v0'][l - 1], 8, st)
        hm = sb(g, ph, "r_hm", [128, 2])
        k.cp('pool', hm[:, 0:1], g.blk[:, 0:1], g.blk.k, hm.k)
        k.cp('pool', hm[:, 1:2], g.blk[:, 127:128], g.blk.k, hm.k)
        Bth = [sb(g, ph, "r_Bth%d" % i, [128, W]) for i in range(2)]
        Kth = [sb(g, ph, "r_Kth%d" % i, [128, W]) for i in range(2)]
        Ath = [sb(g, ph, "r_Ath%d" % i, [128, W]) for i in range(2)]
        VTc = [sb(g, ph, "r_VTc%d" % i, [128, 128]) for i in range(2)]
        VTm = [sb(g, ph, "r_VTm%d" % i, [128, 128]) for i in range(2)]
        cmk = [sb(g, ph, "r_cmk%d" % i, [128, 128]) for i in range(2)]
        for i in range(2):
            k.op('pool', lambda e, i=i: e.memset(cmk[i][:], 0.0), [], cmk[i].k)
            k.op('pool', lambda e, i=i: e.memset(cmk[i][:, i * 64:(i + 1) * 64], 1.0), [], cmk[i].k)
        omka = sb(g, ph, "r_omka", [128, 8])
        k.ts('dve', omka[:], pv['rw_ka'][:], -1.0, 1.0, ALU.mult, ALU.add, pv['rw_ka'].k, omka.k)
        k.barrier()
        state = [sb(g, ph, "r_state%d" % i, [128, 8, 64], F32, n=8) for i in range(2)]
        k.op('pool', lambda e: e.memset(state[0][:], 0.0), [], state[0].k)
        spar = [0] * 8
        hbuf = sb(g, ph, "r_h", [128, KC, W + 1])
        k.op('pool', lambda e: e.memset(hbuf[:, :, 0:1], 0.0), [], hbuf.k)
        xx = sb(g, ph, "r_xx", [128, KC, W])
        xb = xx
        xm = [sb(g, ph, "r_xm%d" % i, [128, KC, W], BF16) for i in range(6)]
        tw = sb(g, ph, "r_tw", [64, W], BF16)
        ta = sb(g, ph, "r_ta", [64, W], BF16)
        tg0 = sb(g, ph, "r_tg0", [128, W], BF16)
        tg1 = sb(g, ph, "r_tg1", [32, W], BF16)
        tv = sb(g, ph, "r_tv", [32, W], BF16)
        names = ['r', 'k', 'v', 'wl', 'sa', 'g', 'kk', 't1', 'rs', 'kkn', 'kf', 'b', 'c', 'd', 'eg', 'em', 'ed', 'ep',
                 'Rt', 'At', 'Kt', 'Bt', 'Kh', 'Bh']
        t = {n: sb(g, ph, "r_t_" + n, [128, W]) for n in names}
        t['y'], t['y2'], t['mn'], t['bon'], t['vf'] = t['eg'], t['em'], t['ed'], t['ep'], t['d']
        gl_ = sb(g, ph, "r_gl", [128, 4])
        KhT = sb(g, ph, "r_KhT", [128, 2, 128])
        BhT = sb(g, ph, "r_BhT", [128, 2, 128])
        VT = sb(g, ph, "r_VT", [128, 2, 128])
        Mall = sb(g, ph, "r_Mall", [128, 4, 512])
        Nall = [sb(g, ph, "r_Nall%d" % i, [128, 4, 128]) for i in range(2)]
        Ntall = [sb(g, ph, "r_Ntall%d" % i, [128, 4, 128]) for i in range(2)]
        Pall = sb(g, ph, "r_Pall", [128, 4, 128])
        sl4 = sb(g, ph, "r_sl4", [128, 4, 128])
        id4 = sb(g, ph, "r_id4", [128, 4, 128])
        for q in range(4):
            k.cp('pool', sl4[:, q, :], g.sl[:], g.sl.k, sl4.k)
            k.cp('pool', id4[:, q, :], g.ident[:], g.ident.k, id4.k)
        Xs2 = sb(g, ph, "r_Xs2", [128, 128])
        UP = sb(g, ph, "r_UP", [128, 2, 128])
        k.op('pool', lambda e: e.memset(UP[:], 0.0), [], UP.k)
        _b = UP.t[:]
        UPdiag = bass.AP(tensor=_b.tensor, offset=_b.offset, ap=[list(_b.ap[0]), [192, 2], [1, 64]])
        SP = sb(g, ph, "r_SP", [128, 128])
        BhTm = [[sb(g, ph, "r_BhTm%d%d" % (a, b_), [128, 128]) for b_ in range(2)] for a in range(2)]
        KhTm = [[sb(g, ph, "r_KhTm%d%d" % (a, b_), [128, 128]) for b_ in range(2)] for a in range(2)]
        mob = sb(g, ph, "r_mo", [128, W], BF16)
        XTv = S['XT'].t.rearrange("(kc p) t -> p kc t", p=128)
        unit_i = 0
        for ti in range(T // W):
            t0 = ti * W
            h = hbuf
            if ti > 0:
                k.cp('pool', h[:, :, 0:1], h[:, :, W:W + 1], h.k, h.k)
            k.dma('sp', xb[:], XTv[:, :, t0:t0 + W], S['XT'].k, xb.k)
            for kc in range(KC):
                k.act(h[:, kc, 1:W + 1], xb[:, kc, :], AF.Identity, xb.k, h.k,
                      scale=m1[:, 8 + kc:9 + kc], bias=mt[:, kc:kc + 1])
            k.tt('dve', xx[:], h[:, :, 0:W], h[:, :, 1:W + 1], ALU.subtract, h.k, xx.k)
            for i in range(6):
                if i == 3 and False:
                    continue
                for kc in range(KC):
                    k.stt(xm[i][:, kc, :], xx[:, kc, :], pv['rw_mu'][:, i * 8 + kc:i * 8 + kc + 1], h[:, kc, 1:W + 1],
                          ALU.mult, ALU.add, xx.k + h.k, xm[i].k)
            p = ps[3]
            for kc in range(KC):
                k.mm(p[0:64, 0:W], w1[:, kc, :], xm[1][:, kc, :], kc == 0, kc == KC - 1, w1.k + xm[1].k, [p.k[0]])
            k.act(tw[:], p[0:64, 0:W], AF.Tanh, [p.k[0]], tw.k)
            for kc in range(KC):
                k.mm(p[0:64, W:2 * W], a1[:, kc, :], xm[4][:, kc, :], kc == 0, kc == KC - 1, a1.k + xm[4].k, [p.k[2]])
            k.cp('act', ta[:], p[0:64, W:2 * W], [p.k[2]], ta.k)
            p = ps[2]
            for kc in range(KC):
                k.mm(p[:, 0:W], g1[:, kc, 0:128], xm[5][:, kc, :], kc == 0, kc == KC - 1, g1.k + xm[5].k, [p.k[0]])
            k.act(tg0[:], p[:, 0:W], AF.Sigmoid, [p.k[0]], tg0.k)
            for kc in range(KC):
                k.mm(p[0:32, W:2 * W], g1[:, kc, 128:160], xm[5][:, kc, :], kc == 0, kc == KC - 1, g1.k + xm[5].k,
                     [p.k[2]])
            k.act(tg1[:], p[0:32, W:2 * W], AF.Sigmoid, [p.k[2]], tg1.k)
            if l > 0:
                p = ps[1]
                for kc in range(KC):
                    k.mm(p[0:32, 0:W], v1[:, kc, :], xm[3][:, kc, :], kc == 0, kc == KC - 1, v1.k + xm[3].k, [p.k[0]])
                k.cp('act', tv[:], p[0:32, 0:W], [p.k[0]], tv.k)
            for hp in range(8):
                if RW_STOP[0] <= 1:
                    break
                oc = hp
                ocs = slice(oc * 128, (oc + 1) * 128)
                col = lambda buf, j: buf[:, j:j + 1]

                def proj(pb, c0, kk_, wt, xi):
                    for kc in range(KC):
                        k.mm(pb[:, c0:c0 + W], wt[:, kc, ocs], xm[xi][:, kc, :], kc == 0, kc == KC - 1,
                             wt.k + xm[xi].k, [pb.k[kk_]])
                proj(ps[0], 0, 0, wr_, 0)
                proj(ps[0], W, 2, wk_, 2)
                proj(ps[1], 0, 0, wvv, 3)
                k.mm(ps[1][:, W:2 * W], w2[:, ocs], tw[:], True, True, w2.k + tw.k, [ps[1].k[2]])
                k.mm(ps[2][:, 0:W], a2[:, ocs], ta[:], True, True, a2.k + ta.k, [ps[2].k[0]])
                k.mm(ps[2][:, W:2 * W], g2[:, 0, ocs], tg0[:], True, False, g2.k + tg0.k, [ps[2].k[2]])
                k.mm(ps[2][:, W:2 * W], g2[0:32, 1, ocs], tg1[:], False, True, g2.k + tg1.k, [ps[2].k[2]])
                if l > 0:
                    k.mm(ps[3][:, 0:W], v2[:, ocs], tv[:], True, True, v2.k + tv.k, [ps[3].k[0]])
                k.cp('act', t['r'][:], ps[0][:, 0:W], [ps[0].k[0]], t['r'].k)
                k.cp('act', t['k'][:], ps[0][:, W:2 * W], [ps[0].k[2]], t['k'].k)
                k.cp('dve', t['v'][:], ps[1][:, 0:W], [ps[1].k[0]], t['v'].k)
                k.cp('act', t['g'][:], ps[2][:, W:2 * W], [ps[2].k[2]], t['g'].k)
                VFv = S['VF'].t[ocs, t0:t0 + W]
                if l == 0:
                    k.dma('sp', VFv, t['v'][:], t['v'].k, S['VF'].k)
                else:
                    k.dma('sp', t['vf'][:], VFv, S['VF'].k, t['vf'].k)
                    k.act(t['y2'][:], ps[3][:, 0:W], AF.Sigmoid, [ps[3].k[0]], t['y2'].k, bias=col(pv['rw_v0'], oc))
                    k.tt('pool', t['vf'][:], t['vf'][:], t['v'][:], ALU.subtract, t['vf'].k + t['v'].k, t['vf'].k)
                    k.tt('pool', t['vf'][:], t['vf'][:], t['y2'][:], ALU.mult, t['vf'].k + t['y2'].k, t['vf'].k)
                    k.tt('pool', t['v'][:], t['v'][:], t['vf'][:], ALU.add, t['vf'].k + t['v'].k, t['v'].k)
                k.act(t['wl'][:], ps[1][:, W:2 * W], AF.Sigmoid, [ps[1].k[2]], t['wl'].k, bias=col(pv['rw_w0'], oc))
                k.act(t['sa'][:], ps[2][:, 0:W], AF.Sigmoid, [ps[2].k[0]], t['sa'].k, bias=col(pv['rw_a0'], oc))
                k.ts('pool', t['wl'][:], t['wl'][:], -0.6065306597126334, None, ALU.mult, None, t['wl'].k, t['wl'].k)
                k.ts('dve', t['kk'][:], t['k'][:], col(pv['rw_kk'], oc), None, ALU.mult, None, t['k'].k, t['kk'].k)
                k.tt('pool', t['t1'][:], t['kk'][:], t['kk'][:], ALU.mult, t['kk'].k, t['t1'].k)
                k.mm(ps[3][:, W:2 * W], g.blk[:], t['t1'][:], True, True, g.blk.k + t['t1'].k, [ps[3].k[2]])
                k.act(t['rs'][:], ps[3][:, W:2 * W], AF.Sqrt, [ps[3].k[2]], t['rs'].k)
                k.ts('dve', t['rs'][:], t['rs'][:], 1e-12, None, ALU.max, None, t['rs'].k, t['rs'].k)
                k.op('dve', lambda e: e.reciprocal(out=t['rs'][:], in_=t['rs'][:]), t['rs'].k, t['rs'].k)
                k.tt('dve', t['kkn'][:], t['kk'][:], t['rs'][:], ALU.mult, t['kk'].k + t['rs'].k, t['kkn'].k)
                k.ts('dve', t['t1'][:], t['sa'][:], col(pv['rw_ka'], oc), col(omka, oc), ALU.mult, ALU.add,
                     t['sa'].k, t['t1'].k)
                k.tt('pool', t['kf'][:], t['k'][:], t['t1'][:], ALU.mult, t['k'].k + t['t1'].k, t['kf'].k)
                k.tt('pool', t['b'][:], t['kkn'][:], t['sa'][:], ALU.mult, t['kkn'].k + t['sa'].k, t['b'].k)
                k.op('dve', lambda e: e.tensor_tensor_scan(out=t['c'][:], data0=g.rm[:], data1=t['wl'][:], initial=0.0,
                                                           op0=ALU.mult, op1=ALU.add),
                     g.rm.k + t['wl'].k, t['c'].k)
                for j in range(4):
                    cs_ = slice(j * 64, (j + 1) * 64)
                    k.ts('dve', t['d'][:, cs_], t['c'][:, cs_], -1.0, t['c'][:, j * 64 + 63:j * 64 + 64],
                         ALU.mult, ALU.add, t['c'].k, t['d'].k)
                k.tt('pool', t['ep'][:], t['c'][:], t['wl'][:], ALU.subtract, t['c'].k + t['wl'].k, t['ep'].k)
                k.act(t['eg'][:], t['c'][:], AF.Exp, t['c'].k, t['eg'].k)
                k.act(t['em'][:], t['c'][:], AF.Exp, t['c'].k, t['em'].k, scale=-1.0)
                k.act(t['ed'][:], t['d'][:], AF.Exp, t['d'].k, t['ed'].k)
                k.act(t['ep'][:], t['ep'][:], AF.Exp, t['ep'].k, t['ep'].k)
                k.act(gl_[:], t['c'][:, :].rearrange("p (j t) -> p j t", t=64)[:, :, 63], AF.Exp, t['c'].k, gl_.k)
                k.tt('dve', t['Rt'][:], t['r'][:], t['eg'][:], ALU.mult, t['r'].k + t['eg'].k, t['Rt'].k)
                k.stt(t['At'][:], t['kkn'][:], -1.0, t['ep'][:], ALU.mult, ALU.mult, t['kkn'].k + t['ep'].k, t['At'].k)
                k.tt('pool', t['Kt'][:], t['kf'][:], t['em'][:], ALU.mult, t['kf'].k + t['em'].k, t['Kt'].k)
                k.tt('pool', t['Bt'][:], t['b'][:], t['em'][:], ALU.mult, t['b'].k + t['em'].k, t['Bt'].k)
                k.tt('dve', t['Kh'][:], t['kf'][:], t['ed'][:], ALU.mult, t['kf'].k + t['ed'].k, t['Kh'].k)
                k.tt('pool', t['Bh'][:], t['b'][:], t['ed'][:], ALU.mult, t['b'].k + t['ed'].k, t['Bh'].k)
                if RW_STOP[0] <= 2:
                    continue
                for hh in range(2):
                    k.ts('pool', Bth[hh][:], t['Bt'][:], hm[:, hh:hh + 1], None, ALU.mult, None, t['Bt'].k + hm.k, Bth[hh].k)
                    k.ts('pool', Kth[hh][:], t['Kt'][:], hm[:, hh:hh + 1], None, ALU.mult, None, t['Kt'].k + hm.k, Kth[hh].k)
                    k.ts('dve', Ath[hh][:], t['At'][:], hm[:, hh:hh + 1], None, ALU.mult, None, t['At'].k + hm.k, Ath[hh].k)
                for cp in range(2):
                    cc = slice(cp * 128, (cp + 1) * 128)
                    k.tr(ps[6][:, cp * 128:(cp + 1) * 128], t['Kh'][:, cc], g.ident[:], t['Kh'].k + g.ident.k, [ps[6].k[0]])
                    k.tr(ps[6][:, 256 + cp * 128:256 + (cp + 1) * 128], t['Bh'][:, cc], g.ident[:],
                         t['Bh'].k + g.ident.k, [ps[6].k[0]])
                    k.tr(ps[7][:, 256 + cp * 128:256 + (cp + 1) * 128], t['v'][:, cc], g.ident[:],
                         t['v'].k + g.ident.k, [ps[7].k[3]])
                k.cp('act', KhT[:], ps[6][:, 0:256].rearrange("p (a b) -> p a b", b=128), [ps[6].k[0]], KhT.k)
                k.cp('dve', BhT[:], ps[6][:, 256:512].rearrange("p (a b) -> p a b", b=128), [ps[6].k[0]], BhT.k)
                k.cp('act', VT[:], ps[7][:, 256:512].rearrange("p (a b) -> p a b", b=128), [ps[7].k[3]], VT.k)
                Yp = ps[0]
                mbank = [ps[4], ps[6], ps[1], ps[2]]
                for cp in range(2):
                    cc = slice(cp * 128, (cp + 1) * 128)
                    for hh in range(2):
                        u = cp * 2 + hh
                        Mp = mbank[u]
                        rd = Bth[hh].k + t['At'].k + Kth[hh].k + t['Rt'].k
                        k.mm(Mp[:, 0:128], Bth[hh][:, cc], t['At'][:, cc], True, True, rd, [Mp.k[0]])
                        k.mm(Mp[:, 128:256], Kth[hh][:, cc], t['At'][:, cc], True, True, rd, [Mp.k[0]])
                        k.mm(Mp[:, 256:384], Bth[hh][:, cc], t['Rt'][:, cc], True, True, rd, [Mp.k[0]])
                        k.mm(Mp[:, 384:512], Kth[hh][:, cc], t['Rt'][:, cc], True, True, rd, [Mp.k[0]])
                        k.mm(ps[5][:, u * 128:(u + 1) * 128], t['At'][:, cc], Bth[hh][:, cc], True, True, rd, [ps[5].k[0]])
                        k.tt('dve', Mall[:, u, :], Mp[:, :], g.mask4[:], ALU.mult, [Mp.k[0]] + g.mask4.k, Mall.k)
                        k.tt('pool', BhTm[cp][hh][:], BhT[:, cp, :], cmk[hh][:], ALU.mult, BhT.k + cmk[hh].k, BhTm[cp][hh].k)
                        k.tt('pool', KhTm[cp][hh][:], KhT[:, cp, :], cmk[hh][:], ALU.mult, KhT.k + cmk[hh].k, KhTm[cp][hh].k)
                k.tt('dve', Nall[0][:].rearrange("p a b -> p (a b)"), ps[5][:, :], sl4[:].rearrange("p a b -> p (a b)"),
                     ALU.mult, [ps[5].k[0]] + sl4.k, Nall[0].k)
                k.cp('pool', Ntall[0][:], Mall[:, :, 0:128], Mall.k, Ntall[0].k)
                k.tt('pool', Pall[:], Mall[:, :, 0:128], id4[:], ALU.add, Mall.k + id4.k, Pall.k)
                pa, pb_, pc = ps[1], ps[2], ps[3]
                for lev in range(1, 6):
                    Np, Ntp = Nall[(lev - 1) % 2], Ntall[(lev - 1) % 2]
                    Nn, Ntn = Nall[lev % 2], Ntall[lev % 2]
                    for u in range(4):
                        k.mm(pa[:, u * 128:(u + 1) * 128], Ntp[:, u, :], Np[:, u, :], True, True, Ntp.k + Np.k, [pa.k[0]])
                    if lev < 5:
                        for u in range(4):
                            k.mm(pb_[:, u * 128:(u + 1) * 128], Np[:, u, :], Ntp[:, u, :], True, True, Ntp.k + Np.k,
                                 [pb_.k[0]])
                    k.cp('act', Nn[:].rearrange("p a b -> p (a b)"), pa[:, :], [pa.k[0]], Nn.k)
                    if lev < 5:
                        k.cp('dve', Ntn[:].rearrange("p a b -> p (a b)"), pb_[:, :], [pb_.k[0]], Ntn.k)
                    for u in range(4):
                        k.mm(pc[:, u * 128:(u + 1) * 128], Nn[:, u, :], Pall[:, u, :], True, True, Nn.k + Pall.k, [pc.k[0]])
                    k.tt('dve', Pall[:].rearrange("p a b -> p (a b)"), pc[:, :], Pall[:].rearrange("p a b -> p (a b)"),
                         ALU.add, [pc.k[0]] + Pall.k, Pall.k)
                for cp in range(2):
                    cc = slice(cp * 128, (cp + 1) * 128)
                    for q in range(2):
                        k.ts('pool', VTc[q][:], VT[:, cp, :], hm[:, q:q + 1], None, ALU.mult, None, VT.k + hm.k, VTc[q].k)
                        k.tt('pool', VTm[q][:], VT[:, cp, :], cmk[q][:], ALU.mult, VT.k + cmk[q].k, VTm[q].k)
                    for ch in range(2):
                        j = cp * 2 + ch
                        c64 = slice(j * 64, (j + 1) * 64)
                        so, sn = state[spar[hp]], state[1 - spar[hp]]
                        sk = [so.k[hp]]
                        p7 = ps[7]
                        for hh in range(2):
                            u = cp * 2 + hh
                            hcol = slice(hh * 64, (hh + 1) * 64)
                            k.mm(p7[:, hcol], Ath[hh][:, cc], so[:, hp, :], True, False, Ath[hh].k + sk, [p7.k[0]])
                            k.mm(p7[:, hcol], Mall[:, u, 128:256], VT[:, cp, hcol], False, True, Mall.k + VT.k, [p7.k[0]])
                        k.cp('act', Xs2[:], p7[:, 0:128], [p7.k[0]], Xs2.k)
                        for hh in range(2):
                            u = cp * 2 + hh
                            hcol = slice(hh * 64, (hh + 1) * 64)
                            k.mm(p7[:, 128 + hh * 64:128 + (hh + 1) * 64], Pall[:, u, :], Xs2[:, hcol], True, True,
                                 Pall.k + Xs2.k, [p7.k[0]])
                        k.ts('dve', UPdiag, p7[:, 128:256].rearrange("p (a b) -> p a b", b=64), hm[:, ch:ch + 1], None,
                             ALU.mult, None, [p7.k[0]] + hm.k, UP.k)
                        for hh in range(2):
                            hcol = slice(hh * 64, (hh + 1) * 64)
                            k.ts('pool', SP[:, hcol], so[:, hp, :], hm[:, hh:hh + 1], None, ALU.mult, None,
                                 sk + hm.k, SP.k)
                        T_ = p7[:, 256:320]
                        for hh in range(2):
                            hcol = slice(hh * 64, (hh + 1) * 64)
                            k.mm(T_, BhTm[cp][hh][:], UP[:, hh, hcol], hh == 0, False, BhTm[cp][hh].k + UP.k, [p7.k[0]])
                            k.mm(T_, KhTm[cp][hh][:], VTc[ch][:, hcol], False, hh == 1, KhTm[cp][hh].k + VTc[ch].k,
                                 [p7.k[0]])
                        k.stt(sn[:, hp, :], so[:, hp, :], gl_[:, j:j + 1], T_, ALU.mult, ALU.add,
                              sk + gl_.k + [p7.k[0]], [sn.k[hp]])
                        Y_ = Yp[:, c64]
                        k.mm(Y_, SP[:], t['Rt'][:, c64], True, False, SP.k + t['Rt'].k, [Yp.k[0]])
                        for hh in range(2):
                            u = cp * 2 + hh
                            k.mm(Y_, UP[:, hh, :], Mall[:, u, 256 + ch * 64:256 + (ch + 1) * 64], False, False,
                                 UP.k + Mall.k, [Yp.k[0]])
                            k.mm(Y_, VTm[hh][:], Mall[:, u, 384 + ch * 64:384 + (ch + 1) * 64], False, hh == 1,
                                 VTm[hh].k + Mall.k, [Yp.k[0]])
                        spar[hp] = 1 - spar[hp]
                if RW_STOP[0] <= 4:
                    continue
                k.cp('act', t['y'][:], Yp[:, 0:W], [Yp.k[0]], t['y'].k)
                k.tt('pool', t['y2'][:], t['y'][:], t['y'][:], ALU.mult, t['y'].k, t['y2'].k)
                k.mm(ps[1][:, 0:W], g.blk[:], t['y'][:], True, True, g.blk.k + t['y'].k, [ps[1].k[0]])
                k.mm(ps[1][:, W:2 * W], g.blk[:], t['y2'][:], True, True, g.blk.k + t['y2'].k, [ps[1].k[2]])
                k.ts('dve', t['mn'][:], ps[1][:, 0:W], 1.0 / 64, None, ALU.mult, None, [ps[1].k[0]], t['mn'].k)
                k.tt('pool', t['y2'][:], t['mn'][:], t['mn'][:], ALU.mult, t['mn'].k, t['y2'].k)
                k.stt(t['y2'][:], ps[1][:, W:2 * W], 1.0 / 64, t['y2'][:], ALU.mult, ALU.subtract,
                      [ps[1].k[2]] + t['y2'].k, t['y2'].k)
                k.ts('dve', t['y2'][:], t['y2'][:], GN_EPS, None, ALU.add, None, t['y2'].k, t['y2'].k)
                k.act(t['y2'][:], t['y2'][:], AF.Sqrt, t['y2'].k, t['y2'].k)
                k.op('dve', lambda e: e.reciprocal(out=t['y2'][:], in_=t['y2'][:]), t['y2'].k, t['y2'].k)
                k.tt('pool', t['y'][:], t['y'][:], t['mn'][:], ALU.subtract, t['y'].k + t['mn'].k, t['y'].k)
                k.tt('pool', t['y'][:], t['y'][:], t['y2'][:], ALU.mult, t['y'].k + t['y2'].k, t['y'].k)
                k.act(t['y'][:], t['y'][:], AF.Identity, t['y'].k, t['y'].k, scale=col(pv['rw_gn_g'], oc),
                      bias=col(pv['rw_gn_b'], oc))
                k.tt('pool', t['bon'][:], t['r'][:], t['kf'][:], ALU.mult, t['r'].k + t['kf'].k, t['bon'].k)
                k.ts('dve', t['bon'][:], t['bon'][:], col(pv['rw_rk'], oc), None, ALU.mult, None, t['bon'].k, t['bon'].k)
                k.mm(ps[3][:, W:2 * W], g.blk[:], t['bon'][:], True, True, g.blk.k + t['bon'].k, [ps[3].k[2]])
                k.tt('dve', t['bon'][:], ps[3][:, W:2 * W], t['v'][:], ALU.mult, [ps[3].k[2]] + t['v'].k, t['bon'].k)
                k.tt('pool', t['y'][:], t['y'][:], t['bon'][:], ALU.add, t['y'].k + t['bon'].k, t['y'].k)
                k.tt('pool', mob[:], t['y'][:], t['g'][:], ALU.mult, t['y'].k + t['g'].k, mob.k)
                k.dma('sp', S['MO'].t[ocs, t0:t0 + W], mob[:], mob.k, S['MO'].k)
        k.barrier()


_CACHE = {}


def prep_inputs(inp, b, T):
    f = lambda a: np.ascontiguousarray(np.asarray(a, dtype=np.float32))
    m = {}
    m['x'] = f(inp['x'][b][:T])
    m['c'] = f(inp['c'][b]).reshape(KC, 128)
    m['ada_w'] = f(inp['ada_w'])
    m['ada_b'] = f(inp['ada_b']).reshape(DEPTH, 48, 128)
    m['ln_g'] = f(inp['ln_g']).reshape(DEPTH, 16, 128)
    m['ln_b'] = f(inp['ln_b']).reshape(DEPTH, 16, 128)
    m['rw_mu'] = f(inp['rw_mu']).reshape(N_A, 48, 128)
    m['rw_rkv'] = f(inp['rw_rkv'])
    for nm in ['rw_w0', 'rw_a0', 'rw_kk', 'rw_ka', 'rw_rk', 'rw_gn_g', 'rw_gn_b']:
        m[nm] = f(inp[nm]).reshape(N_A, 8, 128)
    for nm in ['rw_w1', 'rw_w2', 'rw_a1', 'rw_a2', 'rw_g1', 'rw_g2', 'rw_wo', 'rw_v1', 'rw_v2',
               'kv_ada_w', 'kv_w', 'fx_wqg', 'fx_wo', 'moe_wgrp', 'moe_bgrp', 'moe_wexp', 'moe_bexp',
               'moe_wgate', 'moe_wup', 'moe_wdown']:
        m[nm] = f(inp[nm])
    m['rw_v0'] = f(inp['rw_v0']).reshape(1, 8, 128)
    m['kv_ada_b'] = f(inp['kv_ada_b']).reshape(16, 128)
    m['kv_fb'] = f(inp['kv_fb']).reshape(NH, 1)
    m['kv_knorm'] = f(inp['kv_knorm']).reshape(HD, 1)
    m['fx_qnorm'] = f(inp['fx_qnorm']).reshape(2, HD, 1)
    return m


def kernel(**inputs):
    B, T = inputs['x'].shape[0], inputs['x'].shape[1]
    NG = inputs['moe_wgrp'].shape[-1]
    NE = inputs['moe_wexp'].shape[-1]
    key = (T, NG, NE)
    if key not in _CACHE:
        _CACHE[key] = build(T, NG, NE // NG)[0]
    nc = _CACHE[key]
    shared = None
    maps = []
    for b in range(B):
        m = prep_inputs(inputs, b, T) if shared is None else dict(shared)
        if shared is None:
            shared = m
        else:
            m['x'] = np.ascontiguousarray(np.asarray(inputs['x'][b], dtype=np.float32))
            m['c'] = np.ascontiguousarray(np.asarray(inputs['c'][b], dtype=np.float32)).reshape(KC, 128)
        maps.append(m)
    res = run_bass_kernel_spmd(nc, maps, core_ids=list(range(B)))
    return np.stack([np.asarray(r['out']) for r in res.results]).astype(np.float32)
```

```python
import numpy as np
from contextlib import ExitStack
import concourse.bass as bass
import concourse.mybir as mybir
from concourse.bass_utils import run_bass_kernel_spmd

F32 = mybir.dt.float32
BF16 = mybir.dt.bfloat16
F32R = mybir.dt.float32r
AF = mybir.ActivationFunctionType
ALU = mybir.AluOpType
AX = mybir.AxisListType

D = 1024
KC = 8
HD = 64
NH = 16
DEPTH = 4
N_A = 2
ALPHA = (2 * DEPTH) ** 0.25
LN_EPS = 1e-5
GN_EPS = 64e-5
QK_EPS = 1e-6
DEXP = 512


class Tk:
    __slots__ = ("w", "r", "excl")

    def __init__(s):
        s.w = None
        s.r = {}
        s.excl = False


class Buf:
    def __init__(s, t, n=1):
        s.t = t
        s.k = [Tk() for _ in range(n)]

    def __getitem__(s, idx):
        return s.t[idx]


class _KL(list):
    def __getitem__(s, i):
        return list.__getitem__(s, 0)


class PBuf(Buf):
    def __init__(s, t):
        s.t = t
        s.k = _KL([Tk()])
        s.k[0].excl = True


class K:
    NS = 24

    def __init__(s, nc, es):
        s.nc = nc
        s.eng = {'pe': nc.tensor, 'act': nc.scalar, 'dve': nc.vector, 'pool': nc.gpsimd, 'sp': nc.sync}
        s.esem = {n: es.enter_context(nc.semaphore("s_" + n)) for n in s.eng}
        s.ecnt = {n: 0 for n in s.eng}
        s.seen = {n: {} for n in s.eng}
        s.dsem = [es.enter_context(nc.semaphore("d%d" % i)) for i in range(s.NS)]
        s.dcnt = [0] * s.NS
        s.dnext = {'hw': 0, 'sw': 0}
        s.dpool = {'hw': list(range(0, 16)), 'sw': list(range(16, s.NS))}
        s.same_sync = {'pe': False, 'act': True, 'dve': True, 'pool': True, 'sp': False}
        s.nins = 0

    def _wait(s, en, key, val):
        if s.seen[en].get(key, 0) >= val:
            return
        if key[0] == 'E':
            if key[1] == en and not s.same_sync[en]:
                return
            sem = s.esem[key[1]]
        else:
            sem = s.dsem[key[1]]
        s.eng[en].wait_ge(sem, val)
        s.seen[en][key] = val
        s.nins += 1

    def _need(s, reads, writes):
        need = {}
        for t in reads:
            if t.w is not None:
                k_, v = t.w
                if need.get(k_, 0) < v:
                    need[k_] = v
        for t in writes:
            if t.w is not None:
                k_, v = t.w
                if need.get(k_, 0) < v:
                    need[k_] = v
            for k_, v in t.r.items():
                if need.get(k_, 0) < v:
                    need[k_] = v
        return need

    def op(s, en, fn, reads=(), writes=()):
        ex = [t for t in reads if t.excl]
        if ex:
            reads = [t for t in reads if not t.excl]
            writes = list(writes) + ex
        for k_, v in s._need(reads, writes).items():
            s._wait(en, k_, v)
        ins = fn(s.eng[en])
        s.ecnt[en] += 1
        c = s.ecnt[en]
        ins.then_inc(s.esem[en], 1)
        key = ('E', en)
        for t in reads:
            t.r[key] = c
        for t in writes:
            t.w = (key, c)
            t.r = {}
        s.nins += 1

    def dma(s, q, out, in_, reads=(), writes=(), **kw):
        for k_, v in s._need(reads, writes).items():
            s._wait(q, k_, v)
        pn = 'sw' if q == 'pool' else 'hw'
        pl = s.dpool[pn]
        i = pl[s.dnext[pn] % len(pl)]
        s.dnext[pn] += 1
        if s.dcnt[i]:
            s._wait(q, ('D', i), s.dcnt[i] * 16)
        s.eng[q].dma_start(out=out, in_=in_, **kw).then_inc(s.dsem[i], 16)
        s.dcnt[i] += 1
        key = ('D', i)
        val = s.dcnt[i] * 16
        for t in reads:
            t.r[key] = val
        for t in writes:
            t.w = (key, val)
            t.r = {}
        s.nins += 1

    def barrier(s):
        for en in s.eng:
            for o in s.eng:
                if o != en and s.ecnt[o] > 0:
                    s._wait(en, ('E', o), s.ecnt[o])
            for i in range(s.NS):
                if s.dcnt[i] > 0:
                    s._wait(en, ('D', i), s.dcnt[i] * 16)

    def mm(s, out, lhsT, rhs, start, stop, reads, writes):
        s.op('pe', lambda e: e.matmul(out, lhsT, rhs, start=start, stop=stop), reads, writes)

    def tr(s, out, in_, ident, reads, writes):
        s.op('pe', lambda e: e.transpose(out, in_, ident), reads, writes)

    def act(s, out, in_, func, reads, writes, bias=None, scale=None, accum_out=None, en='act'):
        kw = {}
        if bias is not None:
            kw['bias'] = bias
        if scale is not None:
            kw['scale'] = scale
        if accum_out is not None:
            kw['accum_out'] = accum_out
        s.op('act', lambda e: e.activation(out, in_, func, **kw), reads, writes)

    def tt(s, en, out, in0, in1, op, reads, writes):
        s.op(en, lambda e: e.tensor_tensor(out=out, in0=in0, in1=in1, op=op), reads, writes)

    def ts(s, en, out, in0, s1, s2, op0, op1, reads, writes):
        if op1 is None:
            s.op(en, lambda e: e.tensor_scalar(out=out, in0=in0, scalar1=s1, scalar2=None, op0=op0), reads, writes)
        else:
            s.op(en, lambda e: e.tensor_scalar(out=out, in0=in0, scalar1=s1, scalar2=s2, op0=op0, op1=op1), reads, writes)

    def stt(s, out, in0, scalar, in1, op0, op1, reads, writes):
        s.op('dve', lambda e: e.scalar_tensor_tensor(out=out, in0=in0, scalar=scalar, in1=in1, op0=op0, op1=op1),
             reads, writes)

    def cp(s, en, out, in_, reads, writes):
        if en == 'act':
            s.op('act', lambda e: e.copy(out, in_), reads, writes)
        else:
            s.op(en, lambda e: e.tensor_copy(out=out, in_=in_), reads, writes)


class Ctx:
    pass


def build(T, NG, EPG, layers=DEPTH, dbg=None):
    NE = NG * EPG
    NR = NG + NE
    nc = bass.Bass("TRN2", target_bir_lowering=False)
    g = Ctx()
    g.T, g.NG, g.EPG, g.NE, g.NR = T, NG, EPG, NE, NR
    g.dbg = dbg
    n_a = min(N_A, layers)
    n_b = layers - n_a
    nv = max(n_a - 1, 0)

    def din(name, shape):
        return nc.dram_tensor(name, list(shape), F32, kind="ExternalInput").ap()

    I = {}
    I['x'] = din('x', [T, D])
    I['c'] = din('c', [KC, 128])
    I['ada_w'] = din('ada_w', [DEPTH, D, 6 * D])
    I['ada_b'] = din('ada_b', [DEPTH, 48, 128])
    I['ln_g'] = din('ln_g', [DEPTH, 16, 128])
    I['ln_b'] = din('ln_b', [DEPTH, 16, 128])
    I['rw_mu'] = din('rw_mu', [N_A, 48, 128])
    I['rw_rkv'] = din('rw_rkv', [N_A, 3, D, D])
    for nm in ['rw_w0', 'rw_a0', 'rw_kk', 'rw_ka', 'rw_rk', 'rw_gn_g', 'rw_gn_b']:
        I[nm] = din(nm, [N_A, 8, 128])
    I['rw_w1'] = din('rw_w1', [N_A, D, 64])
    I['rw_w2'] = din('rw_w2', [N_A, 64, D])
    I['rw_a1'] = din('rw_a1', [N_A, D, 64])
    I['rw_a2'] = din('rw_a2', [N_A, 64, D])
    I['rw_g1'] = din('rw_g1', [N_A, D, 160])
    I['rw_g2'] = din('rw_g2', [N_A, 160, D])
    I['rw_wo'] = din('rw_wo', [N_A, D, D])
    I['rw_v0'] = din('rw_v0', [1, 8, 128])
    I['rw_v1'] = din('rw_v1', [1, D, 32])
    I['rw_v2'] = din('rw_v2', [1, 32, D])
    I['kv_ada_w'] = din('kv_ada_w', [D, 2 * D])
    I['kv_ada_b'] = din('kv_ada_b', [16, 128])
    I['kv_w'] = din('kv_w', [D, 2 * D + NH])
    I['kv_fb'] = din('kv_fb', [NH, 1])
    I['kv_knorm'] = din('kv_knorm', [HD, 1])
    I['fx_wqg'] = din('fx_wqg', [2, D, 2 * D])
    I['fx_qnorm'] = din('fx_qnorm', [2, HD, 1])
    I['fx_wo'] = din('fx_wo', [2, D, D])
    I['moe_wgrp'] = din('moe_wgrp', [DEPTH, D, NG])
    I['moe_bgrp'] = din('moe_bgrp', [DEPTH, NG])
    I['moe_wexp'] = din('moe_wexp', [DEPTH, D, NE])
    I['moe_bexp'] = din('moe_bexp', [DEPTH, NE])
    I['moe_wgate'] = din('moe_wgate', [DEPTH, NE, D, DEXP])
    I['moe_wup'] = din('moe_wup', [DEPTH, NE, D, DEXP])
    I['moe_wdown'] = din('moe_wdown', [DEPTH, NE, DEXP, D])
    out_ap = nc.dram_tensor('out', [T, D], F32, kind="ExternalOutput").ap()

    def dscr(name, shape, dt=F32):
        kind = "ExternalOutput" if (dbg and name in dbg) else "Internal"
        return Buf(nc.dram_tensor(name, list(shape), dt, kind=kind).ap())

    S = {}
    S['XT'] = dscr('XT', [D, T])
    S['X1'] = dscr('X1', [D, T])
    S['H2'] = dscr('H2', [D, T], BF16)
    S['MO'] = dscr('MO', [D, T], BF16)
    S['VF'] = dscr('VF', [D, T])
    S['KT'] = dscr('KT', [D, T], BF16)
    S['VK'] = dscr('VK', [T, D], BF16)
    S['FC'] = dscr('FC', [NH, T])
    S['QT'] = dscr('QT', [D, T], BF16)
    S['SG'] = dscr('SG', [D, T], BF16)

    with ExitStack() as es:
        k = K(nc, es)
        g.es = es
        g.k, g.nc, g.I, g.S, g.out = k, nc, I, S, out_ap
        g.ps = [PBuf(es.enter_context(nc.psum_tensor("ps%d" % i, [128, 512], F32))) for i in range(8)]
        setup_consts(g, es)
        g.GT = sb(g, es, "GT", [128, T])
        phase_mod(g, layers, n_a)
        phase_in(g)
        for l in range(layers):
            if l < n_a:
                phase_rwkv(g, l)
            else:
                phase_fox(g, l, l - n_a)
            phase_moe(g, l, last=(l == layers - 1))
            if l == n_a - 1 and n_b > 0:
                phase_kv(g)
        k.barrier()
    g.nc = nc
    return nc, g


_UID = [0]


def sb(g, es, name, shape, dt=F32, n=1):
    _UID[0] += 1
    return Buf(es.enter_context(g.nc.sbuf_tensor("%s_%d" % (name, _UID[0]), list(shape), dt)), n)


def setup_consts(g, es):
    k, nc = g.k, g.nc
    ones = sb(g, es, "c_ones", [128, 512])
    g.ones = ones
    k.op('pool', lambda e: e.memset(ones[:], 1.0), [], ones.k)
    ident = sb(g, es, "c_ident", [128, 128])
    g.ident = ident
    k.op('pool', lambda e: e.affine_select(out=ident[:], in_=ones[:, 0:128], pattern=[[-1, 128]],
                                           compare_op=ALU.is_equal, fill=0.0, base=0, channel_multiplier=1),
         ones.k, ident.k)
    mmean = sb(g, es, "c_mmean", [128, 128])
    g.mmean = mmean
    k.op('pool', lambda e: e.memset(mmean[:], 1.0 / D), [], mmean.k)
    blk = sb(g, es, "c_blk", [128, 128])
    g.blk = blk
    k.op('pool', lambda e: e.memset(blk[:], 0.0), [], blk.k)
    k.op('pool', lambda e: e.memset(blk[0:64, 0:64], 1.0), [], blk.k)
    k.op('pool', lambda e: e.memset(blk[64:128, 64:128], 1.0), [], blk.k)


def dump(g, name, ap, reads):
    if not g.dbg or name not in g.dbg:
        return
    d = g.nc.dram_tensor("dbg_" + name, list(ap.shape), ap.dtype, kind="ExternalOutput").ap()
    g.k.dma('sp', d, ap, reads, [])


def load_vecT(g, out, src2d, R, st):
    k = g.k
    k.dma('sp', st[0:R, :], src2d, [], st.k)
    ps = g.ps[0]
    k.tr(ps[:, 0:R], st[0:R, :], g.ident[0:R, 0:R], st.k + g.ident.k, [ps.k[0]])
    k.cp('dve', out[:, 0:R], ps[:, 0:R], [ps.k[0]], out.k)
    return out


def phase_mod(g, layers, n_a):
    k, nc, I = g.k, g.nc, g.I
    es = g.es
    g.modT = [sb(g, es, "modT%d" % l, [128, 48]) for l in range(layers)]
    g.mod1 = [sb(g, es, "mod1_%d" % l, [128, 48]) for l in range(layers)]
    g.lng = [sb(g, es, "lng%d" % l, [128, 16]) for l in range(layers)]
    g.lnb = [sb(g, es, "lnb%d" % l, [128, 16]) for l in range(layers)]
    if layers > n_a:
        g.kvmod = sb(g, es, "kvmod", [128, 16])
        g.kvmod1 = sb(g, es, "kvmod1", [128, 16])
    with ExitStack() as ph:
        st = sb(g, ph, "lv_st", [128, 128])
        cT = sb(g, ph, "cT", [128, KC])
        bT = sb(g, ph, "bT", [128, 48])
        load_vecT(g, cT, I['c'], KC, st)
        cs2 = sb(g, ph, "cs2", [128, KC, 2])
        k.act(cs2[:, :, 0], cT[:], AF.Silu, cT.k, cs2.k)
        k.act(cs2[:, :, 1], cT[:], AF.Silu, cT.k, cs2.k)
        wb = [sb(g, ph, "adaw%d" % i, [128, KC, 1024]) for i in range(2)]
        nblk = 0

        def matvec(w_ap, ncols, outT):
            nonlocal nblk
            wv = w_ap.rearrange("(kc p) n -> p kc n", p=128)
            for b0 in range(0, ncols, 1024):
                bw = min(1024, ncols - b0)
                w = wb[nblk % 2]
                nblk += 1
                k.dma('sp', w[:, :, 0:bw], wv[:, :, b0:b0 + bw], [], w.k)
                ps = g.ps[1 + (nblk % 2)]
                for j in range(bw // 128):
                    for kc in range(KC):
                        k.mm(ps[:, 2 * j:2 * j + 2], w[:, kc, j * 128:(j + 1) * 128], cs2[:, kc, :],
                             kc == 0, kc == KC - 1, w.k + cs2.k, [ps.k[0]])
                nj = bw // 128
                k.cp('dve', outT[:, b0 // 128:b0 // 128 + nj],
                     ps[:, 0:2 * nj].rearrange("p (j t) -> p j t", t=2)[:, :, 0], [ps.k[0]], outT.k)

        for l in range(layers):
            mt, m1 = g.modT[l], g.mod1[l]
            matvec(I['ada_w'][l], 6 * D, mt)
            load_vecT(g, bT, I['ada_b'][l], 48, st)
            k.tt('dve', mt[:], mt[:], bT[:], ALU.add, mt.k + bT.k, mt.k)
            k.ts('dve', m1[:], mt[:], 1.0, None, ALU.add, None, mt.k, m1.k)
            dump(g, "modT%d" % l, mt[:], mt.k)
            load_vecT(g, g.lng[l], I['ln_g'][l], 16, st)
            load_vecT(g, g.lnb[l], I['ln_b'][l], 16, st)
        if layers > n_a:
            kt, k1 = g.kvmod, g.kvmod1
            matvec(I['kv_ada_w'], 2 * D, kt)
            load_vecT(g, bT, I['kv_ada_b'], 16, st)
            k.tt('dve', kt[:], kt[:], bT[:, 0:16], ALU.add, kt.k + bT.k, kt.k)
            k.ts('dve', k1[:], kt[:], 1.0, None, ALU.add, None, kt.k, k1.k)
        k.barrier()


def phase_in(g):
    k, I, S = g.k, g.I, g.S
    T = g.T
    with ExitStack() as ph:
        xt = [sb(g, ph, "in_x%d" % i, [128, D]) for i in range(2)]
        st = [sb(g, ph, "in_st%d" % i, [128, KC, 512]) for i in range(2)]
        XTv = S['XT'].t.rearrange("(kc p) t -> p kc t", p=128)
        for gi in range(T // 128):
            xb = xt[gi % 2]
            k.dma('sp', xb[:], I['x'][gi * 128:(gi + 1) * 128, :], [], xb.k)
            sg = st[(gi // 4) % 2]
            for half in range(2):
                ps = g.ps[(gi * 2 + half) % 4]
                for j in range(4):
                    kc = half * 4 + j
                    k.tr(ps[:, j * 128:(j + 1) * 128], xb[:, kc * 128:(kc + 1) * 128], g.ident[:],
                         xb.k + g.ident.k, [ps.k[0]])
                dst = sg[:, half * 4:half * 4 + 4, (gi % 4) * 128:(gi % 4 + 1) * 128]
                src = ps[:, :].rearrange("p (j t) -> p j t", t=128)
                k.cp('act' if half else 'dve', dst, src, [ps.k[0]], sg.k)
            if gi % 4 == 3:
                t0 = (gi // 4) * 512
                k.dma('sp', XTv[:, :, t0:t0 + 512], sg[:], sg.k, S['XT'].k)
        k.barrier()


STUB = {'rwkv': False, 'fox': False}
RW_STOP = [99]
RW_SUB = [9]


def bc_rows(ap2d_row, nparts, ncols):
    return bass.AP(tensor=ap2d_row.tensor, offset=ap2d_row.offset, ap=[[0, nparts], [1, ncols]])


def mixer_zero(g):
    k, S, T = g.k, g.S, g.T
    with ExitStack() as ph:
        z = sb(g, ph, "mz", [128, KC, 512], BF16)
        k.op('pool', lambda e: e.memset(z[:], 0.0), [], z.k)
        MOv = S['MO'].t.rearrange("(kc p) t -> p kc t", p=128)
        for t0 in range(0, T, 512):
            k.dma('sp', MOv[:, :, t0:t0 + 512], z[:], z.k, S['MO'].k)
        k.barrier()


def phase_rwkv(g, l):
    if STUB['rwkv']:
        mixer_zero(g)
    else:
        rwkv_mixer(g, l)
    phase_tail(g, l, g.I['rw_wo'][l])


def phase_fox(g, l, j):
    if STUB['fox']:
        mixer_zero(g)
    else:
        fox_mixer(g, l, j)
    phase_tail(g, l, g.I['fx_wo'][j])


def ln_tile(g, zf, zk, sq, mean, rstd, gam, bet, outf, outk, w):
    k = g.k
    psm, psq = g.ps[6], g.ps[7]
    for kc in range(KC):
        k.act(sq[:, kc, 0:w], zf(kc), AF.Square, zk, sq.k)
    for kc in range(KC):
        k.mm(psm[:, 0:w], g.mmean[:], zf(kc), kc == 0, kc == KC - 1, g.mmean.k + zk, [psm.k[0]])
    for kc in range(KC):
        k.mm(psq[:, 0:w], g.mmean[:], sq[:, kc, 0:w], kc == 0, kc == KC - 1, g.mmean.k + sq.k, [psq.k[0]])
    k.cp('act', mean[:, 0:w], psm[:, 0:w], [psm.k[0]], mean.k)
    k.tt('dve', rstd[:, 0:w], mean[:, 0:w], mean[:, 0:w], ALU.mult, mean.k, rstd.k)
    k.tt('dve', rstd[:, 0:w], psq[:, 0:w], rstd[:, 0:w], ALU.subtract, [psq.k[0]] + rstd.k, rstd.k)
    k.ts('dve', rstd[:, 0:w], rstd[:, 0:w], LN_EPS, None, ALU.add, None, rstd.k, rstd.k)
    k.act(rstd[:, 0:w], rstd[:, 0:w], AF.Sqrt, rstd.k, rstd.k)
    k.op('dve', lambda e: e.reciprocal(out=rstd[:, 0:w], in_=rstd[:, 0:w]), rstd.k, rstd.k)
    for kc in range(KC):
        k.tt('dve', zf(kc), zf(kc), mean[:, 0:w], ALU.subtract, zk + mean.k, zk)
        k.tt('pool', zf(kc), zf(kc), rstd[:, 0:w], ALU.mult, zk + rstd.k, zk)
        k.act(outf(kc), zf(kc), AF.Identity, zk, outk, scale=gam(kc), bias=bet(kc))


def phase_tail(g, l, wo_ap):
    k, I, S, T = g.k, g.I, g.S, g.T
    NE, NG, NR, EPG = g.NE, g.NG, g.NR, g.EPG
    mt, m1 = g.modT[l], g.mod1[l]
    GT = g.GT
    with ExitStack() as ph:
        wo = sb(g, ph, "t_wo", [128, KC, D], BF16)
        k.dma('pool', wo[:], wo_ap.rearrange("(kc p) n -> p kc n", p=128), [], wo.k)
        wr = sb(g, ph, "t_wr", [128, KC, NR])
        k.dma('sp', wr[:, :, 0:NG], I['moe_wgrp'][l].rearrange("(kc p) n -> p kc n", p=128), [], wr.k)
        k.dma('sp', wr[:, :, NG:NR], I['moe_wexp'][l].rearrange("(kc p) n -> p kc n", p=128), [], wr.k)
        rb = sb(g, ph, "t_rb", [128, NR])
        k.dma('sp', rb[:, 0:NG], bc_rows(I['moe_bgrp'][l], 128, NG), [], rb.k)
        k.dma('sp', rb[:, NG:NR], bc_rows(I['moe_bexp'][l], 128, NE), [], rb.k)
        xt = [sb(g, ph, "t_x%d" % i, [128, KC, 512]) for i in range(2)]
        zt = [sb(g, ph, "t_z%d" % i, [128, KC, 512]) for i in range(2)]
        mo = [sb(g, ph, "t_mo%d" % i, [128, KC, 512], BF16) for i in range(2)]
        hb = [sb(g, ph, "t_hb%d" % i, [128, KC, 512], BF16) for i in range(2)]
        sq = sb(g, ph, "t_sq", [128, KC, 512])
        mean = sb(g, ph, "t_mean", [128, 512])
        rstd = sb(g, ph, "t_rstd", [128, 512])
        sm = {n: sb(g, ph, "t_r_" + n, [128, w_]) for n, w_ in
              [('lg', NR), ('mg', 1), ('nmg', 1), ('eg', NG), ('sg', 1), ('pg', 1), ('oh', NG), ('pen', NG),
               ('le', NE), ('t8', 8), ('d12', 1), ('s12', 1), ('g1', 1), ('g2', 1), ('G1', NE), ('G2', NE)]}
        XTv = S['XT'].t.rearrange("(kc p) t -> p kc t", p=128)
        X1v = S['X1'].t.rearrange("(kc p) t -> p kc t", p=128)
        MOv = S['MO'].t.rearrange("(kc p) t -> p kc t", p=128)
        H2v = S['H2'].t.rearrange("(kc p) t -> p kc t", p=128)
        for ti in range(T // 512):
            t0 = ti * 512
            x, z, m, h = xt[ti % 2], zt[ti % 2], mo[ti % 2], hb[ti % 2]
            k.dma('sp', x[:], XTv[:, :, t0:t0 + 512], S['XT'].k, x.k)
            k.dma('sp', m[:], MOv[:, :, t0:t0 + 512], S['MO'].k, m.k)
            for oc in range(KC):
                ps = g.ps[oc % 2]
                for kc in range(KC):
                    k.mm(ps[:, :], wo[:, kc, oc * 128:(oc + 1) * 128], m[:, kc, :], kc == 0, kc == KC - 1,
                         wo.k + m.k, [ps.k[0]])
                k.act(z[:, oc, :], ps[:, :], AF.Identity, [ps.k[0]], z.k, scale=m1[:, 16 + oc:17 + oc])
                k.stt(z[:, oc, :], x[:, oc, :], ALPHA, z[:, oc, :], ALU.mult, ALU.add, x.k + z.k, z.k)
            ln_tile(g, lambda kc: z[:, kc, :], z.k, sq, mean, rstd,
                    lambda kc: g.lng[l][:, kc:kc + 1], lambda kc: g.lnb[l][:, kc:kc + 1],
                    lambda kc: x[:, kc, :], x.k, 512)
            k.dma('sp', X1v[:, :, t0:t0 + 512], x[:], x.k, S['X1'].k)
            for kc in range(KC):
                k.act(z[:, kc, :], x[:, kc, :], AF.Identity, x.k, z.k,
                      scale=m1[:, 32 + kc:33 + kc], bias=mt[:, 24 + kc:25 + kc])
            k.cp('pool', h[:], z[:], z.k, h.k)
            k.dma('sp', H2v[:, :, t0:t0 + 512], h[:], h.k, S['H2'].k)
            for tg in range(4):
                pr = g.ps[2 + tg % 2]
                for kc in range(KC):
                    k.mm(pr[:, 0:NR], z[:, kc, tg * 128:(tg + 1) * 128], wr[:, kc, :], kc == 0, kc == KC - 1,
                         z.k + wr.k, [pr.k[0]])
                lg, mg, nmg, eg, sg, pg = sm['lg'], sm['mg'], sm['nmg'], sm['eg'], sm['sg'], sm['pg']
                oh, pen, le, t8 = sm['oh'], sm['pen'], sm['le'], sm['t8']
                k.tt('dve', lg[:], pr[:, 0:NR], rb[:], ALU.add, [pr.k[0]] + rb.k, lg.k)
                k.op('dve', lambda e: e.tensor_reduce(out=mg[:], in_=lg[:, 0:NG], axis=AX.X, op=ALU.max), lg.k, mg.k)
                k.ts('dve', nmg[:], mg[:], -1.0, None, ALU.mult, None, mg.k, nmg.k)
                k.act(eg[:], lg[:, 0:NG], AF.Exp, lg.k + nmg.k, eg.k + sg.k, bias=nmg[:, 0:1], accum_out=sg[:])
                k.op('dve', lambda e: e.reciprocal(out=pg[:], in_=sg[:]), sg.k, pg.k)
                k.ts('dve', oh[:], lg[:, 0:NG], mg[:, 0:1], None, ALU.is_equal, None, lg.k + mg.k, oh.k)
                k.ts('dve', pen[:], oh[:], -1.0, 1e30, ALU.add, ALU.mult, oh.k, pen.k)
                for gi in range(NG):
                    k.ts('dve', le[:, gi * EPG:(gi + 1) * EPG], lg[:, NG + gi * EPG:NG + (gi + 1) * EPG],
                         pen[:, gi:gi + 1], None, ALU.add, None, lg.k + pen.k, le.k)
                k.op('dve', lambda e: e.max(out=t8[:], in_=le[:]), le.k, t8.k)
                d12, s12, g1, g2, G1, G2 = sm['d12'], sm['s12'], sm['g1'], sm['g2'], sm['G1'], sm['G2']
                k.tt('dve', d12[:], t8[:, 0:1], t8[:, 1:2], ALU.subtract, t8.k, d12.k)
                k.act(s12[:], d12[:], AF.Sigmoid, d12.k, s12.k)
                k.tt('dve', g1[:], s12[:], pg[:], ALU.mult, s12.k + pg.k, g1.k)
                k.tt('dve', g2[:], pg[:], g1[:], ALU.subtract, pg.k + g1.k, g2.k)
                k.ts('dve', G1[:], le[:], t8[:, 0:1], g1[:, 0:1], ALU.is_equal, ALU.mult, le.k + t8.k + g1.k, G1.k)
                k.ts('dve', G2[:], le[:], t8[:, 1:2], g2[:, 0:1], ALU.is_equal, ALU.mult, le.k + t8.k + g2.k, G2.k)
                k.tt('dve', G1[:], G1[:], G2[:], ALU.add, G1.k + G2.k, G1.k)
                pt = g.ps[4 + tg % 2]
                k.tr(pt[0:NE, 0:128], G1[:], g.ident[:], G1.k + g.ident.k, [pt.k[0]])
                c0 = t0 + tg * 128
                k.cp('dve', GT[0:NE, c0:c0 + 128], pt[0:NE, 0:128], [pt.k[0]], GT.k)
        dump(g, "GT%d" % l, GT[0:NE, :], GT.k)
        k.barrier()


def phase_moe(g, l, last):
    k, I, S, T = g.k, g.I, g.S, g.T
    NE = g.NE
    mt, m1 = g.modT[l], g.mod1[l]
    GT = g.GT
    HT = min(2048, T)
    NTT = HT // 512
    X1v = S['X1'].t.rearrange("(kc p) t -> p kc t", p=128)
    XTv = S['XT'].t.rearrange("(kc p) t -> p kc t", p=128)
    H2v = S['H2'].t.rearrange("(kc p) t -> p kc t", p=128)
    with ExitStack() as ph:
        h2 = sb(g, ph, "m_h2", [128, KC, HT], BF16)
        yacc = sb(g, ph, "m_y", [128, KC, HT], F32, n=KC * NTT)
        for hf in range(T // HT):
            tb = hf * HT
            for tt in range(NTT):
                k.dma('sp', h2[:, :, tt * 512:(tt + 1) * 512], H2v[:, :, tb + tt * 512:tb + (tt + 1) * 512],
                      S['H2'].k, h2.k)
            with ExitStack() as ex:
                wg = [sb(g, ex, "m_wg%d" % i, [128, KC, DEXP], BF16) for i in range(2)]
                wu = [sb(g, ex, "m_wu%d" % i, [128, KC, DEXP], BF16) for i in range(2)]
                wd = [sb(g, ex, "m_wd%d" % i, [128, 4, D], BF16) for i in range(2)]
                sel = [sb(g, ex, "m_sel%d" % i, [128, 128]) for i in range(2)]
                gbc = sb(g, ex, "m_gbc", [128, 512])
                sl = [sb(g, ex, "m_sl%d" % i, [128, 512]) for i in range(2)]
                tl = sb(g, ex, "m_tl", [128, 512])
                hT = [sb(g, ex, "m_hT%d" % i, [128, 4, 512], BF16) for i in range(2)]
                it = 0

                def load_w(e):
                    bi = e % 2
                    k.dma('pool', wg[bi][:], I['moe_wgate'][l, e].rearrange("(kc p) n -> p kc n", p=128), [], wg[bi].k)
                    k.dma('pool', wu[bi][:], I['moe_wup'][l, e].rearrange("(kc p) n -> p kc n", p=128), [], wu[bi].k)
                    k.dma('pool', wd[bi][:], I['moe_wdown'][l, e].rearrange("(dc p) n -> p dc n", p=128), [], wd[bi].k)
                load_w(0)
                pend = None
                for e in range(NE):
                    bi = e % 2
                    se = sel[bi]
                    k.op('pool', lambda e_, se=se, e=e: e_.affine_select(
                        out=se[0:NE, :], in_=g.ones[0:NE, 0:128], pattern=[[0, 128]], compare_op=ALU.is_equal,
                        fill=0.0, base=-e, channel_multiplier=1), g.ones.k, se.k)
                    for tt in range(NTT):
                        c0 = tt * 512
                        psg = g.ps[0]
                        k.mm(psg[:, :], se[0:NE, :], GT[0:NE, tb + c0:tb + c0 + 512], True, True,
                             se.k + GT.k, [psg.k[0]])
                        k.cp('act', gbc[:], psg[:, :], [psg.k[0]], gbc.k)
                        hh = hT[it % 2]
                        it += 1
                        for dc in range(4):
                            pg_, pu_ = g.ps[1 + dc % 2], g.ps[3 + dc % 2]
                            for kc in range(KC):
                                k.mm(pg_[:, :], wg[bi][:, kc, dc * 128:(dc + 1) * 128], h2[:, kc, c0:c0 + 512],
                                     kc == 0, kc == KC - 1, wg[bi].k + h2.k, [pg_.k[0]])
                            for kc in range(KC):
                                k.mm(pu_[:, :], wu[bi][:, kc, dc * 128:(dc + 1) * 128], h2[:, kc, c0:c0 + 512],
                                     kc == 0, kc == KC - 1, wu[bi].k + h2.k, [pu_.k[0]])
                            s_ = sl[dc % 2]
                            k.act(s_[:], pg_[:, :], AF.Silu, [pg_.k[0]], s_.k)
                            k.tt('dve', tl[:], pu_[:, :], s_[:], ALU.mult, [pu_.k[0]] + s_.k, tl.k)
                            k.tt('dve', hh[:, dc, :], tl[:], gbc[:], ALU.mult, tl.k + gbc.k, hh.k)

                        def down(e=e, bi=bi, tt=tt, c0=c0, hh=hh):
                            for oc in range(KC):
                                py = g.ps[5 + oc % 2]
                                for dc in range(4):
                                    k.mm(py[:, :], wd[bi][:, dc, oc * 128:(oc + 1) * 128], hh[:, dc, :],
                                         dc == 0, dc == 3, wd[bi].k + hh.k, [py.k[0]])
                                yk = [yacc.k[oc * NTT + tt]]
                                ya = yacc[:, oc, c0:c0 + 512]
                                if e == 0:
                                    k.cp('dve', ya, py[:, :], [py.k[0]], yk)
                                else:
                                    k.tt('dve', ya, py[:, :], ya, ALU.add, [py.k[0]] + yk, yk)
                        if pend is not None:
                            pend()
                        pend = down
                        if tt == 0 and e + 1 < NE:
                            load_w(e + 1)
                pend()
                k.barrier()
            with ExitStack() as ex:
                xt = [sb(g, ex, "m_x%d" % i, [128, KC, 512]) for i in range(2)]
                sq = sb(g, ex, "m_sq", [128, KC, 512])
                mean = sb(g, ex, "m_mean", [128, 512])
                rstd = sb(g, ex, "m_rstd", [128, 512])
                ot = [sb(g, ex, "m_ot%d" % i, [128, D]) for i in range(2)] if last else None
                for tt in range(NTT):
                    c0 = tt * 512
                    t0 = tb + c0
                    x = xt[tt % 2]
                    k.dma('sp', x[:], X1v[:, :, t0:t0 + 512], S['X1'].k, x.k)
                    zk = [yacc.k[oc * NTT + tt] for oc in range(KC)]
                    for oc in range(KC):
                        ya = yacc[:, oc, c0:c0 + 512]
                        k.act(ya, ya, AF.Identity, zk, zk, scale=m1[:, 40 + oc:41 + oc])
                        k.stt(ya, x[:, oc, :], ALPHA, ya, ALU.mult, ALU.add, x.k + zk, zk)
                    ln_tile(g, lambda kc: yacc[:, kc, c0:c0 + 512], zk, sq, mean, rstd,
                            lambda kc: g.lng[l][:, 8 + kc:9 + kc], lambda kc: g.lnb[l][:, 8 + kc:9 + kc],
                            lambda kc: x[:, kc, :], x.k, 512)
                    if not last:
                        k.dma('sp', XTv[:, :, t0:t0 + 512], x[:], x.k, S['XT'].k)
                    else:
                        for tg in range(4):
                            o = ot[tg % 2]
                            for half in range(2):
                                ps = g.ps[half]
                                for j in range(4):
                                    kc = half * 4 + j
                                    k.tr(ps[:, j * 128:(j + 1) * 128], x[:, kc, tg * 128:(tg + 1) * 128], g.ident[:],
                                         x.k + g.ident.k, [ps.k[0]])
                                k.cp('act' if half else 'dve', o[:, half * 512:(half + 1) * 512], ps[:, :],
                                     [ps.k[0]], o.k)
                            k.dma('sp', g.out[t0 + tg * 128:t0 + (tg + 1) * 128, :], o[:], o.k, [])
                k.barrier()
        k.barrier()


def head_rms_fm(g, k, pin, outap, outk, sqt, rst, nrm, extra_scale):
    pss = g.ps[7]
    k.act(sqt[:], pin[:, :], AF.Square, [pin.k[0]], sqt.k)
    k.mm(pss[:, :], g.blk[:], sqt[:], True, True, g.blk.k + sqt.k, [pss.k[0]])
    k.ts('dve', rst[:], pss[:, :], 1.0 / 64, QK_EPS, ALU.mult, ALU.add, [pss.k[0]], rst.k)
    k.act(rst[:], rst[:], AF.Sqrt, rst.k, rst.k)
    k.op('dve', lambda e: e.reciprocal(out=rst[:], in_=rst[:]), rst.k, rst.k)
    k.tt('dve', sqt[:], pin[:, :], rst[:], ALU.mult, [pin.k[0]] + rst.k, sqt.k)
    k.ts('pool', outap, sqt[:], nrm[:, 0:1], extra_scale, ALU.mult, ALU.mult, sqt.k + nrm.k, outk)


def phase_kv(g):
    k, I, S, T = g.k, g.I, g.S, g.T
    g.FQ = Buf(g.nc.dram_tensor("FQ", [NH, 2, T], F32, kind="Internal").ap())
    g.FK = Buf(g.nc.dram_tensor("FKn", [NH, 2, T], F32, kind="Internal").ap())
    with ExitStack() as ph:
        wv_ = lambda ap: ap.rearrange("(kc p) n -> p kc n", p=128)
        wk = sb(g, ph, "kv_wk", [128, KC, D], BF16)
        wv = sb(g, ph, "kv_wv", [128, KC, D], BF16)
        wf = sb(g, ph, "kv_wf", [128, KC, NH], BF16)
        k.dma('pool', wk[:], wv_(I['kv_w'][:, 0:D]), [], wk.k)
        k.dma('pool', wv[:], wv_(I['kv_w'][:, D:2 * D]), [], wv.k)
        k.dma('pool', wf[:], wv_(I['kv_w'][:, 2 * D:2 * D + NH]), [], wf.k)
        kn = sb(g, ph, "kv_kn", [128, 1])
        k.dma('sp', kn[0:64, :], I['kv_knorm'], [], kn.k)
        k.dma('sp', kn[64:128, :], I['kv_knorm'], [], kn.k)
        fb = sb(g, ph, "kv_fb", [NH, 1])
        k.dma('sp', fb[:], I['kv_fb'], [], fb.k)
        k.barrier()
        xb = [sb(g, ph, "kv_x%d" % i, [128, KC, 512]) for i in range(2)]
        hk = [sb(g, ph, "kv_h%d" % i, [128, KC, 512], BF16) for i in range(2)]
        kst = [sb(g, ph, "kv_ks%d" % i, [128, KC, 512], BF16) for i in range(2)]
        vst = [sb(g, ph, "kv_vs%d" % i, [128, D], BF16) for i in range(2)]
        sqt = sb(g, ph, "kv_sq", [128, 512])
        rst = sb(g, ph, "kv_rs", [128, 512])
        lf = sb(g, ph, "kv_lf", [NH, 512])
        fc = [sb(g, ph, "kv_fc%d" % i, [NH, 512]) for i in range(2)]
        nfc = [sb(g, ph, "kv_nfc%d" % i, [NH, 512]) for i in range(2)]
        XTv = S['XT'].t.rearrange("(kc p) t -> p kc t", p=128)
        KTv = S['KT'].t.rearrange("(kc p) t -> p kc t", p=128)
        for ti in range(T // 512):
            t0 = ti * 512
            x, h, ks = xb[ti % 2], hk[ti % 2], kst[ti % 2]
            k.dma('sp', x[:], XTv[:, :, t0:t0 + 512], S['XT'].k, x.k)
            for kc in range(KC):
                k.act(h[:, kc, :], x[:, kc, :], AF.Identity, x.k, h.k,
                      scale=g.kvmod1[:, 8 + kc:9 + kc], bias=g.kvmod[:, kc:kc + 1])
            for oc in range(KC):
                p = g.ps[oc % 2]
                for kc in range(KC):
                    k.mm(p[:, :], wk[:, kc, oc * 128:(oc + 1) * 128], h[:, kc, :], kc == 0, kc == KC - 1,
                         wk.k + h.k, [p.k[0]])
                head_rms_fm(g, k, p, ks[:, oc, :], ks.k, sqt, rst, kn, 1.0)
            k.dma('sp', KTv[:, :, t0:t0 + 512], ks[:], ks.k, S['KT'].k)
            for tg in range(4):
                vs = vst[tg % 2]
                for half in range(2):
                    p = g.ps[2 + half]
                    for kc in range(KC):
                        k.mm(p[:, :], h[:, kc, tg * 128:(tg + 1) * 128], wv[:, kc, half * 512:(half + 1) * 512],
                             kc == 0, kc == KC - 1, wv.k + h.k, [p.k[0]])
                    k.cp('act' if half else 'dve', vs[:, half * 512:(half + 1) * 512], p[:, :], [p.k[0]], vs.k)
                k.dma('sp', S['VK'].t[t0 + tg * 128:t0 + (tg + 1) * 128, :], vs[:], vs.k, S['VK'].k)
            p = g.ps[4]
            for kc in range(KC):
                k.mm(p[0:NH, :], wf[:, kc, :], h[:, kc, :], kc == 0, kc == KC - 1, wf.k + h.k, [p.k[0]])
            k.act(lf[:], p[0:NH, :], AF.Sigmoid, [p.k[0]], lf.k, bias=fb[:, 0:1])
            k.act(lf[:], lf[:], AF.Ln, lf.k, lf.k)
            f, fp_, nf = fc[ti % 2], fc[(ti + 1) % 2], nfc[ti % 2]
            init = 0.0 if ti == 0 else fp_[:, 511:512]
            k.op('dve', lambda e, f=f, init=init: e.tensor_tensor_scan(
                out=f[:], data0=g.ones[0:NH, 0:512], data1=lf[:], initial=init, op0=ALU.mult, op1=ALU.add),
                g.ones.k + lf.k + fp_.k, f.k)
            k.ts('pool', nf[:], f[:], -1.0, None, ALU.mult, None, f.k, nf.k)
            k.dma('sp', g.FQ.t[:, 0, t0:t0 + 512], f[:], f.k, g.FQ.k)
            k.dma('sp', g.FQ.t[:, 1, t0:t0 + 512], g.ones[0:NH, 0:512], g.ones.k, g.FQ.k)
            k.dma('sp', g.FK.t[:, 0, t0:t0 + 512], g.ones[0:NH, 0:512], g.ones.k, g.FK.k)
            k.dma('sp', g.FK.t[:, 1, t0:t0 + 512], nf[:], nf.k, g.FK.k)
        k.barrier()


def fox_mixer(g, l, j):
    k, I, S, T = g.k, g.I, g.S, g.T
    mt, m1 = g.modT[l], g.mod1[l]
    XTv = S['XT'].t.rearrange("(kc p) t -> p kc t", p=128)
    QTv = S['QT'].t.rearrange("(kc p) t -> p kc t", p=128)
    SGv = S['SG'].t.rearrange("(kc p) t -> p kc t", p=128)
    with ExitStack() as ph:
        wv_ = lambda ap: ap.rearrange("(kc p) n -> p kc n", p=128)
        wq = sb(g, ph, "f_wq", [128, KC, D], BF16)
        wg = sb(g, ph, "f_wg", [128, KC, D], BF16)
        k.dma('pool', wq[:], wv_(I['fx_wqg'][j][:, 0:D]), [], wq.k)
        k.dma('pool', wg[:], wv_(I['fx_wqg'][j][:, D:2 * D]), [], wg.k)
        qn = sb(g, ph, "f_qn", [128, 1])
        k.dma('sp', qn[0:64, :], I['fx_qnorm'][j], [], qn.k)
        k.dma('sp', qn[64:128, :], I['fx_qnorm'][j], [], qn.k)
        k.barrier()
        xb = [sb(g, ph, "f_x%d" % i, [128, KC, 512]) for i in range(2)]
        hb = [sb(g, ph, "f_h%d" % i, [128, KC, 512], BF16) for i in range(2)]
        qst = [sb(g, ph, "f_qs%d" % i, [128, KC, 512], BF16) for i in range(2)]
        gst = [sb(g, ph, "f_gs%d" % i, [128, KC, 512], BF16) for i in range(2)]
        sqt = sb(g, ph, "f_sq", [128, 512])
        rst = sb(g, ph, "f_rs", [128, 512])
        for ti in range(T // 512):
            t0 = ti * 512
            x, h, qs, gs = xb[ti % 2], hb[ti % 2], qst[ti % 2], gst[ti % 2]
            k.dma('sp', x[:], XTv[:, :, t0:t0 + 512], S['XT'].k, x.k)
            for kc in range(KC):
                k.act(h[:, kc, :], x[:, kc, :], AF.Identity, x.k, h.k,
                      scale=m1[:, 8 + kc:9 + kc], bias=mt[:, kc:kc + 1])
            for oc in range(KC):
                p = g.ps[oc % 2]
                for kc in range(KC):
                    k.mm(p[:, :], wq[:, kc, oc * 128:(oc + 1) * 128], h[:, kc, :], kc == 0, kc == KC - 1,
                         wq.k + h.k, [p.k[0]])
                head_rms_fm(g, k, p, qs[:, oc, :], qs.k, sqt, rst, qn, HD ** -0.5)
                p2 = g.ps[2 + oc % 2]
                for kc in range(KC):
                    k.mm(p2[:, :], wg[:, kc, oc * 128:(oc + 1) * 128], h[:, kc, :], kc == 0, kc == KC - 1,
                         wg.k + h.k, [p2.k[0]])
                k.act(gs[:, oc, :], p2[:, :], AF.Sigmoid, [p2.k[0]], gs.k)
            k.dma('sp', QTv[:, :, t0:t0 + 512], qs[:], qs.k, S['QT'].k)
            k.dma('sp', SGv[:, :, t0:t0 + 512], gs[:], gs.k, S['SG'].k)
        k.barrier()
    with ExitStack() as ph:
        identb = sb(g, ph, "a_identb", [128, 128], BF16)
        k.cp('pool', identb[:], g.ident[:], g.ident.k, identb.k)
        zer = sb(g, ph, "a_zer", [128, 128])
        k.op('pool', lambda e: e.memset(zer[:], 0.0), [], zer.k)
        nmask = sb(g, ph, "a_nmask", [128, 128], BF16)
        k.op('pool', lambda e: e.affine_select(out=nmask[:], in_=zer[:], pattern=[[1, 128]], compare_op=ALU.is_ge,
                                               fill=-30000.0, base=0, channel_multiplier=-1), zer.k, nmask.k)
        onesb = sb(g, ph, "a_onesb", [128, 64], BF16)
        k.op('pool', lambda e: e.memset(onesb[:], 1.0), [], onesb.k)
        k.barrier()
        NTG = T // 128
        KTh = [sb(g, ph, "a_k%d" % i, [64, T], BF16) for i in range(2)]
        QTh = [sb(g, ph, "a_q%d" % i, [64, T], BF16) for i in range(2)]
        SGh = [sb(g, ph, "a_g%d" % i, [64, T], BF16) for i in range(2)]
        Vh = [sb(g, ph, "a_v%d" % i, [128, NTG, 64], BF16) for i in range(2)]
        FQh = [sb(g, ph, "a_fq%d" % i, [2, T]) for i in range(2)]
        FKh = [sb(g, ph, "a_fk%d" % i, [2, T]) for i in range(2)]
        Pb = [sb(g, ph, "a_p%d" % i, [128, 512], BF16) for i in range(3)]
        rl = sb(g, ph, "a_rl", [64, 512])
        of = sb(g, ph, "a_of", [64, 512])
        ost = [sb(g, ph, "a_os%d" % i, [64, 512], BF16) for i in range(2)]
        it = 0
        for hd in range(NH):
            b = hd % 2
            hr = slice(hd * 64, (hd + 1) * 64)
            k.dma('sp', KTh[b][:], S['KT'].t[hr, :], S['KT'].k, KTh[b].k)
            k.dma('sp', QTh[b][:], S['QT'].t[hr, :], S['QT'].k, QTh[b].k)
            k.dma('sp', SGh[b][:], S['SG'].t[hr, :], S['SG'].k, SGh[b].k)
            k.dma('sp', Vh[b][:], S['VK'].t[:, hr].rearrange("(tg p) d -> p tg d", p=128), S['VK'].k, Vh[b].k)
            k.dma('sp', FQh[b][:], g.FQ.t[hd], g.FQ.k, FQh[b].k)
            k.dma('sp', FKh[b][:], g.FK.t[hd], g.FK.k, FKh[b].k)
            for qg in range(T // 512):
                q0 = qg * 512
                Op, Lp = g.ps[2 + 2 * (qg % 2)], g.ps[3 + 2 * (qg % 2)]
                kts = list(range(4 * qg + 4))
                pend = None
                for idx, kt in enumerate(kts):
                    diag = kt >= 4 * qg
                    qlo = (kt - 4 * qg) * 128 if diag else 0
                    n = 512 - qlo
                    Sp = g.ps[idx % 2]
                    kc_ = slice(kt * 128, (kt + 1) * 128)
                    qc_ = slice(q0 + qlo, q0 + 512)
                    k.mm(Sp[:, 0:n], KTh[b][:, kc_], QTh[b][:, qc_], True, False, KTh[b].k + QTh[b].k, [Sp.k[0]])
                    k.mm(Sp[:, 0:n], FKh[b][:, kc_], FQh[b][:, qc_], False, not diag, FKh[b].k + FQh[b].k, [Sp.k[0]])
                    if diag:
                        k.mm(Sp[:, 0:128], identb[:], nmask[:], False, True, identb.k + nmask.k, [Sp.k[0]])
                    P = Pb[it % 3]
                    it += 1
                    k.act(P[:, 0:n], Sp[:, 0:n], AF.Exp, [Sp.k[0]], P.k)
                    first, lastf = idx == 0, idx == len(kts) - 1

                    def pvl(Op=Op, Lp=Lp, P=P, kt=kt, qlo=qlo, n=n, first=first, lastf=lastf, b=b):
                        k.mm(Op[0:64, qlo:512], Vh[b][:, kt, :], P[:, 0:n], first, lastf, Vh[b].k + P.k, [Op.k[0]])
                        k.mm(Lp[0:64, qlo:512], onesb[:], P[:, 0:n], first, lastf, onesb.k + P.k, [Lp.k[0]])
                    if pend is not None:
                        pend()
                    pend = pvl
                pend()
                pend = None
                k.op('dve', lambda e, Lp=Lp: e.reciprocal(out=rl[:], in_=Lp[0:64, :]), [Lp.k[0]], rl.k)
                k.tt('dve', of[:], Op[0:64, :], rl[:], ALU.mult, [Op.k[0]] + rl.k, of.k)
                o = ost[qg % 2]
                k.tt('pool', o[:], of[:], SGh[b][:, q0:q0 + 512], ALU.mult, of.k + SGh[b].k, o.k)
                k.dma('sp', S['MO'].t[hr, q0:q0 + 512], o[:], o.k, S['MO'].k)
        k.barrier()


def setup_rwkv_consts(g, es):
    k = g.k
    ones = g.ones
    su = sb(g, es, "c_su", [128, 128])
    iu = sb(g, es, "c_iu", [128, 128])
    g.mask4 = sb(g, es, "c_mask4", [128, 512])
    g.sl = sb(g, es, "c_sl", [128, 128])
    g.rm = sb(g, es, "c_rm", [128, 256])
    k.op('pool', lambda e: e.affine_select(out=su[:], in_=ones[:, 0:128], pattern=[[1, 128]], compare_op=ALU.is_gt,
                                           fill=0.0, base=0, channel_multiplier=-1), ones.k, su.k)
    k.op('pool', lambda e: e.affine_select(out=iu[:], in_=ones[:, 0:128], pattern=[[1, 128]], compare_op=ALU.is_ge,
                                           fill=0.0, base=0, channel_multiplier=-1), ones.k, iu.k)
    k.op('pool', lambda e: e.affine_select(out=g.sl[:], in_=ones[:, 0:128], pattern=[[-1, 128]], compare_op=ALU.is_gt,
                                           fill=0.0, base=0, channel_multiplier=1), ones.k, g.sl.k)
    k.tt('pool', g.sl[:], g.sl[:], g.blk[:], ALU.mult, g.sl.k + g.blk.k, g.sl.k)
    for q in range(4):
        src = su if q < 2 else iu
        k.tt('pool', g.mask4[:, q * 128:(q + 1) * 128], src[:], g.blk[:], ALU.mult, src.k + g.blk.k, g.mask4.k)
    k.op('pool', lambda e: e.memset(g.rm[:], 1.0), [], g.rm.k)
    for q in range(4):
        k.op('pool', lambda e, q=q: e.memset(g.rm[:, q * 64:q * 64 + 1], 0.0), [], g.rm.k)
    k.barrier()


def rwkv_mixer(g, l):
    k, I, S, T = g.k, g.I, g.S, g.T
    mt, m1 = g.modT[l], g.mod1[l]
    W = 256
    ps = g.ps
    with ExitStack() as ph:
        setup_rwkv_consts(g, ph)
        wv_ = lambda ap: ap.rearrange("(kc p) n -> p kc n", p=128)
        wr_ = sb(g, ph, "r_wr", [128, KC, D], BF16)
        wk_ = sb(g, ph, "r_wk", [128, KC, D], BF16)
        wvv = sb(g, ph, "r_wv", [128, KC, D], BF16)
        k.dma('pool', wr_[:], wv_(I['rw_rkv'][l, 0]), [], wr_.k)
        k.dma('pool', wk_[:], wv_(I['rw_rkv'][l, 1]), [], wk_.k)
        k.dma('pool', wvv[:], wv_(I['rw_rkv'][l, 2]), [], wvv.k)
        w1 = sb(g, ph, "r_w1", [128, KC, 64], BF16)
        a1 = sb(g, ph, "r_a1", [128, KC, 64], BF16)
        g1 = sb(g, ph, "r_g1", [128, KC, 160], BF16)
        k.dma('pool', w1[:], wv_(I['rw_w1'][l]), [], w1.k)
        k.dma('pool', a1[:], wv_(I['rw_a1'][l]), [], a1.k)
        k.dma('pool', g1[:], wv_(I['rw_g1'][l]), [], g1.k)
        w2 = sb(g, ph, "r_w2", [64, D], BF16)
        a2 = sb(g, ph, "r_a2", [64, D], BF16)
        g2 = sb(g, ph, "r_g2", [128, 2, D], BF16)
        k.dma('pool', w2[:], I['rw_w2'][l], [], w2.k)
        k.dma('pool', a2[:], I['rw_a2'][l], [], a2.k)
        k.dma('pool', g2[:, 0, :], I['rw_g2'][l, 0:128, :], [], g2.k)
        k.dma('pool', g2[0:32, 1, :], I['rw_g2'][l, 128:160, :], [], g2.k)
        if l > 0:
            v1 = sb(g, ph, "r_v1", [128, KC, 32], BF16)
            v2 = sb(g, ph, "r_v2", [32, D], BF16)
            k.dma('pool', v1[:], wv_(I['rw_v1'][l - 1]), [], v1.k)
            k.dma('pool', v2[:], I['rw_v2'][l - 1], [], v2.k)
        st = sb(g, ph, "r_lvst", [128, 128])
        pv = {}
        for nm, R in [('rw_mu', 48), ('rw_w0', 8), ('rw_a0', 8), ('rw_kk', 8), ('rw_ka', 8), ('rw_rk', 8),
                      ('rw_gn_g', 8), ('rw_gn_b', 8)]:
            pv[nm] = sb(g, ph, "r_p_" + nm, [128, R])
            load_vecT(g, pv[nm], I[nm][l], R, st)
        if l > 0:
            pv['rw_v0'] = sb(g, ph, "r_p_v0", [128, 8])
            load_vecT(g, pv['rw_v0'], I['rw_v0'][l - 1], 8, st)
        hm = sb(g, ph, "r_hm", [128, 2])
        k.cp('pool', hm[:, 0:1], g.blk[:, 0:1], g.blk.k, hm.k)
        k.cp('pool', hm[:, 1:2], g.blk[:, 127:128], g.blk.k, hm.k)
        Bth = [sb(g, ph, "r_Bth%d" % i, [128, W], F32R) for i in range(2)]
        Kth = [sb(g, ph, "r_Kth%d" % i, [128, W], F32R) for i in range(2)]
        Ath = [sb(g, ph, "r_Ath%d" % i, [128, W], F32R) for i in range(2)]
        VTc = [sb(g, ph, "r_VTc%d" % i, [128, 128], F32R) for i in range(2)]
        VTm = [sb(g, ph, "r_VTm%d" % i, [128, 128], F32R) for i in range(2)]
        cmk = [sb(g, ph, "r_cmk%d" % i, [128, 128]) for i in range(2)]
        for i in range(2):
            k.op('pool', lambda e, i=i: e.memset(cmk[i][:], 0.0), [], cmk[i].k)
            k.op('pool', lambda e, i=i: e.memset(cmk[i][:, i * 64:(i + 1) * 64], 1.0), [], cmk[i].k)
        omka = sb(g, ph, "r_omka", [128, 8])
        k.ts('dve', omka[:], pv['rw_ka'][:], -1.0, 1.0, ALU.mult, ALU.add, pv['rw_ka'].k, omka.k)
        k.barrier()
        state = [sb(g, ph, "r_state%d" % i, [128, 8, 64], F32, n=8) for i in range(2)]
        k.op('pool', lambda e: e.memset(state[0][:], 0.0), [], state[0].k)
        spar = [0] * 8
        hbuf = sb(g, ph, "r_h", [128, KC, W + 1])
        k.op('pool', lambda e: e.memset(hbuf[:, :, 0:1], 0.0), [], hbuf.k)
        xx = sb(g, ph, "r_xx", [128, KC, W])
        xb = xx
        xm = [sb(g, ph, "r_xm%d" % i, [128, KC, W], BF16) for i in range(6)]
        tw = sb(g, ph, "r_tw", [64, W], BF16)
        ta = sb(g, ph, "r_ta", [64, W], BF16)
        tg0 = sb(g, ph, "r_tg0", [128, W], BF16)
        tg1 = sb(g, ph, "r_tg1", [32, W], BF16)
        tv = sb(g, ph, "r_tv", [32, W], BF16)
        names = ['r', 'k', 'v', 'wl', 'sa', 'g', 'kk', 't1', 'rs', 'kkn', 'kf', 'b', 'c', 'd', 'eg', 'em', 'ed', 'ep',
                 'Rt', 'At', 'Kt', 'Bt', 'Kh', 'Bh']
        t = {n: sb(g, ph, "r_t_" + n, [128, W], F32R if n in ('At', 'Rt') else F32) for n in names}
        t['y'], t['y2'], t['mn'], t['bon'], t['vf'] = t['eg'], t['em'], t['ed'], t['ep'], t['d']
        gl_ = sb(g, ph, "r_gl", [128, 4])
        KhT = sb(g, ph, "r_KhT", [128, 2, 128])
        BhT = sb(g, ph, "r_BhT", [128, 2, 128])
        VT = sb(g, ph, "r_VT", [128, 2, 128], F32R)
        Mall = sb(g, ph, "r_Mall", [128, 4, 512], F32R)
        Nall = [sb(g, ph, "r_Nall%d" % i, [128, 4, 128], F32R) for i in range(2)]
        Ntall = [sb(g, ph, "r_Ntall%d" % i, [128, 4, 128], F32R) for i in range(2)]
        Pall = sb(g, ph, "r_Pall", [128, 4, 128], F32R)
        sl4 = sb(g, ph, "r_sl4", [128, 4, 128])
        id4 = sb(g, ph, "r_id4", [128, 4, 128])
        for q in range(4):
            k.cp('pool', sl4[:, q, :], g.sl[:], g.sl.k, sl4.k)
            k.cp('pool', id4[:, q, :], g.ident[:], g.ident.k, id4.k)
        Xs2 = sb(g, ph, "r_Xs2", [128, 128], F32R)
        UP = sb(g, ph, "r_UP", [128, 2, 128], F32R)
        k.op('pool', lambda e: e.memset(UP[:].bitcast(F32), 0.0), [], UP.k)
        _b = UP.t[:]
        UPdiag = bass.AP(tensor=_b.tensor, offset=_b.offset, ap=[list(_b.ap[0]), [192, 2], [1, 64]])
        SP = sb(g, ph, "r_SP", [128, 128], F32R)
        BhTm = [[sb(g, ph, "r_BhTm%d%d" % (a, b_), [128, 128], F32R) for b_ in range(2)] for a in range(2)]
        KhTm = [[sb(g, ph, "r_KhTm%d%d" % (a, b_), [128, 128], F32R) for b_ in range(2)] for a in range(2)]
        mob = sb(g, ph, "r_mo", [128, W], BF16)
        XTv = S['XT'].t.rearrange("(kc p) t -> p kc t", p=128)
        unit_i = 0
        for ti in range(T // W):
            t0 = ti * W
            h = hbuf
            if ti > 0:
                k.cp('pool', h[:, :, 0:1], h[:, :, W:W + 1], h.k, h.k)
            k.dma('sp', xb[:], XTv[:, :, t0:t0 + W], S['XT'].k, xb.k)
            for kc in range(KC):
                k.act(h[:, kc, 1:W + 1], xb[:, kc, :], AF.Identity, xb.k, h.k,
                      scale=m1[:, 8 + kc:9 + kc], bias=mt[:, kc:kc + 1])
            k.tt('dve', xx[:], h[:, :, 0:W], h[:, :, 1:W + 1], ALU.subtract, h.k, xx.k)
            for i in range(6):
                if i == 3 and False:
                    continue
                for kc in range(KC):
                    k.stt(xm[i][:, kc, :], xx[:, kc, :], pv['rw_mu'][:, i * 8 + kc:i * 8 + kc + 1], h[:, kc, 1:W + 1],
                          ALU.mult, ALU.add, xx.k + h.k, xm[i].k)
            p = ps[3]
            for kc in range(KC):
                k.mm(p[0:64, 0:W], w1[:, kc, :], xm[1][:, kc, :], kc == 0, kc == KC - 1, w1.k + xm[1].k, [p.k[0]])
            k.act(tw[:], p[0:64, 0:W], AF.Tanh, [p.k[0]], tw.k)
            for kc in range(KC):
                k.mm(p[0:64, W:2 * W], a1[:, kc, :], xm[4][:, kc, :], kc == 0, kc == KC - 1, a1.k + xm[4].k, [p.k[2]])
            k.cp('act', ta[:], p[0:64, W:2 * W], [p.k[2]], ta.k)
            p = ps[2]
            for kc in range(KC):
                k.mm(p[:, 0:W], g1[:, kc, 0:128], xm[5][:, kc, :], kc == 0, kc == KC - 1, g1.k + xm[5].k, [p.k[0]])
            k.act(tg0[:], p[:, 0:W], AF.Sigmoid, [p.k[0]], tg0.k)
            for kc in range(KC):
                k.mm(p[0:32, W:2 * W], g1[:, kc, 128:160], xm[5][:, kc, :], kc == 0, kc == KC - 1, g1.k + xm[5].k,
                     [p.k[2]])
            k.act(tg1[:], p[0:32, W:2 * W], AF.Sigmoid, [p.k[2]], tg1.k)
            if l > 0:
                p = ps[1]
                for kc in range(KC):
                    k.mm(p[0:32, 0:W], v1[:, kc, :], xm[3][:, kc, :], kc == 0, kc == KC - 1, v1.k + xm[3].k, [p.k[0]])
                k.cp('act', tv[:], p[0:32, 0:W], [p.k[0]], tv.k)
            for hp in range(8):
                if RW_STOP[0] <= 1:
                    break
                oc = hp
                ocs = slice(oc * 128, (oc + 1) * 128)
                col = lambda buf, j: buf[:, j:j + 1]

                def proj(pb, c0, kk_, wt, xi):
                    for kc in range(KC):
                        k.mm(pb[:, c0:c0 + W], wt[:, kc, ocs], xm[xi][:, kc, :], kc == 0, kc == KC - 1,
                             wt.k + xm[xi].k, [pb.k[kk_]])
                proj(ps[0], 0, 0, wr_, 0)
                proj(ps[0], W, 2, wk_, 2)
                proj(ps[1], 0, 0, wvv, 3)
                k.mm(ps[1][:, W:2 * W], w2[:, ocs], tw[:], True, True, w2.k + tw.k, [ps[1].k[2]])
                k.mm(ps[2][:, 0:W], a2[:, ocs], ta[:], True, True, a2.k + ta.k, [ps[2].k[0]])
                k.mm(ps[2][:, W:2 * W], g2[:, 0, ocs], tg0[:], True, False, g2.k + tg0.k, [ps[2].k[2]])
                k.mm(ps[2][:, W:2 * W], g2[0:32, 1, ocs], tg1[:], False, True, g2.k + tg1.k, [ps[2].k[2]])
                if l > 0:
                    k.mm(ps[3][:, 0:W], v2[:, ocs], tv[:], True, True, v2.k + tv.k, [ps[3].k[0]])
                k.cp('act', t['r'][:], ps[0][:, 0:W], [ps[0].k[0]], t['r'].k)
                k.cp('act', t['k'][:], ps[0][:, W:2 * W], [ps[0].k[2]], t['k'].k)
                k.cp('dve', t['v'][:], ps[1][:, 0:W], [ps[1].k[0]], t['v'].k)
                k.cp('act', t['g'][:], ps[2][:, W:2 * W], [ps[2].k[2]], t['g'].k)
                VFv = S['VF'].t[ocs, t0:t0 + W]
                if l == 0:
                    k.dma('sp', VFv, t['v'][:], t['v'].k, S['VF'].k)
                else:
                    k.dma('sp', t['vf'][:], VFv, S['VF'].k, t['vf'].k)
                    k.act(t['y2'][:], ps[3][:, 0:W], AF.Sigmoid, [ps[3].k[0]], t['y2'].k, bias=col(pv['rw_v0'], oc))
                    k.tt('pool', t['vf'][:], t['vf'][:], t['v'][:], ALU.subtract, t['vf'].k + t['v'].k, t['vf'].k)
                    k.tt('pool', t['vf'][:], t['vf'][:], t['y2'][:], ALU.mult, t['vf'].k + t['y2'].k, t['vf'].k)
                    k.tt('pool', t['v'][:], t['v'][:], t['vf'][:], ALU.add, t['vf'].k + t['v'].k, t['v'].k)
                k.act(t['wl'][:], ps[1][:, W:2 * W], AF.Sigmoid, [ps[1].k[2]], t['wl'].k, bias=col(pv['rw_w0'], oc))
                k.act(t['sa'][:], ps[2][:, 0:W], AF.Sigmoid, [ps[2].k[0]], t['sa'].k, bias=col(pv['rw_a0'], oc))
                k.ts('pool', t['wl'][:], t['wl'][:], -0.6065306597126334, None, ALU.mult, None, t['wl'].k, t['wl'].k)
                k.ts('dve', t['kk'][:], t['k'][:], col(pv['rw_kk'], oc), None, ALU.mult, None, t['k'].k, t['kk'].k)
                k.tt('pool', t['t1'][:], t['kk'][:], t['kk'][:], ALU.mult, t['kk'].k, t['t1'].k)
                k.mm(ps[3][:, W:2 * W], g.blk[:], t['t1'][:], True, True, g.blk.k + t['t1'].k, [ps[3].k[2]])
                k.act(t['rs'][:], ps[3][:, W:2 * W], AF.Sqrt, [ps[3].k[2]], t['rs'].k)
                k.ts('dve', t['rs'][:], t['rs'][:], 1e-12, None, ALU.max, None, t['rs'].k, t['rs'].k)
                k.op('dve', lambda e: e.reciprocal(out=t['rs'][:], in_=t['rs'][:]), t['rs'].k, t['rs'].k)
                k.tt('dve', t['kkn'][:], t['kk'][:], t['rs'][:], ALU.mult, t['kk'].k + t['rs'].k, t['kkn'].k)
                k.ts('dve', t['t1'][:], t['sa'][:], col(pv['rw_ka'], oc), col(omka, oc), ALU.mult, ALU.add,
                     t['sa'].k, t['t1'].k)
                k.tt('pool', t['kf'][:], t['k'][:], t['t1'][:], ALU.mult, t['k'].k + t['t1'].k, t['kf'].k)
                k.tt('pool', t['b'][:], t['kkn'][:], t['sa'][:], ALU.mult, t['kkn'].k + t['sa'].k, t['b'].k)
                k.op('dve', lambda e: e.tensor_tensor_scan(out=t['c'][:], data0=g.rm[:], data1=t['wl'][:], initial=0.0,
                                                           op0=ALU.mult, op1=ALU.add),
                     g.rm.k + t['wl'].k, t['c'].k)
                for j in range(4):
                    cs_ = slice(j * 64, (j + 1) * 64)
                    k.ts('dve', t['d'][:, cs_], t['c'][:, cs_], -1.0, t['c'][:, j * 64 + 63:j * 64 + 64],
                         ALU.mult, ALU.add, t['c'].k, t['d'].k)
                k.tt('pool', t['ep'][:], t['c'][:], t['wl'][:], ALU.subtract, t['c'].k + t['wl'].k, t['ep'].k)
                k.act(t['eg'][:], t['c'][:], AF.Exp, t['c'].k, t['eg'].k)
                k.act(t['em'][:], t['c'][:], AF.Exp, t['c'].k, t['em'].k, scale=-1.0)
                k.act(t['ed'][:], t['d'][:], AF.Exp, t['d'].k, t['ed'].k)
                k.act(t['ep'][:], t['ep'][:], AF.Exp, t['ep'].k, t['ep'].k)
                k.act(gl_[:], t['c'][:, :].rearrange("p (j t) -> p j t", t=64)[:, :, 63], AF.Exp, t['c'].k, gl_.k)
                k.tt('dve', t['Rt'][:], t['r'][:], t['eg'][:], ALU.mult, t['r'].k + t['eg'].k, t['Rt'].k)
                k.stt(t['At'][:], t['kkn'][:], -1.0, t['ep'][:], ALU.mult, ALU.mult, t['kkn'].k + t['ep'].k, t['At'].k)
                k.tt('pool', t['Kt'][:], t['kf'][:], t['em'][:], ALU.mult, t['kf'].k + t['em'].k, t['Kt'].k)
                k.tt('pool', t['Bt'][:], t['b'][:], t['em'][:], ALU.mult, t['b'].k + t['em'].k, t['Bt'].k)
                k.tt('dve', t['Kh'][:], t['kf'][:], t['ed'][:], ALU.mult, t['kf'].k + t['ed'].k, t['Kh'].k)
                k.tt('pool', t['Bh'][:], t['b'][:], t['ed'][:], ALU.mult, t['b'].k + t['ed'].k, t['Bh'].k)
                if RW_STOP[0] <= 2:
                    continue
                for hh in range(2):
                    k.ts('pool', Bth[hh][:], t['Bt'][:], hm[:, hh:hh + 1], None, ALU.mult, None, t['Bt'].k + hm.k, Bth[hh].k)
                    k.ts('pool', Kth[hh][:], t['Kt'][:], hm[:, hh:hh + 1], None, ALU.mult, None, t['Kt'].k + hm.k, Kth[hh].k)
                    k.ts('dve', Ath[hh][:], t['At'][:].bitcast(F32), hm[:, hh:hh + 1], None, ALU.mult, None, t['At'].k + hm.k, Ath[hh].k)
                for cp in range(2):
                    cc = slice(cp * 128, (cp + 1) * 128)
                    k.tr(ps[6][:, cp * 128:(cp + 1) * 128], t['Kh'][:, cc], g.ident[:], t['Kh'].k + g.ident.k, [ps[6].k[0]])
                    k.tr(ps[6][:, 256 + cp * 128:256 + (cp + 1) * 128], t['Bh'][:, cc], g.ident[:],
                         t['Bh'].k + g.ident.k, [ps[6].k[0]])
                    k.tr(ps[7][:, 256 + cp * 128:256 + (cp + 1) * 128], t['v'][:, cc], g.ident[:],
                         t['v'].k + g.ident.k, [ps[7].k[3]])
                k.cp('act', KhT[:], ps[6][:, 0:256].rearrange("p (a b) -> p a b", b=128), [ps[6].k[0]], KhT.k)
                k.cp('dve', BhT[:], ps[6][:, 256:512].rearrange("p (a b) -> p a b", b=128), [ps[6].k[0]], BhT.k)
                k.cp('act', VT[:], ps[7][:, 256:512].rearrange("p (a b) -> p a b", b=128), [ps[7].k[3]], VT.k)
                Yp = ps[0]
                mbank = [ps[4], ps[6], ps[1], ps[2]]
                for cp in range(2):
                    cc = slice(cp * 128, (cp + 1) * 128)
                    for hh in range(2):
                        u = cp * 2 + hh
                        Mp = mbank[u]
                        rd = Bth[hh].k + t['At'].k + Kth[hh].k + t['Rt'].k
                        k.mm(Mp[:, 0:128], Bth[hh][:, cc], t['At'][:, cc], True, True, rd, [Mp.k[0]])
                        k.mm(Mp[:, 128:256], Kth[hh][:, cc], t['At'][:, cc], True, True, rd, [Mp.k[0]])
                        k.mm(Mp[:, 256:384], Bth[hh][:, cc], t['Rt'][:, cc], True, True, rd, [Mp.k[0]])
                        k.mm(Mp[:, 384:512], Kth[hh][:, cc], t['Rt'][:, cc], True, True, rd, [Mp.k[0]])
                        k.mm(ps[5][:, u * 128:(u + 1) * 128], t['At'][:, cc], Bth[hh][:, cc], True, True, rd, [ps[5].k[0]])
                        k.tt('dve', Mall[:, u, :], Mp[:, :], g.mask4[:], ALU.mult, [Mp.k[0]] + g.mask4.k, Mall.k)
                        k.tt('pool', BhTm[cp][hh][:], BhT[:, cp, :], cmk[hh][:], ALU.mult, BhT.k + cmk[hh].k, BhTm[cp][hh].k)
                        k.tt('pool', KhTm[cp][hh][:], KhT[:, cp, :], cmk[hh][:], ALU.mult, KhT.k + cmk[hh].k, KhTm[cp][hh].k)
                k.tt('dve', Nall[0][:].rearrange("p a b -> p (a b)"), ps[5][:, :], sl4[:].rearrange("p a b -> p (a b)"),
                     ALU.mult, [ps[5].k[0]] + sl4.k, Nall[0].k)
                k.cp('pool', Ntall[0][:], Mall[:, :, 0:128].bitcast(F32), Mall.k, Ntall[0].k)
                k.tt('pool', Pall[:], Mall[:, :, 0:128].bitcast(F32), id4[:], ALU.add, Mall.k + id4.k, Pall.k)
                pa, pb_, pc = ps[1], ps[2], ps[3]
                for lev in range(1, 6):
                    Np, Ntp = Nall[(lev - 1) % 2], Ntall[(lev - 1) % 2]
                    Nn, Ntn = Nall[lev % 2], Ntall[lev % 2]
                    for u in range(4):
                        k.mm(pa[:, u * 128:(u + 1) * 128], Ntp[:, u, :], Np[:, u, :], True, True, Ntp.k + Np.k, [pa.k[0]])
                    if lev < 5:
                        for u in range(4):
                            k.mm(pb_[:, u * 128:(u + 1) * 128], Np[:, u, :], Ntp[:, u, :], True, True, Ntp.k + Np.k,
                                 [pb_.k[0]])
                    k.cp('act', Nn[:].rearrange("p a b -> p (a b)"), pa[:, :], [pa.k[0]], Nn.k)
                    if lev < 5:
                        k.cp('dve', Ntn[:].rearrange("p a b -> p (a b)"), pb_[:, :], [pb_.k[0]], Ntn.k)
                    for u in range(4):
                        k.mm(pc[:, u * 128:(u + 1) * 128], Nn[:, u, :], Pall[:, u, :], True, True, Nn.k + Pall.k, [pc.k[0]])
                    k.tt('dve', Pall[:].rearrange("p a b -> p (a b)"), pc[:, :], Pall[:].rearrange("p a b -> p (a b)").bitcast(F32),
                         ALU.add, [pc.k[0]] + Pall.k, Pall.k)
                for cp in range(2):
                    cc = slice(cp * 128, (cp + 1) * 128)
                    for q in range(2):
                        k.ts('pool', VTc[q][:], VT[:, cp, :].bitcast(F32), hm[:, q:q + 1], None, ALU.mult, None, VT.k + hm.k, VTc[q].k)
                        k.tt('pool', VTm[q][:], VT[:, cp, :].bitcast(F32), cmk[q][:], ALU.mult, VT.k + cmk[q].k, VTm[q].k)
                    for ch in range(2):
                        j = cp * 2 + ch
                        c64 = slice(j * 64, (j + 1) * 64)
                        so, sn = state[spar[hp]], state[1 - spar[hp]]
                        sk = [so.k[hp]]
                        p7 = ps[7]
                        for hh in range(2):
                            hcol = slice(hh * 64, (hh + 1) * 64)
                            k.ts('pool', SP[:, hcol], so[:, hp, :], hm[:, hh:hh + 1], None, ALU.mult, None,
                                 sk + hm.k, SP.k)
                        for hh in range(2):
                            u = cp * 2 + hh
                            hcol = slice(hh * 64, (hh + 1) * 64)
                            k.mm(p7[:, hcol], Ath[hh][:, cc], SP[:, hcol], True, False, Ath[hh].k + SP.k, [p7.k[0]])
                            k.mm(p7[:, hcol], Mall[:, u, 128:256], VT[:, cp, hcol], False, True, Mall.k + VT.k, [p7.k[0]])
                        k.cp('act', Xs2[:], p7[:, 0:128], [p7.k[0]], Xs2.k)
                        for hh in range(2):
                            u = cp * 2 + hh
                            hcol = slice(hh * 64, (hh + 1) * 64)
                            k.mm(p7[:, 128 + hh * 64:128 + (hh + 1) * 64], Pall[:, u, :], Xs2[:, hcol], True, True,
                                 Pall.k + Xs2.k, [p7.k[0]])
                        k.ts('dve', UPdiag, p7[:, 128:256].rearrange("p (a b) -> p a b", b=64), hm[:, ch:ch + 1], None,
                             ALU.mult, None, [p7.k[0]] + hm.k, UP.k)
                        T_ = p7[:, 256:320]
                        for hh in range(2):
                            hcol = slice(hh * 64, (hh + 1) * 64)
                            k.mm(T_, BhTm[cp][hh][:], UP[:, hh, hcol], hh == 0, False, BhTm[cp][hh].k + UP.k, [p7.k[0]])
                            k.mm(T_, KhTm[cp][hh][:], VTc[ch][:, hcol], False, hh == 1, KhTm[cp][hh].k + VTc[ch].k,
                                 [p7.k[0]])
                        k.stt(sn[:, hp, :], so[:, hp, :], gl_[:, j:j + 1], T_, ALU.mult, ALU.add,
                              sk + gl_.k + [p7.k[0]], [sn.k[hp]])
                        Y_ = Yp[:, c64]
                        k.mm(Y_, SP[:], t['Rt'][:, c64], True, False, SP.k + t['Rt'].k, [Yp.k[0]])
                        for hh in range(2):
                            u = cp * 2 + hh
                            k.mm(Y_, UP[:, hh, :], Mall[:, u, 256 + ch * 64:256 + (ch + 1) * 64], False, False,
                                 UP.k + Mall.k, [Yp.k[0]])
                            k.mm(Y_, VTm[hh][:], Mall[:, u, 384 + ch * 64:384 + (ch + 1) * 64], False, hh == 1,
                                 VTm[hh].k + Mall.k, [Yp.k[0]])
                        spar[hp] = 1 - spar[hp]
                if RW_STOP[0] <= 4:
                    continue
                k.cp('act', t['y'][:], Yp[:, 0:W], [Yp.k[0]], t['y'].k)
                k.tt('pool', t['y2'][:], t['y'][:], t['y'][:], ALU.mult, t['y'].k, t['y2'].k)
                k.mm(ps[1][:, 0:W], g.blk[:], t['y'][:], True, True, g.blk.k + t['y'].k, [ps[1].k[0]])
                k.mm(ps[1][:, W:2 * W], g.blk[:], t['y2'][:], True, True, g.blk.k + t['y2'].k, [ps[1].k[2]])
                k.ts('dve', t['mn'][:], ps[1][:, 0:W], 1.0 / 64, None, ALU.mult, None, [ps[1].k[0]], t['mn'].k)
                k.tt('pool', t['y2'][:], t['mn'][:], t['mn'][:], ALU.mult, t['mn'].k, t['y2'].k)
                k.stt(t['y2'][:], ps[1][:, W:2 * W], 1.0 / 64, t['y2'][:], ALU.mult, ALU.subtract,
                      [ps[1].k[2]] + t['y2'].k, t['y2'].k)
                k.ts('dve', t['y2'][:], t['y2'][:], GN_EPS, None, ALU.add, None, t['y2'].k, t['y2'].k)
                k.act(t['y2'][:], t['y2'][:], AF.Sqrt, t['y2'].k, t['y2'].k)
                k.op('dve', lambda e: e.reciprocal(out=t['y2'][:], in_=t['y2'][:]), t['y2'].k, t['y2'].k)
                k.tt('pool', t['y'][:], t['y'][:], t['mn'][:], ALU.subtract, t['y'].k + t['mn'].k, t['y'].k)
                k.tt('pool', t['y'][:], t['y'][:], t['y2'][:], ALU.mult, t['y'].k + t['y2'].k, t['y'].k)
                k.act(t['y'][:], t['y'][:], AF.Identity, t['y'].k, t['y'].k, scale=col(pv['rw_gn_g'], oc),
                      bias=col(pv['rw_gn_b'], oc))
                k.tt('pool', t['bon'][:], t['r'][:], t['kf'][:], ALU.mult, t['r'].k + t['kf'].k, t['bon'].k)
                k.ts('dve', t['bon'][:], t['bon'][:], col(pv['rw_rk'], oc), None, ALU.mult, None, t['bon'].k, t['bon'].k)
                k.mm(ps[3][:, W:2 * W], g.blk[:], t['bon'][:], True, True, g.blk.k + t['bon'].k, [ps[3].k[2]])
                k.tt('dve', t['bon'][:], ps[3][:, W:2 * W], t['v'][:], ALU.mult, [ps[3].k[2]] + t['v'].k, t['bon'].k)
                k.tt('pool', t['y'][:], t['y'][:], t['bon'][:], ALU.add, t['y'].k + t['bon'].k, t['y'].k)
                k.tt('pool', mob[:], t['y'][:], t['g'][:], ALU.mult, t['y'].k + t['g'].k, mob.k)
                k.dma('sp', S['MO'].t[ocs, t0:t0 + W], mob[:], mob.k, S['MO'].k)
        k.barrier()


_CACHE = {}


def prep_inputs(inp, b, T):
    f = lambda a: np.ascontiguousarray(np.asarray(a, dtype=np.float32))
    m = {}
    m['x'] = f(inp['x'][b][:T])
    m['c'] = f(inp['c'][b]).reshape(KC, 128)
    m['ada_w'] = f(inp['ada_w'])
    m['ada_b'] = f(inp['ada_b']).reshape(DEPTH, 48, 128)
    m['ln_g'] = f(inp['ln_g']).reshape(DEPTH, 16, 128)
    m['ln_b'] = f(inp['ln_b']).reshape(DEPTH, 16, 128)
    m['rw_mu'] = f(inp['rw_mu']).reshape(N_A, 48, 128)
    m['rw_rkv'] = f(inp['rw_rkv'])
    for nm in ['rw_w0', 'rw_a0', 'rw_kk', 'rw_ka', 'rw_rk', 'rw_gn_g', 'rw_gn_b']:
        m[nm] = f(inp[nm]).reshape(N_A, 8, 128)
    for nm in ['rw_w1', 'rw_w2', 'rw_a1', 'rw_a2', 'rw_g1', 'rw_g2', 'rw_wo', 'rw_v1', 'rw_v2',
               'kv_ada_w', 'kv_w', 'fx_wqg', 'fx_wo', 'moe_wgrp', 'moe_bgrp', 'moe_wexp', 'moe_bexp',
               'moe_wgate', 'moe_wup', 'moe_wdown']:
        m[nm] = f(inp[nm])
    m['rw_v0'] = f(inp['rw_v0']).reshape(1, 8, 128)
    m['kv_ada_b'] = f(inp['kv_ada_b']).reshape(16, 128)
    m['kv_fb'] = f(inp['kv_fb']).reshape(NH, 1)
    m['kv_knorm'] = f(inp['kv_knorm']).reshape(HD, 1)
    m['fx_qnorm'] = f(inp['fx_qnorm']).reshape(2, HD, 1)
    return m


def kernel(**inputs):
    B, T = inputs['x'].shape[0], inputs['x'].shape[1]
    NG = inputs['moe_wgrp'].shape[-1]
    NE = inputs['moe_wexp'].shape[-1]
    key = (T, NG, NE)
    if key not in _CACHE:
        _CACHE[key] = build(T, NG, NE // NG)[0]
    nc = _CACHE[key]
    shared = None
    maps = []
    for b in range(B):
        m = prep_inputs(inputs, b, T) if shared is None else dict(shared)
        if shared is None:
            shared = m
        else:
            m['x'] = np.ascontiguousarray(np.asarray(inputs['x'][b], dtype=np.float32))
            m['c'] = np.ascontiguousarray(np.asarray(inputs['c'][b], dtype=np.float32)).reshape(KC, 128)
        maps.append(m)
    res = run_bass_kernel_spmd(nc, maps, core_ids=list(range(B)))
    return np.stack([np.asarray(r['out']) for r in res.results]).astype(np.float32)
```

```python
import numpy as np
from contextlib import ExitStack
import concourse.bass as bass
import concourse.mybir as mybir
from concourse.bass_utils import run_bass_kernel_spmd

F32 = mybir.dt.float32
BF16 = mybir.dt.bfloat16
F32R = mybir.dt.float32r
AF = mybir.ActivationFunctionType
ALU = mybir.AluOpType
AX = mybir.AxisListType

D = 1024
KC = 8
HD = 64
NH = 16
DEPTH = 4
N_A = 2
ALPHA = (2 * DEPTH) ** 0.25
LN_EPS = 1e-5
GN_EPS = 64e-5
QK_EPS = 1e-6
DEXP = 512


class Tk:
    __slots__ = ("w", "r", "excl")

    def __init__(s):
        s.w = None
        s.r = {}
        s.excl = False


class Buf:
    def __init__(s, t, n=1):
        s.t = t
        s.k = [Tk() for _ in range(n)]

    def __getitem__(s, idx):
        return s.t[idx]


class _KL(list):
    def __getitem__(s, i):
        return list.__getitem__(s, 0)


class PBuf(Buf):
    def __init__(s, t):
        s.t = t
        s.k = _KL([Tk()])
        s.k[0].excl = True


class K:
    NS = 24

    def __init__(s, nc, es):
        s.nc = nc
        s.eng = {'pe': nc.tensor, 'act': nc.scalar, 'dve': nc.vector, 'pool': nc.gpsimd, 'sp': nc.sync}
        s.esem = {n: es.enter_context(nc.semaphore("s_" + n)) for n in s.eng}
        s.ecnt = {n: 0 for n in s.eng}
        s.seen = {n: {} for n in s.eng}
        s.dsem = [es.enter_context(nc.semaphore("d%d" % i)) for i in range(s.NS)]
        s.dcnt = [0] * s.NS
        s.dnext = {'hw': 0, 'sw': 0}
        s.dpool = {'hw': list(range(0, 16)), 'sw': list(range(16, s.NS))}
        s.same_sync = {'pe': False, 'act': True, 'dve': True, 'pool': True, 'sp': False}
        s.nins = 0

    def _wait(s, en, key, val):
        if s.seen[en].get(key, 0) >= val:
            return
        if key[0] == 'E':
            if key[1] == en and not s.same_sync[en]:
                return
            sem = s.esem[key[1]]
        else:
            sem = s.dsem[key[1]]
        s.eng[en].wait_ge(sem, val)
        s.seen[en][key] = val
        s.nins += 1

    def _need(s, reads, writes):
        need = {}
        for t in reads:
            if t.w is not None:
                k_, v = t.w
                if need.get(k_, 0) < v:
                    need[k_] = v
        for t in writes:
            if t.w is not None:
                k_, v = t.w
                if need.get(k_, 0) < v:
                    need[k_] = v
            for k_, v in t.r.items():
                if need.get(k_, 0) < v:
                    need[k_] = v
        return need

    def op(s, en, fn, reads=(), writes=()):
        ex = [t for t in reads if t.excl]
        if ex:
            reads = [t for t in reads if not t.excl]
            writes = list(writes) + ex
        for k_, v in s._need(reads, writes).items():
            s._wait(en, k_, v)
        ins = fn(s.eng[en])
        s.ecnt[en] += 1
        c = s.ecnt[en]
        ins.then_inc(s.esem[en], 1)
        key = ('E', en)
        for t in reads:
            t.r[key] = c
        for t in writes:
            t.w = (key, c)
            t.r = {}
        s.nins += 1

    def dma(s, q, out, in_, reads=(), writes=(), **kw):
        for k_, v in s._need(reads, writes).items():
            s._wait(q, k_, v)
        pn = 'sw' if q == 'pool' else 'hw'
        pl = s.dpool[pn]
        i = pl[s.dnext[pn] % len(pl)]
        s.dnext[pn] += 1
        if s.dcnt[i]:
            s._wait(q, ('D', i), s.dcnt[i] * 16)
        s.eng[q].dma_start(out=out, in_=in_, **kw).then_inc(s.dsem[i], 16)
        s.dcnt[i] += 1
        key = ('D', i)
        val = s.dcnt[i] * 16
        for t in reads:
            t.r[key] = val
        for t in writes:
            t.w = (key, val)
            t.r = {}
        s.nins += 1

    def barrier(s):
        for en in s.eng:
            for o in s.eng:
                if o != en and s.ecnt[o] > 0:
                    s._wait(en, ('E', o), s.ecnt[o])
            for i in range(s.NS):
                if s.dcnt[i] > 0:
                    s._wait(en, ('D', i), s.dcnt[i] * 16)

    def mm(s, out, lhsT, rhs, start, stop, reads, writes):
        s.op('pe', lambda e: e.matmul(out, lhsT, rhs, start=start, stop=stop), reads, writes)

    def tr(s, out, in_, ident, reads, writes):
        s.op('pe', lambda e: e.transpose(out, in_, ident), reads, writes)

    def act(s, out, in_, func, reads, writes, bias=None, scale=None, accum_out=None, en='act'):
        kw = {}
        if bias is not None:
            kw['bias'] = bias
        if scale is not None:
            kw['scale'] = scale
        if accum_out is not None:
            kw['accum_out'] = accum_out
        s.op('act', lambda e: e.activation(out, in_, func, **kw), reads, writes)

    def tt(s, en, out, in0, in1, op, reads, writes):
        s.op(en, lambda e: e.tensor_tensor(out=out, in0=in0, in1=in1, op=op), reads, writes)

    def ts(s, en, out, in0, s1, s2, op0, op1, reads, writes):
        if op1 is None:
            s.op(en, lambda e: e.tensor_scalar(out=out, in0=in0, scalar1=s1, scalar2=None, op0=op0), reads, writes)
        else:
            s.op(en, lambda e: e.tensor_scalar(out=out, in0=in0, scalar1=s1, scalar2=s2, op0=op0, op1=op1), reads, writes)

    def stt(s, out, in0, scalar, in1, op0, op1, reads, writes):
        s.op('dve', lambda e: e.scalar_tensor_tensor(out=out, in0=in0, scalar=scalar, in1=in1, op0=op0, op1=op1),
             reads, writes)

    def cp(s, en, out, in_, reads, writes):
        if en == 'act':
            s.op('act', lambda e: e.copy(out, in_), reads, writes)
        else:
            s.op(en, lambda e: e.tensor_copy(out=out, in_=in_), reads, writes)


class Ctx:
    pass


def build(T, NG, EPG, layers=DEPTH, dbg=None):
    NE = NG * EPG
    NR = NG + NE
    nc = bass.Bass("TRN2", target_bir_lowering=False)
    g = Ctx()
    g.T, g.NG, g.EPG, g.NE, g.NR = T, NG, EPG, NE, NR
    g.dbg = dbg
    n_a = min(N_A, layers)
    n_b = layers - n_a
    nv = max(n_a - 1, 0)

    def din(name, shape):
        return nc.dram_tensor(name, list(shape), F32, kind="ExternalInput").ap()

    I = {}
    I['x'] = din('x', [T, D])
    I['c'] = din('c', [KC, 128])
    I['ada_w'] = din('ada_w', [DEPTH, D, 6 * D])
    I['ada_b'] = din('ada_b', [DEPTH, 48, 128])
    I['ln_g'] = din('ln_g', [DEPTH, 16, 128])
    I['ln_b'] = din('ln_b', [DEPTH, 16, 128])
    I['rw_mu'] = din('rw_mu', [N_A, 48, 128])
    I['rw_rkv'] = din('rw_rkv', [N_A, 3, D, D])
    for nm in ['rw_w0', 'rw_a0', 'rw_kk', 'rw_ka', 'rw_rk', 'rw_gn_g', 'rw_gn_b']:
        I[nm] = din(nm, [N_A, 8, 128])
    I['rw_w1'] = din('rw_w1', [N_A, D, 64])
    I['rw_w2'] = din('rw_w2', [N_A, 64, D])
    I['rw_a1'] = din('rw_a1', [N_A, D, 64])
    I['rw_a2'] = din('rw_a2', [N_A, 64, D])
    I['rw_g1'] = din('rw_g1', [N_A, D, 160])
    I['rw_g2'] = din('rw_g2', [N_A, 160, D])
    I['rw_wo'] = din('rw_wo', [N_A, D, D])
    I['rw_v0'] = din('rw_v0', [1, 8, 128])
    I['rw_v1'] = din('rw_v1', [1, D, 32])
    I['rw_v2'] = din('rw_v2', [1, 32, D])
    I['kv_ada_w'] = din('kv_ada_w', [D, 2 * D])
    I['kv_ada_b'] = din('kv_ada_b', [16, 128])
    I['kv_w'] = din('kv_w', [D, 2 * D + NH])
    I['kv_fb'] = din('kv_fb', [NH, 1])
    I['kv_knorm'] = din('kv_knorm', [HD, 1])
    I['fx_wqg'] = din('fx_wqg', [2, D, 2 * D])
    I['fx_qnorm'] = din('fx_qnorm', [2, HD, 1])
    I['fx_wo'] = din('fx_wo', [2, D, D])
    I['moe_wgrp'] = din('moe_wgrp', [DEPTH, D, NG])
    I['moe_bgrp'] = din('moe_bgrp', [DEPTH, NG])
    I['moe_wexp'] = din('moe_wexp', [DEPTH, D, NE])
    I['moe_bexp'] = din('moe_bexp', [DEPTH, NE])
    I['moe_wgate'] = din('moe_wgate', [DEPTH, NE, D, DEXP])
    I['moe_wup'] = din('moe_wup', [DEPTH, NE, D, DEXP])
    I['moe_wdown'] = din('moe_wdown', [DEPTH, NE, DEXP, D])
    out_ap = nc.dram_tensor('out', [T, D], F32, kind="ExternalOutput").ap()

    def dscr(name, shape, dt=F32):
        kind = "ExternalOutput" if (dbg and name in dbg) else "Internal"
        return Buf(nc.dram_tensor(name, list(shape), dt, kind=kind).ap())

    S = {}
    S['XT'] = dscr('XT', [D, T])
    S['X1'] = dscr('X1', [D, T])
    S['H2'] = dscr('H2', [D, T], BF16)
    S['MO'] = dscr('MO', [D, T], BF16)
    S['VF'] = dscr('VF', [D, T])
    S['KT'] = dscr('KT', [D, T], BF16)
    S['VK'] = dscr('VK', [T, D], BF16)
    S['FC'] = dscr('FC', [NH, T])
    S['QT'] = dscr('QT', [D, T], BF16)
    S['SG'] = dscr('SG', [D, T], BF16)

    with ExitStack() as es:
        k = K(nc, es)
        g.es = es
        g.k, g.nc, g.I, g.S, g.out = k, nc, I, S, out_ap
        g.ps = [PBuf(es.enter_context(nc.psum_tensor("ps%d" % i, [128, 512], F32))) for i in range(8)]
        setup_consts(g, es)
        g.GT = sb(g, es, "GT", [128, T])
        phase_mod(g, layers, n_a)
        phase_in(g)
        for l in range(layers):
            if l < n_a:
                phase_rwkv(g, l)
            else:
                phase_fox(g, l, l - n_a)
            phase_moe(g, l, last=(l == layers - 1))
            if l == n_a - 1 and n_b > 0:
                phase_kv(g)
        k.barrier()
    g.nc = nc
    return nc, g


_UID = [0]


def sb(g, es, name, shape, dt=F32, n=1):
    _UID[0] += 1
    return Buf(es.enter_context(g.nc.sbuf_tensor("%s_%d" % (name, _UID[0]), list(shape), dt)), n)


def setup_consts(g, es):
    k, nc = g.k, g.nc
    ones = sb(g, es, "c_ones", [128, 512])
    g.ones = ones
    k.op('pool', lambda e: e.memset(ones[:], 1.0), [], ones.k)
    ident = sb(g, es, "c_ident", [128, 128])
    g.ident = ident
    k.op('pool', lambda e: e.affine_select(out=ident[:], in_=ones[:, 0:128], pattern=[[-1, 128]],
                                           compare_op=ALU.is_equal, fill=0.0, base=0, channel_multiplier=1),
         ones.k, ident.k)
    mmean = sb(g, es, "c_mmean", [128, 128])
    g.mmean = mmean
    k.op('pool', lambda e: e.memset(mmean[:], 1.0 / D), [], mmean.k)
    blk = sb(g, es, "c_blk", [128, 128])
    g.blk = blk
    k.op('pool', lambda e: e.memset(blk[:], 0.0), [], blk.k)
    k.op('pool', lambda e: e.memset(blk[0:64, 0:64], 1.0), [], blk.k)
    k.op('pool', lambda e: e.memset(blk[64:128, 64:128], 1.0), [], blk.k)


def dump(g, name, ap, reads):
    if not g.dbg or name not in g.dbg:
        return
    d = g.nc.dram_tensor("dbg_" + name, list(ap.shape), ap.dtype, kind="ExternalOutput").ap()
    g.k.dma('sp', d, ap, reads, [])


def load_vecT(g, out, src2d, R, st):
    k = g.k
    k.dma('sp', st[0:R, :], src2d, [], st.k)
    ps = g.ps[0]
    k.tr(ps[:, 0:R], st[0:R, :], g.ident[0:R, 0:R], st.k + g.ident.k, [ps.k[0]])
    k.cp('dve', out[:, 0:R], ps[:, 0:R], [ps.k[0]], out.k)
    return out


def phase_mod(g, layers, n_a):
    k, nc, I = g.k, g.nc, g.I
    es = g.es
    g.modT = [sb(g, es, "modT%d" % l, [128, 48]) for l in range(layers)]
    g.mod1 = [sb(g, es, "mod1_%d" % l, [128, 48]) for l in range(layers)]
    g.lng = [sb(g, es, "lng%d" % l, [128, 16]) for l in range(layers)]
    g.lnb = [sb(g, es, "lnb%d" % l, [128, 16]) for l in range(layers)]
    if layers > n_a:
        g.kvmod = sb(g, es, "kvmod", [128, 16])
        g.kvmod1 = sb(g, es, "kvmod1", [128, 16])
    with ExitStack() as ph:
        st = sb(g, ph, "lv_st", [128, 128])
        cT = sb(g, ph, "cT", [128, KC])
        bT = sb(g, ph, "bT", [128, 48])
        load_vecT(g, cT, I['c'], KC, st)
        cs2 = sb(g, ph, "cs2", [128, KC, 2])
        k.act(cs2[:, :, 0], cT[:], AF.Silu, cT.k, cs2.k)
        k.act(cs2[:, :, 1], cT[:], AF.Silu, cT.k, cs2.k)
        wb = [sb(g, ph, "adaw%d" % i, [128, KC, 1024]) for i in range(2)]
        nblk = 0

        def matvec(w_ap, ncols, outT):
            nonlocal nblk
            wv = w_ap.rearrange("(kc p) n -> p kc n", p=128)
            for b0 in range(0, ncols, 1024):
                bw = min(1024, ncols - b0)
                w = wb[nblk % 2]
                nblk += 1
                k.dma('sp', w[:, :, 0:bw], wv[:, :, b0:b0 + bw], [], w.k)
                ps = g.ps[1 + (nblk % 2)]
                for j in range(bw // 128):
                    for kc in range(KC):
                        k.mm(ps[:, 2 * j:2 * j + 2], w[:, kc, j * 128:(j + 1) * 128], cs2[:, kc, :],
                             kc == 0, kc == KC - 1, w.k + cs2.k, [ps.k[0]])
                nj = bw // 128
                k.cp('dve', outT[:, b0 // 128:b0 // 128 + nj],
                     ps[:, 0:2 * nj].rearrange("p (j t) -> p j t", t=2)[:, :, 0], [ps.k[0]], outT.k)

        for l in range(layers):
            mt, m1 = g.modT[l], g.mod1[l]
            matvec(I['ada_w'][l], 6 * D, mt)
            load_vecT(g, bT, I['ada_b'][l], 48, st)
            k.tt('dve', mt[:], mt[:], bT[:], ALU.add, mt.k + bT.k, mt.k)
            k.ts('dve', m1[:], mt[:], 1.0, None, ALU.add, None, mt.k, m1.k)
            dump(g, "modT%d" % l, mt[:], mt.k)
            load_vecT(g, g.lng[l], I['ln_g'][l], 16, st)
            load_vecT(g, g.lnb[l], I['ln_b'][l], 16, st)
        if layers > n_a:
            kt, k1 = g.kvmod, g.kvmod1
            matvec(I['kv_ada_w'], 2 * D, kt)
            load_vecT(g, bT, I['kv_ada_b'], 16, st)
            k.tt('dve', kt[:], kt[:], bT[:, 0:16], ALU.add, kt.k + bT.k, kt.k)
            k.ts('dve', k1[:], kt[:], 1.0, None, ALU.add, None, kt.k, k1.k)
        k.barrier()


def phase_in(g):
    k, I, S = g.k, g.I, g.S
    T = g.T
    with ExitStack() as ph:
        xt = [sb(g, ph, "in_x%d" % i, [128, D]) for i in range(2)]
        st = [sb(g, ph, "in_st%d" % i, [128, KC, 512]) for i in range(2)]
        XTv = S['XT'].t.rearrange("(kc p) t -> p kc t", p=128)
        for gi in range(T // 128):
            xb = xt[gi % 2]
            k.dma('sp', xb[:], I['x'][gi * 128:(gi + 1) * 128, :], [], xb.k)
            sg = st[(gi // 4) % 2]
            for half in range(2):
                ps = g.ps[(gi * 2 + half) % 4]
                for j in range(4):
                    kc = half * 4 + j
                    k.tr(ps[:, j * 128:(j + 1) * 128], xb[:, kc * 128:(kc + 1) * 128], g.ident[:],
                         xb.k + g.ident.k, [ps.k[0]])
                dst = sg[:, half * 4:half * 4 + 4, (gi % 4) * 128:(gi % 4 + 1) * 128]
                src = ps[:, :].rearrange("p (j t) -> p j t", t=128)
                k.cp('act' if half else 'dve', dst, src, [ps.k[0]], sg.k)
            if gi % 4 == 3:
                t0 = (gi // 4) * 512
                k.dma('sp', XTv[:, :, t0:t0 + 512], sg[:], sg.k, S['XT'].k)
        k.barrier()


STUB = {'rwkv': False, 'fox': False}
RW_STOP = [99]
RW_SUB = [9]


def bc_rows(ap2d_row, nparts, ncols):
    return bass.AP(tensor=ap2d_row.tensor, offset=ap2d_row.offset, ap=[[0, nparts], [1, ncols]])


def mixer_zero(g):
    k, S, T = g.k, g.S, g.T
    with ExitStack() as ph:
        z = sb(g, ph, "mz", [128, KC, 512], BF16)
        k.op('pool', lambda e: e.memset(z[:], 0.0), [], z.k)
        MOv = S['MO'].t.rearrange("(kc p) t -> p kc t", p=128)
        for t0 in range(0, T, 512):
            k.dma('sp', MOv[:, :, t0:t0 + 512], z[:], z.k, S['MO'].k)
        k.barrier()


def phase_rwkv(g, l):
    if STUB['rwkv']:
        mixer_zero(g)
    else:
        rwkv_mixer(g, l)
    phase_tail(g, l, g.I['rw_wo'][l])


def phase_fox(g, l, j):
    if STUB['fox']:
        mixer_zero(g)
    else:
        fox_mixer(g, l, j)
    phase_tail(g, l, g.I['fx_wo'][j])


def ln_tile(g, zf, zk, sq, mean, rstd, gam, bet, outf, outk, w):
    k = g.k
    psm, psq = g.ps[6], g.ps[7]
    for kc in range(KC):
        k.act(sq[:, kc, 0:w], zf(kc), AF.Square, zk, sq.k)
    for kc in range(KC):
        k.mm(psm[:, 0:w], g.mmean[:], zf(kc), kc == 0, kc == KC - 1, g.mmean.k + zk, [psm.k[0]])
    for kc in range(KC):
        k.mm(psq[:, 0:w], g.mmean[:], sq[:, kc, 0:w], kc == 0, kc == KC - 1, g.mmean.k + sq.k, [psq.k[0]])
    k.cp('act', mean[:, 0:w], psm[:, 0:w], [psm.k[0]], mean.k)
    k.tt('dve', rstd[:, 0:w], mean[:, 0:w], mean[:, 0:w], ALU.mult, mean.k, rstd.k)
    k.tt('dve', rstd[:, 0:w], psq[:, 0:w], rstd[:, 0:w], ALU.subtract, [psq.k[0]] + rstd.k, rstd.k)
    k.ts('dve', rstd[:, 0:w], rstd[:, 0:w], LN_EPS, None, ALU.add, None, rstd.k, rstd.k)
    k.act(rstd[:, 0:w], rstd[:, 0:w], AF.Sqrt, rstd.k, rstd.k)
    k.op('dve', lambda e: e.reciprocal(out=rstd[:, 0:w], in_=rstd[:, 0:w]), rstd.k, rstd.k)
    for kc in range(KC):
        k.tt('dve', zf(kc), zf(kc), mean[:, 0:w], ALU.subtract, zk + mean.k, zk)
        k.tt('pool', zf(kc), zf(kc), rstd[:, 0:w], ALU.mult, zk + rstd.k, zk)
        k.act(outf(kc), zf(kc), AF.Identity, zk, outk, scale=gam(kc), bias=bet(kc))


def phase_tail(g, l, wo_ap):
    k, I, S, T = g.k, g.I, g.S, g.T
    NE, NG, NR, EPG = g.NE, g.NG, g.NR, g.EPG
    mt, m1 = g.modT[l], g.mod1[l]
    GT = g.GT
    with ExitStack() as ph:
        wo = sb(g, ph, "t_wo", [128, KC, D], BF16)
        k.dma('pool', wo[:], wo_ap.rearrange("(kc p) n -> p kc n", p=128), [], wo.k)
        wr = sb(g, ph, "t_wr", [128, KC, NR])
        k.dma('sp', wr[:, :, 0:NG], I['moe_wgrp'][l].rearrange("(kc p) n -> p kc n", p=128), [], wr.k)
        k.dma('sp', wr[:, :, NG:NR], I['moe_wexp'][l].rearrange("(kc p) n -> p kc n", p=128), [], wr.k)
        rb = sb(g, ph, "t_rb", [128, NR])
        k.dma('sp', rb[:, 0:NG], bc_rows(I['moe_bgrp'][l], 128, NG), [], rb.k)
        k.dma('sp', rb[:, NG:NR], bc_rows(I['moe_bexp'][l], 128, NE), [], rb.k)
        xt = [sb(g, ph, "t_x%d" % i, [128, KC, 512]) for i in range(2)]
        zt = [sb(g, ph, "t_z%d" % i, [128, KC, 512]) for i in range(2)]
        mo = [sb(g, ph, "t_mo%d" % i, [128, KC, 512], BF16) for i in range(2)]
        hb = [sb(g, ph, "t_hb%d" % i, [128, KC, 512], BF16) for i in range(2)]
        sq = sb(g, ph, "t_sq", [128, KC, 512])
        mean = sb(g, ph, "t_mean", [128, 512])
        rstd = sb(g, ph, "t_rstd", [128, 512])
        sm = {n: sb(g, ph, "t_r_" + n, [128, w_]) for n, w_ in
              [('lg', NR), ('mg', 1), ('nmg', 1), ('eg', NG), ('sg', 1), ('pg', 1), ('oh', NG), ('pen', NG),
               ('le', NE), ('t8', 8), ('d12', 1), ('s12', 1), ('g1', 1), ('g2', 1), ('G1', NE), ('G2', NE)]}
        XTv = S['XT'].t.rearrange("(kc p) t -> p kc t", p=128)
        X1v = S['X1'].t.rearrange("(kc p) t -> p kc t", p=128)
        MOv = S['MO'].t.rearrange("(kc p) t -> p kc t", p=128)
        H2v = S['H2'].t.rearrange("(kc p) t -> p kc t", p=128)
        for ti in range(T // 512):
            t0 = ti * 512
            x, z, m, h = xt[ti % 2], zt[ti % 2], mo[ti % 2], hb[ti % 2]
            k.dma('sp', x[:], XTv[:, :, t0:t0 + 512], S['XT'].k, x.k)
            k.dma('sp', m[:], MOv[:, :, t0:t0 + 512], S['MO'].k, m.k)
            for oc in range(KC):
                ps = g.ps[oc % 2]
                for kc in range(KC):
                    k.mm(ps[:, :], wo[:, kc, oc * 128:(oc + 1) * 128], m[:, kc, :], kc == 0, kc == KC - 1,
                         wo.k + m.k, [ps.k[0]])
                k.act(z[:, oc, :], ps[:, :], AF.Identity, [ps.k[0]], z.k, scale=m1[:, 16 + oc:17 + oc])
                k.stt(z[:, oc, :], x[:, oc, :], ALPHA, z[:, oc, :], ALU.mult, ALU.add, x.k + z.k, z.k)
            ln_tile(g, lambda kc: z[:, kc, :], z.k, sq, mean, rstd,
                    lambda kc: g.lng[l][:, kc:kc + 1], lambda kc: g.lnb[l][:, kc:kc + 1],
                    lambda kc: x[:, kc, :], x.k, 512)
            k.dma('sp', X1v[:, :, t0:t0 + 512], x[:], x.k, S['X1'].k)
            for kc in range(KC):
                k.act(z[:, kc, :], x[:, kc, :], AF.Identity, x.k, z.k,
                      scale=m1[:, 32 + kc:33 + kc], bias=mt[:, 24 + kc:25 + kc])
            k.cp('pool', h[:], z[:], z.k, h.k)
            k.dma('sp', H2v[:, :, t0:t0 + 512], h[:], h.k, S['H2'].k)
            for tg in range(4):
                pr = g.ps[2 + tg % 2]
                for kc in range(KC):
                    k.mm(pr[:, 0:NR], z[:, kc, tg * 128:(tg + 1) * 128], wr[:, kc, :], kc == 0, kc == KC - 1,
                         z.k + wr.k, [pr.k[0]])
                lg, mg, nmg, eg, sg, pg = sm['lg'], sm['mg'], sm['nmg'], sm['eg'], sm['sg'], sm['pg']
                oh, pen, le, t8 = sm['oh'], sm['pen'], sm['le'], sm['t8']
                k.tt('dve', lg[:], pr[:, 0:NR], rb[:], ALU.add, [pr.k[0]] + rb.k, lg.k)
                k.op('dve', lambda e: e.tensor_reduce(out=mg[:], in_=lg[:, 0:NG], axis=AX.X, op=ALU.max), lg.k, mg.k)
                k.ts('dve', nmg[:], mg[:], -1.0, None, ALU.mult, None, mg.k, nmg.k)
                k.act(eg[:], lg[:, 0:NG], AF.Exp, lg.k + nmg.k, eg.k + sg.k, bias=nmg[:, 0:1], accum_out=sg[:])
                k.op('dve', lambda e: e.reciprocal(out=pg[:], in_=sg[:]), sg.k, pg.k)
                k.ts('dve', oh[:], lg[:, 0:NG], mg[:, 0:1], None, ALU.is_equal, None, lg.k + mg.k, oh.k)
                k.ts('dve', pen[:], oh[:], -1.0, 1e30, ALU.add, ALU.mult, oh.k, pen.k)
                for gi in range(NG):
                    k.ts('dve', le[:, gi * EPG:(gi + 1) * EPG], lg[:, NG + gi * EPG:NG + (gi + 1) * EPG],
                         pen[:, gi:gi + 1], None, ALU.add, None, lg.k + pen.k, le.k)
                k.op('dve', lambda e: e.max(out=t8[:], in_=le[:]), le.k, t8.k)
                d12, s12, g1, g2, G1, G2 = sm['d12'], sm['s12'], sm['g1'], sm['g2'], sm['G1'], sm['G2']
                k.tt('dve', d12[:], t8[:, 0:1], t8[:, 1:2], ALU.subtract, t8.k, d12.k)
                k.act(s12[:], d12[:], AF.Sigmoid, d12.k, s12.k)
                k.tt('dve', g1[:], s12[:], pg[:], ALU.mult, s12.k + pg.k, g1.k)
                k.tt('dve', g2[:], pg[:], g1[:], ALU.subtract, pg.k + g1.k, g2.k)
                k.ts('dve', G1[:], le[:], t8[:, 0:1], g1[:, 0:1], ALU.is_equal, ALU.mult, le.k + t8.k + g1.k, G1.k)
                k.ts('dve', G2[:], le[:], t8[:, 1:2], g2[:, 0:1], ALU.is_equal, ALU.mult, le.k + t8.k + g2.k, G2.k)
                k.tt('dve', G1[:], G1[:], G2[:], ALU.add, G1.k + G2.k, G1.k)
                pt = g.ps[4 + tg % 2]
                k.tr(pt[0:NE, 0:128], G1[:], g.ident[:], G1.k + g.ident.k, [pt.k[0]])
                c0 = t0 + tg * 128
                k.cp('dve', GT[0:NE, c0:c0 + 128], pt[0:NE, 0:128], [pt.k[0]], GT.k)
        dump(g, "GT%d" % l, GT[0:NE, :], GT.k)
        k.barrier()


def phase_moe(g, l, last):
    k, I, S, T = g.k, g.I, g.S, g.T
    NE = g.NE
    mt, m1 = g.modT[l], g.mod1[l]
    GT = g.GT
    HT = min(2048, T)
    NTT = HT // 512
    X1v = S['X1'].t.rearrange("(kc p) t -> p kc t", p=128)
    XTv = S['XT'].t.rearrange("(kc p) t -> p kc t", p=128)
    H2v = S['H2'].t.rearrange("(kc p) t -> p kc t", p=128)
    with ExitStack() as ph:
        h2 = sb(g, ph, "m_h2", [128, KC, HT], BF16)
        yacc = sb(g, ph, "m_y", [128, KC, HT], F32, n=KC * NTT)
        for hf in range(T // HT):
            tb = hf * HT
            for tt in range(NTT):
                k.dma('sp', h2[:, :, tt * 512:(tt + 1) * 512], H2v[:, :, tb + tt * 512:tb + (tt + 1) * 512],
                      S['H2'].k, h2.k)
            with ExitStack() as ex:
                wg = [sb(g, ex, "m_wg%d" % i, [128, KC, DEXP], BF16) for i in range(2)]
                wu = [sb(g, ex, "m_wu%d" % i, [128, KC, DEXP], BF16) for i in range(2)]
                wd = [sb(g, ex, "m_wd%d" % i, [128, 4, D], BF16) for i in range(2)]
                sel = [sb(g, ex, "m_sel%d" % i, [128, 128]) for i in range(2)]
                gbc = sb(g, ex, "m_gbc", [128, 512])
                sl = [sb(g, ex, "m_sl%d" % i, [128, 512]) for i in range(2)]
                tl = sb(g, ex, "m_tl", [128, 512])
                hT = [sb(g, ex, "m_hT%d" % i, [128, 4, 512], BF16) for i in range(2)]
                it = 0

                def load_w(e):
                    bi = e % 2
                    k.dma('pool', wg[bi][:], I['moe_wgate'][l, e].rearrange("(kc p) n -> p kc n", p=128), [], wg[bi].k)
                    k.dma('pool', wu[bi][:], I['moe_wup'][l, e].rearrange("(kc p) n -> p kc n", p=128), [], wu[bi].k)
                    k.dma('pool', wd[bi][:], I['moe_wdown'][l, e].rearrange("(dc p) n -> p dc n", p=128), [], wd[bi].k)
                load_w(0)
                pend = None
                for e in range(NE):
                    bi = e % 2
                    se = sel[bi]
                    k.op('pool', lambda e_, se=se, e=e: e_.affine_select(
                        out=se[0:NE, :], in_=g.ones[0:NE, 0:128], pattern=[[0, 128]], compare_op=ALU.is_equal,
                        fill=0.0, base=-e, channel_multiplier=1), g.ones.k, se.k)
                    for tt in range(NTT):
                        c0 = tt * 512
                        psg = g.ps[0]
                        k.mm(psg[:, :], se[0:NE, :], GT[0:NE, tb + c0:tb + c0 + 512], True, True,
                             se.k + GT.k, [psg.k[0]])
                        k.cp('act', gbc[:], psg[:, :], [psg.k[0]], gbc.k)
                        hh = hT[it % 2]
                        it += 1
                        for dc in range(4):
                            pg_, pu_ = g.ps[1 + dc % 2], g.ps[3 + dc % 2]
                            for kc in range(KC):
                                k.mm(pg_[:, :], wg[bi][:, kc, dc * 128:(dc + 1) * 128], h2[:, kc, c0:c0 + 512],
                                     kc == 0, kc == KC - 1, wg[bi].k + h2.k, [pg_.k[0]])
                            for kc in range(KC):
                                k.mm(pu_[:, :], wu[bi][:, kc, dc * 128:(dc + 1) * 128], h2[:, kc, c0:c0 + 512],
                                     kc == 0, kc == KC - 1, wu[bi].k + h2.k, [pu_.k[0]])
                            s_ = sl[dc % 2]
                            k.act(s_[:], pg_[:, :], AF.Silu, [pg_.k[0]], s_.k)
                            k.tt('dve', tl[:], pu_[:, :], s_[:], ALU.mult, [pu_.k[0]] + s_.k, tl.k)
                            k.tt('dve', hh[:, dc, :], tl[:], gbc[:], ALU.mult, tl.k + gbc.k, hh.k)

                        def down(e=e, bi=bi, tt=tt, c0=c0, hh=hh):
                            for oc in range(KC):
                                py = g.ps[5 + oc % 2]
                                for dc in range(4):
                                    k.mm(py[:, :], wd[bi][:, dc, oc * 128:(oc + 1) * 128], hh[:, dc, :],
                                         dc == 0, dc == 3, wd[bi].k + hh.k, [py.k[0]])
                                yk = [yacc.k[oc * NTT + tt]]
                                ya = yacc[:, oc, c0:c0 + 512]
                                if e == 0:
                                    k.cp('dve', ya, py[:, :], [py.k[0]], yk)
                                else:
                                    k.tt('dve', ya, py[:, :], ya, ALU.add, [py.k[0]] + yk, yk)
                        if pend is not None:
                            pend()
                        pend = down
                        if tt == 0 and e + 1 < NE:
                            load_w(e + 1)
                pend()
                k.barrier()
            with ExitStack() as ex:
                xt = [sb(g, ex, "m_x%d" % i, [128, KC, 512]) for i in range(2)]
                sq = sb(g, ex, "m_sq", [128, KC, 512])
                mean = sb(g, ex, "m_mean", [128, 512])
                rstd = sb(g, ex, "m_rstd", [128, 512])
                ot = [sb(g, ex, "m_ot%d" % i, [128, D]) for i in range(2)] if last else None
                for tt in range(NTT):
                    c0 = tt * 512
                    t0 = tb + c0
                    x = xt[tt % 2]
                    k.dma('sp', x[:], X1v[:, :, t0:t0 + 512], S['X1'].k, x.k)
                    zk = [yacc.k[oc * NTT + tt] for oc in range(KC)]
                    for oc in range(KC):
                        ya = yacc[:, oc, c0:c0 + 512]
                        k.act(ya, ya, AF.Identity, zk, zk, scale=m1[:, 40 + oc:41 + oc])
                        k.stt(ya, x[:, oc, :], ALPHA, ya, ALU.mult, ALU.add, x.k + zk, zk)
                    ln_tile(g, lambda kc: yacc[:, kc, c0:c0 + 512], zk, sq, mean, rstd,
                            lambda kc: g.lng[l][:, 8 + kc:9 + kc], lambda kc: g.lnb[l][:, 8 + kc:9 + kc],
                            lambda kc: x[:, kc, :], x.k, 512)
                    if not last:
                        k.dma('sp', XTv[:, :, t0:t0 + 512], x[:], x.k, S['XT'].k)
                    else:
                        for tg in range(4):
                            o = ot[tg % 2]
                            for half in range(2):
                                ps = g.ps[half]
                                for j in range(4):
                                    kc = half * 4 + j
                                    k.tr(ps[:, j * 128:(j + 1) * 128], x[:, kc, tg * 128:(tg + 1) * 128], g.ident[:],
                                         x.k + g.ident.k, [ps.k[0]])
                                k.cp('act' if half else 'dve', o[:, half * 512:(half + 1) * 512], ps[:, :],
                                     [ps.k[0]], o.k)
                            k.dma('sp', g.out[t0 + tg * 128:t0 + (tg + 1) * 128, :], o[:], o.k, [])
                k.barrier()
        k.barrier()


def head_rms_fm(g, k, pin, outap, outk, sqt, rst, nrm, extra_scale):
    pss = g.ps[7]
    k.act(sqt[:], pin[:, :], AF.Square, [pin.k[0]], sqt.k)
    k.mm(pss[:, :], g.blk[:], sqt[:], True, True, g.blk.k + sqt.k, [pss.k[0]])
    k.ts('dve', rst[:], pss[:, :], 1.0 / 64, QK_EPS, ALU.mult, ALU.add, [pss.k[0]], rst.k)
    k.act(rst[:], rst[:], AF.Sqrt, rst.k, rst.k)
    k.op('dve', lambda e: e.reciprocal(out=rst[:], in_=rst[:]), rst.k, rst.k)
    k.tt('dve', sqt[:], pin[:, :], rst[:], ALU.mult, [pin.k[0]] + rst.k, sqt.k)
    k.ts('pool', outap, sqt[:], nrm[:, 0:1], extra_scale, ALU.mult, ALU.mult, sqt.k + nrm.k, outk)


def phase_kv(g):
    k, I, S, T = g.k, g.I, g.S, g.T
    NTG = T // 128
    g.FQ = Buf(g.nc.dram_tensor("FQ3", [NH, 3, T], BF16, kind="Internal").ap())
    g.FK = Buf(g.nc.dram_tensor("NFD", [NH, 128, NTG], F32, kind="Internal").ap())
    with ExitStack() as ph:
        wv_ = lambda ap: ap.rearrange("(kc p) n -> p kc n", p=128)
        wk = sb(g, ph, "kv_wk", [128, KC, D], BF16)
        wv = sb(g, ph, "kv_wv", [128, KC, D], BF16)
        wf = sb(g, ph, "kv_wf", [128, KC, NH], BF16)
        k.dma('pool', wk[:], wv_(I['kv_w'][:, 0:D]), [], wk.k)
        k.dma('pool', wv[:], wv_(I['kv_w'][:, D:2 * D]), [], wv.k)
        k.dma('pool', wf[:], wv_(I['kv_w'][:, 2 * D:2 * D + NH]), [], wf.k)
        kn = sb(g, ph, "kv_kn", [128, 1])
        k.dma('sp', kn[0:64, :], I['kv_knorm'], [], kn.k)
        k.dma('sp', kn[64:128, :], I['kv_knorm'], [], kn.k)
        fb = sb(g, ph, "kv_fb", [NH, 1])
        k.dma('sp', fb[:], I['kv_fb'], [], fb.k)
        k.barrier()
        xb = [sb(g, ph, "kv_x%d" % i, [128, KC, 512]) for i in range(2)]
        hk = [sb(g, ph, "kv_h%d" % i, [128, KC, 512], BF16) for i in range(2)]
        kst = [sb(g, ph, "kv_ks%d" % i, [128, KC, 512], BF16) for i in range(2)]
        vst = [sb(g, ph, "kv_vs%d" % i, [128, D], BF16) for i in range(2)]
        sqt = sb(g, ph, "kv_sq", [128, 512])
        rst = sb(g, ph, "kv_rs", [128, 512])
        lf = sb(g, ph, "kv_lf", [NH, 512])
        fc = [sb(g, ph, "kv_fc%d" % i, [NH, 512]) for i in range(2)]
        nfc = [sb(g, ph, "kv_nfc%d" % i, [NH, 512]) for i in range(2)]
        fsp = [[sb(g, ph, "kv_fsp%d_%d" % (i, q), [NH, 512], BF16) for q in range(3)] for i in range(2)]
        fr = sb(g, ph, "kv_fr", [NH, 512])
        NFs = sb(g, ph, "kv_NFs", [128, NH, NTG])
        XTv = S['XT'].t.rearrange("(kc p) t -> p kc t", p=128)
        KTv = S['KT'].t.rearrange("(kc p) t -> p kc t", p=128)
        for ti in range(T // 512):
            t0 = ti * 512
            x, h, ks = xb[ti % 2], hk[ti % 2], kst[ti % 2]
            k.dma('sp', x[:], XTv[:, :, t0:t0 + 512], S['XT'].k, x.k)
            for kc in range(KC):
                k.act(h[:, kc, :], x[:, kc, :], AF.Identity, x.k, h.k,
                      scale=g.kvmod1[:, 8 + kc:9 + kc], bias=g.kvmod[:, kc:kc + 1])
            for oc in range(KC):
                p = g.ps[oc % 2]
                for kc in range(KC):
                    k.mm(p[:, :], wk[:, kc, oc * 128:(oc + 1) * 128], h[:, kc, :], kc == 0, kc == KC - 1,
                         wk.k + h.k, [p.k[0]])
                head_rms_fm(g, k, p, ks[:, oc, :], ks.k, sqt, rst, kn, 1.0)
            k.dma('sp', KTv[:, :, t0:t0 + 512], ks[:], ks.k, S['KT'].k)
            for tg in range(4):
                vs = vst[tg % 2]
                for half in range(2):
                    p = g.ps[2 + half]
                    for kc in range(KC):
                        k.mm(p[:, :], h[:, kc, tg * 128:(tg + 1) * 128], wv[:, kc, half * 512:(half + 1) * 512],
                             kc == 0, kc == KC - 1, wv.k + h.k, [p.k[0]])
                    k.cp('act' if half else 'dve', vs[:, half * 512:(half + 1) * 512], p[:, :], [p.k[0]], vs.k)
                k.dma('sp', S['VK'].t[t0 + tg * 128:t0 + (tg + 1) * 128, :], vs[:], vs.k, S['VK'].k)
            p = g.ps[4]
            for kc in range(KC):
                k.mm(p[0:NH, :], wf[:, kc, :], h[:, kc, :], kc == 0, kc == KC - 1, wf.k + h.k, [p.k[0]])
            k.act(lf[:], p[0:NH, :], AF.Sigmoid, [p.k[0]], lf.k, bias=fb[:, 0:1])
            k.act(lf[:], lf[:], AF.Ln, lf.k, lf.k)
            f, fp_, nf = fc[ti % 2], fc[(ti + 1) % 2], nfc[ti % 2]
            init = 0.0 if ti == 0 else fp_[:, 511:512]
            k.op('dve', lambda e, f=f, init=init: e.tensor_tensor_scan(
                out=f[:], data0=g.ones[0:NH, 0:512], data1=lf[:], initial=init, op0=ALU.mult, op1=ALU.add),
                g.ones.k + lf.k + fp_.k, f.k)
            k.ts('pool', nf[:], f[:], -1.0, None, ALU.mult, None, f.k, nf.k)
            sp3 = fsp[ti % 2]
            k.cp('dve', sp3[0][:], f[:], f.k, sp3[0].k)
            k.tt('dve', fr[:], f[:], sp3[0][:], ALU.subtract, f.k + sp3[0].k, fr.k)
            k.cp('dve', sp3[1][:], fr[:], fr.k, sp3[1].k)
            k.tt('dve', fr[:], fr[:], sp3[1][:], ALU.subtract, fr.k + sp3[1].k, fr.k)
            k.cp('dve', sp3[2][:], fr[:], fr.k, sp3[2].k)
            for q in range(3):
                k.dma('sp', g.FQ.t[:, q, t0:t0 + 512], sp3[q][:], sp3[q].k, g.FQ.k)
            for tg in range(4):
                pt = g.ps[5]
                k.tr(pt[:, 0:NH], nf[:, tg * 128:(tg + 1) * 128], g.ident[0:NH, 0:NH], nf.k + g.ident.k, [pt.k[0]])
                k.cp('dve', NFs[:, :, ti * 4 + tg], pt[:, 0:NH], [pt.k[0]], NFs.k)
        k.dma('sp', g.FK.t.rearrange("h p k -> p h k"), NFs[:], NFs.k, g.FK.k)
        k.barrier()


def fox_mixer(g, l, j):
    k, I, S, T = g.k, g.I, g.S, g.T
    mt, m1 = g.modT[l], g.mod1[l]
    XTv = S['XT'].t.rearrange("(kc p) t -> p kc t", p=128)
    QTv = S['QT'].t.rearrange("(kc p) t -> p kc t", p=128)
    SGv = S['SG'].t.rearrange("(kc p) t -> p kc t", p=128)
    with ExitStack() as ph:
        wv_ = lambda ap: ap.rearrange("(kc p) n -> p kc n", p=128)
        wq = sb(g, ph, "f_wq", [128, KC, D], BF16)
        wg = sb(g, ph, "f_wg", [128, KC, D], BF16)
        k.dma('pool', wq[:], wv_(I['fx_wqg'][j][:, 0:D]), [], wq.k)
        k.dma('pool', wg[:], wv_(I['fx_wqg'][j][:, D:2 * D]), [], wg.k)
        qn = sb(g, ph, "f_qn", [128, 1])
        k.dma('sp', qn[0:64, :], I['fx_qnorm'][j], [], qn.k)
        k.dma('sp', qn[64:128, :], I['fx_qnorm'][j], [], qn.k)
        k.barrier()
        xb = [sb(g, ph, "f_x%d" % i, [128, KC, 512]) for i in range(2)]
        hb = [sb(g, ph, "f_h%d" % i, [128, KC, 512], BF16) for i in range(2)]
        qst = [sb(g, ph, "f_qs%d" % i, [128, KC, 512], BF16) for i in range(2)]
        gst = [sb(g, ph, "f_gs%d" % i, [128, KC, 512], BF16) for i in range(2)]
        sqt = sb(g, ph, "f_sq", [128, 512])
        rst = sb(g, ph, "f_rs", [128, 512])
        for ti in range(T // 512):
            t0 = ti * 512
            x, h, qs, gs = xb[ti % 2], hb[ti % 2], qst[ti % 2], gst[ti % 2]
            k.dma('sp', x[:], XTv[:, :, t0:t0 + 512], S['XT'].k, x.k)
            for kc in range(KC):
                k.act(h[:, kc, :], x[:, kc, :], AF.Identity, x.k, h.k,
                      scale=m1[:, 8 + kc:9 + kc], bias=mt[:, kc:kc + 1])
            for oc in range(KC):
                p = g.ps[oc % 2]
                for kc in range(KC):
                    k.mm(p[:, :], wq[:, kc, oc * 128:(oc + 1) * 128], h[:, kc, :], kc == 0, kc == KC - 1,
                         wq.k + h.k, [p.k[0]])
                head_rms_fm(g, k, p, qs[:, oc, :], qs.k, sqt, rst, qn, HD ** -0.5)
                p2 = g.ps[2 + oc % 2]
                for kc in range(KC):
                    k.mm(p2[:, :], wg[:, kc, oc * 128:(oc + 1) * 128], h[:, kc, :], kc == 0, kc == KC - 1,
                         wg.k + h.k, [p2.k[0]])
                k.act(gs[:, oc, :], p2[:, :], AF.Sigmoid, [p2.k[0]], gs.k)
            k.dma('sp', QTv[:, :, t0:t0 + 512], qs[:], qs.k, S['QT'].k)
            k.dma('sp', SGv[:, :, t0:t0 + 512], gs[:], gs.k, S['SG'].k)
        k.barrier()
    with ExitStack() as ph:
        identb = sb(g, ph, "a_identb", [128, 128], BF16)
        k.cp('pool', identb[:], g.ident[:], g.ident.k, identb.k)
        zer = sb(g, ph, "a_zer", [128, 128])
        k.op('pool', lambda e: e.memset(zer[:], 0.0), [], zer.k)
        nmask = sb(g, ph, "a_nmask", [128, 128], BF16)
        k.op('pool', lambda e: e.affine_select(out=nmask[:], in_=zer[:], pattern=[[1, 128]], compare_op=ALU.is_ge,
                                               fill=-30000.0, base=0, channel_multiplier=-1), zer.k, nmask.k)
        onesb = sb(g, ph, "a_onesb", [128, 64], BF16)
        k.op('pool', lambda e: e.memset(onesb[:], 1.0), [], onesb.k)
        k.barrier()
        NTG = T // 128
        KTh = [sb(g, ph, "a_k%d" % i, [67, T], BF16) for i in range(2)]
        QTh = [sb(g, ph, "a_q%d" % i, [67, T], BF16) for i in range(2)]
        for i in range(2):
            k.op('pool', lambda e, i=i: e.memset(KTh[i][64:67, :], 1.0), [], KTh[i].k)
        SGh = [sb(g, ph, "a_g%d" % i, [64, T], BF16) for i in range(2)]
        Vh = [sb(g, ph, "a_v%d" % i, [128, NTG, 64], BF16) for i in range(2)]
        nfk = [sb(g, ph, "a_nfk%d" % i, [128, NTG]) for i in range(2)]
        Pb = [sb(g, ph, "a_p%d" % i, [128, 512], BF16) for i in range(3)]
        rl = sb(g, ph, "a_rl", [64, 512])
        of = sb(g, ph, "a_of", [64, 512])
        ost = [sb(g, ph, "a_os%d" % i, [64, 512], BF16) for i in range(2)]
        it = 0
        for hd in range(NH):
            b = hd % 2
            hr = slice(hd * 64, (hd + 1) * 64)
            k.dma('sp', KTh[b][0:64, :], S['KT'].t[hr, :], S['KT'].k, KTh[b].k)
            k.dma('sp', QTh[b][0:64, :], S['QT'].t[hr, :], S['QT'].k, QTh[b].k)
            k.dma('sp', QTh[b][64:67, :], g.FQ.t[hd], g.FQ.k, QTh[b].k)
            k.dma('sp', nfk[b][:], g.FK.t[hd], g.FK.k, nfk[b].k)
            k.dma('sp', SGh[b][:], S['SG'].t[hr, :], S['SG'].k, SGh[b].k)
            k.dma('sp', Vh[b][:], S['VK'].t[:, hr].rearrange("(tg p) d -> p tg d", p=128), S['VK'].k, Vh[b].k)
            for qg in range(T // 512):
                q0 = qg * 512
                Op, Lp = g.ps[2 + 2 * (qg % 2)], g.ps[3 + 2 * (qg % 2)]
                kts = list(range(4 * qg + 4))
                pend = None
                for idx, kt in enumerate(kts):
                    diag = kt >= 4 * qg
                    qlo = (kt - 4 * qg) * 128 if diag else 0
                    n = 512 - qlo
                    Sp = g.ps[idx % 2]
                    kc_ = slice(kt * 128, (kt + 1) * 128)
                    qc_ = slice(q0 + qlo, q0 + 512)
                    k.mm(Sp[:, 0:n], KTh[b][:, kc_], QTh[b][:, qc_], True, not diag, KTh[b].k + QTh[b].k, [Sp.k[0]])
                    if diag:
                        k.mm(Sp[:, 0:128], identb[:], nmask[:], False, True, identb.k + nmask.k, [Sp.k[0]])
                    P = Pb[it % 3]
                    it += 1
                    k.act(P[:, 0:n], Sp[:, 0:n], AF.Exp, [Sp.k[0]] + nfk[b].k, P.k, bias=nfk[b][:, kt:kt + 1])
                    first, lastf = idx == 0, idx == len(kts) - 1

                    def pvl(Op=Op, Lp=Lp, P=P, kt=kt, qlo=qlo, n=n, first=first, lastf=lastf, b=b):
                        k.mm(Op[0:64, qlo:512], Vh[b][:, kt, :], P[:, 0:n], first, lastf, Vh[b].k + P.k, [Op.k[0]])
                        k.mm(Lp[0:64, qlo:512], onesb[:], P[:, 0:n], first, lastf, onesb.k + P.k, [Lp.k[0]])
                    if pend is not None:
                        pend()
                    pend = pvl
                pend()
                pend = None
                k.op('dve', lambda e, Lp=Lp: e.reciprocal(out=rl[:], in_=Lp[0:64, :]), [Lp.k[0]], rl.k)
                k.tt('dve', of[:], Op[0:64, :], rl[:], ALU.mult, [Op.k[0]] + rl.k, of.k)
                o = ost[qg % 2]
                k.tt('pool', o[:], of[:], SGh[b][:, q0:q0 + 512], ALU.mult, of.k + SGh[b].k, o.k)
                k.dma('sp', S['MO'].t[hr, q0:q0 + 512], o[:], o.k, S['MO'].k)
        k.barrier()


def setup_rwkv_consts(g, es):
    k = g.k
    ones = g.ones
    su = sb(g, es, "c_su", [128, 128])
    iu = sb(g, es, "c_iu", [128, 128])
    g.mask4 = sb(g, es, "c_mask4", [128, 512])
    g.sl = sb(g, es, "c_sl", [128, 128])
    g.rm = sb(g, es, "c_rm", [128, 256])
    k.op('pool', lambda e: e.affine_select(out=su[:], in_=ones[:, 0:128], pattern=[[1, 128]], compare_op=ALU.is_gt,
                                           fill=0.0, base=0, channel_multiplier=-1), ones.k, su.k)
    k.op('pool', lambda e: e.affine_select(out=iu[:], in_=ones[:, 0:128], pattern=[[1, 128]], compare_op=ALU.is_ge,
                                           fill=0.0, base=0, channel_multiplier=-1), ones.k, iu.k)
    k.op('pool', lambda e: e.affine_select(out=g.sl[:], in_=ones[:, 0:128], pattern=[[-1, 128]], compare_op=ALU.is_gt,
                                           fill=0.0, base=0, channel_multiplier=1), ones.k, g.sl.k)
    k.tt('pool', g.sl[:], g.sl[:], g.blk[:], ALU.mult, g.sl.k + g.blk.k, g.sl.k)
    for q in range(4):
        src = su if q < 2 else iu
        k.tt('pool', g.mask4[:, q * 128:(q + 1) * 128], src[:], g.blk[:], ALU.mult, src.k + g.blk.k, g.mask4.k)
    k.op('pool', lambda e: e.memset(g.rm[:], 1.0), [], g.rm.k)
    for q in range(4):
        k.op('pool', lambda e, q=q: e.memset(g.rm[:, q * 64:q * 64 + 1], 0.0), [], g.rm.k)
    k.barrier()


def rwkv_mixer(g, l):
    k, I, S, T = g.k, g.I, g.S, g.T
    mt, m1 = g.modT[l], g.mod1[l]
    W = 256
    ps = g.ps
    with ExitStack() as ph:
        setup_rwkv_consts(g, ph)
        wv_ = lambda ap: ap.rearrange("(kc p) n -> p kc n", p=128)
        wr_ = sb(g, ph, "r_wr", [128, KC, D], BF16)
        wk_ = sb(g, ph, "r_wk", [128, KC, D], BF16)
        wvv = sb(g, ph, "r_wv", [128, KC, D], BF16)
        k.dma('pool', wr_[:], wv_(I['rw_rkv'][l, 0]), [], wr_.k)
        k.dma('pool', wk_[:], wv_(I['rw_rkv'][l, 1]), [], wk_.k)
        k.dma('pool', wvv[:], wv_(I['rw_rkv'][l, 2]), [], wvv.k)
        w1 = sb(g, ph, "r_w1", [128, KC, 64], BF16)
        a1 = sb(g, ph, "r_a1", [128, KC, 64], BF16)
        g1 = sb(g, ph, "r_g1", [128, KC, 160], BF16)
        k.dma('pool', w1[:], wv_(I['rw_w1'][l]), [], w1.k)
        k.dma('pool', a1[:], wv_(I['rw_a1'][l]), [], a1.k)
        k.dma('pool', g1[:], wv_(I['rw_g1'][l]), [], g1.k)
        w2 = sb(g, ph, "r_w2", [64, D], BF16)
        a2 = sb(g, ph, "r_a2", [64, D], BF16)
        g2 = sb(g, ph, "r_g2", [128, 2, D], BF16)
        k.dma('pool', w2[:], I['rw_w2'][l], [], w2.k)
        k.dma('pool', a2[:], I['rw_a2'][l], [], a2.k)
        k.dma('pool', g2[:, 0, :], I['rw_g2'][l, 0:128, :], [], g2.k)
        k.dma('pool', g2[0:32, 1, :], I['rw_g2'][l, 128:160, :], [], g2.k)
        if l > 0:
            v1 = sb(g, ph, "r_v1", [128, KC, 32], BF16)
            v2 = sb(g, ph, "r_v2", [32, D], BF16)
            k.dma('pool', v1[:], wv_(I['rw_v1'][l - 1]), [], v1.k)
            k.dma('pool', v2[:], I['rw_v2'][l - 1], [], v2.k)
        st = sb(g, ph, "r_lvst", [128, 128])
        pv = {}
        for nm, R in [('rw_mu', 48), ('rw_w0', 8), ('rw_a0', 8), ('rw_kk', 8), ('rw_ka', 8), ('rw_rk', 8),
                      ('rw_gn_g', 8), ('rw_gn_b', 8)]:
            pv[nm] = sb(g, ph, "r_p_" + nm, [128, R])
            load_vecT(g, pv[nm], I[nm][l], R, st)
        if l > 0:
            pv['rw_v0'] = sb(g, ph, "r_p_v0", [128, 8])
            load_vecT(g, pv['rw_v0'], I['rw_v0'][l - 1], 8, st)
        hm = sb(g, ph, "r_hm", [128, 2])
        k.cp('pool', hm[:, 0:1], g.blk[:, 0:1], g.blk.k, hm.k)
        k.cp('pool', hm[:, 1:2], g.blk[:, 127:128], g.blk.k, hm.k)
        Bth = [sb(g, ph, "r_Bth%d" % i, [128, W], F32R) for i in range(2)]
        Kth = [sb(g, ph, "r_Kth%d" % i, [128, W], F32R) for i in range(2)]
        Ath = [sb(g, ph, "r_Ath%d" % i, [128, W], F32R) for i in range(2)]
        VTc = [sb(g, ph, "r_VTc%d" % i, [128, 128], F32R) for i in range(2)]
        VTm = [sb(g, ph, "r_VTm%d" % i, [128, 128], F32R) for i in range(2)]
        cmk = [sb(g, ph, "r_cmk%d" % i, [128, 128]) for i in range(2)]
        for i in range(2):
            k.op('pool', lambda e, i=i: e.memset(cmk[i][:], 0.0), [], cmk[i].k)
            k.op('pool', lambda e, i=i: e.memset(cmk[i][:, i * 64:(i + 1) * 64], 1.0), [], cmk[i].k)
        omka = sb(g, ph, "r_omka", [128, 8])
        k.ts('dve', omka[:], pv['rw_ka'][:], -1.0, 1.0, ALU.mult, ALU.add, pv['rw_ka'].k, omka.k)
        k.barrier()
        state = [sb(g, ph, "r_state%d" % i, [128, 8, 64], F32, n=8) for i in range(2)]
        k.op('pool', lambda e: e.memset(state[0][:], 0.0), [], state[0].k)
        spar = [0] * 8
        hbuf = sb(g, ph, "r_h", [128, KC, W + 1])
        k.op('pool', lambda e: e.memset(hbuf[:, :, 0:1], 0.0), [], hbuf.k)
        xx = sb(g, ph, "r_xx", [128, KC, W])
        xb = xx
        xm = [sb(g, ph, "r_xm%d" % i, [128, KC, W], BF16) for i in range(6)]
        tw = sb(g, ph, "r_tw", [64, W], BF16)
        ta = sb(g, ph, "r_ta", [64, W], BF16)
        tg0 = sb(g, ph, "r_tg0", [128, W], BF16)
        tg1 = sb(g, ph, "r_tg1", [32, W], BF16)
        tv = sb(g, ph, "r_tv", [32, W], BF16)
        names = ['r', 'k', 'v', 'wl', 'sa', 'g', 'kk', 't1', 'rs', 'kkn', 'kf', 'b', 'c', 'd', 'eg', 'em', 'ed', 'ep',
                 'Rt', 'At', 'Kt', 'Bt', 'Kh', 'Bh']
        t = {n: sb(g, ph, "r_t_" + n, [128, W], F32R if n in ('At', 'Rt') else F32) for n in names}
        t['y'], t['y2'], t['mn'], t['bon'], t['vf'] = t['eg'], t['em'], t['ed'], t['ep'], t['d']
        gl_ = sb(g, ph, "r_gl", [128, 4])
        KhT = sb(g, ph, "r_KhT", [128, 2, 128])
        BhT = sb(g, ph, "r_BhT", [128, 2, 128])
        VT = sb(g, ph, "r_VT", [128, 2, 128], F32R)
        Mall = sb(g, ph, "r_Mall", [128, 4, 512], F32R)
        Nall = [sb(g, ph, "r_Nall%d" % i, [128, 4, 128], F32R) for i in range(2)]
        Ntall = [sb(g, ph, "r_Ntall%d" % i, [128, 4, 128], F32R) for i in range(2)]
        Pall = sb(g, ph, "r_Pall", [128, 4, 128], F32R)
        sl4 = sb(g, ph, "r_sl4", [128, 4, 128])
        id4 = sb(g, ph, "r_id4", [128, 4, 128])
        for q in range(4):
            k.cp('pool', sl4[:, q, :], g.sl[:], g.sl.k, sl4.k)
            k.cp('pool', id4[:, q, :], g.ident[:], g.ident.k, id4.k)
        Xs2 = sb(g, ph, "r_Xs2", [128, 128], F32R)
        UP = sb(g, ph, "r_UP", [128, 2, 128], F32R)
        k.op('pool', lambda e: e.memset(UP[:].bitcast(F32), 0.0), [], UP.k)
        _b = UP.t[:]
        UPdiag = bass.AP(tensor=_b.tensor, offset=_b.offset, ap=[list(_b.ap[0]), [192, 2], [1, 64]])
        SP = sb(g, ph, "r_SP", [128, 128], F32R)
        BhTm = [[sb(g, ph, "r_BhTm%d%d" % (a, b_), [128, 128], F32R) for b_ in range(2)] for a in range(2)]
        KhTm = [[sb(g, ph, "r_KhTm%d%d" % (a, b_), [128, 128], F32R) for b_ in range(2)] for a in range(2)]
        mob = sb(g, ph, "r_mo", [128, W], BF16)
        XTv = S['XT'].t.rearrange("(kc p) t -> p kc t", p=128)
        unit_i = 0
        for ti in range(T // W):
            t0 = ti * W
            h = hbuf
            if ti > 0:
                k.cp('pool', h[:, :, 0:1], h[:, :, W:W + 1], h.k, h.k)
            k.dma('sp', xb[:], XTv[:, :, t0:t0 + W], S['XT'].k, xb.k)
            for kc in range(KC):
                k.act(h[:, kc, 1:W + 1], xb[:, kc, :], AF.Identity, xb.k, h.k,
                      scale=m1[:, 8 + kc:9 + kc], bias=mt[:, kc:kc + 1])
            k.tt('dve', xx[:], h[:, :, 0:W], h[:, :, 1:W + 1], ALU.subtract, h.k, xx.k)
            for i in range(6):
                if i == 3 and False:
                    continue
                for kc in range(KC):
                    k.stt(xm[i][:, kc, :], xx[:, kc, :], pv['rw_mu'][:, i * 8 + kc:i * 8 + kc + 1], h[:, kc, 1:W + 1],
                          ALU.mult, ALU.add, xx.k + h.k, xm[i].k)
            p = ps[3]
            for kc in range(KC):
                k.mm(p[0:64, 0:W], w1[:, kc, :], xm[1][:, kc, :], kc == 0, kc == KC - 1, w1.k + xm[1].k, [p.k[0]])
            k.act(tw[:], p[0:64, 0:W], AF.Tanh, [p.k[0]], tw.k)
            for kc in range(KC):
                k.mm(p[0:64, W:2 * W], a1[:, kc, :], xm[4][:, kc, :], kc == 0, kc == KC - 1, a1.k + xm[4].k, [p.k[2]])
            k.cp('act', ta[:], p[0:64, W:2 * W], [p.k[2]], ta.k)
            p = ps[2]
            for kc in range(KC):
                k.mm(p[:, 0:W], g1[:, kc, 0:128], xm[5][:, kc, :], kc == 0, kc == KC - 1, g1.k + xm[5].k, [p.k[0]])
            k.act(tg0[:], p[:, 0:W], AF.Sigmoid, [p.k[0]], tg0.k)
            for kc in range(KC):
                k.mm(p[0:32, W:2 * W], g1[:, kc, 128:160], xm[5][:, kc, :], kc == 0, kc == KC - 1, g1.k + xm[5].k,
                     [p.k[2]])
            k.act(tg1[:], p[0:32, W:2 * W], AF.Sigmoid, [p.k[2]], tg1.k)
            if l > 0:
                p = ps[1]
                for kc in range(KC):
                    k.mm(p[0:32, 0:W], v1[:, kc, :], xm[3][:, kc, :], kc == 0, kc == KC - 1, v1.k + xm[3].k, [p.k[0]])
                k.cp('act', tv[:], p[0:32, 0:W], [p.k[0]], tv.k)
            for hp in range(8):
                if RW_STOP[0] <= 1:
                    break
                oc = hp
                ocs = slice(oc * 128, (oc + 1) * 128)
                col = lambda buf, j: buf[:, j:j + 1]

                def proj(pb, c0, kk_, wt, xi):
                    for kc in range(KC):
                        k.mm(pb[:, c0:c0 + W], wt[:, kc, ocs], xm[xi][:, kc, :], kc == 0, kc == KC - 1,
                             wt.k + xm[xi].k, [pb.k[kk_]])
                proj(ps[0], 0, 0, wr_, 0)
                proj(ps[0], W, 2, wk_, 2)
                proj(ps[1], 0, 0, wvv, 3)
                k.mm(ps[1][:, W:2 * W], w2[:, ocs], tw[:], True, True, w2.k + tw.k, [ps[1].k[2]])
                k.mm(ps[2][:, 0:W], a2[:, ocs], ta[:], True, True, a2.k + ta.k, [ps[2].k[0]])
                k.mm(ps[2][:, W:2 * W], g2[:, 0, ocs], tg0[:], True, False, g2.k + tg0.k, [ps[2].k[2]])
                k.mm(ps[2][:, W:2 * W], g2[0:32, 1, ocs], tg1[:], False, True, g2.k + tg1.k, [ps[2].k[2]])
                if l > 0:
                    k.mm(ps[3][:, 0:W], v2[:, ocs], tv[:], True, True, v2.k + tv.k, [ps[3].k[0]])
                k.cp('act', t['r'][:], ps[0][:, 0:W], [ps[0].k[0]], t['r'].k)
                k.cp('act', t['k'][:], ps[0][:, W:2 * W], [ps[0].k[2]], t['k'].k)
                k.cp('dve', t['v'][:], ps[1][:, 0:W], [ps[1].k[0]], t['v'].k)
                k.cp('act', t['g'][:], ps[2][:, W:2 * W], [ps[2].k[2]], t['g'].k)
                VFv = S['VF'].t[ocs, t0:t0 + W]
                if l == 0:
                    k.dma('sp', VFv, t['v'][:], t['v'].k, S['VF'].k)
                else:
                    k.dma('sp', t['vf'][:], VFv, S['VF'].k, t['vf'].k)
                    k.act(t['y2'][:], ps[3][:, 0:W], AF.Sigmoid, [ps[3].k[0]], t['y2'].k, bias=col(pv['rw_v0'], oc))
                    k.tt('pool', t['vf'][:], t['vf'][:], t['v'][:], ALU.subtract, t['vf'].k + t['v'].k, t['vf'].k)
                    k.tt('pool', t['vf'][:], t['vf'][:], t['y2'][:], ALU.mult, t['vf'].k + t['y2'].k, t['vf'].k)
                    k.tt('pool', t['v'][:], t['v'][:], t['vf'][:], ALU.add, t['vf'].k + t['v'].k, t['v'].k)
                k.act(t['wl'][:], ps[1][:, W:2 * W], AF.Sigmoid, [ps[1].k[2]], t['wl'].k, bias=col(pv['rw_w0'], oc))
                k.act(t['sa'][:], ps[2][:, 0:W], AF.Sigmoid, [ps[2].k[0]], t['sa'].k, bias=col(pv['rw_a0'], oc))
                k.ts('pool', t['wl'][:], t['wl'][:], -0.6065306597126334, None, ALU.mult, None, t['wl'].k, t['wl'].k)
                k.ts('dve', t['kk'][:], t['k'][:], col(pv['rw_kk'], oc), None, ALU.mult, None, t['k'].k, t['kk'].k)
                k.tt('pool', t['t1'][:], t['kk'][:], t['kk'][:], ALU.mult, t['kk'].k, t['t1'].k)
                k.mm(ps[3][:, W:2 * W], g.blk[:], t['t1'][:], True, True, g.blk.k + t['t1'].k, [ps[3].k[2]])
                k.act(t['rs'][:], ps[3][:, W:2 * W], AF.Sqrt, [ps[3].k[2]], t['rs'].k)
                k.ts('dve', t['rs'][:], t['rs'][:], 1e-12, None, ALU.max, None, t['rs'].k, t['rs'].k)
                k.op('dve', lambda e: e.reciprocal(out=t['rs'][:], in_=t['rs'][:]), t['rs'].k, t['rs'].k)
                k.tt('dve', t['kkn'][:], t['kk'][:], t['rs'][:], ALU.mult, t['kk'].k + t['rs'].k, t['kkn'].k)
                k.ts('dve', t['t1'][:], t['sa'][:], col(pv['rw_ka'], oc), col(omka, oc), ALU.mult, ALU.add,
                     t['sa'].k, t['t1'].k)
                k.tt('pool', t['kf'][:], t['k'][:], t['t1'][:], ALU.mult, t['k'].k + t['t1'].k, t['kf'].k)
                k.tt('pool', t['b'][:], t['kkn'][:], t['sa'][:], ALU.mult, t['kkn'].k + t['sa'].k, t['b'].k)
                k.op('dve', lambda e: e.tensor_tensor_scan(out=t['c'][:], data0=g.rm[:], data1=t['wl'][:], initial=0.0,
                                                           op0=ALU.mult, op1=ALU.add),
                     g.rm.k + t['wl'].k, t['c'].k)
                for j in range(4):
                    cs_ = slice(j * 64, (j + 1) * 64)
                    k.ts('dve', t['d'][:, cs_], t['c'][:, cs_], -1.0, t['c'][:, j * 64 + 63:j * 64 + 64],
                         ALU.mult, ALU.add, t['c'].k, t['d'].k)
                k.tt('pool', t['ep'][:], t['c'][:], t['wl'][:], ALU.subtract, t['c'].k + t['wl'].k, t['ep'].k)
                k.act(t['eg'][:], t['c'][:], AF.Exp, t['c'].k, t['eg'].k)
                k.act(t['em'][:], t['c'][:], AF.Exp, t['c'].k, t['em'].k, scale=-1.0)
                k.act(t['ed'][:], t['d'][:], AF.Exp, t['d'].k, t['ed'].k)
                k.act(t['ep'][:], t['ep'][:], AF.Exp, t['ep'].k, t['ep'].k)
                k.act(gl_[:], t['c'][:, :].rearrange("p (j t) -> p j t", t=64)[:, :, 63], AF.Exp, t['c'].k, gl_.k)
                k.tt('dve', t['Rt'][:], t['r'][:], t['eg'][:], ALU.mult, t['r'].k + t['eg'].k, t['Rt'].k)
                k.stt(t['At'][:], t['kkn'][:], -1.0, t['ep'][:], ALU.mult, ALU.mult, t['kkn'].k + t['ep'].k, t['At'].k)
                k.tt('pool', t['Kt'][:], t['kf'][:], t['em'][:], ALU.mult, t['kf'].k + t['em'].k, t['Kt'].k)
                k.tt('pool', t['Bt'][:], t['b'][:], t['em'][:], ALU.mult, t['b'].k + t['em'].k, t['Bt'].k)
                k.tt('dve', t['Kh'][:], t['kf'][:], t['ed'][:], ALU.mult, t['kf'].k + t['ed'].k, t['Kh'].k)
                k.tt('pool', t['Bh'][:], t['b'][:], t['ed'][:], ALU.mult, t['b'].k + t['ed'].k, t['Bh'].k)
                if RW_STOP[0] <= 2:
                    continue
                for hh in range(2):
                    k.ts('pool', Bth[hh][:], t['Bt'][:], hm[:, hh:hh + 1], None, ALU.mult, None, t['Bt'].k + hm.k, Bth[hh].k)
                    k.ts('pool', Kth[hh][:], t['Kt'][:], hm[:, hh:hh + 1], None, ALU.mult, None, t['Kt'].k + hm.k, Kth[hh].k)
                    k.ts('dve', Ath[hh][:], t['At'][:].bitcast(F32), hm[:, hh:hh + 1], None, ALU.mult, None, t['At'].k + hm.k, Ath[hh].k)
                for cp in range(2):
                    cc = slice(cp * 128, (cp + 1) * 128)
                    k.tr(ps[6][:, cp * 128:(cp + 1) * 128], t['Kh'][:, cc], g.ident[:], t['Kh'].k + g.ident.k, [ps[6].k[0]])
                    k.tr(ps[6][:, 256 + cp * 128:256 + (cp + 1) * 128], t['Bh'][:, cc], g.ident[:],
                         t['Bh'].k + g.ident.k, [ps[6].k[0]])
                    k.tr(ps[7][:, 256 + cp * 128:256 + (cp + 1) * 128], t['v'][:, cc], g.ident[:],
                         t['v'].k + g.ident.k, [ps[7].k[3]])
                k.cp('act', KhT[:], ps[6][:, 0:256].rearrange("p (a b) -> p a b", b=128), [ps[6].k[0]], KhT.k)
                k.cp('dve', BhT[:], ps[6][:, 256:512].rearrange("p (a b) -> p a b", b=128), [ps[6].k[0]], BhT.k)
                k.cp('act', VT[:], ps[7][:, 256:512].rearrange("p (a b) -> p a b", b=128), [ps[7].k[3]], VT.k)
                Yp = ps[0]
                mbank = [ps[4], ps[6], ps[1], ps[2]]
                for cp in range(2):
                    cc = slice(cp * 128, (cp + 1) * 128)
                    for hh in range(2):
                        u = cp * 2 + hh
                        Mp = mbank[u]
                        rd = Bth[hh].k + t['At'].k + Kth[hh].k + t['Rt'].k
                        k.mm(Mp[:, 0:128], Bth[hh][:, cc], t['At'][:, cc], True, True, rd, [Mp.k[0]])
                        k.mm(Mp[:, 128:256], Kth[hh][:, cc], t['At'][:, cc], True, True, rd, [Mp.k[0]])
                        k.mm(Mp[:, 256:384], Bth[hh][:, cc], t['Rt'][:, cc], True, True, rd, [Mp.k[0]])
                        k.mm(Mp[:, 384:512], Kth[hh][:, cc], t['Rt'][:, cc], True, True, rd, [Mp.k[0]])
                        k.mm(ps[5][:, u * 128:(u + 1) * 128], t['At'][:, cc], Bth[hh][:, cc], True, True, rd, [ps[5].k[0]])
                        k.tt('dve', Mall[:, u, :], Mp[:, :], g.mask4[:], ALU.mult, [Mp.k[0]] + g.mask4.k, Mall.k)
                        k.tt('pool', BhTm[cp][hh][:], BhT[:, cp, :], cmk[hh][:], ALU.mult, BhT.k + cmk[hh].k, BhTm[cp][hh].k)
                        k.tt('pool', KhTm[cp][hh][:], KhT[:, cp, :], cmk[hh][:], ALU.mult, KhT.k + cmk[hh].k, KhTm[cp][hh].k)
                k.tt('dve', Nall[0][:].rearrange("p a b -> p (a b)"), ps[5][:, :], sl4[:].rearrange("p a b -> p (a b)"),
                     ALU.mult, [ps[5].k[0]] + sl4.k, Nall[0].k)
                k.cp('pool', Ntall[0][:], Mall[:, :, 0:128].bitcast(F32), Mall.k, Ntall[0].k)
                k.tt('pool', Pall[:], Mall[:, :, 0:128].bitcast(F32), id4[:], ALU.add, Mall.k + id4.k, Pall.k)
                pa, pb_, pc = ps[1], ps[2], ps[3]
                for lev in range(1, 6):
                    Np, Ntp = Nall[(lev - 1) % 2], Ntall[(lev - 1) % 2]
                    Nn, Ntn = Nall[lev % 2], Ntall[lev % 2]
                    for u in range(4):
                        k.mm(pa[:, u * 128:(u + 1) * 128], Ntp[:, u, :], Np[:, u, :], True, True, Ntp.k + Np.k, [pa.k[0]])
                    if lev < 5:
                        for u in range(4):
                            k.mm(pb_[:, u * 128:(u + 1) * 128], Np[:, u, :], Ntp[:, u, :], True, True, Ntp.k + Np.k,
                                 [pb_.k[0]])
                    k.cp('act', Nn[:].rearrange("p a b -> p (a b)"), pa[:, :], [pa.k[0]], Nn.k)
                    if lev < 5:
                        k.cp('dve', Ntn[:].rearrange("p a b -> p (a b)"), pb_[:, :], [pb_.k[0]], Ntn.k)
                    for u in range(4):
                        k.mm(pc[:, u * 128:(u + 1) * 128], Nn[:, u, :], Pall[:, u, :], True, True, Nn.k + Pall.k, [pc.k[0]])
                    k.tt('dve', Pall[:].rearrange("p a b -> p (a b)"), pc[:, :], Pall[:].rearrange("p a b -> p (a b)").bitcast(F32),
                         ALU.add, [pc.k[0]] + Pall.k, Pall.k)
                for cp in range(2):
                    cc = slice(cp * 128, (cp + 1) * 128)
                    for q in range(2):
                        k.ts('pool', VTc[q][:], VT[:, cp, :].bitcast(F32), hm[:, q:q + 1], None, ALU.mult, None, VT.k + hm.k, VTc[q].k)
                        k.tt('pool', VTm[q][:], VT[:, cp, :].bitcast(F32), cmk[q][:], ALU.mult, VT.k + cmk[q].k, VTm[q].k)
                    for ch in range(2):
                        j = cp * 2 + ch
                        c64 = slice(j * 64, (j + 1) * 64)
                        so, sn = state[spar[hp]], state[1 - spar[hp]]
                        sk = [so.k[hp]]
                        p7 = ps[7]
                        for hh in range(2):
                            hcol = slice(hh * 64, (hh + 1) * 64)
                            k.ts('pool', SP[:, hcol], so[:, hp, :], hm[:, hh:hh + 1], None, ALU.mult, None,
                                 sk + hm.k, SP.k)
                        for hh in range(2):
                            u = cp * 2 + hh
                            hcol = slice(hh * 64, (hh + 1) * 64)
                            k.mm(p7[:, hcol], Ath[hh][:, cc], SP[:, hcol], True, False, Ath[hh].k + SP.k, [p7.k[0]])
                            k.mm(p7[:, hcol], Mall[:, u, 128:256], VT[:, cp, hcol], False, True, Mall.k + VT.k, [p7.k[0]])
                        k.cp('act', Xs2[:], p7[:, 0:128], [p7.k[0]], Xs2.k)
                        for hh in range(2):
                            u = cp * 2 + hh
                            hcol = slice(hh * 64, (hh + 1) * 64)
                            k.mm(p7[:, 128 + hh * 64:128 + (hh + 1) * 64], Pall[:, u, :], Xs2[:, hcol], True, True,
                                 Pall.k + Xs2.k, [p7.k[0]])
                        k.ts('dve', UPdiag, p7[:, 128:256].rearrange("p (a b) -> p a b", b=64), hm[:, ch:ch + 1], None,
                             ALU.mult, None, [p7.k[0]] + hm.k, UP.k)
                        T_ = p7[:, 256:320]
                        for hh in range(2):
                            hcol = slice(hh * 64, (hh + 1) * 64)
                            k.mm(T_, BhTm[cp][hh][:], UP[:, hh, hcol], hh == 0, False, BhTm[cp][hh].k + UP.k, [p7.k[0]])
                            k.mm(T_, KhTm[cp][hh][:], VTc[ch][:, hcol], False, hh == 1, KhTm[cp][hh].k + VTc[ch].k,
                                 [p7.k[0]])
                        k.stt(sn[:, hp, :], so[:, hp, :], gl_[:, j:j + 1], T_, ALU.mult, ALU.add,
                              sk + gl_.k + [p7.k[0]], [sn.k[hp]])
                        Y_ = Yp[:, c64]
                        k.mm(Y_, SP[:], t['Rt'][:, c64], True, False, SP.k + t['Rt'].k, [Yp.k[0]])
                        for hh in range(2):
                            u = cp * 2 + hh
                            k.mm(Y_, UP[:, hh, :], Mall[:, u, 256 + ch * 64:256 + (ch + 1) * 64], False, False,
                                 UP.k + Mall.k, [Yp.k[0]])
                            k.mm(Y_, VTm[hh][:], Mall[:, u, 384 + ch * 64:384 + (ch + 1) * 64], False, hh == 1,
                                 VTm[hh].k + Mall.k, [Yp.k[0]])
                        spar[hp] = 1 - spar[hp]
                if RW_STOP[0] <= 4:
                    continue
                k.cp('act', t['y'][:], Yp[:, 0:W], [Yp.k[0]], t['y'].k)
                k.tt('pool', t['y2'][:], t['y'][:], t['y'][:], ALU.mult, t['y'].k, t['y2'].k)
                k.mm(ps[1][:, 0:W], g.blk[:], t['y'][:], True, True, g.blk.k + t['y'].k, [ps[1].k[0]])
                k.mm(ps[1][:, W:2 * W], g.blk[:], t['y2'][:], True, True, g.blk.k + t['y2'].k, [ps[1].k[2]])
                k.ts('dve', t['mn'][:], ps[1][:, 0:W], 1.0 / 64, None, ALU.mult, None, [ps[1].k[0]], t['mn'].k)
                k.tt('pool', t['y2'][:], t['mn'][:], t['mn'][:], ALU.mult, t['mn'].k, t['y2'].k)
                k.stt(t['y2'][:], ps[1][:, W:2 * W], 1.0 / 64, t['y2'][:], ALU.mult, ALU.subtract,
                      [ps[1].k[2]] + t['y2'].k, t['y2'].k)
                k.ts('dve', t['y2'][:], t['y2'][:], GN_EPS, None, ALU.add, None, t['y2'].k, t['y2'].k)
                k.act(t['y2'][:], t['y2'][:], AF.Sqrt, t['y2'].k, t['y2'].k)
                k.op('dve', lambda e: e.reciprocal(out=t['y2'][:], in_=t['y2'][:]), t['y2'].k, t['y2'].k)
                k.tt('pool', t['y'][:], t['y'][:], t['mn'][:], ALU.subtract, t['y'].k + t['mn'].k, t['y'].k)
                k.tt('pool', t['y'][:], t['y'][:], t['y2'][:], ALU.mult, t['y'].k + t['y2'].k, t['y'].k)
                k.act(t['y'][:], t['y'][:], AF.Identity, t['y'].k, t['y'].k, scale=col(pv['rw_gn_g'], oc),
                      bias=col(pv['rw_gn_b'], oc))
                k.tt('pool', t['bon'][:], t['r'][:], t['kf'][:], ALU.mult, t['r'].k + t['kf'].k, t['bon'].k)
                k.ts('dve', t['bon'][:], t['bon'][:], col(pv['rw_rk'], oc), None, ALU.mult, None, t['bon'].k, t['bon'].k)
                k.mm(ps[3][:, W:2 * W], g.blk[:], t['bon'][:], True, True, g.blk.k + t['bon'].k, [ps[3].k[2]])
                k.tt('dve', t['bon'][:], ps[3][:, W:2 * W], t['v'][:], ALU.mult, [ps[3].k[2]] + t['v'].k, t['bon'].k)
                k.tt('pool', t['y'][:], t['y'][:], t['bon'][:], ALU.add, t['y'].k + t['bon'].k, t['y'].k)
                k.tt('pool', mob[:], t['y'][:], t['g'][:], ALU.mult, t['y'].k + t['g'].k, mob.k)
                k.dma('sp', S['MO'].t[ocs, t0:t0 + W], mob[:], mob.k, S['MO'].k)
        k.barrier()


_CACHE = {}


def prep_inputs(inp, b, T):
    f = lambda a: np.ascontiguousarray(np.asarray(a, dtype=np.float32))
    m = {}
    m['x'] = f(inp['x'][b][:T])
    m['c'] = f(inp['c'][b]).reshape(KC, 128)
    m['ada_w'] = f(inp['ada_w'])
    m['ada_b'] = f(inp['ada_b']).reshape(DEPTH, 48, 128)
    m['ln_g'] = f(inp['ln_g']).reshape(DEPTH, 16, 128)
    m['ln_b'] = f(inp['ln_b']).reshape(DEPTH, 16, 128)
    m['rw_mu'] = f(inp['rw_mu']).reshape(N_A, 48, 128)
    m['rw_rkv'] = f(inp['rw_rkv'])
    for nm in ['rw_w0', 'rw_a0', 'rw_kk', 'rw_ka', 'rw_rk', 'rw_gn_g', 'rw_gn_b']:
        m[nm] = f(inp[nm]).reshape(N_A, 8, 128)
    for nm in ['rw_w1', 'rw_w2', 'rw_a1', 'rw_a2', 'rw_g1', 'rw_g2', 'rw_wo', 'rw_v1', 'rw_v2',
               'kv_ada_w', 'kv_w', 'fx_wqg', 'fx_wo', 'moe_wgrp', 'moe_bgrp', 'moe_wexp', 'moe_bexp',
               'moe_wgate', 'moe_wup', 'moe_wdown']:
        m[nm] = f(inp[nm])
    m['rw_v0'] = f(inp['rw_v0']).reshape(1, 8, 128)
    m['kv_ada_b'] = f(inp['kv_ada_b']).reshape(16, 128)
    m['kv_fb'] = f(inp['kv_fb']).reshape(NH, 1)
    m['kv_knorm'] = f(inp['kv_knorm']).reshape(HD, 1)
    m['fx_qnorm'] = f(inp['fx_qnorm']).reshape(2, HD, 1)
    return m


def kernel(**inputs):
    B, T = inputs['x'].shape[0], inputs['x'].shape[1]
    NG = inputs['moe_wgrp'].shape[-1]
    NE = inputs['moe_wexp'].shape[-1]
    key = (T, NG, NE)
    if key not in _CACHE:
        _CACHE[key] = build(T, NG, NE // NG)[0]
    nc = _CACHE[key]
    shared = None
    maps = []
    for b in range(B):
        m = prep_inputs(inputs, b, T) if shared is None else dict(shared)
        if shared is None:
            shared = m
        else:
            m['x'] = np.ascontiguousarray(np.asarray(inputs['x'][b], dtype=np.float32))
            m['c'] = np.ascontiguousarray(np.asarray(inputs['c'][b], dtype=np.float32)).reshape(KC, 128)
        maps.append(m)
    res = run_bass_kernel_spmd(nc, maps, core_ids=list(range(B)))
    return np.stack([np.asarray(r['out']) for r in res.results]).astype(np.float32)
```

```python
import numpy as np
from contextlib import ExitStack
import concourse.bass as bass
import concourse.mybir as mybir
from concourse.bass_utils import run_bass_kernel_spmd

F32 = mybir.dt.float32
BF16 = mybir.dt.bfloat16
F32R = mybir.dt.float32r
AF = mybir.ActivationFunctionType
ALU = mybir.AluOpType
AX = mybir.AxisListType

D = 1024
KC = 8
HD = 64
NH = 16
DEPTH = 4
N_A = 2
ALPHA = (2 * DEPTH) ** 0.25
LN_EPS = 1e-5
GN_EPS = 64e-5
QK_EPS = 1e-6
DEXP = 512


class Tk:
    __slots__ = ("w", "r", "excl")

    def __init__(s):
        s.w = None
        s.r = {}
        s.excl = False


class Buf:
    def __init__(s, t, n=1):
        s.t = t
        s.k = [Tk() for _ in range(n)]

    def __getitem__(s, idx):
        return s.t[idx]


class _KL(list):
    def __getitem__(s, i):
        return list.__getitem__(s, 0)


class PBuf(Buf):
    def __init__(s, t):
        s.t = t
        s.k = _KL([Tk()])
        s.k[0].excl = True


class K:
    NS = 24

    def __init__(s, nc, es):
        s.nc = nc
        s.eng = {'pe': nc.tensor, 'act': nc.scalar, 'dve': nc.vector, 'pool': nc.gpsimd, 'sp': nc.sync}
        s.esem = {n: es.enter_context(nc.semaphore("s_" + n)) for n in s.eng}
        s.ecnt = {n: 0 for n in s.eng}
        s.seen = {n: {} for n in s.eng}
        s.dsem = [es.enter_context(nc.semaphore("d%d" % i)) for i in range(s.NS)]
        s.dcnt = [0] * s.NS
        s.dnext = {'hw': 0, 'sw': 0}
        s.dpool = {'hw': list(range(0, 16)), 'sw': list(range(16, s.NS))}
        s.same_sync = {'pe': False, 'act': True, 'dve': True, 'pool': True, 'sp': False}
        s.nins = 0

    def _wait(s, en, key, val):
        if s.seen[en].get(key, 0) >= val:
            return
        if key[0] == 'E':
            if key[1] == en and not s.same_sync[en]:
                return
            sem = s.esem[key[1]]
        else:
            sem = s.dsem[key[1]]
        s.eng[en].wait_ge(sem, val)
        s.seen[en][key] = val
        s.nins += 1

    def _need(s, reads, writes):
        need = {}
        for t in reads:
            if t.w is not None:
                k_, v = t.w
                if need.get(k_, 0) < v:
                    need[k_] = v
        for t in writes:
            if t.w is not None:
                k_, v = t.w
                if need.get(k_, 0) < v:
                    need[k_] = v
            for k_, v in t.r.items():
                if need.get(k_, 0) < v:
                    need[k_] = v
        return need

    def op(s, en, fn, reads=(), writes=()):
        ex = [t for t in reads if t.excl]
        if ex:
            reads = [t for t in reads if not t.excl]
            writes = list(writes) + ex
        for k_, v in s._need(reads, writes).items():
            s._wait(en, k_, v)
        ins = fn(s.eng[en])
        s.ecnt[en] += 1
        c = s.ecnt[en]
        ins.then_inc(s.esem[en], 1)
        key = ('E', en)
        for t in reads:
            t.r[key] = c
        for t in writes:
            t.w = (key, c)
            t.r = {}
        s.nins += 1

    def dma(s, q, out, in_, reads=(), writes=(), **kw):
        for k_, v in s._need(reads, writes).items():
            s._wait(q, k_, v)
        pn = 'sw' if q == 'pool' else 'hw'
        pl = s.dpool[pn]
        i = pl[s.dnext[pn] % len(pl)]
        s.dnext[pn] += 1
        if s.dcnt[i]:
            s._wait(q, ('D', i), s.dcnt[i] * 16)
        s.eng[q].dma_start(out=out, in_=in_, **kw).then_inc(s.dsem[i], 16)
        s.dcnt[i] += 1
        key = ('D', i)
        val = s.dcnt[i] * 16
        for t in reads:
            t.r[key] = val
        for t in writes:
            t.w = (key, val)
            t.r = {}
        s.nins += 1

    def barrier(s):
        for en in s.eng:
            for o in s.eng:
                if o != en and s.ecnt[o] > 0:
                    s._wait(en, ('E', o), s.ecnt[o])
            for i in range(s.NS):
                if s.dcnt[i] > 0:
                    s._wait(en, ('D', i), s.dcnt[i] * 16)

    def mm(s, out, lhsT, rhs, start, stop, reads, writes):
        s.op('pe', lambda e: e.matmul(out, lhsT, rhs, start=start, stop=stop), reads, writes)

    def tr(s, out, in_, ident, reads, writes):
        s.op('pe', lambda e: e.transpose(out, in_, ident), reads, writes)

    def act(s, out, in_, func, reads, writes, bias=None, scale=None, accum_out=None, en='act'):
        kw = {}
        if bias is not None:
            kw['bias'] = bias
        if scale is not None:
            kw['scale'] = scale
        if accum_out is not None:
            kw['accum_out'] = accum_out
        s.op('act', lambda e: e.activation(out, in_, func, **kw), reads, writes)

    def tt(s, en, out, in0, in1, op, reads, writes):
        s.op(en, lambda e: e.tensor_tensor(out=out, in0=in0, in1=in1, op=op), reads, writes)

    def ts(s, en, out, in0, s1, s2, op0, op1, reads, writes):
        if op1 is None:
            s.op(en, lambda e: e.tensor_scalar(out=out, in0=in0, scalar1=s1, scalar2=None, op0=op0), reads, writes)
        else:
            s.op(en, lambda e: e.tensor_scalar(out=out, in0=in0, scalar1=s1, scalar2=s2, op0=op0, op1=op1), reads, writes)

    def stt(s, out, in0, scalar, in1, op0, op1, reads, writes):
        s.op('dve', lambda e: e.scalar_tensor_tensor(out=out, in0=in0, scalar=scalar, in1=in1, op0=op0, op1=op1),
             reads, writes)

    def cp(s, en, out, in_, reads, writes):
        if en == 'act':
            s.op('act', lambda e: e.copy(out, in_), reads, writes)
        else:
            s.op(en, lambda e: e.tensor_copy(out=out, in_=in_), reads, writes)


class Ctx:
    pass


def build(T, NG, EPG, layers=DEPTH, dbg=None):
    NE = NG * EPG
    NR = NG + NE
    nc = bass.Bass("TRN2", target_bir_lowering=False)
    g = Ctx()
    g.T, g.NG, g.EPG, g.NE, g.NR = T, NG, EPG, NE, NR
    g.dbg = dbg
    n_a = min(N_A, layers)
    n_b = layers - n_a
    nv = max(n_a - 1, 0)

    def din(name, shape):
        return nc.dram_tensor(name, list(shape), F32, kind="ExternalInput").ap()

    I = {}
    I['x'] = din('x', [T, D])
    I['c'] = din('c', [KC, 128])
    I['ada_w'] = din('ada_w', [DEPTH, D, 6 * D])
    I['ada_b'] = din('ada_b', [DEPTH, 48, 128])
    I['ln_g'] = din('ln_g', [DEPTH, 16, 128])
    I['ln_b'] = din('ln_b', [DEPTH, 16, 128])
    I['rw_mu'] = din('rw_mu', [N_A, 48, 128])
    I['rw_rkv'] = din('rw_rkv', [N_A, 3, D, D])
    for nm in ['rw_w0', 'rw_a0', 'rw_kk', 'rw_ka', 'rw_rk', 'rw_gn_g', 'rw_gn_b']:
        I[nm] = din(nm, [N_A, 8, 128])
    I['rw_w1'] = din('rw_w1', [N_A, D, 64])
    I['rw_w2'] = din('rw_w2', [N_A, 64, D])
    I['rw_a1'] = din('rw_a1', [N_A, D, 64])
    I['rw_a2'] = din('rw_a2', [N_A, 64, D])
    I['rw_g1'] = din('rw_g1', [N_A, D, 160])
    I['rw_g2'] = din('rw_g2', [N_A, 160, D])
    I['rw_wo'] = din('rw_wo', [N_A, D, D])
    I['rw_v0'] = din('rw_v0', [1, 8, 128])
    I['rw_v1'] = din('rw_v1', [1, D, 32])
    I['rw_v2'] = din('rw_v2', [1, 32, D])
    I['kv_ada_w'] = din('kv_ada_w', [D, 2 * D])
    I['kv_ada_b'] = din('kv_ada_b', [16, 128])
    I['kv_w'] = din('kv_w', [D, 2 * D + NH])
    I['kv_fb'] = din('kv_fb', [NH, 1])
    I['kv_knorm'] = din('kv_knorm', [HD, 1])
    I['fx_wqg'] = din('fx_wqg', [2, D, 2 * D])
    I['fx_qnorm'] = din('fx_qnorm', [2, HD, 1])
    I['fx_wo'] = din('fx_wo', [2, D, D])
    I['moe_wgrp'] = din('moe_wgrp', [DEPTH, D, NG])
    I['moe_bgrp'] = din('moe_bgrp', [DEPTH, NG])
    I['moe_wexp'] = din('moe_wexp', [DEPTH, D, NE])
    I['moe_bexp'] = din('moe_bexp', [DEPTH, NE])
    I['moe_wgate'] = din('moe_wgate', [DEPTH, NE, D, DEXP])
    I['moe_wup'] = din('moe_wup', [DEPTH, NE, D, DEXP])
    I['moe_wdown'] = din('moe_wdown', [DEPTH, NE, DEXP, D])
    out_ap = nc.dram_tensor('out', [T, D], F32, kind="ExternalOutput").ap()

    def dscr(name, shape, dt=F32):
        kind = "ExternalOutput" if (dbg and name in dbg) else "Internal"
        return Buf(nc.dram_tensor(name, list(shape), dt, kind=kind).ap())

    S = {}
    S['XT'] = dscr('XT', [D, T])
    S['X1'] = dscr('X1', [D, T])
    S['H2'] = dscr('H2', [D, T], BF16)
    S['MO'] = dscr('MO', [D, T], BF16)
    S['VF'] = dscr('VF', [D, T])
    S['KT'] = dscr('KT', [D, T], BF16)
    S['VK'] = dscr('VK', [T, D], BF16)
    S['FC'] = dscr('FC', [NH, T])
    S['QT'] = dscr('QT', [D, T], BF16)
    S['SG'] = dscr('SG', [D, T], BF16)

    with ExitStack() as es:
        k = K(nc, es)
        g.es = es
        g.k, g.nc, g.I, g.S, g.out = k, nc, I, S, out_ap
        g.ps = [PBuf(es.enter_context(nc.psum_tensor("ps%d" % i, [128, 512], F32))) for i in range(8)]
        setup_consts(g, es)
        g.GT = sb(g, es, "GT", [128, T])
        phase_mod(g, layers, n_a)
        phase_in(g)
        for l in range(layers):
            if l < n_a:
                phase_rwkv(g, l)
            else:
                phase_fox(g, l, l - n_a)
            phase_moe(g, l, last=(l == layers - 1))
            if l == n_a - 1 and n_b > 0:
                phase_kv(g)
        k.barrier()
    g.nc = nc
    return nc, g


_UID = [0]


def sb(g, es, name, shape, dt=F32, n=1):
    _UID[0] += 1
    return Buf(es.enter_context(g.nc.sbuf_tensor("%s_%d" % (name, _UID[0]), list(shape), dt)), n)


def setup_consts(g, es):
    k, nc = g.k, g.nc
    ones = sb(g, es, "c_ones", [128, 512])
    g.ones = ones
    k.op('pool', lambda e: e.memset(ones[:], 1.0), [], ones.k)
    ident = sb(g, es, "c_ident", [128, 128])
    g.ident = ident
    k.op('pool', lambda e: e.affine_select(out=ident[:], in_=ones[:, 0:128], pattern=[[-1, 128]],
                                           compare_op=ALU.is_equal, fill=0.0, base=0, channel_multiplier=1),
         ones.k, ident.k)
    mmean = sb(g, es, "c_mmean", [128, 128])
    g.mmean = mmean
    k.op('pool', lambda e: e.memset(mmean[:], 1.0 / D), [], mmean.k)
    blk = sb(g, es, "c_blk", [128, 128])
    g.blk = blk
    k.op('pool', lambda e: e.memset(blk[:], 0.0), [], blk.k)
    k.op('pool', lambda e: e.memset(blk[0:64, 0:64], 1.0), [], blk.k)
    k.op('pool', lambda e: e.memset(blk[64:128, 64:128], 1.0), [], blk.k)


def dump(g, name, ap, reads):
    if not g.dbg or name not in g.dbg:
        return
    d = g.nc.dram_tensor("dbg_" + name, list(ap.shape), ap.dtype, kind="ExternalOutput").ap()
    g.k.dma('sp', d, ap, reads, [])


def load_vecT(g, out, src2d, R, st):
    k = g.k
    k.dma('sp', st[0:R, :], src2d, [], st.k)
    ps = g.ps[0]
    k.tr(ps[:, 0:R], st[0:R, :], g.ident[0:R, 0:R], st.k + g.ident.k, [ps.k[0]])
    k.cp('dve', out[:, 0:R], ps[:, 0:R], [ps.k[0]], out.k)
    return out


def phase_mod(g, layers, n_a):
    k, nc, I = g.k, g.nc, g.I
    es = g.es
    g.modT = [sb(g, es, "modT%d" % l, [128, 48]) for l in range(layers)]
    g.mod1 = [sb(g, es, "mod1_%d" % l, [128, 48]) for l in range(layers)]
    g.lng = [sb(g, es, "lng%d" % l, [128, 16]) for l in range(layers)]
    g.lnb = [sb(g, es, "lnb%d" % l, [128, 16]) for l in range(layers)]
    if layers > n_a:
        g.kvmod = sb(g, es, "kvmod", [128, 16])
        g.kvmod1 = sb(g, es, "kvmod1", [128, 16])
    with ExitStack() as ph:
        st = sb(g, ph, "lv_st", [128, 128])
        cT = sb(g, ph, "cT", [128, KC])
        bT = sb(g, ph, "bT", [128, 48])
        load_vecT(g, cT, I['c'], KC, st)
        cs2 = sb(g, ph, "cs2", [128, KC, 2])
        k.act(cs2[:, :, 0], cT[:], AF.Silu, cT.k, cs2.k)
        k.act(cs2[:, :, 1], cT[:], AF.Silu, cT.k, cs2.k)
        wb = [sb(g, ph, "adaw%d" % i, [128, KC, 1024]) for i in range(2)]
        nblk = 0

        def matvec(w_ap, ncols, outT):
            nonlocal nblk
            wv = w_ap.rearrange("(kc p) n -> p kc n", p=128)
            for b0 in range(0, ncols, 1024):
                bw = min(1024, ncols - b0)
                w = wb[nblk % 2]
                nblk += 1
                k.dma('sp', w[:, :, 0:bw], wv[:, :, b0:b0 + bw], [], w.k)
                ps = g.ps[1 + (nblk % 2)]
                for j in range(bw // 128):
                    for kc in range(KC):
                        k.mm(ps[:, 2 * j:2 * j + 2], w[:, kc, j * 128:(j + 1) * 128], cs2[:, kc, :],
                             kc == 0, kc == KC - 1, w.k + cs2.k, [ps.k[0]])
                nj = bw // 128
                k.cp('dve', outT[:, b0 // 128:b0 // 128 + nj],
                     ps[:, 0:2 * nj].rearrange("p (j t) -> p j t", t=2)[:, :, 0], [ps.k[0]], outT.k)

        for l in range(layers):
            mt, m1 = g.modT[l], g.mod1[l]
            matvec(I['ada_w'][l], 6 * D, mt)
            load_vecT(g, bT, I['ada_b'][l], 48, st)
            k.tt('dve', mt[:], mt[:], bT[:], ALU.add, mt.k + bT.k, mt.k)
            k.ts('dve', m1[:], mt[:], 1.0, None, ALU.add, None, mt.k, m1.k)
            dump(g, "modT%d" % l, mt[:], mt.k)
            load_vecT(g, g.lng[l], I['ln_g'][l], 16, st)
            load_vecT(g, g.lnb[l], I['ln_b'][l], 16, st)
        if layers > n_a:
            kt, k1 = g.kvmod, g.kvmod1
            matvec(I['kv_ada_w'], 2 * D, kt)
            load_vecT(g, bT, I['kv_ada_b'], 16, st)
            k.tt('dve', kt[:], kt[:], bT[:, 0:16], ALU.add, kt.k + bT.k, kt.k)
            k.ts('dve', k1[:], kt[:], 1.0, None, ALU.add, None, kt.k, k1.k)
        k.barrier()


def phase_in(g):
    k, I, S = g.k, g.I, g.S
    T = g.T
    with ExitStack() as ph:
        xt = [sb(g, ph, "in_x%d" % i, [128, D]) for i in range(2)]
        st = [sb(g, ph, "in_st%d" % i, [128, KC, 512]) for i in range(2)]
        XTv = S['XT'].t.rearrange("(kc p) t -> p kc t", p=128)
        for gi in range(T // 128):
            xb = xt[gi % 2]
            k.dma('sp', xb[:], I['x'][gi * 128:(gi + 1) * 128, :], [], xb.k)
            sg = st[(gi // 4) % 2]
            for half in range(2):
                ps = g.ps[(gi * 2 + half) % 4]
                for j in range(4):
                    kc = half * 4 + j
                    k.tr(ps[:, j * 128:(j + 1) * 128], xb[:, kc * 128:(kc + 1) * 128], g.ident[:],
                         xb.k + g.ident.k, [ps.k[0]])
                dst = sg[:, half * 4:half * 4 + 4, (gi % 4) * 128:(gi % 4 + 1) * 128]
                src = ps[:, :].rearrange("p (j t) -> p j t", t=128)
                k.cp('act' if half else 'dve', dst, src, [ps.k[0]], sg.k)
            if gi % 4 == 3:
                t0 = (gi // 4) * 512
                k.dma('sp', XTv[:, :, t0:t0 + 512], sg[:], sg.k, S['XT'].k)
        k.barrier()


STUB = {'rwkv': False, 'fox': False}
RW_STOP = [99]
RW_SUB = [9]


def bc_rows(ap2d_row, nparts, ncols):
    return bass.AP(tensor=ap2d_row.tensor, offset=ap2d_row.offset, ap=[[0, nparts], [1, ncols]])


def mixer_zero(g):
    k, S, T = g.k, g.S, g.T
    with ExitStack() as ph:
        z = sb(g, ph, "mz", [128, KC, 512], BF16)
        k.op('pool', lambda e: e.memset(z[:], 0.0), [], z.k)
        MOv = S['MO'].t.rearrange("(kc p) t -> p kc t", p=128)
        for t0 in range(0, T, 512):
            k.dma('sp', MOv[:, :, t0:t0 + 512], z[:], z.k, S['MO'].k)
        k.barrier()


def phase_rwkv(g, l):
    if STUB['rwkv']:
        mixer_zero(g)
    else:
        rwkv_mixer(g, l)
    phase_tail(g, l, g.I['rw_wo'][l])


def phase_fox(g, l, j):
    if STUB['fox']:
        mixer_zero(g)
    else:
        fox_mixer(g, l, j)
    phase_tail(g, l, g.I['fx_wo'][j])


def ln_tile(g, zf, zk, sq, mean, rstd, gam, bet, outf, outk, w):
    k = g.k
    psm, psq = g.ps[6], g.ps[7]
    for kc in range(KC):
        k.act(sq[:, kc, 0:w], zf(kc), AF.Square, zk, sq.k)
    for kc in range(KC):
        k.mm(psm[:, 0:w], g.mmean[:], zf(kc), kc == 0, kc == KC - 1, g.mmean.k + zk, [psm.k[0]])
    for kc in range(KC):
        k.mm(psq[:, 0:w], g.mmean[:], sq[:, kc, 0:w], kc == 0, kc == KC - 1, g.mmean.k + sq.k, [psq.k[0]])
    k.cp('act', mean[:, 0:w], psm[:, 0:w], [psm.k[0]], mean.k)
    k.tt('dve', rstd[:, 0:w], mean[:, 0:w], mean[:, 0:w], ALU.mult, mean.k, rstd.k)
    k.tt('dve', rstd[:, 0:w], psq[:, 0:w], rstd[:, 0:w], ALU.subtract, [psq.k[0]] + rstd.k, rstd.k)
    k.ts('dve', rstd[:, 0:w], rstd[:, 0:w], LN_EPS, None, ALU.add, None, rstd.k, rstd.k)
    k.act(rstd[:, 0:w], rstd[:, 0:w], AF.Sqrt, rstd.k, rstd.k)
    k.op('dve', lambda e: e.reciprocal(out=rstd[:, 0:w], in_=rstd[:, 0:w]), rstd.k, rstd.k)
    for kc in range(KC):
        k.tt('dve', zf(kc), zf(kc), mean[:, 0:w], ALU.subtract, zk + mean.k, zk)
        k.tt('pool', zf(kc), zf(kc), rstd[:, 0:w], ALU.mult, zk + rstd.k, zk)
        k.act(outf(kc), zf(kc), AF.Identity, zk, outk, scale=gam(kc), bias=bet(kc))


def phase_tail(g, l, wo_ap):
    k, I, S, T = g.k, g.I, g.S, g.T
    NE, NG, NR, EPG = g.NE, g.NG, g.NR, g.EPG
    mt, m1 = g.modT[l], g.mod1[l]
    GT = g.GT
    with ExitStack() as ph:
        wo = sb(g, ph, "t_wo", [128, KC, D], BF16)
        k.dma('pool', wo[:], wo_ap.rearrange("(kc p) n -> p kc n", p=128), [], wo.k)
        wr = sb(g, ph, "t_wr", [128, KC, NR])
        k.dma('sp', wr[:, :, 0:NG], I['moe_wgrp'][l].rearrange("(kc p) n -> p kc n", p=128), [], wr.k)
        k.dma('sp', wr[:, :, NG:NR], I['moe_wexp'][l].rearrange("(kc p) n -> p kc n", p=128), [], wr.k)
        rb = sb(g, ph, "t_rb", [128, NR])
        k.dma('sp', rb[:, 0:NG], bc_rows(I['moe_bgrp'][l], 128, NG), [], rb.k)
        k.dma('sp', rb[:, NG:NR], bc_rows(I['moe_bexp'][l], 128, NE), [], rb.k)
        xt = [sb(g, ph, "t_x%d" % i, [128, KC, 512]) for i in range(2)]
        zt = [sb(g, ph, "t_z%d" % i, [128, KC, 512]) for i in range(2)]
        mo = [sb(g, ph, "t_mo%d" % i, [128, KC, 512], BF16) for i in range(2)]
        hb = [sb(g, ph, "t_hb%d" % i, [128, KC, 512], BF16) for i in range(2)]
        sq = sb(g, ph, "t_sq", [128, KC, 512])
        mean = sb(g, ph, "t_mean", [128, 512])
        rstd = sb(g, ph, "t_rstd", [128, 512])
        sm = {n: sb(g, ph, "t_r_" + n, [128, w_]) for n, w_ in
              [('lg', NR), ('mg', 1), ('nmg', 1), ('eg', NG), ('sg', 1), ('pg', 1), ('oh', NG), ('pen', NG),
               ('le', NE), ('t8', 8), ('d12', 1), ('s12', 1), ('g1', 1), ('g2', 1), ('G1', NE), ('G2', NE)]}
        XTv = S['XT'].t.rearrange("(kc p) t -> p kc t", p=128)
        X1v = S['X1'].t.rearrange("(kc p) t -> p kc t", p=128)
        MOv = S['MO'].t.rearrange("(kc p) t -> p kc t", p=128)
        H2v = S['H2'].t.rearrange("(kc p) t -> p kc t", p=128)
        for ti in range(T // 512):
            t0 = ti * 512
            x, z, m, h = xt[ti % 2], zt[ti % 2], mo[ti % 2], hb[ti % 2]
            k.dma('sp', x[:], XTv[:, :, t0:t0 + 512], S['XT'].k, x.k)
            k.dma('sp', m[:], MOv[:, :, t0:t0 + 512], S['MO'].k, m.k)
            for oc in range(KC):
                ps = g.ps[oc % 2]
                for kc in range(KC):
                    k.mm(ps[:, :], wo[:, kc, oc * 128:(oc + 1) * 128], m[:, kc, :], kc == 0, kc == KC - 1,
                         wo.k + m.k, [ps.k[0]])
                k.act(z[:, oc, :], ps[:, :], AF.Identity, [ps.k[0]], z.k, scale=m1[:, 16 + oc:17 + oc])
                k.stt(z[:, oc, :], x[:, oc, :], ALPHA, z[:, oc, :], ALU.mult, ALU.add, x.k + z.k, z.k)
            ln_tile(g, lambda kc: z[:, kc, :], z.k, sq, mean, rstd,
                    lambda kc: g.lng[l][:, kc:kc + 1], lambda kc: g.lnb[l][:, kc:kc + 1],
                    lambda kc: x[:, kc, :], x.k, 512)
            k.dma('sp', X1v[:, :, t0:t0 + 512], x[:], x.k, S['X1'].k)
            for kc in range(KC):
                k.act(z[:, kc, :], x[:, kc, :], AF.Identity, x.k, z.k,
                      scale=m1[:, 32 + kc:33 + kc], bias=mt[:, 24 + kc:25 + kc])
            k.cp('pool', h[:], z[:], z.k, h.k)
            k.dma('sp', H2v[:, :, t0:t0 + 512], h[:], h.k, S['H2'].k)
            for tg in range(4):
                pr = g.ps[2 + tg % 2]
                for kc in range(KC):
                    k.mm(pr[:, 0:NR], z[:, kc, tg * 128:(tg + 1) * 128], wr[:, kc, :], kc == 0, kc == KC - 1,
                         z.k + wr.k, [pr.k[0]])
                lg, mg, nmg, eg, sg, pg = sm['lg'], sm['mg'], sm['nmg'], sm['eg'], sm['sg'], sm['pg']
                oh, pen, le, t8 = sm['oh'], sm['pen'], sm['le'], sm['t8']
                k.tt('dve', lg[:], pr[:, 0:NR], rb[:], ALU.add, [pr.k[0]] + rb.k, lg.k)
                k.op('dve', lambda e: e.tensor_reduce(out=mg[:], in_=lg[:, 0:NG], axis=AX.X, op=ALU.max), lg.k, mg.k)
                k.ts('dve', nmg[:], mg[:], -1.0, None, ALU.mult, None, mg.k, nmg.k)
                k.act(eg[:], lg[:, 0:NG], AF.Exp, lg.k + nmg.k, eg.k + sg.k, bias=nmg[:, 0:1], accum_out=sg[:])
                k.op('dve', lambda e: e.reciprocal(out=pg[:], in_=sg[:]), sg.k, pg.k)
                k.ts('dve', oh[:], lg[:, 0:NG], mg[:, 0:1], None, ALU.is_equal, None, lg.k + mg.k, oh.k)
                k.ts('dve', pen[:], oh[:], -1.0, 1e30, ALU.add, ALU.mult, oh.k, pen.k)
                for gi in range(NG):
                    k.ts('dve', le[:, gi * EPG:(gi + 1) * EPG], lg[:, NG + gi * EPG:NG + (gi + 1) * EPG],
                         pen[:, gi:gi + 1], None, ALU.add, None, lg.k + pen.k, le.k)
                k.op('dve', lambda e: e.max(out=t8[:], in_=le[:]), le.k, t8.k)
                d12, s12, g1, g2, G1, G2 = sm['d12'], sm['s12'], sm['g1'], sm['g2'], sm['G1'], sm['G2']
                k.tt('dve', d12[:], t8[:, 0:1], t8[:, 1:2], ALU.subtract, t8.k, d12.k)
                k.act(s12[:], d12[:], AF.Sigmoid, d12.k, s12.k)
                k.tt('dve', g1[:], s12[:], pg[:], ALU.mult, s12.k + pg.k, g1.k)
                k.tt('dve', g2[:], pg[:], g1[:], ALU.subtract, pg.k + g1.k, g2.k)
                k.ts('dve', G1[:], le[:], t8[:, 0:1], g1[:, 0:1], ALU.is_equal, ALU.mult, le.k + t8.k + g1.k, G1.k)
                k.ts('dve', G2[:], le[:], t8[:, 1:2], g2[:, 0:1], ALU.is_equal, ALU.mult, le.k + t8.k + g2.k, G2.k)
                k.tt('dve', G1[:], G1[:], G2[:], ALU.add, G1.k + G2.k, G1.k)
                pt = g.ps[4 + tg % 2]
                k.tr(pt[0:NE, 0:128], G1[:], g.ident[:], G1.k + g.ident.k, [pt.k[0]])
                c0 = t0 + tg * 128
                k.cp('dve', GT[0:NE, c0:c0 + 128], pt[0:NE, 0:128], [pt.k[0]], GT.k)
        dump(g, "GT%d" % l, GT[0:NE, :], GT.k)
        k.barrier()


def phase_moe(g, l, last):
    k, I, S, T = g.k, g.I, g.S, g.T
    NE = g.NE
    mt, m1 = g.modT[l], g.mod1[l]
    GT = g.GT
    HT = min(2048, T)
    NTT = HT // 512
    X1v = S['X1'].t.rearrange("(kc p) t -> p kc t", p=128)
    XTv = S['XT'].t.rearrange("(kc p) t -> p kc t", p=128)
    H2v = S['H2'].t.rearrange("(kc p) t -> p kc t", p=128)
    with ExitStack() as ph:
        h2 = sb(g, ph, "m_h2", [128, KC, HT], BF16)
        yacc = sb(g, ph, "m_y", [128, KC, HT], F32, n=KC * NTT)
        for hf in range(T // HT):
            tb = hf * HT
            for tt in range(NTT):
                k.dma('sp', h2[:, :, tt * 512:(tt + 1) * 512], H2v[:, :, tb + tt * 512:tb + (tt + 1) * 512],
                      S['H2'].k, h2.k)
            with ExitStack() as ex:
                wg = [sb(g, ex, "m_wg%d" % i, [128, KC, DEXP], BF16) for i in range(2)]
                wu = [sb(g, ex, "m_wu%d" % i, [128, KC, DEXP], BF16) for i in range(2)]
                wd = [sb(g, ex, "m_wd%d" % i, [128, 4, D], BF16) for i in range(2)]
                sel = [sb(g, ex, "m_sel%d" % i, [128, 128]) for i in range(2)]
                gbc = sb(g, ex, "m_gbc", [128, 512])
                sl = [sb(g, ex, "m_sl%d" % i, [128, 512]) for i in range(2)]
                tl = sb(g, ex, "m_tl", [128, 512])
                hT = [sb(g, ex, "m_hT%d" % i, [128, 4, 512], BF16) for i in range(2)]
                it = 0

                def load_w(e):
                    bi = e % 2
                    k.dma('pool', wg[bi][:], I['moe_wgate'][l, e].rearrange("(kc p) n -> p kc n", p=128), [], wg[bi].k)
                    k.dma('pool', wu[bi][:], I['moe_wup'][l, e].rearrange("(kc p) n -> p kc n", p=128), [], wu[bi].k)
                    k.dma('pool', wd[bi][:], I['moe_wdown'][l, e].rearrange("(dc p) n -> p dc n", p=128), [], wd[bi].k)
                load_w(0)
                pend = None
                for e in range(NE):
                    bi = e % 2
                    se = sel[bi]
                    k.op('pool', lambda e_, se=se, e=e: e_.affine_select(
                        out=se[0:NE, :], in_=g.ones[0:NE, 0:128], pattern=[[0, 128]], compare_op=ALU.is_equal,
                        fill=0.0, base=-e, channel_multiplier=1), g.ones.k, se.k)
                    for tt in range(NTT):
                        c0 = tt * 512
                        psg = g.ps[0]
                        k.mm(psg[:, :], se[0:NE, :], GT[0:NE, tb + c0:tb + c0 + 512], True, True,
                             se.k + GT.k, [psg.k[0]])
                        k.cp('act', gbc[:], psg[:, :], [psg.k[0]], gbc.k)
                        hh = hT[it % 2]
                        it += 1
                        for dc in range(4):
                            pg_, pu_ = g.ps[1 + dc % 2], g.ps[3 + dc % 2]
                            for kc in range(KC):
                                k.mm(pg_[:, :], wg[bi][:, kc, dc * 128:(dc + 1) * 128], h2[:, kc, c0:c0 + 512],
                                     kc == 0, kc == KC - 1, wg[bi].k + h2.k, [pg_.k[0]])
                            for kc in range(KC):
                                k.mm(pu_[:, :], wu[bi][:, kc, dc * 128:(dc + 1) * 128], h2[:, kc, c0:c0 + 512],
                                     kc == 0, kc == KC - 1, wu[bi].k + h2.k, [pu_.k[0]])
                            s_ = sl[dc % 2]
                            k.act(s_[:], pg_[:, :], AF.Silu, [pg_.k[0]], s_.k)
                            k.tt('dve', tl[:], pu_[:, :], s_[:], ALU.mult, [pu_.k[0]] + s_.k, tl.k)
                            k.tt('dve', hh[:, dc, :], tl[:], gbc[:], ALU.mult, tl.k + gbc.k, hh.k)

                        def down(e=e, bi=bi, tt=tt, c0=c0, hh=hh):
                            for oc in range(KC):
                                py = g.ps[5 + oc % 2]
                                for dc in range(4):
                                    k.mm(py[:, :], wd[bi][:, dc, oc * 128:(oc + 1) * 128], hh[:, dc, :],
                                         dc == 0, dc == 3, wd[bi].k + hh.k, [py.k[0]])
                                yk = [yacc.k[oc * NTT + tt]]
                                ya = yacc[:, oc, c0:c0 + 512]
                                if e == 0:
                                    k.cp('dve', ya, py[:, :], [py.k[0]], yk)
                                else:
                                    k.tt('dve', ya, py[:, :], ya, ALU.add, [py.k[0]] + yk, yk)
                        if pend is not None:
                            pend()
                        pend = down
                        if tt == 0 and e + 1 < NE:
                            load_w(e + 1)
                pend()
                k.barrier()
            with ExitStack() as ex:
                xt = [sb(g, ex, "m_x%d" % i, [128, KC, 512]) for i in range(2)]
                sq = sb(g, ex, "m_sq", [128, KC, 512])
                mean = sb(g, ex, "m_mean", [128, 512])
                rstd = sb(g, ex, "m_rstd", [128, 512])
                ot = [sb(g, ex, "m_ot%d" % i, [128, D]) for i in range(2)] if last else None
                for tt in range(NTT):
                    c0 = tt * 512
                    t0 = tb + c0
                    x = xt[tt % 2]
                    k.dma('sp', x[:], X1v[:, :, t0:t0 + 512], S['X1'].k, x.k)
                    zk = [yacc.k[oc * NTT + tt] for oc in range(KC)]
                    for oc in range(KC):
                        ya = yacc[:, oc, c0:c0 + 512]
                        k.act(ya, ya, AF.Identity, zk, zk, scale=m1[:, 40 + oc:41 + oc])
                        k.stt(ya, x[:, oc, :], ALPHA, ya, ALU.mult, ALU.add, x.k + zk, zk)
                    ln_tile(g, lambda kc: yacc[:, kc, c0:c0 + 512], zk, sq, mean, rstd,
                            lambda kc: g.lng[l][:, 8 + kc:9 + kc], lambda kc: g.lnb[l][:, 8 + kc:9 + kc],
                            lambda kc: x[:, kc, :], x.k, 512)
                    if not last:
                        k.dma('sp', XTv[:, :, t0:t0 + 512], x[:], x.k, S['XT'].k)
                    else:
                        for tg in range(4):
                            o = ot[tg % 2]
                            for half in range(2):
                                ps = g.ps[half]
                                for j in range(4):
                                    kc = half * 4 + j
                                    k.tr(ps[:, j * 128:(j + 1) * 128], x[:, kc, tg * 128:(tg + 1) * 128], g.ident[:],
                                         x.k + g.ident.k, [ps.k[0]])
                                k.cp('act' if half else 'dve', o[:, half * 512:(half + 1) * 512], ps[:, :],
                                     [ps.k[0]], o.k)
                            k.dma('sp', g.out[t0 + tg * 128:t0 + (tg + 1) * 128, :], o[:], o.k, [])
                k.barrier()
        k.barrier()


def head_rms_fm(g, k, pin, outap, outk, sqt, rst, nrm, extra_scale):
    pss = g.ps[7]
    k.act(sqt[:], pin[:, :], AF.Square, [pin.k[0]], sqt.k)
    k.mm(pss[:, :], g.blk[:], sqt[:], True, True, g.blk.k + sqt.k, [pss.k[0]])
    k.ts('dve', rst[:], pss[:, :], 1.0 / 64, QK_EPS, ALU.mult, ALU.add, [pss.k[0]], rst.k)
    k.act(rst[:], rst[:], AF.Sqrt, rst.k, rst.k)
    k.op('dve', lambda e: e.reciprocal(out=rst[:], in_=rst[:]), rst.k, rst.k)
    k.tt('dve', sqt[:], pin[:, :], rst[:], ALU.mult, [pin.k[0]] + rst.k, sqt.k)
    k.ts('pool', outap, sqt[:], nrm[:, 0:1], extra_scale, ALU.mult, ALU.mult, sqt.k + nrm.k, outk)


def phase_kv(g):
    k, I, S, T = g.k, g.I, g.S, g.T
    NTG = T // 128
    g.FQ = Buf(g.nc.dram_tensor("FQ3", [NH, 3, T], BF16, kind="Internal").ap())
    g.FK = Buf(g.nc.dram_tensor("NFD", [NH, 128, NTG], F32, kind="Internal").ap())
    with ExitStack() as ph:
        wv_ = lambda ap: ap.rearrange("(kc p) n -> p kc n", p=128)
        wk = sb(g, ph, "kv_wk", [128, KC, D], BF16)
        wv = sb(g, ph, "kv_wv", [128, KC, D], BF16)
        wf = sb(g, ph, "kv_wf", [128, KC, NH], BF16)
        k.dma('pool', wk[:], wv_(I['kv_w'][:, 0:D]), [], wk.k)
        k.dma('pool', wv[:], wv_(I['kv_w'][:, D:2 * D]), [], wv.k)
        k.dma('pool', wf[:], wv_(I['kv_w'][:, 2 * D:2 * D + NH]), [], wf.k)
        kn = sb(g, ph, "kv_kn", [128, 1])
        k.dma('sp', kn[0:64, :], I['kv_knorm'], [], kn.k)
        k.dma('sp', kn[64:128, :], I['kv_knorm'], [], kn.k)
        fb = sb(g, ph, "kv_fb", [NH, 1])
        k.dma('sp', fb[:], I['kv_fb'], [], fb.k)
        k.barrier()
        xb = [sb(g, ph, "kv_x%d" % i, [128, KC, 512]) for i in range(2)]
        hk = [sb(g, ph, "kv_h%d" % i, [128, KC, 512], BF16) for i in range(2)]
        kst = [sb(g, ph, "kv_ks%d" % i, [128, KC, 512], BF16) for i in range(2)]
        vst = [sb(g, ph, "kv_vs%d" % i, [128, D], BF16) for i in range(2)]
        sqt = sb(g, ph, "kv_sq", [128, 512])
        rst = sb(g, ph, "kv_rs", [128, 512])
        lf = sb(g, ph, "kv_lf", [NH, 512])
        fc = [sb(g, ph, "kv_fc%d" % i, [NH, 512]) for i in range(2)]
        nfc = [sb(g, ph, "kv_nfc%d" % i, [NH, 512]) for i in range(2)]
        fsp = [[sb(g, ph, "kv_fsp%d_%d" % (i, q), [NH, 512], BF16) for q in range(3)] for i in range(2)]
        fr = sb(g, ph, "kv_fr", [NH, 512])
        NFs = sb(g, ph, "kv_NFs", [128, NH, NTG])
        XTv = S['XT'].t.rearrange("(kc p) t -> p kc t", p=128)
        KTv = S['KT'].t.rearrange("(kc p) t -> p kc t", p=128)
        for ti in range(T // 512):
            t0 = ti * 512
            x, h, ks = xb[ti % 2], hk[ti % 2], kst[ti % 2]
            k.dma('sp', x[:], XTv[:, :, t0:t0 + 512], S['XT'].k, x.k)
            for kc in range(KC):
                k.act(h[:, kc, :], x[:, kc, :], AF.Identity, x.k, h.k,
                      scale=g.kvmod1[:, 8 + kc:9 + kc], bias=g.kvmod[:, kc:kc + 1])
            for oc in range(KC):
                p = g.ps[oc % 2]
                for kc in range(KC):
                    k.mm(p[:, :], wk[:, kc, oc * 128:(oc + 1) * 128], h[:, kc, :], kc == 0, kc == KC - 1,
                         wk.k + h.k, [p.k[0]])
                head_rms_fm(g, k, p, ks[:, oc, :], ks.k, sqt, rst, kn, 1.0)
            k.dma('sp', KTv[:, :, t0:t0 + 512], ks[:], ks.k, S['KT'].k)
            for tg in range(4):
                vs = vst[tg % 2]
                for half in range(2):
                    p = g.ps[2 + half]
                    for kc in range(KC):
                        k.mm(p[:, :], h[:, kc, tg * 128:(tg + 1) * 128], wv[:, kc, half * 512:(half + 1) * 512],
                             kc == 0, kc == KC - 1, wv.k + h.k, [p.k[0]])
                    k.cp('act' if half else 'dve', vs[:, half * 512:(half + 1) * 512], p[:, :], [p.k[0]], vs.k)
                k.dma('sp', S['VK'].t[t0 + tg * 128:t0 + (tg + 1) * 128, :], vs[:], vs.k, S['VK'].k)
            p = g.ps[4]
            for kc in range(KC):
                k.mm(p[0:NH, :], wf[:, kc, :], h[:, kc, :], kc == 0, kc == KC - 1, wf.k + h.k, [p.k[0]])
            k.act(lf[:], p[0:NH, :], AF.Sigmoid, [p.k[0]], lf.k, bias=fb[:, 0:1])
            k.act(lf[:], lf[:], AF.Ln, lf.k, lf.k)
            f, fp_, nf = fc[ti % 2], fc[(ti + 1) % 2], nfc[ti % 2]
            init = 0.0 if ti == 0 else fp_[:, 511:512]
            k.op('dve', lambda e, f=f, init=init: e.tensor_tensor_scan(
                out=f[:], data0=g.ones[0:NH, 0:512], data1=lf[:], initial=init, op0=ALU.mult, op1=ALU.add),
                g.ones.k + lf.k + fp_.k, f.k)
            k.ts('pool', nf[:], f[:], -1.0, None, ALU.mult, None, f.k, nf.k)
            sp3 = fsp[ti % 2]
            k.cp('dve', sp3[0][:], f[:], f.k, sp3[0].k)
            k.tt('dve', fr[:], f[:], sp3[0][:], ALU.subtract, f.k + sp3[0].k, fr.k)
            k.cp('dve', sp3[1][:], fr[:], fr.k, sp3[1].k)
            k.tt('dve', fr[:], fr[:], sp3[1][:], ALU.subtract, fr.k + sp3[1].k, fr.k)
            k.cp('dve', sp3[2][:], fr[:], fr.k, sp3[2].k)
            for q in range(3):
                k.dma('sp', g.FQ.t[:, q, t0:t0 + 512], sp3[q][:], sp3[q].k, g.FQ.k)
            for tg in range(4):
                pt = g.ps[5]
                k.tr(pt[:, 0:NH], nf[:, tg * 128:(tg + 1) * 128], g.ident[0:NH, 0:NH], nf.k + g.ident.k, [pt.k[0]])
                k.cp('dve', NFs[:, :, ti * 4 + tg], pt[:, 0:NH], [pt.k[0]], NFs.k)
        k.dma('sp', g.FK.t.rearrange("h p k -> p h k"), NFs[:], NFs.k, g.FK.k)
        k.barrier()


def fox_mixer(g, l, j):
    k, I, S, T = g.k, g.I, g.S, g.T
    mt, m1 = g.modT[l], g.mod1[l]
    XTv = S['XT'].t.rearrange("(kc p) t -> p kc t", p=128)
    QTv = S['QT'].t.rearrange("(kc p) t -> p kc t", p=128)
    SGv = S['SG'].t.rearrange("(kc p) t -> p kc t", p=128)
    with ExitStack() as ph:
        wv_ = lambda ap: ap.rearrange("(kc p) n -> p kc n", p=128)
        wq = sb(g, ph, "f_wq", [128, KC, D], BF16)
        wg = sb(g, ph, "f_wg", [128, KC, D], BF16)
        k.dma('pool', wq[:], wv_(I['fx_wqg'][j][:, 0:D]), [], wq.k)
        k.dma('pool', wg[:], wv_(I['fx_wqg'][j][:, D:2 * D]), [], wg.k)
        qn = sb(g, ph, "f_qn", [128, 1])
        k.dma('sp', qn[0:64, :], I['fx_qnorm'][j], [], qn.k)
        k.dma('sp', qn[64:128, :], I['fx_qnorm'][j], [], qn.k)
        k.barrier()
        xb = [sb(g, ph, "f_x%d" % i, [128, KC, 512]) for i in range(2)]
        hb = [sb(g, ph, "f_h%d" % i, [128, KC, 512], BF16) for i in range(2)]
        qst = [sb(g, ph, "f_qs%d" % i, [128, KC, 512], BF16) for i in range(2)]
        gst = [sb(g, ph, "f_gs%d" % i, [128, KC, 512], BF16) for i in range(2)]
        sqt = sb(g, ph, "f_sq", [128, 512])
        rst = sb(g, ph, "f_rs", [128, 512])
        for ti in range(T // 512):
            t0 = ti * 512
            x, h, qs, gs = xb[ti % 2], hb[ti % 2], qst[ti % 2], gst[ti % 2]
            k.dma('sp', x[:], XTv[:, :, t0:t0 + 512], S['XT'].k, x.k)
            for kc in range(KC):
                k.act(h[:, kc, :], x[:, kc, :], AF.Identity, x.k, h.k,
                      scale=m1[:, 8 + kc:9 + kc], bias=mt[:, kc:kc + 1])
            for oc in range(KC):
                p = g.ps[oc % 2]
                for kc in range(KC):
                    k.mm(p[:, :], wq[:, kc, oc * 128:(oc + 1) * 128], h[:, kc, :], kc == 0, kc == KC - 1,
                         wq.k + h.k, [p.k[0]])
                head_rms_fm(g, k, p, qs[:, oc, :], qs.k, sqt, rst, qn, HD ** -0.5)
                p2 = g.ps[2 + oc % 2]
                for kc in range(KC):
                    k.mm(p2[:, :], wg[:, kc, oc * 128:(oc + 1) * 128], h[:, kc, :], kc == 0, kc == KC - 1,
                         wg.k + h.k, [p2.k[0]])
                k.act(gs[:, oc, :], p2[:, :], AF.Sigmoid, [p2.k[0]], gs.k)
            k.dma('sp', QTv[:, :, t0:t0 + 512], qs[:], qs.k, S['QT'].k)
            k.dma('sp', SGv[:, :, t0:t0 + 512], gs[:], gs.k, S['SG'].k)
        k.barrier()
    with ExitStack() as ph:
        identb = sb(g, ph, "a_identb", [128, 128], BF16)
        k.cp('pool', identb[:], g.ident[:], g.ident.k, identb.k)
        zer = sb(g, ph, "a_zer", [128, 128])
        k.op('pool', lambda e: e.memset(zer[:], 0.0), [], zer.k)
        nmask = sb(g, ph, "a_nmask", [128, 128], BF16)
        k.op('pool', lambda e: e.affine_select(out=nmask[:], in_=zer[:], pattern=[[1, 128]], compare_op=ALU.is_ge,
                                               fill=-30000.0, base=0, channel_multiplier=-1), zer.k, nmask.k)
        onesb = sb(g, ph, "a_onesb", [128, 64], BF16)
        k.op('pool', lambda e: e.memset(onesb[:], 1.0), [], onesb.k)
        k.barrier()
        NTG = T // 128
        KTh = [sb(g, ph, "a_k%d" % i, [67, T], BF16) for i in range(2)]
        QTh = [sb(g, ph, "a_q%d" % i, [67, T], BF16) for i in range(2)]
        for i in range(2):
            k.op('pool', lambda e, i=i: e.memset(KTh[i][64:67, :], 1.0), [], KTh[i].k)
        SGh = [sb(g, ph, "a_g%d" % i, [64, T], BF16) for i in range(2)]
        Vh = [sb(g, ph, "a_v%d" % i, [128, NTG, 64], BF16) for i in range(2)]
        nfk = [sb(g, ph, "a_nfk%d" % i, [128, NTG]) for i in range(2)]
        Pb = [sb(g, ph, "a_p%d" % i, [128, 512], BF16) for i in range(3)]
        rl = sb(g, ph, "a_rl", [64, 512])
        of = sb(g, ph, "a_of", [64, 512])
        ost = [sb(g, ph, "a_os%d" % i, [64, 512], BF16) for i in range(2)]
        it = 0
        for hd in range(NH):
            b = hd % 2
            hr = slice(hd * 64, (hd + 1) * 64)
            k.dma('sp', KTh[b][0:64, :], S['KT'].t[hr, :], S['KT'].k, KTh[b].k)
            k.dma('sp', QTh[b][0:64, :], S['QT'].t[hr, :], S['QT'].k, QTh[b].k)
            k.dma('sp', QTh[b][64:67, :], g.FQ.t[hd], g.FQ.k, QTh[b].k)
            k.dma('sp', nfk[b][:], g.FK.t[hd], g.FK.k, nfk[b].k)
            k.dma('sp', SGh[b][:], S['SG'].t[hr, :], S['SG'].k, SGh[b].k)
            k.dma('sp', Vh[b][:], S['VK'].t[:, hr].rearrange("(tg p) d -> p tg d", p=128), S['VK'].k, Vh[b].k)
            for qg in range(T // 512):
                q0 = qg * 512
                Op, Lp = g.ps[2 + 2 * (qg % 2)], g.ps[3 + 2 * (qg % 2)]
                kts = list(range(4 * qg + 4))
                pend = None
                for idx, kt in enumerate(kts):
                    diag = kt >= 4 * qg
                    qlo = (kt - 4 * qg) * 128 if diag else 0
                    n = 512 - qlo
                    Sp = g.ps[idx % 2]
                    kc_ = slice(kt * 128, (kt + 1) * 128)
                    qc_ = slice(q0 + qlo, q0 + 512)
                    k.mm(Sp[:, 0:n], KTh[b][:, kc_], QTh[b][:, qc_], True, not diag, KTh[b].k + QTh[b].k, [Sp.k[0]])
                    if diag:
                        k.mm(Sp[:, 0:128], identb[:], nmask[:], False, True, identb.k + nmask.k, [Sp.k[0]])
                    P = Pb[it % 3]
                    it += 1
                    k.act(P[:, 0:n], Sp[:, 0:n], AF.Exp, [Sp.k[0]] + nfk[b].k, P.k, bias=nfk[b][:, kt:kt + 1])
                    first, lastf = idx == 0, idx == len(kts) - 1

                    def pvl(Op=Op, Lp=Lp, P=P, kt=kt, qlo=qlo, n=n, first=first, lastf=lastf, b=b):
                        k.mm(Op[0:64, qlo:512], Vh[b][:, kt, :], P[:, 0:n], first, lastf, Vh[b].k + P.k, [Op.k[0]])
                        k.mm(Lp[0:64, qlo:512], onesb[:], P[:, 0:n], first, lastf, onesb.k + P.k, [Lp.k[0]])
                    if pend is not None:
                        pend()
                    pend = pvl
                pend()
                pend = None
                k.op('dve', lambda e, Lp=Lp: e.reciprocal(out=rl[:], in_=Lp[0:64, :]), [Lp.k[0]], rl.k)
                k.tt('dve', of[:], Op[0:64, :], rl[:], ALU.mult, [Op.k[0]] + rl.k, of.k)
                o = ost[qg % 2]
                k.tt('pool', o[:], of[:], SGh[b][:, q0:q0 + 512], ALU.mult, of.k + SGh[b].k, o.k)
                k.dma('sp', S['MO'].t[hr, q0:q0 + 512], o[:], o.k, S['MO'].k)
        k.barrier()


def setup_rwkv_consts(g, es):
    k = g.k
    ones = g.ones
    su = sb(g, es, "c_su", [128, 128])
    iu = sb(g, es, "c_iu", [128, 128])
    g.mask4 = sb(g, es, "c_mask4", [128, 512])
    g.sl = sb(g, es, "c_sl", [128, 128])
    g.rm = sb(g, es, "c_rm", [128, 256])
    k.op('pool', lambda e: e.affine_select(out=su[:], in_=ones[:, 0:128], pattern=[[1, 128]], compare_op=ALU.is_gt,
                                           fill=0.0, base=0, channel_multiplier=-1), ones.k, su.k)
    k.op('pool', lambda e: e.affine_select(out=iu[:], in_=ones[:, 0:128], pattern=[[1, 128]], compare_op=ALU.is_ge,
                                           fill=0.0, base=0, channel_multiplier=-1), ones.k, iu.k)
    k.op('pool', lambda e: e.affine_select(out=g.sl[:], in_=ones[:, 0:128], pattern=[[-1, 128]], compare_op=ALU.is_gt,
                                           fill=0.0, base=0, channel_multiplier=1), ones.k, g.sl.k)
    k.tt('pool', g.sl[:], g.sl[:], g.blk[:], ALU.mult, g.sl.k + g.blk.k, g.sl.k)
    for q in range(4):
        src = su if q < 2 else iu
        k.tt('pool', g.mask4[:, q * 128:(q + 1) * 128], src[:], g.blk[:], ALU.mult, src.k + g.blk.k, g.mask4.k)
    k.op('pool', lambda e: e.memset(g.rm[:], 1.0), [], g.rm.k)
    for q in range(4):
        k.op('pool', lambda e, q=q: e.memset(g.rm[:, q * 64:q * 64 + 1], 0.0), [], g.rm.k)
    k.barrier()


def rwkv_mixer(g, l):
    k, I, S, T = g.k, g.I, g.S, g.T
    mt, m1 = g.modT[l], g.mod1[l]
    W = 256
    ps = g.ps
    with ExitStack() as ph:
        setup_rwkv_consts(g, ph)
        wv_ = lambda ap: ap.rearrange("(kc p) n -> p kc n", p=128)
        wr_ = sb(g, ph, "r_wr", [128, KC, D], BF16)
        wk_ = sb(g, ph, "r_wk", [128, KC, D], BF16)
        wvv = sb(g, ph, "r_wv", [128, KC, D], BF16)
        k.dma('pool', wr_[:], wv_(I['rw_rkv'][l, 0]), [], wr_.k)
        k.dma('pool', wk_[:], wv_(I['rw_rkv'][l, 1]), [], wk_.k)
        k.dma('pool', wvv[:], wv_(I['rw_rkv'][l, 2]), [], wvv.k)
        w1 = sb(g, ph, "r_w1", [128, KC, 64], BF16)
        a1 = sb(g, ph, "r_a1", [128, KC, 64], BF16)
        g1 = sb(g, ph, "r_g1", [128, KC, 160], BF16)
        k.dma('pool', w1[:], wv_(I['rw_w1'][l]), [], w1.k)
        k.dma('pool', a1[:], wv_(I['rw_a1'][l]), [], a1.k)
        k.dma('pool', g1[:], wv_(I['rw_g1'][l]), [], g1.k)
        w2 = sb(g, ph, "r_w2", [64, D], BF16)
        a2 = sb(g, ph, "r_a2", [64, D], BF16)
        g2 = sb(g, ph, "r_g2", [128, 2, D], BF16)
        k.dma('pool', w2[:], I['rw_w2'][l], [], w2.k)
        k.dma('pool', a2[:], I['rw_a2'][l], [], a2.k)
        k.dma('pool', g2[:, 0, :], I['rw_g2'][l, 0:128, :], [], g2.k)
        k.dma('pool', g2[0:32, 1, :], I['rw_g2'][l, 128:160, :], [], g2.k)
        if l > 0:
            v1 = sb(g, ph, "r_v1", [128, KC, 32], BF16)
            v2 = sb(g, ph, "r_v2", [32, D], BF16)
            k.dma('pool', v1[:], wv_(I['rw_v1'][l - 1]), [], v1.k)
            k.dma('pool', v2[:], I['rw_v2'][l - 1], [], v2.k)
        st = sb(g, ph, "r_lvst", [128, 128])
        pv = {}
        for nm, R in [('rw_mu', 48), ('rw_w0', 8), ('rw_a0', 8), ('rw_kk', 8), ('rw_ka', 8), ('rw_rk', 8),
                      ('rw_gn_g', 8), ('rw_gn_b', 8)]:
            pv[nm] = sb(g, ph, "r_p_" + nm, [128, R])
            load_vecT(g, pv[nm], I[nm][l], R, st)
        if l > 0:
            pv['rw_v0'] = sb(g, ph, "r_p_v0", [128, 8])
            load_vecT(g, pv['rw_v0'], I['rw_v0'][l - 1], 8, st)
        hm = sb(g, ph, "r_hm", [128, 2])
        k.cp('pool', hm[:, 0:1], g.blk[:, 0:1], g.blk.k, hm.k)
        k.cp('pool', hm[:, 1:2], g.blk[:, 127:128], g.blk.k, hm.k)
        Bth = [sb(g, ph, "r_Bth%d" % i, [128, W], F32R) for i in range(2)]
        Kth = [sb(g, ph, "r_Kth%d" % i, [128, W], F32R) for i in range(2)]
        Ath = [sb(g, ph, "r_Ath%d" % i, [128, W], F32R) for i in range(2)]
        VTc = [sb(g, ph, "r_VTc%d" % i, [128, 128], F32R) for i in range(2)]
        VTm = [sb(g, ph, "r_VTm%d" % i, [128, 128], F32R) for i in range(2)]
        cmk = [sb(g, ph, "r_cmk%d" % i, [128, 128]) for i in range(2)]
        for i in range(2):
            k.op('pool', lambda e, i=i: e.memset(cmk[i][:], 0.0), [], cmk[i].k)
            k.op('pool', lambda e, i=i: e.memset(cmk[i][:, i * 64:(i + 1) * 64], 1.0), [], cmk[i].k)
            k.op('pool', lambda e, i=i: e.memset(VTm[i][:].bitcast(F32), 0.0), [], VTm[i].k)
        omka = sb(g, ph, "r_omka", [128, 8])
        k.ts('dve', omka[:], pv['rw_ka'][:], -1.0, 1.0, ALU.mult, ALU.add, pv['rw_ka'].k, omka.k)
        k.barrier()
        state = [sb(g, ph, "r_state%d" % i, [128, 8, 64], F32, n=8) for i in range(2)]
        k.op('pool', lambda e: e.memset(state[0][:], 0.0), [], state[0].k)
        spar = [0] * 8
        hbuf = sb(g, ph, "r_h", [128, KC, W + 1])
        k.op('pool', lambda e: e.memset(hbuf[:, :, 0:1], 0.0), [], hbuf.k)
        xx = sb(g, ph, "r_xx", [128, KC, W])
        xb = xx
        xm = [sb(g, ph, "r_xm%d" % i, [128, KC, W], BF16) for i in range(6)]
        tw = sb(g, ph, "r_tw", [64, W], BF16)
        ta = sb(g, ph, "r_ta", [64, W], BF16)
        tg0 = sb(g, ph, "r_tg0", [128, W], BF16)
        tg1 = sb(g, ph, "r_tg1", [32, W], BF16)
        tv = sb(g, ph, "r_tv", [32, W], BF16)
        names = ['r', 'k', 'v', 'wl', 'sa', 'g', 'kk', 't1', 'rs', 'kkn', 'kf', 'b', 'c', 'd', 'eg', 'em', 'ed', 'ep',
                 'Rt', 'At', 'Kt', 'Bt', 'Kh', 'Bh']
        t = {n: sb(g, ph, "r_t_" + n, [128, W], F32R if n in ('At', 'Rt') else F32) for n in names}
        t['y'], t['y2'], t['mn'], t['bon'], t['vf'] = t['eg'], t['em'], t['ed'], t['ep'], t['d']
        gl_ = sb(g, ph, "r_gl", [128, 4])
        KhT = sb(g, ph, "r_KhT", [128, 2, 128])
        BhT = sb(g, ph, "r_BhT", [128, 2, 128])
        VT = sb(g, ph, "r_VT", [128, 2, 128], F32R)
        Mall = sb(g, ph, "r_Mall", [128, 4, 512], F32R)
        Nall = [sb(g, ph, "r_Nall%d" % i, [128, 4, 128], F32R) for i in range(2)]
        Ntall = [sb(g, ph, "r_Ntall%d" % i, [128, 4, 128], F32R) for i in range(2)]
        Pall = sb(g, ph, "r_Pall", [128, 4, 128], F32R)
        sl4 = sb(g, ph, "r_sl4", [128, 4, 128])
        id4 = sb(g, ph, "r_id4", [128, 4, 128])
        for q in range(4):
            k.cp('pool', sl4[:, q, :], g.sl[:], g.sl.k, sl4.k)
            k.cp('pool', id4[:, q, :], g.ident[:], g.ident.k, id4.k)
        Xs2 = sb(g, ph, "r_Xs2", [128, 128], F32R)
        UP = sb(g, ph, "r_UP", [128, 2, 128], F32R)
        k.op('pool', lambda e: e.memset(UP[:].bitcast(F32), 0.0), [], UP.k)
        _b = UP.t[:]
        UPdiag = bass.AP(tensor=_b.tensor, offset=_b.offset, ap=[list(_b.ap[0]), [192, 2], [1, 64]])
        SP = sb(g, ph, "r_SP", [128, 128], F32R)
        BhTm = [[sb(g, ph, "r_BhTm%d%d" % (a, b_), [128, 128], F32R) for b_ in range(2)] for a in range(2)]
        KhTm = [[sb(g, ph, "r_KhTm%d%d" % (a, b_), [128, 128], F32R) for b_ in range(2)] for a in range(2)]
        for a in range(2):
            for b_ in range(2):
                k.op('pool', lambda e, a=a, b_=b_: e.memset(BhTm[a][b_][:].bitcast(F32), 0.0), [], BhTm[a][b_].k)
                k.op('pool', lambda e, a=a, b_=b_: e.memset(KhTm[a][b_][:].bitcast(F32), 0.0), [], KhTm[a][b_].k)
        mob = sb(g, ph, "r_mo", [128, W], BF16)
        XTv = S['XT'].t.rearrange("(kc p) t -> p kc t", p=128)
        unit_i = 0
        for ti in range(T // W):
            t0 = ti * W
            h = hbuf
            if ti > 0:
                k.cp('pool', h[:, :, 0:1], h[:, :, W:W + 1], h.k, h.k)
            k.dma('sp', xb[:], XTv[:, :, t0:t0 + W], S['XT'].k, xb.k)
            for kc in range(KC):
                k.act(h[:, kc, 1:W + 1], xb[:, kc, :], AF.Identity, xb.k, h.k,
                      scale=m1[:, 8 + kc:9 + kc], bias=mt[:, kc:kc + 1])
            k.tt('dve', xx[:], h[:, :, 0:W], h[:, :, 1:W + 1], ALU.subtract, h.k, xx.k)
            for i in range(6):
                if i == 3 and False:
                    continue
                for kc in range(KC):
                    k.stt(xm[i][:, kc, :], xx[:, kc, :], pv['rw_mu'][:, i * 8 + kc:i * 8 + kc + 1], h[:, kc, 1:W + 1],
                          ALU.mult, ALU.add, xx.k + h.k, xm[i].k)
            p = ps[3]
            for kc in range(KC):
                k.mm(p[0:64, 0:W], w1[:, kc, :], xm[1][:, kc, :], kc == 0, kc == KC - 1, w1.k + xm[1].k, [p.k[0]])
            k.act(tw[:], p[0:64, 0:W], AF.Tanh, [p.k[0]], tw.k)
            for kc in range(KC):
                k.mm(p[0:64, W:2 * W], a1[:, kc, :], xm[4][:, kc, :], kc == 0, kc == KC - 1, a1.k + xm[4].k, [p.k[2]])
            k.cp('act', ta[:], p[0:64, W:2 * W], [p.k[2]], ta.k)
            p = ps[2]
            for kc in range(KC):
                k.mm(p[:, 0:W], g1[:, kc, 0:128], xm[5][:, kc, :], kc == 0, kc == KC - 1, g1.k + xm[5].k, [p.k[0]])
            k.act(tg0[:], p[:, 0:W], AF.Sigmoid, [p.k[0]], tg0.k)
            for kc in range(KC):
                k.mm(p[0:32, W:2 * W], g1[:, kc, 128:160], xm[5][:, kc, :], kc == 0, kc == KC - 1, g1.k + xm[5].k,
                     [p.k[2]])
            k.act(tg1[:], p[0:32, W:2 * W], AF.Sigmoid, [p.k[2]], tg1.k)
            if l > 0:
                p = ps[1]
                for kc in range(KC):
                    k.mm(p[0:32, 0:W], v1[:, kc, :], xm[3][:, kc, :], kc == 0, kc == KC - 1, v1.k + xm[3].k, [p.k[0]])
                k.cp('act', tv[:], p[0:32, 0:W], [p.k[0]], tv.k)
            for hp in range(8):
                if RW_STOP[0] <= 1:
                    break
                oc = hp
                ocs = slice(oc * 128, (oc + 1) * 128)
                col = lambda buf, j: buf[:, j:j + 1]

                def proj(pb, c0, kk_, wt, xi):
                    for kc in range(KC):
                        k.mm(pb[:, c0:c0 + W], wt[:, kc, ocs], xm[xi][:, kc, :], kc == 0, kc == KC - 1,
                             wt.k + xm[xi].k, [pb.k[kk_]])
                proj(ps[0], 0, 0, wr_, 0)
                proj(ps[0], W, 2, wk_, 2)
                proj(ps[1], 0, 0, wvv, 3)
                k.mm(ps[1][:, W:2 * W], w2[:, ocs], tw[:], True, True, w2.k + tw.k, [ps[1].k[2]])
                k.mm(ps[2][:, 0:W], a2[:, ocs], ta[:], True, True, a2.k + ta.k, [ps[2].k[0]])
                k.mm(ps[2][:, W:2 * W], g2[:, 0, ocs], tg0[:], True, False, g2.k + tg0.k, [ps[2].k[2]])
                k.mm(ps[2][:, W:2 * W], g2[0:32, 1, ocs], tg1[:], False, True, g2.k + tg1.k, [ps[2].k[2]])
                if l > 0:
                    k.mm(ps[3][:, 0:W], v2[:, ocs], tv[:], True, True, v2.k + tv.k, [ps[3].k[0]])
                k.cp('act', t['r'][:], ps[0][:, 0:W], [ps[0].k[0]], t['r'].k)
                k.cp('act', t['k'][:], ps[0][:, W:2 * W], [ps[0].k[2]], t['k'].k)
                k.cp('dve', t['v'][:], ps[1][:, 0:W], [ps[1].k[0]], t['v'].k)
                k.cp('act', t['g'][:], ps[2][:, W:2 * W], [ps[2].k[2]], t['g'].k)
                VFv = S['VF'].t[ocs, t0:t0 + W]
                if l == 0:
                    k.dma('sp', VFv, t['v'][:], t['v'].k, S['VF'].k)
                else:
                    k.dma('sp', t['vf'][:], VFv, S['VF'].k, t['vf'].k)
                    k.act(t['y2'][:], ps[3][:, 0:W], AF.Sigmoid, [ps[3].k[0]], t['y2'].k, bias=col(pv['rw_v0'], oc))
                    k.tt('pool', t['vf'][:], t['vf'][:], t['v'][:], ALU.subtract, t['vf'].k + t['v'].k, t['vf'].k)
                    k.tt('pool', t['vf'][:], t['vf'][:], t['y2'][:], ALU.mult, t['vf'].k + t['y2'].k, t['vf'].k)
                    k.tt('pool', t['v'][:], t['v'][:], t['vf'][:], ALU.add, t['vf'].k + t['v'].k, t['v'].k)
                k.act(t['wl'][:], ps[1][:, W:2 * W], AF.Sigmoid, [ps[1].k[2]], t['wl'].k, bias=col(pv['rw_w0'], oc))
                k.act(t['sa'][:], ps[2][:, 0:W], AF.Sigmoid, [ps[2].k[0]], t['sa'].k, bias=col(pv['rw_a0'], oc))
                k.act(t['wl'][:], t['wl'][:], AF.Copy, t['wl'].k, t['wl'].k, scale=-0.6065306597126334)
                k.ts('dve', t['kk'][:], t['k'][:], col(pv['rw_kk'], oc), None, ALU.mult, None, t['k'].k, t['kk'].k)
                k.act(t['t1'][:], t['kk'][:], AF.Square, t['kk'].k, t['t1'].k)
                k.mm(ps[3][:, W:2 * W], g.blk[:], t['t1'][:], True, True, g.blk.k + t['t1'].k, [ps[3].k[2]])
                k.act(t['rs'][:], ps[3][:, W:2 * W], AF.Sqrt, [ps[3].k[2]], t['rs'].k)
                k.ts('dve', t['rs'][:], t['rs'][:], 1e-12, None, ALU.max, None, t['rs'].k, t['rs'].k)
                k.op('dve', lambda e: e.reciprocal(out=t['rs'][:], in_=t['rs'][:]), t['rs'].k, t['rs'].k)
                k.tt('dve', t['kkn'][:], t['kk'][:], t['rs'][:], ALU.mult, t['kk'].k + t['rs'].k, t['kkn'].k)
                k.ts('dve', t['t1'][:], t['sa'][:], col(pv['rw_ka'], oc), col(omka, oc), ALU.mult, ALU.add,
                     t['sa'].k, t['t1'].k)
                k.tt('dve', t['kf'][:], t['k'][:], t['t1'][:], ALU.mult, t['k'].k + t['t1'].k, t['kf'].k)
                k.tt('dve', t['b'][:], t['kkn'][:], t['sa'][:], ALU.mult, t['kkn'].k + t['sa'].k, t['b'].k)
                k.op('dve', lambda e: e.tensor_tensor_scan(out=t['c'][:], data0=g.rm[:], data1=t['wl'][:], initial=0.0,
                                                           op0=ALU.mult, op1=ALU.add),
                     g.rm.k + t['wl'].k, t['c'].k)
                for j in range(4):
                    cs_ = slice(j * 64, (j + 1) * 64)
                    k.ts('dve', t['d'][:, cs_], t['c'][:, cs_], -1.0, t['c'][:, j * 64 + 63:j * 64 + 64],
                         ALU.mult, ALU.add, t['c'].k, t['d'].k)
                k.tt('pool', t['ep'][:], t['c'][:], t['wl'][:], ALU.subtract, t['c'].k + t['wl'].k, t['ep'].k)
                k.act(t['eg'][:], t['c'][:], AF.Exp, t['c'].k, t['eg'].k)
                k.act(t['em'][:], t['c'][:], AF.Exp, t['c'].k, t['em'].k, scale=-1.0)
                k.act(t['ed'][:], t['d'][:], AF.Exp, t['d'].k, t['ed'].k)
                k.act(t['ep'][:], t['ep'][:], AF.Exp, t['ep'].k, t['ep'].k)
                k.act(gl_[:], t['c'][:, :].rearrange("p (j t) -> p j t", t=64)[:, :, 63], AF.Exp, t['c'].k, gl_.k)
                k.tt('dve', t['Rt'][:], t['r'][:], t['eg'][:], ALU.mult, t['r'].k + t['eg'].k, t['Rt'].k)
                k.stt(t['At'][:], t['kkn'][:], -1.0, t['ep'][:], ALU.mult, ALU.mult, t['kkn'].k + t['ep'].k, t['At'].k)
                k.tt('pool', t['Kt'][:], t['kf'][:], t['em'][:], ALU.mult, t['kf'].k + t['em'].k, t['Kt'].k)
                k.tt('pool', t['Bt'][:], t['b'][:], t['em'][:], ALU.mult, t['b'].k + t['em'].k, t['Bt'].k)
                k.tt('dve', t['Kh'][:], t['kf'][:], t['ed'][:], ALU.mult, t['kf'].k + t['ed'].k, t['Kh'].k)
                k.tt('pool', t['Bh'][:], t['b'][:], t['ed'][:], ALU.mult, t['b'].k + t['ed'].k, t['Bh'].k)
                if RW_STOP[0] <= 2:
                    continue
                for hh in range(2):
                    k.act(Bth[hh][:], t['Bt'][:], AF.Copy, t['Bt'].k + hm.k, Bth[hh].k, scale=hm[:, hh:hh + 1])
                    k.act(Kth[hh][:], t['Kt'][:], AF.Copy, t['Kt'].k + hm.k, Kth[hh].k, scale=hm[:, hh:hh + 1])
                    k.ts('dve', Ath[hh][:], t['At'][:].bitcast(F32), hm[:, hh:hh + 1], None, ALU.mult, None, t['At'].k + hm.k, Ath[hh].k)
                for cp in range(2):
                    cc = slice(cp * 128, (cp + 1) * 128)
                    k.tr(ps[6][:, cp * 128:(cp + 1) * 128], t['Kh'][:, cc], g.ident[:], t['Kh'].k + g.ident.k, [ps[6].k[0]])
                    k.tr(ps[6][:, 256 + cp * 128:256 + (cp + 1) * 128], t['Bh'][:, cc], g.ident[:],
                         t['Bh'].k + g.ident.k, [ps[6].k[0]])
                    k.tr(ps[7][:, 256 + cp * 128:256 + (cp + 1) * 128], t['v'][:, cc], g.ident[:],
                         t['v'].k + g.ident.k, [ps[7].k[3]])
                k.cp('act', KhT[:], ps[6][:, 0:256].rearrange("p (a b) -> p a b", b=128), [ps[6].k[0]], KhT.k)
                k.cp('dve', BhT[:], ps[6][:, 256:512].rearrange("p (a b) -> p a b", b=128), [ps[6].k[0]], BhT.k)
                k.cp('act', VT[:], ps[7][:, 256:512].rearrange("p (a b) -> p a b", b=128), [ps[7].k[3]], VT.k)
                Yp = ps[0]
                mbank = [ps[4], ps[6], ps[1], ps[2]]
                for cp in range(2):
                    cc = slice(cp * 128, (cp + 1) * 128)
                    for hh in range(2):
                        u = cp * 2 + hh
                        Mp = mbank[u]
                        rd = Bth[hh].k + t['At'].k + Kth[hh].k + t['Rt'].k
                        k.mm(Mp[:, 0:128], Bth[hh][:, cc], t['At'][:, cc], True, True, rd, [Mp.k[0]])
                        k.mm(Mp[:, 128:256], Kth[hh][:, cc], t['At'][:, cc], True, True, rd, [Mp.k[0]])
                        k.mm(Mp[:, 256:384], Bth[hh][:, cc], t['Rt'][:, cc], True, True, rd, [Mp.k[0]])
                        k.mm(Mp[:, 384:512], Kth[hh][:, cc], t['Rt'][:, cc], True, True, rd, [Mp.k[0]])
                        k.mm(ps[5][:, u * 128:(u + 1) * 128], t['At'][:, cc], Bth[hh][:, cc], True, True, rd, [ps[5].k[0]])
                        k.tt('dve', Mall[:, u, :], Mp[:, :], g.mask4[:], ALU.mult, [Mp.k[0]] + g.mask4.k, Mall.k)
                        hc_ = slice(hh * 64, (hh + 1) * 64)
                        k.cp('act', BhTm[cp][hh][:, hc_], BhT[:, cp, hc_], BhT.k, BhTm[cp][hh].k)
                        k.cp('act', KhTm[cp][hh][:, hc_], KhT[:, cp, hc_], KhT.k, KhTm[cp][hh].k)
                k.tt('dve', Nall[0][:].rearrange("p a b -> p (a b)"), ps[5][:, :], sl4[:].rearrange("p a b -> p (a b)"),
                     ALU.mult, [ps[5].k[0]] + sl4.k, Nall[0].k)
                k.cp('act', Ntall[0][:], Mall[:, :, 0:128].bitcast(F32), Mall.k, Ntall[0].k)
                k.tt('dve', Pall[:], Mall[:, :, 0:128].bitcast(F32), id4[:], ALU.add, Mall.k + id4.k, Pall.k)
                pa, pb_, pc = ps[1], ps[2], ps[3]
                for lev in range(1, 6):
                    Np, Ntp = Nall[(lev - 1) % 2], Ntall[(lev - 1) % 2]
                    Nn, Ntn = Nall[lev % 2], Ntall[lev % 2]
                    for u in range(4):
                        k.mm(pa[:, u * 128:(u + 1) * 128], Ntp[:, u, :], Np[:, u, :], True, True, Ntp.k + Np.k, [pa.k[0]])
                    if lev < 5:
                        for u in range(4):
                            k.mm(pb_[:, u * 128:(u + 1) * 128], Np[:, u, :], Ntp[:, u, :], True, True, Ntp.k + Np.k,
                                 [pb_.k[0]])
                    k.cp('act', Nn[:].rearrange("p a b -> p (a b)"), pa[:, :], [pa.k[0]], Nn.k)
                    if lev < 5:
                        k.cp('dve', Ntn[:].rearrange("p a b -> p (a b)"), pb_[:, :], [pb_.k[0]], Ntn.k)
                    for u in range(4):
                        k.mm(pc[:, u * 128:(u + 1) * 128], Nn[:, u, :], Pall[:, u, :], True, True, Nn.k + Pall.k, [pc.k[0]])
                    k.tt('dve', Pall[:].rearrange("p a b -> p (a b)"), pc[:, :], Pall[:].rearrange("p a b -> p (a b)").bitcast(F32),
                         ALU.add, [pc.k[0]] + Pall.k, Pall.k)
                for cp in range(2):
                    cc = slice(cp * 128, (cp + 1) * 128)
                    for q in range(2):
                        k.act(VTc[q][:], VT[:, cp, :].bitcast(F32), AF.Copy, VT.k + hm.k, VTc[q].k, scale=hm[:, q:q + 1])
                        k.cp('act', VTm[q][:, q * 64:(q + 1) * 64], VT[:, cp, q * 64:(q + 1) * 64].bitcast(F32), VT.k, VTm[q].k)
                    for ch in range(2):
                        j = cp * 2 + ch
                        c64 = slice(j * 64, (j + 1) * 64)
                        so, sn = state[spar[hp]], state[1 - spar[hp]]
                        sk = [so.k[hp]]
                        p7 = ps[7]
                        for hh in range(2):
                            hcol = slice(hh * 64, (hh + 1) * 64)
                            k.act(SP[:, hcol], so[:, hp, :], AF.Copy, sk + hm.k, SP.k, scale=hm[:, hh:hh + 1])
                        for hh in range(2):
                            u = cp * 2 + hh
                            hcol = slice(hh * 64, (hh + 1) * 64)
                            k.mm(p7[:, hcol], Ath[hh][:, cc], SP[:, hcol], True, False, Ath[hh].k + SP.k, [p7.k[0]])
                            k.mm(p7[:, hcol], Mall[:, u, 128:256], VT[:, cp, hcol], False, True, Mall.k + VT.k, [p7.k[0]])
                        k.cp('act', Xs2[:], p7[:, 0:128], [p7.k[0]], Xs2.k)
                        for hh in range(2):
                            u = cp * 2 + hh
                            hcol = slice(hh * 64, (hh + 1) * 64)
                            k.mm(p7[:, 128 + hh * 64:128 + (hh + 1) * 64], Pall[:, u, :], Xs2[:, hcol], True, True,
                                 Pall.k + Xs2.k, [p7.k[0]])
                        k.ts('dve', UPdiag, p7[:, 128:256].rearrange("p (a b) -> p a b", b=64), hm[:, ch:ch + 1], None,
                             ALU.mult, None, [p7.k[0]] + hm.k, UP.k)
                        T_ = p7[:, 256:320]
                        for hh in range(2):
                            hcol = slice(hh * 64, (hh + 1) * 64)
                            k.mm(T_, BhTm[cp][hh][:], UP[:, hh, hcol], hh == 0, False, BhTm[cp][hh].k + UP.k, [p7.k[0]])
                            k.mm(T_, KhTm[cp][hh][:], VTc[ch][:, hcol], False, hh == 1, KhTm[cp][hh].k + VTc[ch].k,
                                 [p7.k[0]])
                        k.stt(sn[:, hp, :], so[:, hp, :], gl_[:, j:j + 1], T_, ALU.mult, ALU.add,
                              sk + gl_.k + [p7.k[0]], [sn.k[hp]])
                        Y_ = Yp[:, c64]
                        k.mm(Y_, SP[:], t['Rt'][:, c64], True, False, SP.k + t['Rt'].k, [Yp.k[0]])
                        for hh in range(2):
                            u = cp * 2 + hh
                            k.mm(Y_, UP[:, hh, :], Mall[:, u, 256 + ch * 64:256 + (ch + 1) * 64], False, False,
                                 UP.k + Mall.k, [Yp.k[0]])
                            k.mm(Y_, VTm[hh][:], Mall[:, u, 384 + ch * 64:384 + (ch + 1) * 64], False, hh == 1,
                                 VTm[hh].k + Mall.k, [Yp.k[0]])
                        spar[hp] = 1 - spar[hp]
                if RW_STOP[0] <= 4:
                    continue
                k.cp('act', t['y'][:], Yp[:, 0:W], [Yp.k[0]], t['y'].k)
                k.act(t['y2'][:], t['y'][:], AF.Square, t['y'].k, t['y2'].k)
                k.mm(ps[1][:, 0:W], g.blk[:], t['y'][:], True, True, g.blk.k + t['y'].k, [ps[1].k[0]])
                k.mm(ps[1][:, W:2 * W], g.blk[:], t['y2'][:], True, True, g.blk.k + t['y2'].k, [ps[1].k[2]])
                k.ts('dve', t['mn'][:], ps[1][:, 0:W], 1.0 / 64, None, ALU.mult, None, [ps[1].k[0]], t['mn'].k)
                k.act(t['y2'][:], t['mn'][:], AF.Square, t['mn'].k, t['y2'].k)
                k.stt(t['y2'][:], ps[1][:, W:2 * W], 1.0 / 64, t['y2'][:], ALU.mult, ALU.subtract,
                      [ps[1].k[2]] + t['y2'].k, t['y2'].k)
                k.ts('dve', t['y2'][:], t['y2'][:], GN_EPS, None, ALU.add, None, t['y2'].k, t['y2'].k)
                k.act(t['y2'][:], t['y2'][:], AF.Sqrt, t['y2'].k, t['y2'].k)
                k.op('dve', lambda e: e.reciprocal(out=t['y2'][:], in_=t['y2'][:]), t['y2'].k, t['y2'].k)
                k.tt('pool', t['y'][:], t['y'][:], t['mn'][:], ALU.subtract, t['y'].k + t['mn'].k, t['y'].k)
                k.tt('pool', t['y'][:], t['y'][:], t['y2'][:], ALU.mult, t['y'].k + t['y2'].k, t['y'].k)
                k.act(t['y'][:], t['y'][:], AF.Identity, t['y'].k, t['y'].k, scale=col(pv['rw_gn_g'], oc),
                      bias=col(pv['rw_gn_b'], oc))
                k.tt('pool', t['bon'][:], t['r'][:], t['kf'][:], ALU.mult, t['r'].k + t['kf'].k, t['bon'].k)
                k.ts('dve', t['bon'][:], t['bon'][:], col(pv['rw_rk'], oc), None, ALU.mult, None, t['bon'].k, t['bon'].k)
                k.mm(ps[3][:, W:2 * W], g.blk[:], t['bon'][:], True, True, g.blk.k + t['bon'].k, [ps[3].k[2]])
                k.tt('dve', t['bon'][:], ps[3][:, W:2 * W], t['v'][:], ALU.mult, [ps[3].k[2]] + t['v'].k, t['bon'].k)
                k.tt('pool', t['y'][:], t['y'][:], t['bon'][:], ALU.add, t['y'].k + t['bon'].k, t['y'].k)
                k.tt('pool', mob[:], t['y'][:], t['g'][:], ALU.mult, t['y'].k + t['g'].k, mob.k)
                k.dma('sp', S['MO'].t[ocs, t0:t0 + W], mob[:], mob.k, S['MO'].k)
        k.barrier()


_CACHE = {}


def prep_inputs(inp, b, T):
    f = lambda a: np.ascontiguousarray(np.asarray(a, dtype=np.float32))
    m = {}
    m['x'] = f(inp['x'][b][:T])
    m['c'] = f(inp['c'][b]).reshape(KC, 128)
    m['ada_w'] = f(inp['ada_w'])
    m['ada_b'] = f(inp['ada_b']).reshape(DEPTH, 48, 128)
    m['ln_g'] = f(inp['ln_g']).reshape(DEPTH, 16, 128)
    m['ln_b'] = f(inp['ln_b']).reshape(DEPTH, 16, 128)
    m['rw_mu'] = f(inp['rw_mu']).reshape(N_A, 48, 128)
    m['rw_rkv'] = f(inp['rw_rkv'])
    for nm in ['rw_w0', 'rw_a0', 'rw_kk', 'rw_ka', 'rw_rk', 'rw_gn_g', 'rw_gn_b']:
        m[nm] = f(inp[nm]).reshape(N_A, 8, 128)
    for nm in ['rw_w1', 'rw_w2', 'rw_a1', 'rw_a2', 'rw_g1', 'rw_g2', 'rw_wo', 'rw_v1', 'rw_v2',
               'kv_ada_w', 'kv_w', 'fx_wqg', 'fx_wo', 'moe_wgrp', 'moe_bgrp', 'moe_wexp', 'moe_bexp',
               'moe_wgate', 'moe_wup', 'moe_wdown']:
        m[nm] = f(inp[nm])
    m['rw_v0'] = f(inp['rw_v0']).reshape(1, 8, 128)
    m['kv_ada_b'] = f(inp['kv_ada_b']).reshape(16, 128)
    m['kv_fb'] = f(inp['kv_fb']).reshape(NH, 1)
    m['kv_knorm'] = f(inp['kv_knorm']).reshape(HD, 1)
    m['fx_qnorm'] = f(inp['fx_qnorm']).reshape(2, HD, 1)
    return m


def kernel(**inputs):
    B, T = inputs['x'].shape[0], inputs['x'].shape[1]
    NG = inputs['moe_wgrp'].shape[-1]
    NE = inputs['moe_wexp'].shape[-1]
    key = (T, NG, NE)
    if key not in _CACHE:
        _CACHE[key] = build(T, NG, NE // NG)[0]
    nc = _CACHE[key]
    shared = None
    maps = []
    for b in range(B):
        m = prep_inputs(inputs, b, T) if shared is None else dict(shared)
        if shared is None:
            shared = m
        else:
            m['x'] = np.ascontiguousarray(np.asarray(inputs['x'][b], dtype=np.float32))
            m['c'] = np.ascontiguousarray(np.asarray(inputs['c'][b], dtype=np.float32)).reshape(KC, 128)
        maps.append(m)
    res = run_bass_kernel_spmd(nc, maps, core_ids=list(range(B)))
    return np.stack([np.asarray(r['out']) for r in res.results]).astype(np.float32)
```
